# Optimizing a Trainium2 kernel written in Bass

```python
import jax, jax.numpy as jnp
from jax import lax
import numpy as np

D_MODEL = 1024
BATCH = 8
SEQ = 4096
DEPTH = 1

N_HEADS_ATTN = 8
HEAD_DIM = 64
ATTN_WIDTH = N_HEADS_ATTN * HEAD_DIM
MOBA_BLOCK = 256
MOBA_TOPK = 3
Q_CHUNK = 32
ROPE_THETA = 500000.0
ROT_DIM = HEAD_DIM // 4
CONV_WIDTH = 512
CONV_K = 3
N_BRANCH = 2
IN_COLS = 3 * ATTN_WIDTH + 3 * CONV_WIDTH + N_BRANCH * D_MODEL
N_GROUPS = 4
EXPERTS_PER_GROUP = 8
N_EXPERTS = N_GROUPS * EXPERTS_PER_GROUP
TOPK_IN_GROUP = 2
D_EXPERT = 512
MOE_BLOCK = 128
N_MOD = 6
EPS = 1e-6

kernel_name = "hybrid_moba_shortconv_hmoe_block"


def rms_norm(x, g):
    xf = x.astype(jnp.float32)
    y = xf * lax.rsqrt(jnp.mean(xf * xf, axis=-1, keepdims=True) + EPS)
    return (y * g.astype(jnp.float32)).astype(x.dtype)


def modulate(h, shift, scale):
    return h * (1 + scale[:, None, :]) + shift[:, None, :]


def rope_tables(seq_len, dtype):
    pos = jnp.arange(seq_len, dtype=jnp.float32)
    inv_freq = ROPE_THETA ** (-jnp.arange(0, ROT_DIM, 2, dtype=jnp.float32) / ROT_DIM)
    ang = pos[:, None] * inv_freq[None, :]
    return jnp.cos(ang).astype(dtype), jnp.sin(ang).astype(dtype)


def partial_rope(x, cos, sin):
    half = ROT_DIM // 2
    x1, x2, xp = x[..., :half], x[..., half:ROT_DIM], x[..., ROT_DIM:]
    return jnp.concatenate([x1 * cos - x2 * sin, x2 * cos + x1 * sin, xp], axis=-1)


def moba_attention(q, k, v):
    B, H, S, dh = q.shape
    nb = -(-S // MOBA_BLOCK)
    pad = nb * MOBA_BLOCK - S
    kb = jnp.pad(k, ((0, 0), (0, 0), (0, pad), (0, 0))).reshape(B, H, nb, MOBA_BLOCK, dh)
    vb = jnp.pad(v, ((0, 0), (0, 0), (0, pad), (0, 0))).reshape(B, H, nb, MOBA_BLOCK, dh)
    kmean = jnp.mean(kb.astype(jnp.float32), axis=3)
    topk = min(MOBA_TOPK, nb)
    scale = HEAD_DIM ** -0.5
    b_ix = jnp.arange(B)[:, None, None, None]
    h_ix = jnp.arange(H)[None, :, None, None]
    blk_ids = jnp.arange(nb)
    t_in_blk = jnp.arange(MOBA_BLOCK)

    def chunk(ci):
        q0 = ci * Q_CHUNK
        qc = lax.dynamic_slice_in_dim(q, q0, Q_CHUNK, axis=2)
        qpos = q0 + jnp.arange(Q_CHUNK)
        cur = q0 // MOBA_BLOCK
        gate = jnp.einsum('bhqd,bhnd->bhqn', qc.astype(jnp.float32), kmean)
        gate = jnp.where(blk_ids < cur, gate, -jnp.inf)
        _, sel = lax.top_k(gate, topk)
        sel_valid = jnp.arange(topk) < cur
        k_sel = kb[b_ix, h_ix, sel]
        v_sel = vb[b_ix, h_ix, sel]
        s_sel = jnp.einsum('bhqd,bhqntd->bhqnt', qc, k_sel).astype(jnp.float32) * scale
        s_sel = jnp.where(sel_valid[:, None], s_sel, -jnp.inf)
        k_own = lax.dynamic_index_in_dim(kb, cur, axis=2, keepdims=False)
        v_own = lax.dynamic_index_in_dim(vb, cur, axis=2, keepdims=False)
        s_own = jnp.einsum('bhqd,bhtd->bhqt', qc, k_own).astype(jnp.float32) * scale
        kpos = cur * MOBA_BLOCK + t_in_blk
        s_own = jnp.where(kpos[None, :] <= qpos[:, None], s_own, -jnp.inf)
        logits = jnp.concatenate([s_sel.reshape(B, H, Q_CHUNK, topk * MOBA_BLOCK), s_own], axis=-1)
        p = jax.nn.softmax(logits, axis=-1).astype(v.dtype)
        p_sel = p[..., :topk * MOBA_BLOCK].reshape(B, H, Q_CHUNK, topk, MOBA_BLOCK)
        p_own = p[..., topk * MOBA_BLOCK:]
        return (jnp.einsum('bhqnt,bhqntd->bhqd', p_sel, v_sel)
                + jnp.einsum('bhqt,bhtd->bhqd', p_own, v_own))

    out = lax.map(chunk, jnp.arange(S // Q_CHUNK))
    return out.transpose(1, 2, 0, 3, 4).reshape(B, H, S, dh)


def short_gated_conv(xb, bg, cg, conv_w, conv_b):
    u = cg * xb
    y = lax.conv_general_dilated(u, conv_w[:, None, :], window_strides=(1,), padding=[(CONV_K - 1, 0)],
                                 dimension_numbers=('NWC', 'WIO', 'NWC'), feature_group_count=CONV_WIDTH)
    return bg * (y + conv_b)


def hierarchical_moe(h, w_rg, b_rg, w_re, b_re, w1, w3, w2):
    B, S, D = h.shape
    T = B * S
    xt = h.reshape(T, D)
    g_logits = (xt @ w_rg).astype(jnp.float32) + b_rg.astype(jnp.float32)
    g_prob = jax.nn.softmax(g_logits, axis=-1)
    g_idx = jnp.argmax(g_logits, axis=-1)
    g_w = jnp.take_along_axis(g_prob, g_idx[:, None], axis=-1)
    e_logits = ((xt @ w_re).astype(jnp.float32) + b_re.astype(jnp.float32)).reshape(T, N_GROUPS, EXPERTS_PER_GROUP)
    e_logits = jnp.take_along_axis(e_logits, g_idx[:, None, None], axis=1)[:, 0]
    e_val, e_loc = lax.top_k(e_logits, TOPK_IN_GROUP)
    e_w = jax.nn.softmax(e_val, axis=-1) * g_w
    e_id = g_idx[:, None].astype(jnp.int32) * EXPERTS_PER_GROUP + e_loc.astype(jnp.int32)

    n_assign = T * TOPK_IN_GROUP
    flat_e = e_id.reshape(-1)
    flat_w = e_w.reshape(-1)
    order = jnp.argsort(flat_e)
    sorted_e = flat_e[order]
    counts = jnp.bincount(flat_e, length=N_EXPERTS)
    padded = (counts + MOE_BLOCK - 1) // MOE_BLOCK * MOE_BLOCK
    start = jnp.cumsum(counts) - counts
    pad_end = jnp.cumsum(padded)
    pad_start = pad_end - padded
    dest = pad_start[sorted_e] + (jnp.arange(n_assign) - start[sorted_e])
    n_pad = (-(-n_assign // MOE_BLOCK) + N_EXPERTS) * MOE_BLOCK
    n_blocks = n_pad // MOE_BLOCK
    tok = jnp.full((n_pad,), T, jnp.int32).at[dest].set((order // TOPK_IN_GROUP).astype(jnp.int32))
    wpad = jnp.zeros((n_pad,), jnp.float32).at[dest].set(flat_w[order])
    blk_start = jnp.arange(n_blocks) * MOE_BLOCK
    blk_expert = jnp.minimum(jnp.sum(pad_end[None, :] <= blk_start[:, None], axis=-1), N_EXPERTS - 1)
    x_ext = jnp.concatenate([xt, jnp.zeros((1, D), xt.dtype)], axis=0)
    xs = x_ext[tok].reshape(n_blocks, MOE_BLOCK, D)

    def expert_block(args):
        xb, e = args
        hid = jax.nn.silu(xb @ w1[e]) * (xb @ w3[e])
        return hid @ w2[e]

    ys = lax.map(expert_block, (xs, blk_expert)).reshape(n_pad, D)
    ys = ys * wpad[:, None].astype(ys.dtype)
    out = jnp.zeros((T + 1, D), ys.dtype).at[tok].add(ys)[:T]
    return out.reshape(B, S, D)


def setup_inputs(seed: int = 0) -> dict:
    key = jax.random.key(seed)
    ks = jax.random.split(key, 24)
    L, D = DEPTH, D_MODEL
    f32 = jnp.float32
    nrm = lambda k, shape, s: jax.random.normal(k, shape, f32) * s
    return {
        "x": nrm(ks[0], (BATCH, SEQ, D), 1.0),
        "c": nrm(ks[1], (BATCH, D), 1.0),
        "w_ada": nrm(ks[2], (L, D, N_MOD * D), 0.5 * D ** -0.5),
        "b_ada": nrm(ks[3], (L, N_MOD * D), 0.02),
        "g_norm1": 1.0 + nrm(ks[4], (L, D), 0.02),
        "g_norm2": 1.0 + nrm(ks[5], (L, D), 0.02),
        "w_in": nrm(ks[6], (L, D, IN_COLS), D ** -0.5),
        "g_q": 1.0 + nrm(ks[7], (L, HEAD_DIM), 0.02),
        "g_k": 1.0 + nrm(ks[8], (L, HEAD_DIM), 0.02),
        "conv_w": nrm(ks[9], (L, CONV_K, CONV_WIDTH), CONV_K ** -0.5),
        "conv_b": nrm(ks[10], (L, CONV_WIDTH), 0.02),
        "w_pa": nrm(ks[11], (L, ATTN_WIDTH, D), ATTN_WIDTH ** -0.5),
        "w_pb": nrm(ks[12], (L, CONV_WIDTH, D), CONV_WIDTH ** -0.5),
        "w_o": nrm(ks[13], (L, D, D), D ** -0.5),
        "w_rg": nrm(ks[14], (L, D, N_GROUPS), D ** -0.5),
        "b_rg": nrm(ks[15], (L, N_GROUPS), 0.01),
        "w_re": nrm(ks[16], (L, D, N_EXPERTS), D ** -0.5),
        "b_re": nrm(ks[17], (L, N_EXPERTS), 0.01),
        "w1": nrm(ks[18], (L, N_EXPERTS, D, D_EXPERT), D ** -0.5),
        "w3": nrm(ks[19], (L, N_EXPERTS, D, D_EXPERT), D ** -0.5),
        "w2": nrm(ks[20], (L, N_EXPERTS, D_EXPERT, D), D_EXPERT ** -0.5),
    }


def reference(x, c, w_ada, b_ada, g_norm1, g_norm2, w_in, g_q, g_k, conv_w, conv_b,
              w_pa, w_pb, w_o, w_rg, b_rg, w_re, b_re, w1, w3, w2):
    B, S, D = x.shape
    cos, sin = rope_tables(S, x.dtype)
    splits = [ATTN_WIDTH, 2 * ATTN_WIDTH, 3 * ATTN_WIDTH,
              3 * ATTN_WIDTH + CONV_WIDTH, 3 * ATTN_WIDTH + 2 * CONV_WIDTH, 3 * ATTN_WIDTH + 3 * CONV_WIDTH]
    for l in range(DEPTH):
        mod = jax.nn.silu(c) @ w_ada[l] + b_ada[l]
        sh1, sc1, ga1, sh2, sc2, ga2 = jnp.split(mod, N_MOD, axis=-1)

        h = modulate(rms_norm(x, g_norm1[l]), sh1, sc1)
        z = h @ w_in[l]
        q, k, v, xb, bg, cg, gates = jnp.split(z, splits, axis=-1)
        to_heads = lambda t: t.reshape(B, S, N_HEADS_ATTN, HEAD_DIM).transpose(0, 2, 1, 3)
        q = partial_rope(rms_norm(to_heads(q), g_q[l]), cos, sin)
        k = partial_rope(rms_norm(to_heads(k), g_k[l]), cos, sin)
        v = to_heads(v)
        y_a = moba_attention(q, k, v).transpose(0, 2, 1, 3).reshape(B, S, ATTN_WIDTH)
        y_b = short_gated_conv(xb, bg, cg, conv_w[l], conv_b[l])
        gate_a, gate_b = jnp.split(jax.nn.sigmoid(gates), N_BRANCH, axis=-1)
        merged = gate_a * (y_a @ w_pa[l]) + gate_b * (y_b @ w_pb[l])
        x = x + ga1[:, None, :] * (merged @ w_o[l])

        h2 = modulate(rms_norm(x, g_norm2[l]), sh2, sc2)
        x = x + ga2[:, None, :] * hierarchical_moe(h2, w_rg[l], b_rg[l], w_re[l], b_re[l], w1[l], w3[l], w2[l])
    return x
```

```python
import numpy as np
import concourse.bass as bass
import concourse.mybir as mybir
from concourse.bass_utils import run_bass_kernel_spmd

F32 = mybir.dt.float32
BF16 = mybir.dt.bfloat16
ALU = mybir.AluOpType
AF = mybir.ActivationFunctionType
AX = mybir.AxisListType

S_LEN = 4096
D = 1024
NT = 32
BIG = 1.0e30
MASKV = 30000.0
_STOP_AFTER = None
_NCORES = 8
_DBG = {}


class Buf:
    def __init__(self, name):
        self.name = name
        self.lw = None
        self.rd = {}
        self.dsem = None
        self.dcnt = 0


class Sched:
    def __init__(self, nc):
        self.nc = nc
        self.engs = ['pe', 'act', 'dve', 'pool', 'sp']
        self.q = {e: [] for e in self.engs}
        self.sems = []
        self.esem = {}
        for e in ['pe', 'act', 'dve', 'pool']:
            self.esem[e] = self.new_sem('s_' + e)
        self.cnt = {e: 0 for e in self.esem}
        self.seen = {e: {} for e in self.engs}
        self.bufs = []

    def new_sem(self, name):
        s = self.nc.alloc_semaphore('%s_%d' % (name, len(self.sems)))
        self.sems.append(s)
        return len(self.sems) - 1

    def buf(self, name):
        b = Buf(name)
        self.bufs.append(b)
        return b

    def _waits(self, eng, reads, writes):
        need = {}

        def add(s, v):
            if need.get(s, 0) < v:
                need[s] = v
        for b in reads:
            if b.lw is not None:
                add(*b.lw)
        for b in writes:
            if b.lw is not None:
                add(*b.lw)
            for s, v in b.rd.items():
                add(s, v)
        seen = self.seen[eng]
        out = []
        for s, v in need.items():
            if eng == 'pe' and s == self.esem['pe']:
                continue
            if seen.get(s, 0) < v:
                seen[s] = v
                out.append((s, v))
        return out

    def _commit(self, ev, reads, writes):
        s, v = ev
        for b in reads:
            if b.rd.get(s, 0) < v:
                b.rd[s] = v
        for b in writes:
            b.lw = ev
            b.rd = {}

    def op(self, eng, fns, reads=(), writes=()):
        if callable(fns):
            fns = [fns]
        waits = self._waits(eng, reads, writes)
        self.cnt[eng] += 1
        ev = (self.esem[eng], self.cnt[eng])
        self.q[eng].append((waits, fns, ev[0], 1))
        self._commit(ev, reads, writes)

    def dma(self, fn, own, reads=(), writes=(), q='sp'):
        if own.dsem is None:
            own.dsem = self.new_sem('d_' + own.name)
        waits = self._waits(q, reads, writes)
        own.dcnt += 16
        ev = (own.dsem, own.dcnt)
        self.q[q].append((waits, [fn], ev[0], 16))
        self._commit(ev, reads, writes)

    def barrier(self):
        evs = [(self.esem[e], self.cnt[e]) for e in self.esem if self.cnt[e] > 0]
        for b in self.bufs:
            if b.dsem is not None and b.dcnt > 0:
                evs.append((b.dsem, b.dcnt))
        for e in self.engs:
            seen = self.seen[e]
            waits = []
            for s, v in evs:
                if e == 'pe' and s == self.esem['pe']:
                    continue
                if seen.get(s, 0) < v:
                    seen[s] = v
                    waits.append((s, v))
            if waits:
                self.q[e].append((waits, [], None, 0))

    def emit(self):
        nc = self.nc
        sems = self.sems

        def replay(name, e):
            for waits, fns, s, inc in self.q[name]:
                for (ws, wv) in waits:
                    e.wait_ge(sems[ws], wv)
                ins = None
                for fn in fns:
                    ins = fn(e)
                if ins is not None and s is not None:
                    ins.then_inc(sems[s], inc)
        with nc.Block() as block:
            @block.tensor
            def _(e):
                replay('pe', e)

            @block.scalar
            def _(e):
                replay('act', e)

            @block.vector
            def _(e):
                replay('dve', e)

            @block.gpsimd
            def _(e):
                replay('pool', e)

            @block.sync
            def _(e):
                replay('sp', e)


class Arena:
    LO = 16640
    HI = 229376

    def __init__(self, nc):
        self.nc = nc
        self.off = self.LO
        self.n = 0

    def alloc(self, name, shape, dt):
        esz = 4 if dt == F32 else 2
        nbytes = int(np.prod(shape[1:])) * esz
        off = (self.off + 31) // 32 * 32
        assert off + nbytes <= self.HI, ("SBUF overflow", name, off, nbytes)
        self.n += 1
        t = self.nc.alloc_sbuf_tensor_at("%s_%d" % (name, self.n), list(shape), dt, offset=off)
        self.off = off + nbytes
        return t


def MM(out, lhsT, rhs, start=True, stop=True):
    return lambda e: e.matmul(out, lhsT=lhsT, rhs=rhs, start=start, stop=stop)


def TR(out, in_, ident):
    return lambda e: e.transpose(out=out, in_=in_, identity=ident)


def ACTV(out, in_, func, **kw):
    return lambda e: e.activation(out=out, in_=in_, func=func, **kw)


def TT(out, in0, in1, op):
    return lambda e: e.tensor_tensor(out=out, in0=in0, in1=in1, op=op)


def TS(out, in0, s1, s2, op0, op1=None):
    if op1 is None:
        return lambda e: e.tensor_scalar(out=out, in0=in0, scalar1=s1, scalar2=None, op0=op0)
    return lambda e: e.tensor_scalar(out=out, in0=in0, scalar1=s1, scalar2=s2, op0=op0, op1=op1)


def STT(out, in0, scalar, in1, op0, op1):
    return lambda e: e.scalar_tensor_tensor(out=out, in0=in0, scalar=scalar, in1=in1, op0=op0, op1=op1)


def CP(out, in_):
    return lambda e: e.tensor_copy(out=out, in_=in_)


def MS(ap, v):
    return lambda e: e.memset(ap, v)


def RED(out, in_, op=None):
    return lambda e: e.tensor_reduce(out=out, in_=in_, axis=AX.X, op=(op or ALU.add))


def RECIP(out, in_):
    return lambda e: e.reciprocal(out=out, in_=in_)


def MAX8(out, in_):
    return lambda e: e.max(out=out, in_=in_)


def DMA(out, in_):
    return lambda e: e.dma_start(out=out, in_=in_)


def CAST(eng, out, in_):
    if eng == 'act':
        return ACTV(out, in_, AF.Copy)
    return CP(out, in_)


def build_program(stop_after=None):
    nc = bass.Bass("TRN2", target_bir_lowering=False)
    din = lambda name, shape: nc.dram_tensor(name, list(shape), F32, kind="ExternalInput").ap()
    x_d = din("x", [S_LEN, D])
    cT_d = din("cT", [128, 8])
    wada_d = din("w_ada", [D, 6 * D])
    bada_d = din("b_ada", [1, 6 * D])
    g1_d = din("g1", [1, D])
    g2_d = din("g2", [1, D])
    win_d = din("w_in", [D, 5120])
    gqk_d = din("gqk", [128, 512])
    cs1_d = din("cs1", [128, NT * 16])
    sn2_d = din("sn2", [128, NT * 16])
    convw_d = din("convw", [128, 12])
    convb_d = din("convb", [128, 4])
    wpa_d = din("w_pa", [512, D])
    wpb_d = din("w_pb", [512, D])
    wo_d = din("w_o", [D, D])
    wr_d = din("wr", [D, 36])
    br_d = din("br", [128, 36])
    if stop_after is None:
        w1_d = din("w1", [32, D, 512])
        w3_d = din("w3", [32, D, 512])
        w2_d = din("w2", [32, 512, D])
    ident_d = din("ident", [128, 128])
    tri_d = din("tri", [128, 512])
    oneh_d = din("oneh", [16, S_LEN])
    out_d = nc.dram_tensor("out", [S_LEN, D], F32, kind="ExternalOutput").ap()
    ya_d = nc.dram_tensor("ya_scratch", [512, S_LEN], BF16, kind="ExternalOutput").ap()

    S = Sched(nc)
    A = Arena(nc)
    pbank = [nc.alloc_psum_tensor("pb%d" % i, [128, 512], F32) for i in range(8)]
    pbuf = [S.buf("pb%d" % i) for i in range(8)]

    def pbf(i):
        return pbank[i][:, :].bitcast(BF16)

    identb = A.alloc("identb", [128, 128], BF16); b_identb = S.buf("identb")
    cs1 = A.alloc("cs1", [128, NT, 16], F32); b_cs1 = S.buf("cs1")
    sn2 = A.alloc("sn2", [128, NT, 16], F32); b_sn2 = S.buf("sn2")
    trib = A.alloc("trib", [128, 2, 256], BF16); b_trib = S.buf("trib")
    modT = A.alloc("modT", [128, 32], F32); b_modT = S.buf("modT")
    gabc = A.alloc("gabc", [128, 2, D], F32); b_gabc = S.buf("gabc")
    epsb = A.alloc("epsb", [128, 1], F32); b_epsb = S.buf("epsb")
    onesf = A.alloc("onesf", [128, 128], F32); b_onesf = S.buf("onesf")
    gqk = A.alloc("gqk", [128, 512], F32); b_gqk = S.buf("gqk")
    convw = A.alloc("convw", [128, 4, 3], F32); b_convw = S.buf("convw")
    convb = A.alloc("convb", [128, 4], F32); b_convb = S.buf("convb")
    brbc = A.alloc("brbc", [128, 36], F32); b_brbc = S.buf("brbc")
    wrb = A.alloc("wrb", [128, 8, 36], BF16); b_wrb = S.buf("wrb")
    PERSIST = A.off

    stgc = A.alloc("stgc", [128, 512], F32); b_stgc = S.buf("stgc")
    stgi = A.alloc("stgi", [128, 128], F32); b_stgi = S.buf("stgi")
    stgr = A.alloc("stgr", [128, 8, 36], F32); b_stgr = S.buf("stgr")
    cT = A.alloc("cT", [128, 8], F32); b_cT = S.buf("cT")
    sT = A.alloc("sT", [128, 8], F32); b_sT = S.buf("sT")
    wst = [A.alloc("wst", [128, 8, 512], F32) for _ in range(2)]
    b_wst = [S.buf("wst%d" % i) for i in range(2)]
    modrow = A.alloc("modrow", [1, 6 * D], F32); b_modrow = S.buf("modrow")
    badar = A.alloc("badar", [1, 6 * D], F32); b_badar = S.buf("badar")
    grow = A.alloc("grow", [1, 2 * D], F32); b_grow = S.buf("grow")
    arow = A.alloc("arow", [1, 2 * D], F32); b_arow = S.buf("arow")

    S.op('pool', [MS(epsb[:], 1e-6), MS(onesf[:], 1.0)], writes=[b_epsb, b_onesf])
    S.dma(DMA(stgi[:], ident_d[:, :]), b_stgi, writes=[b_stgi])
    S.op('dve', CP(identb[:], stgi[:]), reads=[b_stgi], writes=[b_identb])
    S.dma(DMA(stgc[:], tri_d[:, :]), b_stgc, writes=[b_stgc])
    S.op('dve', CP(trib[:].rearrange("p a b -> p (a b)"), stgc[:]), reads=[b_stgc], writes=[b_trib])
    S.dma(DMA(cs1[:].rearrange("p a b -> p (a b)"), cs1_d[:, :]), b_cs1, writes=[b_cs1])
    S.dma(DMA(sn2[:].rearrange("p a b -> p (a b)"), sn2_d[:, :]), b_sn2, writes=[b_sn2])
    S.dma(DMA(gqk[:], gqk_d[:, :]), b_gqk, writes=[b_gqk])
    S.dma(DMA(convw[:].rearrange("p a b -> p (a b)"), convw_d[:, :]), b_convw, writes=[b_convw])
    S.dma(DMA(convb[:], convb_d[:, :]), b_convb, writes=[b_convb])
    S.dma(DMA(brbc[:], br_d[:, :]), b_brbc, writes=[b_brbc])
    S.dma(DMA(stgr[:], wr_d.rearrange("(kc p) n -> p kc n", p=128)), b_stgr, writes=[b_stgr])
    S.op('dve', CP(wrb[:], stgr[:]), reads=[b_stgr], writes=[b_wrb])
    S.dma(DMA(cT[:], cT_d[:, :]), b_cT, writes=[b_cT])
    S.op('act', ACTV(sT[:], cT[:], AF.Silu), reads=[b_cT], writes=[b_sT])
    S.dma(DMA(badar[:], bada_d[:, :]), b_badar, writes=[b_badar])
    S.dma(DMA(grow[0:1, 0:D], g1_d[:, :]), b_grow, writes=[b_grow])
    S.dma(DMA(grow[0:1, D:2 * D], g2_d[:, :]), b_grow, writes=[b_grow])
    for j in range(12):
        wb = j % 2
        S.dma(DMA(wst[wb][:], wada_d[:, j * 512:(j + 1) * 512].rearrange("(kc p) n -> p kc n", p=128)),
              b_wst[wb], writes=[b_wst[wb]])
        S.op('pe', [MM(pbank[0][0:1, :], sT[:, kc:kc + 1], wst[wb][:, kc, :], kc == 0, kc == 7) for kc in range(8)],
             reads=[b_sT, b_wst[wb]], writes=[pbuf[0]])
        S.op('dve', TT(modrow[0:1, j * 512:(j + 1) * 512], pbank[0][0:1, :], badar[0:1, j * 512:(j + 1) * 512], ALU.add),
             reads=[pbuf[0], b_badar], writes=[b_modrow])
    S.op('dve', STT(arow[0:1, 0:D], modrow[0:1, D:2 * D], 1.0, grow[0:1, 0:D], ALU.add, ALU.mult),
         reads=[b_modrow, b_grow], writes=[b_arow])
    S.op('dve', STT(arow[0:1, D:2 * D], modrow[0:1, 4 * D:5 * D], 1.0, grow[0:1, D:2 * D], ALU.add, ALU.mult),
         reads=[b_modrow, b_grow], writes=[b_arow])
    fns = []
    srcs = [(arow, 0), (modrow, 0), (arow, D), (modrow, 3 * D)]
    for r, (src, off) in enumerate(srcs):
        for kc in range(8):
            fns.append(MM(pbank[1][:, r * 8 + kc:r * 8 + kc + 1], src[0:1, off + kc * 128:off + (kc + 1) * 128],
                          onesf[0:1, 0:1]))
    S.op('pe', fns, reads=[b_arow, b_modrow, b_onesf], writes=[pbuf[1]])
    S.op('dve', CP(modT[:], pbank[1][:, 0:32]), reads=[pbuf[1]], writes=[b_modT])
    for gi, off in enumerate([2 * D, 5 * D]):
        for hf in range(2):
            S.op('pe', MM(pbank[2][:, :], onesf[0:1, 0:128], modrow[0:1, off + hf * 512:off + (hf + 1) * 512]),
                 reads=[b_onesf, b_modrow], writes=[pbuf[2]])
            S.op('act', ACTV(gabc[:, gi, hf * 512:(hf + 1) * 512], pbank[2][:, :], AF.Copy),
                 reads=[pbuf[2]], writes=[b_gabc])
    S.barrier()
    A.off = PERSIST
    if stop_after == '0':
        S.emit()
        return nc

    def make_norm_bufs(with_xt=True, with_junk=True):
        d = {}
        d['xt'] = [A.alloc("xt", [128, D], F32) for _ in range(2)] if with_xt else None
        d['b_xt'] = [S.buf("xt%d" % i) for i in range(2)]
        if with_junk:
            d['junk'] = A.alloc("junk", [128, D], BF16); d['b_junk'] = S.buf("junk")
        d['ss'] = [A.alloc("ss", [128, 1], F32) for _ in range(2)]
        d['b_ss'] = [S.buf("ss%d" % i) for i in range(2)]
        d['xn'] = [A.alloc("xn", [128, D], BF16) for _ in range(2)]
        d['b_xn'] = [S.buf("xn%d" % i) for i in range(2)]
        return d

    def norm_tile(nb, par, xsrc, b_xsrc, hT_dst, b_hT, col0, ptr_i=0):
        k = par % 2
        S.op('act', ACTV(nb['junk'][:], xsrc, AF.Square, scale=1.0 / 32.0, accum_out=nb['ss'][k][:]),
             reads=[b_xsrc], writes=[nb['b_junk'], nb['b_ss'][k]])
        S.op('act', ACTV(nb['ss'][k][:], nb['ss'][k][:], AF.Sqrt, bias=epsb[:]),
             reads=[nb['b_ss'][k], b_epsb], writes=[nb['b_ss'][k]])
        S.op('dve', RECIP(nb['ss'][k][:], nb['ss'][k][:]), reads=[nb['b_ss'][k]], writes=[nb['b_ss'][k]])
        S.op('dve', TS(nb['xn'][k][:], xsrc, nb['ss'][k][:, 0:1], None, ALU.mult),
             reads=[b_xsrc, nb['b_ss'][k]], writes=[nb['b_xn'][k]])
        pv = pbf(ptr_i).rearrange("p (a b) -> p a b", a=8)
        S.op('pe', [TR(pv[:, kc, :], nb['xn'][k][:, kc * 128:(kc + 1) * 128], identb[:]) for kc in range(8)],
             reads=[nb['b_xn'][k], b_identb], writes=[pbuf[ptr_i]])
        S.op('act', [ACTV(hT_dst[:, kc, :], pv[:, kc, :], AF.Identity, scale=modT[:, col0 + kc:col0 + kc + 1],
                          bias=modT[:, col0 + 8 + kc:col0 + 9 + kc]) for kc in range(8)],
             reads=[pbuf[ptr_i], b_modT], writes=[b_hT])

    engrr = [0]

    def cast_eng():
        engrr[0] += 1
        return ['dve', 'act'][engrr[0] % 2]

    KxT = A.alloc("KxT", [128, 4, S_LEN], BF16)
    b_Kx = [S.buf("Kx%d" % c) for c in range(8)]
    Vx = A.alloc("Vx", [128, NT, 4, 65], BF16)
    b_Vx = [S.buf("Vx%d" % c) for c in range(8)]
    kmT = A.alloc("kmT", [128, 4, 16], BF16)
    b_km = [S.buf("km%d" % c) for c in range(16)]
    kms = A.alloc("kms", [128, 4], F32); b_kms = S.buf("kms")
    wqkv = A.alloc("wqkv", [128, 8, 768], BF16); b_wqkv = S.buf("wqkv")
    stgA = [A.alloc("stgA", [128, 8, 256], F32) for _ in range(2)]
    b_stgA = [S.buf("stgA%d" % i) for i in range(2)]
    stgo = A.alloc("stgo", [128, S_LEN], F32); b_stgo = S.buf("stgo")
    nb = make_norm_bufs()
    hT = [A.alloc("hT", [128, 8, 512], BF16) for _ in range(2)]
    b_hT = [S.buf("hT%d" % i) for i in range(2)]
    QxT = [A.alloc("QxT", [128, 4, 512], BF16) for _ in range(2)]
    b_Qx = [S.buf("Qx%d" % i) for i in range(2)]
    sq = A.alloc("sq", [128, 512], F32); b_sq = S.buf("sq")
    ssq = [A.alloc("ssq", [128, 8], F32) for _ in range(2)]; b_ssq = [S.buf("ssq%d" % i) for i in range(2)]
    qn = [A.alloc("qn", [128, 512], F32) for _ in range(2)]
    b_qn = [S.buf("qn%d" % i) for i in range(2)]
    tA = [A.alloc("tA", [128, 8, 16], F32) for _ in range(2)]; b_tA = [S.buf("tA%d" % i) for i in range(2)]
    tB = [A.alloc("tB", [128, 8, 16], F32) for _ in range(2)]; b_tB = [S.buf("tB%d" % i) for i in range(2)]
    qkb = [A.alloc("qkb", [128, 8, 128], BF16) for _ in range(2)]
    b_qkb = [S.buf("qkb%d" % i) for i in range(2)]
    gsb = A.alloc("gsb", [128, 4, 16], F32); b_gsb = S.buf("gsb")
    mx8 = A.alloc("mx8", [128, 4, 8], F32); b_mx8 = S.buf("mx8")
    sel = A.alloc("sel", [128, 4, 16], F32); b_sel = S.buf("sel")
    mbp = [A.alloc("mbp", [128, 4, 128], BF16) for _ in range(2)]
    b_mbp = [S.buf("mbp%d" % i) for i in range(2)]
    pT = [A.alloc("pT", [128, 512], BF16) for _ in range(3)]
    b_pT = [S.buf("pT%d" % i) for i in range(3)]
    rd = A.alloc("rd", [128, 512], F32); b_rd = S.buf("rd")
    bcs = A.alloc("bcs", [128, 512], F32); b_bcs = S.buf("bcs")
    yo = [A.alloc("yo", [128, 512], BF16) for _ in range(2)]
    b_yo = [S.buf("yo%d" % i) for i in range(2)]

    S.dma(DMA(stgo[64:80, :], oneh_d[:, :]), b_stgo, writes=[b_stgo])
    S.op('dve', [CP(KxT[64:80, h, :], stgo[64:80, :]) for h in range(4)], reads=[b_stgo], writes=b_Kx)
    S.op('dve', [MS(Vx[:, :, :, 64:65], 1.0), MS(mbp[0][:], 0.0), MS(mbp[1][:], 0.0),
                  MS(qkb[0][:], 0.0), MS(qkb[1][:], 0.0)],
         writes=b_Vx + b_mbp + b_qkb)

    rot = [0]
    L = _DBG.get('lvl', 9)
    b_pg = S.buf('pg')
    for hh in range(_DBG.get('nhh', 2)):
        for part, c0 in enumerate([hh * 256, 512 + hh * 256, 1024 + hh * 256]):
            sb = part % 2
            S.dma(DMA(stgA[sb][:], win_d[:, c0:c0 + 256].rearrange("(kc p) n -> p kc n", p=128)),
                  b_stgA[sb], writes=[b_stgA[sb]])
            ce = cast_eng()
            S.op(ce, CAST(ce, wqkv[:, :, part * 256:(part + 1) * 256], stgA[sb][:]),
                 reads=[b_stgA[sb]], writes=[b_wqkv])
        NCH = _DBG.get('nch', 8)

        def stageA(c, j):
            i = 4 * c + j
            k = i % 2
            hb = c % 2
            S.dma(DMA(nb['xt'][k][:], x_d[i * 128:(i + 1) * 128, :]), nb['b_xt'][k], writes=[nb['b_xt'][k]])
            norm_tile(nb, i, nb['xt'][k][:], nb['b_xt'][k], hT[hb][:, :, j * 128:(j + 1) * 128], b_hT[hb], 0)

        def stageB(c, j):
            i = 4 * c + j
            k = i % 2
            hb = c % 2
            S.op('pe', [MM(pbank[1][:, :], hT[hb][:, kc, j * 128:(j + 1) * 128], wqkv[:, kc, 0:512], kc == 0, kc == 7)
                        for kc in range(8)], reads=[b_hT[hb], b_wqkv], writes=[pbuf[1]])
            S.op('pe', [MM(pbank[2][:, 0:256], hT[hb][:, kc, j * 128:(j + 1) * 128], wqkv[:, kc, 512:768], kc == 0, kc == 7)
                        for kc in range(8)], reads=[b_hT[hb], b_wqkv], writes=[pbuf[2]])
            S.op('act', ACTV(Vx[:, i, :, 0:64], pbank[2][:, 0:256].rearrange("p (h d) -> p h d", h=4), AF.Copy),
                 reads=[pbuf[2]], writes=[b_Vx[c]])
            S.op('act', ACTV(sq[:], pbank[1][:, :], AF.Square), reads=[pbuf[1]], writes=[b_sq])
            S.op('dve', RED(ssq[k][:], sq[:].rearrange("p (h d) -> p h d", h=8)), reads=[b_sq], writes=[b_ssq[k]])
            S.op('act', ACTV(ssq[k][:], ssq[k][:], AF.Sqrt, scale=1.0 / 64.0, bias=epsb[:]),
                 reads=[b_ssq[k], b_epsb], writes=[b_ssq[k]])
            S.op('dve', RECIP(ssq[k][:], ssq[k][:]), reads=[b_ssq[k]], writes=[b_ssq[k]])
            qv = qn[k][:].rearrange("p (h d) -> p h d", h=8)
            S.op('dve', TT(qv, pbank[1][:, :].rearrange("p (h d) -> p h d", h=8),
                           ssq[k][:, :].unsqueeze(2).to_broadcast([128, 8, 64]), ALU.mult),
                 reads=[pbuf[1], b_ssq[k]], writes=[b_qn[k]])

        def stageC(c, j):
            i = 4 * c + j
            k = i % 2
            qb = c % 2
            qv = qn[k][:].rearrange("p (h d) -> p h d", h=8)
            S.op('dve', TT(qn[k][:], qn[k][:], gqk[:], ALU.mult), reads=[b_qn[k], b_gqk], writes=[b_qn[k]])
            S.op('dve', [TT(tA[k][:], qv[:, :, 0:16], cs1[:, i, :].unsqueeze(1).to_broadcast([128, 8, 16]), ALU.mult),
                         TT(tB[k][:, :, 0:8], qv[:, :, 8:16], sn2[:, i, 0:8].unsqueeze(1).to_broadcast([128, 8, 8]), ALU.mult),
                         TT(tB[k][:, :, 8:16], qv[:, :, 0:8], sn2[:, i, 8:16].unsqueeze(1).to_broadcast([128, 8, 8]), ALU.mult)],
                 reads=[b_qn[k], b_cs1, b_sn2], writes=[b_tA[k], b_tB[k]])
            S.op('act', ACTV(qkb[k][:, :, 16:64], qv[:, :, 16:64], AF.Copy), reads=[b_qn[k]], writes=[b_qkb[k]])
            S.op('dve', TT(qkb[k][:, :, 0:16], tA[k][:], tB[k][:], ALU.add), reads=[b_tA[k], b_tB[k]], writes=[b_qkb[k]])
            ptq = pbf(0).rearrange("p (a b) -> p a b", a=8)
            S.op('pe', [TR(ptq[:, s, :], qkb[k][:, s, :], identb[:]) for s in range(8)],
                 reads=[b_qkb[k], b_identb], writes=[pbuf[0]])
            S.op('act', [ACTV(QxT[qb][0:64, h4, j * 128:(j + 1) * 128], ptq[0:64, h4, :], AF.Copy) for h4 in range(4)],
                 reads=[pbuf[0]], writes=[b_Qx[qb]])
            S.op('act', [ACTV(KxT[0:64, h4, i * 128:(i + 1) * 128], ptq[0:64, 4 + h4, :], AF.Copy) for h4 in range(4)],
                 reads=[pbuf[0]], writes=[b_Kx[c]])
            if i % 2 == 1:
                blk = i // 2
                S.op('dve', RED(kms[0:64, :], KxT[0:64, :, blk * 256:(blk + 1) * 256]),
                     reads=[b_Kx[c]], writes=[b_kms])
                S.op('dve', TS(kmT[0:64, :, blk], kms[0:64, :], 1.0 / 256.0, None, ALU.mult),
                     reads=[b_kms], writes=[b_km[blk]])

        def prep_items(c):
            seq = [(stageA, 0), (stageA, 1), (stageB, 0), (stageA, 2), (stageB, 1), (stageC, 0),
                   (stageA, 3), (stageB, 2), (stageC, 1), (stageB, 3), (stageC, 2), (stageC, 3)]
            return [(f, c, j) for f, j in seq]

        pending = prep_items(0)
        for c in range(NCH):
            hb = c % 2
            qb = c % 2
            for f, cc_, j_ in pending:
                f(cc_, j_)
            pending = prep_items(c + 1) if c + 1 < NCH else []
            for j in range(4 if _DBG.get('gate', True) else 0):
                i = 4 * c + j
                cur = i // 2
                m = j % 2
                pg = pbank[2][:, 256:320].rearrange("p (h n) -> p h n", h=4)
                fl = [MS(gsb[:, :, cur:cur + 1], BIG)] + ([MS(gsb[:, :, cur + 1:16], -BIG)] if cur < 15 else [])
                S.op('dve', fl, writes=[b_gsb])
                if cur > 0:
                    S.op('pe', [MM(pg[:, h, 0:cur], QxT[qb][0:64, h, j * 128:(j + 1) * 128], kmT[0:64, h, 0:cur])
                                for h in range(4)], reads=[b_Qx[qb]] + b_km[0:cur], writes=[b_pg])
                    S.op('dve', CP(gsb[:, :, 0:cur], pg[:, :, 0:cur]), reads=[b_pg], writes=[b_gsb])
                S.op('dve', [MAX8(mx8[:, h, :], gsb[:, h, :]) for h in range(4)], reads=[b_gsb], writes=[b_mx8])
                S.op('dve', TT(sel[:], gsb[:], mx8[:, :, 3:4].to_broadcast([128, 4, 16]), ALU.is_ge),
                     reads=[b_gsb, b_mx8], writes=[b_sel])
                S.op('dve', TS(mbp[m][:, :, 64:80], sel[:], MASKV, -MASKV, ALU.mult, ALU.add),
                     reads=[b_sel], writes=[b_mbp[m]])
                pmb = pbf(0).rearrange("p (a b) -> p a b", a=8)
                S.op('pe', [TR(pmb[:, h, :], mbp[m][:, h, :], identb[:]) for h in range(4)],
                     reads=[b_mbp[m], b_identb], writes=[pbuf[0]])
                S.op('act', [ACTV(QxT[qb][64:80, h4, j * 128:(j + 1) * 128], pmb[64:80, h4, :], AF.Copy) for h4 in range(4)],
                     reads=[pbuf[0]], writes=[b_Qx[qb]])
            nsteps = 4 * (4 * c + 4)
            stride = max(1, nsteps // (len(pending) + 1)) if pending else 0
            stepc = [0]
            for h in range(4 if _DBG.get('attn', True) else 0):
                nk = 4 * c + 4
                pyi = 6 + (h % 2)

                def cols_of(kt):
                    return (0, 512) if kt < 4 * c + 2 else (256, 512)

                def emit_S(kt, r):
                    c0, c1 = cols_of(kt)
                    S.op('pe', MM(pbank[3 + r][:, c0:c1], KxT[0:80, h, kt * 128:(kt + 1) * 128], QxT[qb][0:80, h, c0:c1]),
                         reads=[b_Kx[kt // 4], b_Qx[qb]], writes=[pbuf[3 + r]])
                rs = []
                for kt in range(nk):
                    rs.append(rot[0] % 3)
                    rot[0] += 1
                emit_S(0, rs[0])
                if nk > 1:
                    emit_S(1, rs[1])
                for kt in range(nk):
                    if kt + 2 < nk:
                        emit_S(kt + 2, rs[kt + 2])
                    r = rs[kt]
                    c0, c1 = cols_of(kt)
                    S.op('act', ACTV(pT[r][:, c0:c1], pbank[3 + r][:, c0:c1], AF.Exp, scale=0.125),
                         reads=[pbuf[3 + r]], writes=[b_pT[r]])
                    if kt >= 4 * c:
                        d0 = 0 if kt < 4 * c + 2 else 256
                        S.op('dve', TT(pT[r][:, d0:d0 + 256], pT[r][:, d0:d0 + 256], trib[:, kt % 2, :], ALU.mult),
                             reads=[b_pT[r], b_trib], writes=[b_pT[r]])
                    S.op('pe', MM(pbank[pyi][0:65, c0:c1], Vx[:, kt, h, :], pT[r][:, c0:c1], kt == 0, kt == nk - 1),
                         reads=[b_Vx[kt // 4], b_pT[r]], writes=[pbuf[pyi]])
                    stepc[0] += 1
                    if pending and _DBG.get('ilv', True) and stepc[0] % stride == 0:
                        f, cc_, j_ = pending.pop(0)
                        f(cc_, j_)
                yb = h % 2
                S.op('dve', RECIP(rd[64:65, :], pbank[pyi][64:65, :]), reads=[pbuf[pyi]], writes=[b_rd])
                S.op('pe', MM(pbank[1][0:64, :], onesf[64:65, 0:64], rd[64:65, :]),
                     reads=[b_onesf, b_rd], writes=[pbuf[1]])
                S.op('act', ACTV(bcs[0:64, :], pbank[1][0:64, :], AF.Copy), reads=[pbuf[1]], writes=[b_bcs])
                S.op('dve', TT(yo[yb][0:64, :], pbank[pyi][0:64, :], bcs[0:64, :], ALU.mult),
                     reads=[pbuf[pyi], b_bcs], writes=[b_yo[yb]])
                hg = hh * 4 + h
                S.dma(DMA(ya_d[hg * 64:(hg + 1) * 64, c * 512:(c + 1) * 512], yo[yb][0:64, :]),
                      b_yo[yb], reads=[b_yo[yb]])
    S.barrier()
    A.off = PERSIST
    if stop_after == 'A':
        S.emit()
        return nc
    wB = A.alloc("wB", [128, 8, 3584], BF16); b_wB = S.buf("wB")
    wpa = A.alloc("wpa", [128, 4, D], BF16); b_wpa = S.buf("wpa")
    wpb = A.alloc("wpb", [128, 4, D], BF16); b_wpb = S.buf("wpb")
    wo = A.alloc("wo", [128, 8, D], BF16); b_wo = S.buf("wo")
    stgB = [A.alloc("stgB", [128, 2048], F32) for _ in range(2)]
    b_stgB = [S.buf("stgB%d" % i) for i in range(2)]
    sidx = [0]

    def load_cast(dst, src_ap, shape3, b_dst, extra=None):
        k = sidx[0] % len(stgB)
        sidx[0] += 1
        a, bb = shape3
        view = stgB[k][:, 0:a * bb].rearrange("p (a b) -> p a b", a=a)
        S.dma(DMA(view, src_ap), b_stgB[k], writes=[b_stgB[k]])
        if extra is None:
            ce = cast_eng()
            S.op(ce, CAST(ce, dst, view), reads=[b_stgB[k]], writes=[b_dst])
        else:
            ex, b_ex = extra
            S.op('dve', [TT(dst[:, q, :], view[:, q, :], ex, ALU.mult) for q in range(a)],
                 reads=[b_stgB[k], b_ex], writes=[b_dst])

    for p in range(14):
        c0 = 1536 + p * 256
        load_cast(wB[:, :, p * 256:(p + 1) * 256], win_d[:, c0:c0 + 256].rearrange("(kc p) n -> p kc n", p=128),
                  (8, 256), b_wB)
    for q2 in range(2):
        load_cast(wpa[:, 2 * q2:2 * q2 + 2, :], wpa_d[q2 * 256:(q2 + 1) * 256, :].rearrange("(cc p) n -> p cc n", p=128),
                  (2, D), b_wpa)
        load_cast(wpb[:, 2 * q2:2 * q2 + 2, :], wpb_d[q2 * 256:(q2 + 1) * 256, :].rearrange("(cc p) n -> p cc n", p=128),
                  (2, D), b_wpb)
    for q4 in range(4):
        load_cast(wo[:, 2 * q4:2 * q4 + 2, :], wo_d[q4 * 256:(q4 + 1) * 256, :].rearrange("(cc p) n -> p cc n", p=128),
                  (2, D), b_wo)
    nbB = make_norm_bufs()
    hTB = A.alloc("hTB", [128, 8, 512], BF16); b_hTB = S.buf("hTB")
    yaT = A.alloc("yaT", [128, 4, 512], BF16); b_yaT = S.buf("yaT")
    xbs = A.alloc("xbs", [128, 512], F32); b_xbs = S.buf("xbs")
    bgs = A.alloc("bgs", [128, 512], F32); b_bgs = S.buf("bgs")
    u = A.alloc("u", [128, 4, 514], F32); b_u = [S.buf("u%d" % i) for i in range(4)]
    tcv = A.alloc("tcv", [128, 512], F32); b_tcv = S.buf("tcv")
    ybT = A.alloc("ybT", [128, 4, 512], BF16); b_ybT = S.buf("ybT")
    gas = A.alloc("gas", [128, 512], F32); b_gas = S.buf("gas")
    gbs = A.alloc("gbs", [128, 512], F32); b_gbs = S.buf("gbs")
    t1 = A.alloc("t1", [128, 512], F32); b_t1 = S.buf("t1")
    t2 = A.alloc("t2", [128, 512], F32); b_t2 = S.buf("t2")
    mT = A.alloc("mT", [128, 8, 512], BF16); b_mT = S.buf("mT")
    xr = [A.alloc("xr", [128, D], F32) for _ in range(2)]
    b_xr = [S.buf("xr%d" % i) for i in range(2)]
    to = A.alloc("to", [128, 512], F32); b_to = S.buf("to")
    x1t = [A.alloc("x1t", [128, D], F32) for _ in range(2)]
    b_x1t = [S.buf("x1t%d" % i) for i in range(2)]
    S.op('dve', MS(u[:], 0.0), writes=b_u)
    for c in range(8):
        for j in range(4):
            i = 4 * c + j
            k = i % 2
            S.dma(DMA(nbB['xt'][k][:], x_d[i * 128:(i + 1) * 128, :]), nbB['b_xt'][k], writes=[nbB['b_xt'][k]])
            norm_tile(nbB, i, nbB['xt'][k][:], nbB['b_xt'][k], hTB[:, :, j * 128:(j + 1) * 128], b_hTB, 0)
        S.dma(DMA(yaT[:], ya_d[:, c * 512:(c + 1) * 512].rearrange("(cc p) n -> p cc n", p=128)), b_yaT, writes=[b_yaT])
        for cc in range(4):
            for bank, col0 in [(1, 0), (2, 512), (3, 1024)]:
                S.op('pe', [MM(pbank[bank][:, :], wB[:, kc, col0 + cc * 128:col0 + (cc + 1) * 128], hTB[:, kc, :], kc == 0, kc == 7)
                            for kc in range(8)], reads=[b_wB, b_hTB], writes=[pbuf[bank]])
            S.op('act', ACTV(xbs[:], pbank[1][:, :], AF.Copy), reads=[pbuf[1]], writes=[b_xbs])
            if c > 0:
                S.op('dve', CP(u[:, cc, 0:2], u[:, cc, 512:514]), reads=[b_u[cc]], writes=[b_u[cc]])
            S.op('dve', TT(u[:, cc, 2:514], pbank[3][:, :], xbs[:], ALU.mult), reads=[pbuf[3], b_xbs], writes=[b_u[cc]])
            S.op('act', ACTV(bgs[:], pbank[2][:, :], AF.Copy), reads=[pbuf[2]], writes=[b_bgs])
            S.op('dve', TS(tcv[:], u[:, cc, 0:512], convw[:, cc, 0:1], None, ALU.mult),
                 reads=[b_u[cc], b_convw], writes=[b_tcv])
            S.op('dve', STT(tcv[:], u[:, cc, 1:513], convw[:, cc, 1:2], tcv[:], ALU.mult, ALU.add),
                 reads=[b_u[cc], b_convw, b_tcv], writes=[b_tcv])
            S.op('dve', STT(tcv[:], u[:, cc, 2:514], convw[:, cc, 2:3], tcv[:], ALU.mult, ALU.add),
                 reads=[b_u[cc], b_convw, b_tcv], writes=[b_tcv])
            S.op('dve', STT(ybT[:, cc, :], tcv[:], convb[:, cc:cc + 1], bgs[:], ALU.add, ALU.mult),
                 reads=[b_tcv, b_convb, b_bgs], writes=[b_ybT])
        for m in range(8):
            S.op('pe', [MM(pbank[4][:, :], wB[:, kc, 1536 + m * 128:1536 + (m + 1) * 128], hTB[:, kc, :], kc == 0, kc == 7)
                        for kc in range(8)], reads=[b_wB, b_hTB], writes=[pbuf[4]])
            S.op('pe', [MM(pbank[5][:, :], wB[:, kc, 2560 + m * 128:2560 + (m + 1) * 128], hTB[:, kc, :], kc == 0, kc == 7)
                        for kc in range(8)], reads=[b_wB, b_hTB], writes=[pbuf[5]])
            S.op('pe', [MM(pbank[6][:, :], wpa[:, cc, m * 128:(m + 1) * 128], yaT[:, cc, :], cc == 0, cc == 3)
                        for cc in range(4)], reads=[b_wpa, b_yaT], writes=[pbuf[6]])
            S.op('pe', [MM(pbank[7][:, :], wpb[:, cc, m * 128:(m + 1) * 128], ybT[:, cc, :], cc == 0, cc == 3)
                        for cc in range(4)], reads=[b_wpb, b_ybT], writes=[pbuf[7]])
            S.op('act', ACTV(gas[:], pbank[4][:, :], AF.Sigmoid), reads=[pbuf[4]], writes=[b_gas])
            S.op('act', ACTV(gbs[:], pbank[5][:, :], AF.Sigmoid), reads=[pbuf[5]], writes=[b_gbs])
            S.op('dve', TT(t1[:], pbank[6][:, :], gas[:], ALU.mult), reads=[pbuf[6], b_gas], writes=[b_t1])
            S.op('dve', TT(t2[:], pbank[7][:, :], gbs[:], ALU.mult), reads=[pbuf[7], b_gbs], writes=[b_t2])
            S.op('dve', TT(mT[:, m, :], t1[:], t2[:], ALU.add), reads=[b_t1, b_t2], writes=[b_mT])
        for j in range(4):
            i = 4 * c + j
            k = i % 2
            S.dma(DMA(xr[k][:], x_d[i * 128:(i + 1) * 128, :]), b_xr[k], writes=[b_xr[k]])
            for hf in range(2):
                S.op('pe', [MM(pbank[1][:, :], mT[:, m, j * 128:(j + 1) * 128], wo[:, m, hf * 512:(hf + 1) * 512], m == 0, m == 7)
                            for m in range(8)], reads=[b_mT, b_wo], writes=[pbuf[1]])
                S.op('dve', TT(to[:], pbank[1][:, :], gabc[:, 0, hf * 512:(hf + 1) * 512], ALU.mult),
                     reads=[pbuf[1], b_gabc], writes=[b_to])
                S.op('dve', TT(x1t[k][:, hf * 512:(hf + 1) * 512], to[:], xr[k][:, hf * 512:(hf + 1) * 512], ALU.add),
                     reads=[b_to, b_xr[k]], writes=[b_x1t[k]])
            S.dma(DMA(out_d[i * 128:(i + 1) * 128, :], x1t[k][:]), b_x1t[k], reads=[b_x1t[k]])
    S.barrier()
    A.off = PERSIST
    if stop_after == 'B':
        S.emit()
        return nc
    h2T = A.alloc("h2T", [128, 8, 2048], BF16); b_h2T = [S.buf("h2T%d" % i) for i in range(4)]
    acc = A.alloc("acc", [128, 16, D], F32); b_acc = [S.buf("acc%d" % i) for i in range(16)]
    cw = A.alloc("cw", [128, 16, 32], F32); b_cw = [S.buf("cw%d" % i) for i in range(16)]
    w1b = [A.alloc("w1b", [128, 8, 512], BF16) for _ in range(2)]; b_w1b = [S.buf("w1b%d" % i) for i in range(2)]
    w3b = [A.alloc("w3b", [128, 8, 512], BF16) for _ in range(2)]; b_w3b = [S.buf("w3b%d" % i) for i in range(2)]
    w2b = [A.alloc("w2b", [128, 4, D], BF16) for _ in range(2)]; b_w2b = [S.buf("w2b%d" % i) for i in range(2)]
    stgC = [A.alloc("stgC", [128, 2048], F32) for _ in range(2)]
    b_stgC = [S.buf("stgC%d" % i) for i in range(2)]
    stgB[:] = stgC
    b_stgB[:] = b_stgC
    nbC = make_norm_bufs(with_xt=True, with_junk=False)
    hidT = [A.alloc("hidT", [128, 4, 512], BF16) for _ in range(2)]; b_hid = [S.buf("hid%d" % i) for i in range(2)]
    sil = [A.alloc("sil", [128, 512], F32) for _ in range(2)]; b_sil = [S.buf("sil%d" % i) for i in range(2)]
    nbC['junk'] = hidT[0][:, :, :].rearrange("p a b -> p (a b)")[:, 0:D]
    nbC['b_junk'] = b_hid[0]
    lg = A.alloc("lg", [128, 36], F32); b_lg = S.buf("lg")
    rt = A.alloc("rt", [128, 16], F32); b_rt = S.buf("rt")
    goh = A.alloc("goh", [128, 4], F32); b_goh = S.buf("goh")
    gex = A.alloc("gex", [128, 4], F32); b_gex = S.buf("gex")
    pen = A.alloc("pen", [128, 4], F32); b_pen = S.buf("pen")
    em = A.alloc("em", [128, 32], F32); b_em = S.buf("em")
    emc = A.alloc("emc", [128, 32], F32); b_emc = S.buf("emc")
    mx8c = A.alloc("mx8c", [128, 8], F32); b_mx8c = S.buf("mx8c")
    selc = A.alloc("selc", [128, 32], F32); b_selc = S.buf("selc")
    ex = A.alloc("ex", [128, 32], F32); b_ex = S.buf("ex")
    exs = A.alloc("exs", [128, 32], F32); b_exs = S.buf("exs")
    gmax, ngmax, gsum, nm1, den, fsc = [rt[:, q:q + 1] for q in range(6)]
    PEN = 1.0e4
    for hf in range(2):
        for ti in range(16):
            i = hf * 16 + ti
            k = i % 2
            S.dma(DMA(nbC['xt'][k][:], out_d[i * 128:(i + 1) * 128, :]), nbC['b_xt'][k], writes=[nbC['b_xt'][k]])
            norm_tile(nbC, i, nbC['xt'][k][:], nbC['b_xt'][k], h2T[:, :, ti * 128:(ti + 1) * 128], b_h2T[ti // 4], 16)
            S.op('pool', MS(acc[:, ti, :], 0.0), writes=[b_acc[ti]])
            S.op('pe', [MM(pbank[1][:, 0:36], h2T[:, kc, ti * 128:(ti + 1) * 128], wrb[:, kc, :], kc == 0, kc == 7)
                        for kc in range(8)], reads=[b_h2T[ti // 4], b_wrb], writes=[pbuf[1]])
            S.op('dve', TT(lg[:], pbank[1][:, 0:36], brbc[:], ALU.add), reads=[pbuf[1], b_brbc], writes=[b_lg])
            S.op('dve', RED(gmax, lg[:, 0:4], ALU.max), reads=[b_lg], writes=[b_rt])
            S.op('dve', TS(ngmax, gmax, -1.0, None, ALU.mult), reads=[b_rt], writes=[b_rt])
            S.op('dve', TS(goh[:], lg[:, 0:4], gmax, None, ALU.is_ge), reads=[b_lg, b_rt], writes=[b_goh])
            S.op('act', ACTV(gex[:], lg[:, 0:4], AF.Exp, bias=ngmax, accum_out=gsum),
                 reads=[b_lg, b_rt], writes=[b_gex, b_rt])
            S.op('dve', RECIP(gsum, gsum), reads=[b_rt], writes=[b_rt])
            S.op('dve', TS(pen[:], goh[:], PEN, -PEN, ALU.mult, ALU.add), reads=[b_goh], writes=[b_pen])
            S.op('dve', TT(em[:].rearrange("p (g e) -> p g e", g=4), lg[:, 4:36].rearrange("p (g e) -> p g e", g=4),
                           pen[:, :].unsqueeze(2).to_broadcast([128, 4, 8]), ALU.add),
                 reads=[b_lg, b_pen], writes=[b_em])
            S.op('dve', MAX8(mx8c[:], em[:]), reads=[b_em], writes=[b_mx8c])
            S.op('dve', TS(nm1, mx8c[:, 0:1], -1.0, None, ALU.mult), reads=[b_mx8c], writes=[b_rt])
            S.op('dve', TS(selc[:], em[:], mx8c[:, 1:2], None, ALU.is_ge), reads=[b_em, b_mx8c], writes=[b_selc])
            S.op('dve', TS(emc[:], em[:], mx8c[:, 1:2], None, ALU.max), reads=[b_em, b_mx8c], writes=[b_emc])
            S.op('act', ACTV(ex[:], emc[:], AF.Exp, bias=nm1), reads=[b_emc, b_rt], writes=[b_ex])
            S.op('dve', TT(exs[:], ex[:], selc[:], ALU.mult), reads=[b_ex, b_selc], writes=[b_exs])
            S.op('dve', RED(den, exs[:]), reads=[b_exs], writes=[b_rt])
            S.op('dve', RECIP(den, den), reads=[b_rt], writes=[b_rt])
            S.op('dve', TT(fsc, den, gsum, ALU.mult), reads=[b_rt], writes=[b_rt])
            S.op('dve', TS(cw[:, ti, :], exs[:], fsc, None, ALU.mult), reads=[b_exs, b_rt], writes=[b_cw[ti]])
        for e in range(32):
            wb = e % 2
            for q2 in range(2):
                load_cast(w1b[wb][:, 4 * q2:4 * q2 + 4, :],
                          w1_d[e, q2 * 512:(q2 + 1) * 512, :].rearrange("(kc p) n -> p kc n", p=128), (4, 512), b_w1b[wb])
                load_cast(w3b[wb][:, 4 * q2:4 * q2 + 4, :],
                          w3_d[e, q2 * 512:(q2 + 1) * 512, :].rearrange("(kc p) n -> p kc n", p=128), (4, 512), b_w3b[wb])
            for q2 in range(2):
                load_cast(w2b[wb][:, 2 * q2:2 * q2 + 2, :],
                          w2_d[e, q2 * 256:(q2 + 1) * 256, :].rearrange("(fc p) n -> p fc n", p=128), (2, D), b_w2b[wb])
            for ch in range(4):
                hk = (e * 4 + ch) % 2
                for fc in range(4):
                    pb1 = 2 + (fc % 2)
                    pb3 = 4 + (fc % 2)
                    sk = fc % 2
                    S.op('pe', [MM(pbank[pb1][:, :], w1b[wb][:, kc, fc * 128:(fc + 1) * 128], h2T[:, kc, ch * 512:(ch + 1) * 512],
                                   kc == 0, kc == 7) for kc in range(8)], reads=[b_w1b[wb], b_h2T[ch]], writes=[pbuf[pb1]])
                    S.op('pe', [MM(pbank[pb3][:, :], w3b[wb][:, kc, fc * 128:(fc + 1) * 128], h2T[:, kc, ch * 512:(ch + 1) * 512],
                                   kc == 0, kc == 7) for kc in range(8)], reads=[b_w3b[wb], b_h2T[ch]], writes=[pbuf[pb3]])
                    S.op('act', ACTV(sil[sk][:], pbank[pb1][:, :], AF.Silu), reads=[pbuf[pb1]], writes=[b_sil[sk]])
                    S.op('dve', TT(hidT[hk][:, fc, :], pbank[pb3][:, :], sil[sk][:], ALU.mult),
                         reads=[pbuf[pb3], b_sil[sk]], writes=[b_hid[hk]])
                for j in range(4):
                    ti = ch * 4 + j
                    for hf2 in range(2):
                        po = 6 + hf2
                        S.op('pe', [MM(pbank[po][:, :], hidT[hk][:, fc, j * 128:(j + 1) * 128],
                                       w2b[wb][:, fc, hf2 * 512:(hf2 + 1) * 512], fc == 0, fc == 3) for fc in range(4)],
                             reads=[b_hid[hk], b_w2b[wb]], writes=[pbuf[po]])
                        S.op('dve', STT(acc[:, ti, hf2 * 512:(hf2 + 1) * 512], pbank[po][:, :], cw[:, ti, e:e + 1],
                                        acc[:, ti, hf2 * 512:(hf2 + 1) * 512], ALU.mult, ALU.add),
                             reads=[pbuf[po], b_cw[ti], b_acc[ti]], writes=[b_acc[ti]])
        for ti in range(16):
            i = hf * 16 + ti
            k = i % 2
            S.dma(DMA(nbC['xt'][k][:], out_d[i * 128:(i + 1) * 128, :]), nbC['b_xt'][k], writes=[nbC['b_xt'][k]])
            S.op('dve', TT(acc[:, ti, :], acc[:, ti, :], gabc[:, 1, :], ALU.mult), reads=[b_acc[ti], b_gabc], writes=[b_acc[ti]])
            S.op('dve', TT(acc[:, ti, :], acc[:, ti, :], nbC['xt'][k][:], ALU.add),
                 reads=[b_acc[ti], nbC['b_xt'][k]], writes=[b_acc[ti]])
            S.dma(DMA(out_d[i * 128:(i + 1) * 128, :], acc[:, ti, :]), b_acc[ti], reads=[b_acc[ti]])
    S.barrier()
    S.emit()
    return nc


def _consts():
    pos = np.arange(S_LEN, dtype=np.float32)
    inv = (np.float32(500000.0) ** (-np.arange(0, 16, 2, dtype=np.float32) / np.float32(16))).astype(np.float32)
    ang = (pos[:, None] * inv[None, :]).astype(np.float32)
    cos = np.cos(ang).astype(np.float32).reshape(NT, 128, 8).transpose(1, 0, 2)
    sin = np.sin(ang).astype(np.float32).reshape(NT, 128, 8).transpose(1, 0, 2)
    cs1 = np.concatenate([cos, cos], -1).reshape(128, NT * 16)
    sn2 = np.concatenate([-sin, sin], -1).reshape(128, NT * 16)
    kp = np.arange(128)[:, None, None]
    jj = np.arange(2)[None, :, None]
    qq = np.arange(256)[None, None, :]
    tri = (jj * 128 + kp <= qq).astype(np.float32).reshape(128, 512)
    oneh = (np.arange(S_LEN)[None, :] // 256 == np.arange(16)[:, None]).astype(np.float32)
    return dict(cs1=np.ascontiguousarray(cs1), sn2=np.ascontiguousarray(sn2), tri=tri, oneh=oneh,
                ident=np.eye(128, dtype=np.float32))


def kernel(x, c, w_ada, b_ada, g_norm1, g_norm2, w_in, g_q, g_k, conv_w, conv_b,
           w_pa, w_pb, w_o, w_rg, b_rg, w_re, b_re, w1, w3, w2):
    f = lambda a: np.ascontiguousarray(np.asarray(a, dtype=np.float32))
    x = f(x); c = f(c)
    cst = _consts()
    gqk = np.concatenate([np.tile(f(g_q)[0], 4), np.tile(f(g_k)[0], 4)])
    gqk = np.ascontiguousarray(np.broadcast_to(gqk[None, :], (128, 512)))
    convw = np.ascontiguousarray(f(conv_w)[0].reshape(3, 4, 128).transpose(2, 1, 0).reshape(128, 12))
    convb = np.ascontiguousarray(f(conv_b)[0].reshape(4, 128).T)
    wr = np.ascontiguousarray(np.concatenate([f(w_rg)[0], f(w_re)[0]], axis=1))
    br = np.concatenate([f(b_rg)[0], f(b_re)[0]])
    br = np.ascontiguousarray(np.broadcast_to(br[None, :], (128, 36)))
    shared = dict(w_ada=f(w_ada)[0], b_ada=f(b_ada)[0:1], g1=f(g_norm1)[0:1], g2=f(g_norm2)[0:1], w_in=f(w_in)[0],
                  gqk=gqk, convw=convw, convb=convb, w_pa=f(w_pa)[0], w_pb=f(w_pb)[0], w_o=f(w_o)[0],
                  wr=wr, br=br, w1=f(w1)[0], w3=f(w3)[0], w2=f(w2)[0], **cst)
    n = _NCORES
    in_maps = []
    for b in range(n):
        m = dict(shared)
        m["x"] = x[b]
        m["cT"] = np.ascontiguousarray(c[b].reshape(8, 128).T)
        in_maps.append(m)
    if _STOP_AFTER is not None:
        for m in in_maps:
            for kk in ('w1', 'w3', 'w2'):
                m.pop(kk)
    nc = build_program(_STOP_AFTER)
    res = run_bass_kernel_spmd(nc, in_maps, core_ids=list(range(n)))
    if _STOP_AFTER is not None:
        global _DBG_RES
        _DBG_RES = res.results
    out = np.stack([np.asarray(res.results[b]["out"], dtype=np.float32).reshape(S_LEN, D) for b in range(n)])
    return out
```

```python
import numpy as np
import concourse.bass as bass
import concourse.mybir as mybir
from concourse.bass_utils import run_bass_kernel_spmd

F32 = mybir.dt.float32
BF16 = mybir.dt.bfloat16
ALU = mybir.AluOpType
AF = mybir.ActivationFunctionType
AX = mybir.AxisListType

S_LEN = 4096
D = 1024
NT = 32
BIG = 1.0e30
MASKV = 30000.0
_STOP_AFTER = None
_NCORES = 8
_DBG = {}
_SPARSE = True


class Buf:
    def __init__(self, name):
        self.name = name
        self.lw = None
        self.rd = {}
        self.dsem = {}
        self.dcnt = {}


class Sched:
    def __init__(self, nc):
        self.nc = nc
        self.engs = ['pe', 'act', 'dve', 'pool', 'sp']
        self.q = {e: [] for e in self.engs}
        self.sems = []
        self.esem = {}
        for e in ['pe', 'act', 'dve', 'pool']:
            self.esem[e] = self.new_sem('s_' + e)
        self.cnt = {e: 0 for e in self.esem}
        self.seen = {e: {} for e in self.engs}
        self.bufs = []

    def new_sem(self, name):
        s = self.nc.alloc_semaphore('%s_%d' % (name, len(self.sems)))
        self.sems.append(s)
        return len(self.sems) - 1

    def buf(self, name):
        b = Buf(name)
        self.bufs.append(b)
        return b

    def _waits(self, eng, reads, writes):
        need = {}

        def add(s, v):
            if need.get(s, 0) < v:
                need[s] = v
        for b in reads:
            if b.lw is not None:
                add(*b.lw)
        for b in writes:
            if b.lw is not None:
                add(*b.lw)
            for s, v in b.rd.items():
                add(s, v)
        seen = self.seen[eng]
        out = []
        for s, v in need.items():
            if eng == 'pe' and s == self.esem['pe']:
                continue
            if seen.get(s, 0) < v:
                seen[s] = v
                out.append((s, v))
        return out

    def _commit(self, ev, reads, writes):
        s, v = ev
        for b in reads:
            if b.rd.get(s, 0) < v:
                b.rd[s] = v
        for b in writes:
            b.lw = ev
            b.rd = {}

    def op(self, eng, fns, reads=(), writes=()):
        if callable(fns):
            fns = [fns]
        waits = self._waits(eng, reads, writes)
        self.cnt[eng] += 1
        ev = (self.esem[eng], self.cnt[eng])
        self.q[eng].append((waits, fns, ev[0], 1))
        self._commit(ev, reads, writes)

    def dma(self, fn, own, reads=(), writes=(), q='sp'):
        if q not in own.dsem:
            own.dsem[q] = self.new_sem('d_' + own.name + '_' + q)
            own.dcnt[q] = 0
        waits = self._waits(q, reads, writes)
        own.dcnt[q] += 16
        ev = (own.dsem[q], own.dcnt[q])
        self.q[q].append((waits, [fn], ev[0], 16))
        self._commit(ev, reads, writes)

    def barrier(self):
        evs = [(self.esem[e], self.cnt[e]) for e in self.esem if self.cnt[e] > 0]
        for b in self.bufs:
            for qq, sm in b.dsem.items():
                evs.append((sm, b.dcnt[qq]))
        for e in self.engs:
            seen = self.seen[e]
            waits = []
            for s, v in evs:
                if e == 'pe' and s == self.esem['pe']:
                    continue
                if seen.get(s, 0) < v:
                    seen[s] = v
                    waits.append((s, v))
            if waits:
                self.q[e].append((waits, [], None, 0))

    def emit(self):
        nc = self.nc
        sems = self.sems

        def replay(name, e):
            for waits, fns, s, inc in self.q[name]:
                for (ws, wv) in waits:
                    e.wait_ge(sems[ws], wv)
                ins = None
                for fn in fns:
                    ins = fn(e)
                if ins is not None and s is not None:
                    ins.then_inc(sems[s], inc)
        with nc.Block() as block:
            @block.tensor
            def _(e):
                replay('pe', e)

            @block.scalar
            def _(e):
                replay('act', e)

            @block.vector
            def _(e):
                replay('dve', e)

            @block.gpsimd
            def _(e):
                replay('pool', e)

            @block.sync
            def _(e):
                replay('sp', e)


class Arena:
    LO = 16640
    HI = 229376

    def __init__(self, nc):
        self.nc = nc
        self.off = self.LO
        self.n = 0

    def alloc(self, name, shape, dt):
        esz = 2 if dt == BF16 else 4
        nbytes = int(np.prod(shape[1:])) * esz
        off = (self.off + 31) // 32 * 32
        assert off + nbytes <= self.HI, ("SBUF overflow", name, off, nbytes)
        self.n += 1
        t = self.nc.alloc_sbuf_tensor_at("%s_%d" % (name, self.n), list(shape), dt, offset=off)
        self.off = off + nbytes
        return t


def MM(out, lhsT, rhs, start=True, stop=True):
    return lambda e: e.matmul(out, lhsT=lhsT, rhs=rhs, start=start, stop=stop)


def TR(out, in_, ident):
    return lambda e: e.transpose(out=out, in_=in_, identity=ident)


def ACTV(out, in_, func, **kw):
    return lambda e: e.activation(out=out, in_=in_, func=func, **kw)


def TT(out, in0, in1, op):
    return lambda e: e.tensor_tensor(out=out, in0=in0, in1=in1, op=op)


def TS(out, in0, s1, s2, op0, op1=None):
    if op1 is None:
        return lambda e: e.tensor_scalar(out=out, in0=in0, scalar1=s1, scalar2=None, op0=op0)
    return lambda e: e.tensor_scalar(out=out, in0=in0, scalar1=s1, scalar2=s2, op0=op0, op1=op1)


def STT(out, in0, scalar, in1, op0, op1):
    return lambda e: e.scalar_tensor_tensor(out=out, in0=in0, scalar=scalar, in1=in1, op0=op0, op1=op1)


def CP(out, in_):
    return lambda e: e.tensor_copy(out=out, in_=in_)


def MS(ap, v):
    return lambda e: e.memset(ap, v)


def RED(out, in_, op=None):
    return lambda e: e.tensor_reduce(out=out, in_=in_, axis=AX.X, op=(op or ALU.add))


def RECIP(out, in_):
    return lambda e: e.reciprocal(out=out, in_=in_)


def MAX8(out, in_):
    return lambda e: e.max(out=out, in_=in_)


def DMA(out, in_):
    return lambda e: e.dma_start(out=out, in_=in_)


def CAST(eng, out, in_):
    if eng == 'act':
        return ACTV(out, in_, AF.Copy)
    return CP(out, in_)


def IDMA_G(out, table, idx):
    return lambda e: e.indirect_dma_start(out=out, out_offset=None, in_=table,
                                          in_offset=bass.IndirectOffsetOnAxis(ap=idx, axis=0))


def IDMA_S(table, idx, in_):
    return lambda e: e.indirect_dma_start(out=table, out_offset=bass.IndirectOffsetOnAxis(ap=idx, axis=0),
                                          in_=in_, in_offset=None)


I32 = mybir.dt.int32


def _sparse_moe(nc, S, A, G):
    pbank, pbuf, identb, b_identb = G['pbank'], G['pbuf'], G['identb'], G['b_identb']
    modT, b_modT, gabc, b_gabc = G['modT'], G['b_modT'], G['gabc'], G['b_gabc']
    epsb, b_epsb, wrb, b_wrb, brbc, b_brbc = G['epsb'], G['b_epsb'], G['wrb'], G['b_wrb'], G['brbc'], G['b_brbc']
    out_d, mc_d, w1_d, w3_d, w2_d = G['out_d'], G['mc_d'], G['w1_d'], G['w3_d'], G['w2_d']
    pbf = G['pbf']
    NBLK, BLKR = 48, 512
    xs_d = nc.dram_tensor("xs_scratch", [NBLK * BLKR, D], BF16).ap()
    ys_d = nc.dram_tensor("ys_scratch", [NBLK * BLKR, D], F32).ap()
    w1t = w1_d.rearrange("e (p k) n -> (e p) (k n)", k=8)
    w3t = w3_d.rearrange("e (p k) n -> (e p) (k n)", k=8)
    w2t = w2_d.rearrange("e (p k) n -> (e p) (k n)", k=4)
    T = lambda name, shape, dt: (A.alloc(name, shape, dt), S.buf(name))
    mc, b_mc = T("mc", [128, 225], F32)
    ltri, b_ltri = T("ltri", [128, 128], BF16)
    onesb, b_onesb = T("onesb", [128, 128], BF16)
    ustr, b_ustr = T("ustr", [128, 32], BF16)
    modTp, b_modTp = G['modTp'], G['b_modTp']
    selall, b_selall = T("selall", [128, 32, 32], F32)
    selAall, b_selAall = T("selAall", [128, 32, 32], F32)
    cwall, b_cwall = T("cwall", [128, 32, 32], F32)
    rankall, b_rankall = T("rankall", [128, 32, 32], F32)
    csum, b_csum = T("csum", [128, 32], F32)
    dallf, b_dallf = T("dallf", [128, 64], F32)
    dalli, b_dalli = T("dalli", [128, 64], I32)
    wall, b_wall = T("wall", [128, 64], F32)
    widxf, b_widxf = T("widxf", [128, NBLK], F32)
    widxi, b_widxi = T("widxi", [128, NBLK], I32)
    xt = [A.alloc("xtC", [128, D], F32) for _ in range(2)]; b_xt = [S.buf("xtC%d" % i) for i in range(2)]
    ss = [A.alloc("ssC", [128, 1], F32) for _ in range(2)]; b_ss = [S.buf("ssC%d" % i) for i in range(2)]
    junk, b_junk = T("junkC", [128, D], BF16)
    h2Tt, b_h2Tt = T("h2Tt", [128, 8, 128], BF16)
    lg, b_lg = T("lgC", [128, 36], F32)
    rt, b_rt = T("rtC", [128, 16], F32)
    goh, b_goh = T("gohC", [128, 4], F32)
    gex, b_gex = T("gexC", [128, 4], F32)
    pen, b_pen = T("penC", [128, 4], F32)
    em, b_em = T("emC", [128, 32], F32)
    emc, b_emc = T("emcC", [128, 32], F32)
    mx8c, b_mx8c = T("mx8cC", [128, 8], F32)
    ex, b_ex = T("exC", [128, 32], F32)
    exs, b_exs = T("exsC", [128, 32], F32)
    selb, b_selb = T("selbC", [128, 32], BF16)
    tm, b_tm = T("tmC", [128, 32], F32)
    tm2, b_tm2 = T("tm2C", [128, 32], F32)
    big3, b_big3 = T("big3C", [128, 48 * 32], F32)
    nblk, b_nblk = T("nblkC", [128, 32], F32)
    nbpad, b_nbpad = T("nbpadC", [128, 128], BF16)
    nbT, b_nbT = T("nbTC", [128, 128], BF16)
    pss, b_pss = T("pssC", [128, 32], F32)
    pend, b_pend = T("pendC", [128, 32], F32)
    bex, b_bex = T("bexC", [128, NBLK], F32)
    gmax, ngmax, gsum, nm1, den, fsc = [rt[:, q:q + 1] for q in range(6)]
    PEN = 1.0e4
    MARK = A.off
    xnall = A.alloc("xnall", [128, 32, D], BF16); b_xnall = [S.buf("xnall%d" % i) for i in range(32)]

    zt, b_zt = T("zt", [128, 4, D], BF16)
    S.op('pool', MS(zt[:], 0.0), writes=[b_zt])
    for b in range(NBLK):
        S.dma(DMA(xs_d[b * BLKR:(b + 1) * BLKR, :].rearrange("(s p) d -> p s d", p=128), zt[:]), b_zt, reads=[b_zt])
    S.dma(DMA(mc[:], mc_d[:, :]), b_mc, writes=[b_mc])
    S.op('dve', [CP(ltri[:], mc[:, 0:128]), CP(ustr[0:32, :], mc[0:32, 128:160])], reads=[b_mc], writes=[b_ltri, b_ustr])
    S.op('pool', [MS(onesb[:], 1.0), MS(csum[:], 0.0), MS(nbpad[:], 0.0)], writes=[b_onesb, b_csum, b_nbpad])
    for i in range(32):
        k = i % 2
        S.dma(DMA(xt[k][:], out_d[i * 128:(i + 1) * 128, :]), b_xt[k], writes=[b_xt[k]])
        S.op('act', ACTV(junk[:], xt[k][:], AF.Square, scale=1.0 / 32.0, accum_out=ss[k][:]),
             reads=[b_xt[k]], writes=[b_junk, b_ss[k]])
        S.op('act', ACTV(ss[k][:], ss[k][:], AF.Sqrt, bias=epsb[:]), reads=[b_ss[k], b_epsb], writes=[b_ss[k]])
        S.op('dve', RECIP(ss[k][:], ss[k][:]), reads=[b_ss[k]], writes=[b_ss[k]])
        S.op('dve', TS(xnall[:, i, :], xt[k][:], ss[k][:, 0:1], None, ALU.mult), reads=[b_xt[k], b_ss[k]], writes=[b_xnall[i]])
        pv = pbf(0).rearrange("p (a b) -> p a b", a=8)
        S.op('pe', [TR(pv[:, kc, :], xnall[:, i, kc * 128:(kc + 1) * 128], identb[:]) for kc in range(8)],
             reads=[b_xnall[i], b_identb], writes=[pbuf[0]])
        S.op('act', [ACTV(h2Tt[:, kc, :], pv[:, kc, :], AF.Identity, scale=modT[:, 16 + kc:17 + kc],
                          bias=modT[:, 24 + kc:25 + kc]) for kc in range(8)], reads=[pbuf[0], b_modT], writes=[b_h2Tt])
        S.op('pe', [MM(pbank[1][:, 0:36], h2Tt[:, kc, :], wrb[:, kc, :], kc == 0, kc == 7) for kc in range(8)],
             reads=[b_h2Tt, b_wrb], writes=[pbuf[1]])
        S.op('dve', TT(lg[:], pbank[1][:, 0:36], brbc[:], ALU.add), reads=[pbuf[1], b_brbc], writes=[b_lg])
        S.op('dve', RED(gmax, lg[:, 0:4], ALU.max), reads=[b_lg], writes=[b_rt])
        S.op('dve', TS(ngmax, gmax, -1.0, None, ALU.mult), reads=[b_rt], writes=[b_rt])
        S.op('dve', TS(goh[:], lg[:, 0:4], gmax, None, ALU.is_ge), reads=[b_lg, b_rt], writes=[b_goh])
        S.op('act', ACTV(gex[:], lg[:, 0:4], AF.Exp, bias=ngmax, accum_out=gsum), reads=[b_lg, b_rt], writes=[b_gex, b_rt])
        S.op('dve', RECIP(gsum, gsum), reads=[b_rt], writes=[b_rt])
        S.op('dve', TS(pen[:], goh[:], PEN, -PEN, ALU.mult, ALU.add), reads=[b_goh], writes=[b_pen])
        S.op('dve', TT(em[:].rearrange("p (g e) -> p g e", g=4), lg[:, 4:36].rearrange("p (g e) -> p g e", g=4),
                       pen[:, :].unsqueeze(2).to_broadcast([128, 4, 8]), ALU.add), reads=[b_lg, b_pen], writes=[b_em])
        S.op('dve', MAX8(mx8c[:], em[:]), reads=[b_em], writes=[b_mx8c])
        S.op('dve', TS(nm1, mx8c[:, 0:1], -1.0, None, ALU.mult), reads=[b_mx8c], writes=[b_rt])
        S.op('dve', TS(selall[:, i, :], em[:], mx8c[:, 1:2], None, ALU.is_ge), reads=[b_em, b_mx8c], writes=[b_selall])
        S.op('dve', TS(selAall[:, i, :], em[:], mx8c[:, 0:1], None, ALU.is_ge), reads=[b_em, b_mx8c], writes=[b_selAall])
        S.op('dve', TS(emc[:], em[:], mx8c[:, 1:2], None, ALU.max), reads=[b_em, b_mx8c], writes=[b_emc])
        S.op('act', ACTV(ex[:], emc[:], AF.Exp, bias=nm1), reads=[b_emc, b_rt], writes=[b_ex])
        S.op('dve', TT(exs[:], ex[:], selall[:, i, :], ALU.mult), reads=[b_ex, b_selall], writes=[b_exs])
        S.op('dve', RED(den, exs[:]), reads=[b_exs], writes=[b_rt])
        S.op('dve', RECIP(den, den), reads=[b_rt], writes=[b_rt])
        S.op('dve', TT(fsc, den, gsum, ALU.mult), reads=[b_rt], writes=[b_rt])
        S.op('dve', TS(cwall[:, i, :], exs[:], fsc, None, ALU.mult), reads=[b_exs, b_rt], writes=[b_cwall])
        S.op('dve', CP(selb[:], selall[:, i, :]), reads=[b_selall], writes=[b_selb])
        S.op('pe', MM(pbank[2][:, 0:32], ltri[:], selb[:]), reads=[b_ltri, b_selb], writes=[pbuf[2]])
        S.op('pe', MM(pbank[3][:, 0:32], onesb[:], selb[:]), reads=[b_onesb, b_selb], writes=[pbuf[3]])
        S.op('dve', TT(rankall[:, i, :], pbank[2][:, 0:32], csum[:], ALU.add), reads=[pbuf[2], b_csum], writes=[b_rankall])
        S.op('dve', TT(csum[:], pbank[3][:, 0:32], csum[:], ALU.add), reads=[pbuf[3], b_csum], writes=[b_csum])
    b3 = big3[:, 0:512].rearrange("p (e k) -> p e k", k=16)
    S.op('dve', TT(b3, csum[:, :].unsqueeze(2).to_broadcast([128, 32, 16]),
                   mc[:, 160:176].unsqueeze(1).to_broadcast([128, 32, 16]), ALU.is_gt), reads=[b_csum, b_mc], writes=[b_big3])
    S.op('dve', RED(nblk[:], b3), reads=[b_big3], writes=[b_nblk])
    S.op('dve', CP(nbpad[:, 0:32], nblk[:]), reads=[b_nblk], writes=[b_nbpad])
    pvn = pbf(0)
    S.op('pe', TR(pvn[:, 0:128], nbpad[:], identb[:]), reads=[b_nbpad, b_identb], writes=[pbuf[0]])
    S.op('act', ACTV(nbT[0:32, :], pvn[0:32, 0:128], AF.Copy), reads=[pbuf[0]], writes=[b_nbT])
    S.op('pe', MM(pbank[2][:, 0:32], nbT[0:32, :], ustr[0:32, :]), reads=[b_nbT, b_ustr], writes=[pbuf[2]])
    S.op('dve', CP(pss[:], pbank[2][:, 0:32]), reads=[pbuf[2]], writes=[b_pss])
    S.op('dve', TT(pend[:], pss[:], nblk[:], ALU.add), reads=[b_pss, b_nblk], writes=[b_pend])
    b4 = big3[:, :].rearrange("p (b e) -> p b e", e=32)
    S.op('dve', TT(b4, pend[:, :].unsqueeze(1).to_broadcast([128, NBLK, 32]),
                   mc[:, 176:224].unsqueeze(2).to_broadcast([128, NBLK, 32]), ALU.is_le), reads=[b_pend, b_mc], writes=[b_big3])
    S.op('dve', RED(bex[:], b4), reads=[b_big3], writes=[b_bex])
    S.op('dve', TS(bex[:], bex[:], 31.0, None, ALU.min), reads=[b_bex], writes=[b_bex])
    S.op('dve', TS(widxf[:], bex[:], 128.0, None, ALU.mult), reads=[b_bex], writes=[b_widxf])
    S.op('dve', TT(widxf[:], widxf[:], mc[:, 224:225].to_broadcast([128, NBLK]), ALU.add), reads=[b_widxf, b_mc], writes=[b_widxf])
    S.op('dve', CP(widxi[:], widxf[:]), reads=[b_widxf], writes=[b_widxi])
    S.op('dve', TS(pss[:], pss[:], float(BLKR), None, ALU.mult), reads=[b_pss], writes=[b_pss])
    for i in range(32):
        S.op('dve', TT(tm[:], rankall[:, i, :], pss[:], ALU.add), reads=[b_rankall, b_pss], writes=[b_tm])
        S.op('dve', TT(tm2[:], tm[:], selAall[:, i, :], ALU.mult), reads=[b_tm, b_selAall], writes=[b_tm2])
        S.op('dve', RED(dallf[:, 2 * i:2 * i + 1], tm2[:]), reads=[b_tm2], writes=[b_dallf])
        S.op('dve', TT(tm2[:], selall[:, i, :], selAall[:, i, :], ALU.subtract), reads=[b_selall, b_selAall], writes=[b_tm2])
        S.op('dve', TT(tm[:], tm[:], tm2[:], ALU.mult), reads=[b_tm, b_tm2], writes=[b_tm])
        S.op('dve', RED(dallf[:, 2 * i + 1:2 * i + 2], tm[:]), reads=[b_tm], writes=[b_dallf])
        S.op('dve', TT(tm[:], cwall[:, i, :], tm2[:], ALU.mult), reads=[b_cwall, b_tm2], writes=[b_tm])
        S.op('dve', RED(wall[:, 2 * i + 1:2 * i + 2], tm[:]), reads=[b_tm], writes=[b_wall])
        S.op('dve', TT(tm[:], cwall[:, i, :], selAall[:, i, :], ALU.mult), reads=[b_cwall, b_selAall], writes=[b_tm])
        S.op('dve', RED(wall[:, 2 * i:2 * i + 1], tm[:]), reads=[b_tm], writes=[b_wall])
    S.op('dve', CP(dalli[:], dallf[:]), reads=[b_dallf], writes=[b_dalli])
    S.barrier()
    for i in range(32):
        for kk in range(2):
            S.dma(IDMA_S(xs_d[:, :], dalli[:, 2 * i + kk:2 * i + kk + 1], xnall[:, i, :]), b_xnall[i],
                  reads=[b_xnall[i], b_dalli], q='pool')
    S.barrier()
    A.off = MARK
    w1b = [A.alloc("w1s", [128, 8, 512], BF16) for _ in range(2)]; b_w1b = [S.buf("w1s%d" % i) for i in range(2)]
    w3b = [A.alloc("w3s", [128, 8, 512], BF16) for _ in range(2)]; b_w3b = [S.buf("w3s%d" % i) for i in range(2)]
    w2b = [A.alloc("w2s", [128, 4, D], BF16) for _ in range(2)]; b_w2b = [S.buf("w2s%d" % i) for i in range(2)]
    stg = [A.alloc("stgS", [128, 4096], F32) for _ in range(2)]; b_stg = [S.buf("stgS%d" % i) for i in range(2)]
    xs = [A.alloc("xs", [128, 4, D], BF16) for _ in range(2)]; b_xs = [S.buf("xs%d" % i) for i in range(2)]
    xsT = [A.alloc("xsT", [128, 8, 512], BF16) for _ in range(2)]; b_xsT = [S.buf("xsT%d" % i) for i in range(2)]
    hidT = [A.alloc("hidS", [128, 4, 512], BF16) for _ in range(2)]; b_hid = [S.buf("hidS%d" % i) for i in range(2)]
    sil = [A.alloc("silS", [128, 512], F32) for _ in range(2)]; b_sil = [S.buf("silS%d" % i) for i in range(2)]
    ysb = [A.alloc("ysb", [128, D], F32) for _ in range(2)]; b_ysb = [S.buf("ysb%d" % i) for i in range(2)]
    yg = [A.alloc("yg", [128, D], F32) for _ in range(4)]; b_yg = [S.buf("yg%d" % i) for i in range(4)]
    sidx = [0]
    crr = [0]

    def prep(b):
        wb = b % 2
        xb = b % 2
        S.dma(DMA(xs[xb][:], xs_d[b * BLKR:(b + 1) * BLKR, :].rearrange("(s p) d -> p s d", p=128)), b_xs[xb], writes=[b_xs[xb]])
        items = []
        for (tab, dst, b_dst) in [(w1t, w1b[wb], b_w1b[wb]), (w3t, w3b[wb], b_w3b[wb]), (w2t, w2b[wb], b_w2b[wb])]:
            sk = sidx[0] % 2
            sidx[0] += 1
            S.dma(IDMA_G(stg[sk][:], tab, widxi[:, b:b + 1]), b_stg[sk], reads=[b_widxi], writes=[b_stg[sk]], q='pool')
            crr[0] += 1
            ce = ['dve', 'act'][crr[0] % 2]

            def cast_item(ce=ce, dst=dst, sk=sk, b_dst=b_dst):
                S.op(ce, CAST(ce, dst[:].rearrange("p a b -> p (a b)"), stg[sk][:]), reads=[b_stg[sk]], writes=[b_dst])
            cast_item()
        for sub in range(4):
            def tr_item(sub=sub, xb=xb):
                pv = pbf(0).rearrange("p (a b) -> p a b", a=8)
                S.op('pe', [TR(pv[:, kc, :], xs[xb][:, sub, :].rearrange("p (q k) -> p q k", k=8)[:, :, kc], identb[:])
                            for kc in range(8)], reads=[b_xs[xb], b_identb], writes=[pbuf[0]])
                S.op('act', [ACTV(xsT[xb][:, kc, sub * 128:(sub + 1) * 128], pv[:, kc, :], AF.Identity,
                                  scale=modTp[:, kc:kc + 1], bias=modTp[:, 8 + kc:9 + kc]) for kc in range(8)],
                     reads=[pbuf[0], b_modTp], writes=[b_xsT[xb]])
            items.append(tr_item)
        return items

    for it in prep(0):
        it()
    for b in range(NBLK):
        wb = b % 2
        xb = b % 2
        hk = b % 2
        nxt = prep(b + 1) if b + 1 < NBLK else []
        for fc in range(4):
            pb1 = 2 + (fc % 2)
            pb3 = 4 + (fc % 2)
            sk2 = fc % 2
            S.op('pe', [MM(pbank[pb1][:, :], w1b[wb][:, kc, :].rearrange("p (m f) -> p m f", f=4)[:, :, fc], xsT[xb][:, kc, :],
                           kc == 0, kc == 7) for kc in range(8)], reads=[b_w1b[wb], b_xsT[xb]], writes=[pbuf[pb1]])
            S.op('pe', [MM(pbank[pb3][:, :], w3b[wb][:, kc, :].rearrange("p (m f) -> p m f", f=4)[:, :, fc], xsT[xb][:, kc, :],
                           kc == 0, kc == 7) for kc in range(8)], reads=[b_w3b[wb], b_xsT[xb]], writes=[pbuf[pb3]])
            S.op('act', ACTV(sil[sk2][:], pbank[pb1][:, :], AF.Silu), reads=[pbuf[pb1]], writes=[b_sil[sk2]])
            S.op('dve', TT(hidT[hk][:, fc, :], pbank[pb3][:, :], sil[sk2][:], ALU.mult),
                 reads=[pbuf[pb3], b_sil[sk2]], writes=[b_hid[hk]])
            if nxt:
                nxt.pop(0)()
        for j in range(4):
            yk = j % 2
            for hf2 in range(2):
                po = 6 + hf2
                S.op('pe', [MM(pbank[po][:, :], hidT[hk][:, fc, j * 128:(j + 1) * 128],
                               w2b[wb][:, fc, hf2 * 512:(hf2 + 1) * 512], fc == 0, fc == 3) for fc in range(4)],
                     reads=[b_hid[hk], b_w2b[wb]], writes=[pbuf[po]])
                S.op('dve' if hf2 else 'act',
                     CAST('dve' if hf2 else 'act', ysb[yk][:, hf2 * 512:(hf2 + 1) * 512], pbank[po][:, :]),
                     reads=[pbuf[po]], writes=[b_ysb[yk]])
            r0 = b * BLKR + j * 128
            S.dma(DMA(ys_d[r0:r0 + 128, :], ysb[yk][:]), b_ysb[yk], reads=[b_ysb[yk]])
        while nxt:
            nxt.pop(0)()
    S.barrier()
    for i in range(32):
        k = i % 2
        g0, g1 = yg[2 * k], yg[2 * k + 1]
        S.dma(IDMA_G(g0[:], ys_d[:, :], dalli[:, 2 * i:2 * i + 1]), b_yg[2 * k], reads=[b_dalli], writes=[b_yg[2 * k]], q='pool')
        S.dma(IDMA_G(g1[:], ys_d[:, :], dalli[:, 2 * i + 1:2 * i + 2]), b_yg[2 * k + 1], reads=[b_dalli], writes=[b_yg[2 * k + 1]], q='pool')
        S.dma(DMA(xt[k][:], out_d[i * 128:(i + 1) * 128, :]), b_xt[k], writes=[b_xt[k]])
        S.op('dve', TS(g0[:], g0[:], wall[:, 2 * i:2 * i + 1], None, ALU.mult), reads=[b_yg[2 * k], b_wall], writes=[b_yg[2 * k]])
        S.op('dve', STT(g0[:], g1[:], wall[:, 2 * i + 1:2 * i + 2], g0[:], ALU.mult, ALU.add),
             reads=[b_yg[2 * k], b_yg[2 * k + 1], b_wall], writes=[b_yg[2 * k]])
        S.op('dve', TT(g0[:], g0[:], gabc[:, 1, :], ALU.mult), reads=[b_yg[2 * k], b_gabc], writes=[b_yg[2 * k]])
        S.op('dve', TT(g0[:], g0[:], xt[k][:], ALU.add), reads=[b_yg[2 * k], b_xt[k]], writes=[b_yg[2 * k]])
        S.dma(DMA(out_d[i * 128:(i + 1) * 128, :], g0[:]), b_yg[2 * k], reads=[b_yg[2 * k]])
    S.barrier()


def build_program(stop_after=None):
    nc = bass.Bass("TRN2", target_bir_lowering=False)
    din = lambda name, shape: nc.dram_tensor(name, list(shape), F32, kind="ExternalInput").ap()
    x_d = din("x", [S_LEN, D])
    cT_d = din("cT", [128, 8])
    wada_d = din("w_ada", [D, 6 * D])
    bada_d = din("b_ada", [1, 6 * D])
    g1_d = din("g1", [1, D])
    g2_d = din("g2", [1, D])
    win_d = din("w_in", [D, 5120])
    gqk_d = din("gqk", [128, 512])
    cs1_d = din("cs1", [128, NT * 16])
    sn2_d = din("sn2", [128, NT * 16])
    convw_d = din("convw", [128, 12])
    convb_d = din("convb", [128, 4])
    wpa_d = din("w_pa", [512, D])
    wpb_d = din("w_pb", [512, D])
    wo_d = din("w_o", [D, D])
    wr_d = din("wr", [D, 36])
    br_d = din("br", [128, 36])
    if stop_after is None:
        w1_d = din("w1", [32, D, 512])
        w3_d = din("w3", [32, D, 512])
        w2_d = din("w2", [32, 512, D])
    ident_d = din("ident", [128, 128])
    tri_d = din("tri", [128, 512])
    oneh_d = din("oneh", [16, S_LEN])
    mc_d = din("mconst", [128, 225])
    out_d = nc.dram_tensor("out", [S_LEN, D], F32, kind="ExternalOutput").ap()
    ya_d = nc.dram_tensor("ya_scratch", [512, S_LEN], BF16, kind="ExternalOutput").ap()

    S = Sched(nc)
    A = Arena(nc)
    pbank = [nc.alloc_psum_tensor("pb%d" % i, [128, 512], F32) for i in range(8)]
    pbuf = [S.buf("pb%d" % i) for i in range(8)]

    def pbf(i):
        return pbank[i][:, :].bitcast(BF16)

    identb = A.alloc("identb", [128, 128], BF16); b_identb = S.buf("identb")
    cs1 = A.alloc("cs1", [128, NT, 16], F32); b_cs1 = S.buf("cs1")
    sn2 = A.alloc("sn2", [128, NT, 16], F32); b_sn2 = S.buf("sn2")
    trib = A.alloc("trib", [128, 2, 256], BF16); b_trib = S.buf("trib")
    modT = A.alloc("modT", [128, 32], F32); b_modT = S.buf("modT")
    gabc = A.alloc("gabc", [128, 2, D], F32); b_gabc = S.buf("gabc")
    epsb = A.alloc("epsb", [128, 1], F32); b_epsb = S.buf("epsb")
    onesf = A.alloc("onesf", [128, 128], F32); b_onesf = S.buf("onesf")
    gqk = A.alloc("gqk", [128, 512], F32); b_gqk = S.buf("gqk")
    convw = A.alloc("convw", [128, 4, 3], F32); b_convw = S.buf("convw")
    convb = A.alloc("convb", [128, 4], F32); b_convb = S.buf("convb")
    brbc = A.alloc("brbc", [128, 36], F32); b_brbc = S.buf("brbc")
    wrb = A.alloc("wrb", [128, 8, 36], BF16); b_wrb = S.buf("wrb")
    modTp = A.alloc("modTp", [128, 16], F32); b_modTp = S.buf("modTp")
    PERSIST = A.off

    stgc = A.alloc("stgc", [128, 512], F32); b_stgc = S.buf("stgc")
    stgi = A.alloc("stgi", [128, 128], F32); b_stgi = S.buf("stgi")
    stgr = A.alloc("stgr", [128, 8, 36], F32); b_stgr = S.buf("stgr")
    cT = A.alloc("cT", [128, 8], F32); b_cT = S.buf("cT")
    sT = A.alloc("sT", [128, 8], F32); b_sT = S.buf("sT")
    wst = [A.alloc("wst", [128, 8, 512], F32) for _ in range(2)]
    b_wst = [S.buf("wst%d" % i) for i in range(2)]
    modrow = A.alloc("modrow", [1, 6 * D], F32); b_modrow = S.buf("modrow")
    badar = A.alloc("badar", [1, 6 * D], F32); b_badar = S.buf("badar")
    grow = A.alloc("grow", [1, 2 * D], F32); b_grow = S.buf("grow")
    arow = A.alloc("arow", [1, 2 * D], F32); b_arow = S.buf("arow")

    S.op('pool', [MS(epsb[:], 1e-6), MS(onesf[:], 1.0)], writes=[b_epsb, b_onesf])
    S.dma(DMA(stgi[:], ident_d[:, :]), b_stgi, writes=[b_stgi])
    S.op('dve', CP(identb[:], stgi[:]), reads=[b_stgi], writes=[b_identb])
    S.dma(DMA(stgc[:], tri_d[:, :]), b_stgc, writes=[b_stgc])
    S.op('dve', CP(trib[:].rearrange("p a b -> p (a b)"), stgc[:]), reads=[b_stgc], writes=[b_trib])
    S.dma(DMA(cs1[:].rearrange("p a b -> p (a b)"), cs1_d[:, :]), b_cs1, writes=[b_cs1])
    S.dma(DMA(sn2[:].rearrange("p a b -> p (a b)"), sn2_d[:, :]), b_sn2, writes=[b_sn2])
    S.dma(DMA(gqk[:], gqk_d[:, :]), b_gqk, writes=[b_gqk])
    S.dma(DMA(convw[:].rearrange("p a b -> p (a b)"), convw_d[:, :]), b_convw, writes=[b_convw])
    S.dma(DMA(convb[:], convb_d[:, :]), b_convb, writes=[b_convb])
    S.dma(DMA(brbc[:], br_d[:, :]), b_brbc, writes=[b_brbc])
    S.dma(DMA(stgr[:], wr_d.rearrange("(kc p) n -> p kc n", p=128)), b_stgr, writes=[b_stgr])
    S.op('dve', CP(wrb[:], stgr[:]), reads=[b_stgr], writes=[b_wrb])
    S.dma(DMA(cT[:], cT_d[:, :]), b_cT, writes=[b_cT])
    S.op('act', ACTV(sT[:], cT[:], AF.Silu), reads=[b_cT], writes=[b_sT])
    S.dma(DMA(badar[:], bada_d[:, :]), b_badar, writes=[b_badar])
    S.dma(DMA(grow[0:1, 0:D], g1_d[:, :]), b_grow, writes=[b_grow])
    S.dma(DMA(grow[0:1, D:2 * D], g2_d[:, :]), b_grow, writes=[b_grow])
    for j in range(12):
        wb = j % 2
        S.dma(DMA(wst[wb][:], wada_d[:, j * 512:(j + 1) * 512].rearrange("(kc p) n -> p kc n", p=128)),
              b_wst[wb], writes=[b_wst[wb]])
        S.op('pe', [MM(pbank[0][0:1, :], sT[:, kc:kc + 1], wst[wb][:, kc, :], kc == 0, kc == 7) for kc in range(8)],
             reads=[b_sT, b_wst[wb]], writes=[pbuf[0]])
        S.op('dve', TT(modrow[0:1, j * 512:(j + 1) * 512], pbank[0][0:1, :], badar[0:1, j * 512:(j + 1) * 512], ALU.add),
             reads=[pbuf[0], b_badar], writes=[b_modrow])
    S.op('dve', STT(arow[0:1, 0:D], modrow[0:1, D:2 * D], 1.0, grow[0:1, 0:D], ALU.add, ALU.mult),
         reads=[b_modrow, b_grow], writes=[b_arow])
    S.op('dve', STT(arow[0:1, D:2 * D], modrow[0:1, 4 * D:5 * D], 1.0, grow[0:1, D:2 * D], ALU.add, ALU.mult),
         reads=[b_modrow, b_grow], writes=[b_arow])
    fns = []
    srcs = [(arow, 0), (modrow, 0), (arow, D), (modrow, 3 * D)]
    for r, (src, off) in enumerate(srcs):
        for kc in range(8):
            fns.append(MM(pbank[1][:, r * 8 + kc:r * 8 + kc + 1], src[0:1, off + kc * 128:off + (kc + 1) * 128],
                          onesf[0:1, 0:1]))
    S.op('pe', fns, reads=[b_arow, b_modrow, b_onesf], writes=[pbuf[1]])
    S.op('dve', CP(modT[:], pbank[1][:, 0:32]), reads=[pbuf[1]], writes=[b_modT])
    fns = []
    for r, (src, off) in enumerate([(arow, D), (modrow, 3 * D)]):
        for kc in range(8):
            fns.append(MM(pbank[1][:, r * 8 + kc:r * 8 + kc + 1],
                          src[0:1, off:off + D].rearrange("o (p k) -> o p k", k=8)[:, :, kc], onesf[0:1, 0:1]))
    S.op('pe', fns, reads=[b_arow, b_modrow, b_onesf], writes=[pbuf[1]])
    S.op('dve', CP(modTp[:], pbank[1][:, 0:16]), reads=[pbuf[1]], writes=[b_modTp])
    for gi, off in enumerate([2 * D, 5 * D]):
        for hf in range(2):
            S.op('pe', MM(pbank[2][:, :], onesf[0:1, 0:128], modrow[0:1, off + hf * 512:off + (hf + 1) * 512]),
                 reads=[b_onesf, b_modrow], writes=[pbuf[2]])
            S.op('act', ACTV(gabc[:, gi, hf * 512:(hf + 1) * 512], pbank[2][:, :], AF.Copy),
                 reads=[pbuf[2]], writes=[b_gabc])
    S.barrier()
    A.off = PERSIST
    if stop_after == '0':
        S.emit()
        return nc

    def make_norm_bufs(with_xt=True, with_junk=True):
        d = {}
        d['xt'] = [A.alloc("xt", [128, D], F32) for _ in range(2)] if with_xt else None
        d['b_xt'] = [S.buf("xt%d" % i) for i in range(2)]
        if with_junk:
            d['junk'] = A.alloc("junk", [128, D], BF16); d['b_junk'] = S.buf("junk")
        d['ss'] = [A.alloc("ss", [128, 1], F32) for _ in range(2)]
        d['b_ss'] = [S.buf("ss%d" % i) for i in range(2)]
        d['xn'] = [A.alloc("xn", [128, D], BF16) for _ in range(2)]
        d['b_xn'] = [S.buf("xn%d" % i) for i in range(2)]
        return d

    def norm_tile(nb, par, xsrc, b_xsrc, hT_dst, b_hT, col0, ptr_i=0):
        k = par % 2
        S.op('act', ACTV(nb['junk'][:], xsrc, AF.Square, scale=1.0 / 32.0, accum_out=nb['ss'][k][:]),
             reads=[b_xsrc], writes=[nb['b_junk'], nb['b_ss'][k]])
        S.op('act', ACTV(nb['ss'][k][:], nb['ss'][k][:], AF.Sqrt, bias=epsb[:]),
             reads=[nb['b_ss'][k], b_epsb], writes=[nb['b_ss'][k]])
        S.op('dve', RECIP(nb['ss'][k][:], nb['ss'][k][:]), reads=[nb['b_ss'][k]], writes=[nb['b_ss'][k]])
        S.op('dve', TS(nb['xn'][k][:], xsrc, nb['ss'][k][:, 0:1], None, ALU.mult),
             reads=[b_xsrc, nb['b_ss'][k]], writes=[nb['b_xn'][k]])
        pv = pbf(ptr_i).rearrange("p (a b) -> p a b", a=8)
        S.op('pe', [TR(pv[:, kc, :], nb['xn'][k][:, kc * 128:(kc + 1) * 128], identb[:]) for kc in range(8)],
             reads=[nb['b_xn'][k], b_identb], writes=[pbuf[ptr_i]])
        S.op('act', [ACTV(hT_dst[:, kc, :], pv[:, kc, :], AF.Identity, scale=modT[:, col0 + kc:col0 + kc + 1],
                          bias=modT[:, col0 + 8 + kc:col0 + 9 + kc]) for kc in range(8)],
             reads=[pbuf[ptr_i], b_modT], writes=[b_hT])

    engrr = [0]

    def cast_eng():
        engrr[0] += 1
        return ['dve', 'act'][engrr[0] % 2]

    KxT = A.alloc("KxT", [128, 4, S_LEN], BF16)
    b_Kx = [S.buf("Kx%d" % c) for c in range(8)]
    Vx = A.alloc("Vx", [128, NT, 4, 65], BF16)
    b_Vx = [S.buf("Vx%d" % c) for c in range(8)]
    kmT = A.alloc("kmT", [128, 4, 16], BF16)
    b_km = [S.buf("km%d" % c) for c in range(16)]
    kms = A.alloc("kms", [128, 4], F32); b_kms = S.buf("kms")
    wqkv = A.alloc("wqkv", [128, 8, 768], BF16); b_wqkv = S.buf("wqkv")
    stgA = [A.alloc("stgA", [128, 8, 256], F32) for _ in range(2)]
    b_stgA = [S.buf("stgA%d" % i) for i in range(2)]
    stgo = A.alloc("stgo", [128, S_LEN], F32); b_stgo = S.buf("stgo")
    nb = make_norm_bufs()
    hT = [A.alloc("hT", [128, 8, 512], BF16) for _ in range(2)]
    b_hT = [S.buf("hT%d" % i) for i in range(2)]
    QxT = [A.alloc("QxT", [128, 4, 512], BF16) for _ in range(2)]
    b_Qx = [S.buf("Qx%d" % i) for i in range(2)]
    sq = A.alloc("sq", [128, 512], F32); b_sq = S.buf("sq")
    ssq = [A.alloc("ssq", [128, 8], F32) for _ in range(2)]; b_ssq = [S.buf("ssq%d" % i) for i in range(2)]
    qn = [A.alloc("qn", [128, 512], F32) for _ in range(2)]
    b_qn = [S.buf("qn%d" % i) for i in range(2)]
    tA = [A.alloc("tA", [128, 8, 16], F32) for _ in range(2)]; b_tA = [S.buf("tA%d" % i) for i in range(2)]
    tB = [A.alloc("tB", [128, 8, 16], F32) for _ in range(2)]; b_tB = [S.buf("tB%d" % i) for i in range(2)]
    qkb = [A.alloc("qkb", [128, 8, 128], BF16) for _ in range(2)]
    b_qkb = [S.buf("qkb%d" % i) for i in range(2)]
    gsb = A.alloc("gsb", [128, 4, 16], F32); b_gsb = S.buf("gsb")
    mx8 = A.alloc("mx8", [128, 4, 8], F32); b_mx8 = S.buf("mx8")
    sel = A.alloc("sel", [128, 4, 16], F32); b_sel = S.buf("sel")
    mbp = [A.alloc("mbp", [128, 4, 128], BF16) for _ in range(2)]
    b_mbp = [S.buf("mbp%d" % i) for i in range(2)]
    pT = [A.alloc("pT", [128, 512], BF16) for _ in range(3)]
    b_pT = [S.buf("pT%d" % i) for i in range(3)]
    rd = A.alloc("rd", [128, 512], F32); b_rd = S.buf("rd")
    bcs = A.alloc("bcs", [128, 512], F32); b_bcs = S.buf("bcs")
    yo = [A.alloc("yo", [128, 512], BF16) for _ in range(2)]
    b_yo = [S.buf("yo%d" % i) for i in range(2)]

    S.dma(DMA(stgo[64:80, :], oneh_d[:, :]), b_stgo, writes=[b_stgo])
    S.op('dve', [CP(KxT[64:80, h, :], stgo[64:80, :]) for h in range(4)], reads=[b_stgo], writes=b_Kx)
    S.op('dve', [MS(Vx[:, :, :, 64:65], 1.0), MS(mbp[0][:], 0.0), MS(mbp[1][:], 0.0),
                  MS(qkb[0][:], 0.0), MS(qkb[1][:], 0.0)],
         writes=b_Vx + b_mbp + b_qkb)

    rot = [0]
    L = _DBG.get('lvl', 9)
    b_pg = S.buf('pg')
    for hh in range(_DBG.get('nhh', 2)):
        for part, c0 in enumerate([hh * 256, 512 + hh * 256, 1024 + hh * 256]):
            sb = part % 2
            S.dma(DMA(stgA[sb][:], win_d[:, c0:c0 + 256].rearrange("(kc p) n -> p kc n", p=128)),
                  b_stgA[sb], writes=[b_stgA[sb]])
            ce = cast_eng()
            S.op(ce, CAST(ce, wqkv[:, :, part * 256:(part + 1) * 256], stgA[sb][:]),
                 reads=[b_stgA[sb]], writes=[b_wqkv])
        NCH = _DBG.get('nch', 8)

        def stageA(c, j):
            i = 4 * c + j
            k = i % 2
            hb = c % 2
            S.dma(DMA(nb['xt'][k][:], x_d[i * 128:(i + 1) * 128, :]), nb['b_xt'][k], writes=[nb['b_xt'][k]])
            norm_tile(nb, i, nb['xt'][k][:], nb['b_xt'][k], hT[hb][:, :, j * 128:(j + 1) * 128], b_hT[hb], 0)

        def stageB(c, j):
            i = 4 * c + j
            k = i % 2
            hb = c % 2
            S.op('pe', [MM(pbank[1][:, :], hT[hb][:, kc, j * 128:(j + 1) * 128], wqkv[:, kc, 0:512], kc == 0, kc == 7)
                        for kc in range(8)], reads=[b_hT[hb], b_wqkv], writes=[pbuf[1]])
            S.op('pe', [MM(pbank[2][:, 0:256], hT[hb][:, kc, j * 128:(j + 1) * 128], wqkv[:, kc, 512:768], kc == 0, kc == 7)
                        for kc in range(8)], reads=[b_hT[hb], b_wqkv], writes=[pbuf[2]])
            S.op('act', ACTV(Vx[:, i, :, 0:64], pbank[2][:, 0:256].rearrange("p (h d) -> p h d", h=4), AF.Copy),
                 reads=[pbuf[2]], writes=[b_Vx[c]])
            S.op('act', ACTV(sq[:], pbank[1][:, :], AF.Square), reads=[pbuf[1]], writes=[b_sq])
            S.op('dve', RED(ssq[k][:], sq[:].rearrange("p (h d) -> p h d", h=8)), reads=[b_sq], writes=[b_ssq[k]])
            S.op('act', ACTV(ssq[k][:], ssq[k][:], AF.Sqrt, scale=1.0 / 64.0, bias=epsb[:]),
                 reads=[b_ssq[k], b_epsb], writes=[b_ssq[k]])
            S.op('dve', RECIP(ssq[k][:], ssq[k][:]), reads=[b_ssq[k]], writes=[b_ssq[k]])
            qv = qn[k][:].rearrange("p (h d) -> p h d", h=8)
            S.op('dve', TT(qv, pbank[1][:, :].rearrange("p (h d) -> p h d", h=8),
                           ssq[k][:, :].unsqueeze(2).to_broadcast([128, 8, 64]), ALU.mult),
                 reads=[pbuf[1], b_ssq[k]], writes=[b_qn[k]])

        def stageC(c, j):
            i = 4 * c + j
            k = i % 2
            qb = c % 2
            qv = qn[k][:].rearrange("p (h d) -> p h d", h=8)
            S.op('dve', TT(qn[k][:], qn[k][:], gqk[:], ALU.mult), reads=[b_qn[k], b_gqk], writes=[b_qn[k]])
            S.op('dve', [TT(tA[k][:], qv[:, :, 0:16], cs1[:, i, :].unsqueeze(1).to_broadcast([128, 8, 16]), ALU.mult),
                         TT(tB[k][:, :, 0:8], qv[:, :, 8:16], sn2[:, i, 0:8].unsqueeze(1).to_broadcast([128, 8, 8]), ALU.mult),
                         TT(tB[k][:, :, 8:16], qv[:, :, 0:8], sn2[:, i, 8:16].unsqueeze(1).to_broadcast([128, 8, 8]), ALU.mult)],
                 reads=[b_qn[k], b_cs1, b_sn2], writes=[b_tA[k], b_tB[k]])
            S.op('act', ACTV(qkb[k][:, :, 16:64], qv[:, :, 16:64], AF.Copy), reads=[b_qn[k]], writes=[b_qkb[k]])
            S.op('dve', TT(qkb[k][:, :, 0:16], tA[k][:], tB[k][:], ALU.add), reads=[b_tA[k], b_tB[k]], writes=[b_qkb[k]])
            ptq = pbf(0).rearrange("p (a b) -> p a b", a=8)
            S.op('pe', [TR(ptq[:, s, :], qkb[k][:, s, :], identb[:]) for s in range(8)],
                 reads=[b_qkb[k], b_identb], writes=[pbuf[0]])
            S.op('act', [ACTV(QxT[qb][0:64, h4, j * 128:(j + 1) * 128], ptq[0:64, h4, :], AF.Copy) for h4 in range(4)],
                 reads=[pbuf[0]], writes=[b_Qx[qb]])
            S.op('act', [ACTV(KxT[0:64, h4, i * 128:(i + 1) * 128], ptq[0:64, 4 + h4, :], AF.Copy) for h4 in range(4)],
                 reads=[pbuf[0]], writes=[b_Kx[c]])
            if i % 2 == 1:
                blk = i // 2
                S.op('dve', RED(kms[0:64, :], KxT[0:64, :, blk * 256:(blk + 1) * 256]),
                     reads=[b_Kx[c]], writes=[b_kms])
                S.op('dve', TS(kmT[0:64, :, blk], kms[0:64, :], 1.0 / 256.0, None, ALU.mult),
                     reads=[b_kms], writes=[b_km[blk]])

        def prep_items(c):
            seq = [(stageA, 0), (stageA, 1), (stageB, 0), (stageA, 2), (stageB, 1), (stageC, 0),
                   (stageA, 3), (stageB, 2), (stageC, 1), (stageB, 3), (stageC, 2), (stageC, 3)]
            return [(f, c, j) for f, j in seq]

        pending = prep_items(0)
        for c in range(NCH):
            hb = c % 2
            qb = c % 2
            for f, cc_, j_ in pending:
                f(cc_, j_)
            pending = prep_items(c + 1) if c + 1 < NCH else []
            for j in range(4 if _DBG.get('gate', True) else 0):
                i = 4 * c + j
                cur = i // 2
                m = j % 2
                pg = pbank[2][:, 256:320].rearrange("p (h n) -> p h n", h=4)
                fl = [MS(gsb[:, :, cur:cur + 1], BIG)] + ([MS(gsb[:, :, cur + 1:16], -BIG)] if cur < 15 else [])
                S.op('dve', fl, writes=[b_gsb])
                if cur > 0:
                    S.op('pe', [MM(pg[:, h, 0:cur], QxT[qb][0:64, h, j * 128:(j + 1) * 128], kmT[0:64, h, 0:cur])
                                for h in range(4)], reads=[b_Qx[qb]] + b_km[0:cur], writes=[b_pg])
                    S.op('dve', CP(gsb[:, :, 0:cur], pg[:, :, 0:cur]), reads=[b_pg], writes=[b_gsb])
                S.op('dve', [MAX8(mx8[:, h, :], gsb[:, h, :]) for h in range(4)], reads=[b_gsb], writes=[b_mx8])
                S.op('dve', TT(sel[:], gsb[:], mx8[:, :, 3:4].to_broadcast([128, 4, 16]), ALU.is_ge),
                     reads=[b_gsb, b_mx8], writes=[b_sel])
                S.op('dve', TS(mbp[m][:, :, 64:80], sel[:], MASKV, -MASKV, ALU.mult, ALU.add),
                     reads=[b_sel], writes=[b_mbp[m]])
                pmb = pbf(0).rearrange("p (a b) -> p a b", a=8)
                S.op('pe', [TR(pmb[:, h, :], mbp[m][:, h, :], identb[:]) for h in range(4)],
                     reads=[b_mbp[m], b_identb], writes=[pbuf[0]])
                S.op('act', [ACTV(QxT[qb][64:80, h4, j * 128:(j + 1) * 128], pmb[64:80, h4, :], AF.Copy) for h4 in range(4)],
                     reads=[pbuf[0]], writes=[b_Qx[qb]])
            nsteps = 4 * (4 * c + 4)
            stride = max(1, nsteps // (len(pending) + 1)) if pending else 0
            stepc = [0]
            for h in range(4 if _DBG.get('attn', True) else 0):
                nk = 4 * c + 4
                pyi = 6 + (h % 2)

                def cols_of(kt):
                    return (0, 512) if kt < 4 * c + 2 else (256, 512)

                def emit_S(kt, r):
                    c0, c1 = cols_of(kt)
                    S.op('pe', MM(pbank[3 + r][:, c0:c1], KxT[0:80, h, kt * 128:(kt + 1) * 128], QxT[qb][0:80, h, c0:c1]),
                         reads=[b_Kx[kt // 4], b_Qx[qb]], writes=[pbuf[3 + r]])
                rs = []
                for kt in range(nk):
                    rs.append(rot[0] % 3)
                    rot[0] += 1
                emit_S(0, rs[0])
                if nk > 1:
                    emit_S(1, rs[1])
                for kt in range(nk):
                    if kt + 2 < nk:
                        emit_S(kt + 2, rs[kt + 2])
                    r = rs[kt]
                    c0, c1 = cols_of(kt)
                    S.op('act', ACTV(pT[r][:, c0:c1], pbank[3 + r][:, c0:c1], AF.Exp, scale=0.125),
                         reads=[pbuf[3 + r]], writes=[b_pT[r]])
                    if kt >= 4 * c:
                        d0 = 0 if kt < 4 * c + 2 else 256
                        S.op('dve', TT(pT[r][:, d0:d0 + 256], pT[r][:, d0:d0 + 256], trib[:, kt % 2, :], ALU.mult),
                             reads=[b_pT[r], b_trib], writes=[b_pT[r]])
                    S.op('pe', MM(pbank[pyi][0:65, c0:c1], Vx[:, kt, h, :], pT[r][:, c0:c1], kt == 0, kt == nk - 1),
                         reads=[b_Vx[kt // 4], b_pT[r]], writes=[pbuf[pyi]])
                    stepc[0] += 1
                    if pending and _DBG.get('ilv', True) and stepc[0] % stride == 0:
                        f, cc_, j_ = pending.pop(0)
                        f(cc_, j_)
                yb = h % 2
                S.op('dve', RECIP(rd[64:65, :], pbank[pyi][64:65, :]), reads=[pbuf[pyi]], writes=[b_rd])
                S.op('pe', MM(pbank[1][0:64, :], onesf[64:65, 0:64], rd[64:65, :]),
                     reads=[b_onesf, b_rd], writes=[pbuf[1]])
                S.op('act', ACTV(bcs[0:64, :], pbank[1][0:64, :], AF.Copy), reads=[pbuf[1]], writes=[b_bcs])
                S.op('dve', TT(yo[yb][0:64, :], pbank[pyi][0:64, :], bcs[0:64, :], ALU.mult),
                     reads=[pbuf[pyi], b_bcs], writes=[b_yo[yb]])
                hg = hh * 4 + h
                S.dma(DMA(ya_d[hg * 64:(hg + 1) * 64, c * 512:(c + 1) * 512], yo[yb][0:64, :]),
                      b_yo[yb], reads=[b_yo[yb]])
    S.barrier()
    A.off = PERSIST
    if stop_after == 'A':
        S.emit()
        return nc
    wB = A.alloc("wB", [128, 8, 3584], BF16); b_wB = S.buf("wB")
    wpa = A.alloc("wpa", [128, 4, D], BF16); b_wpa = S.buf("wpa")
    wpb = A.alloc("wpb", [128, 4, D], BF16); b_wpb = S.buf("wpb")
    wo = A.alloc("wo", [128, 8, D], BF16); b_wo = S.buf("wo")
    stgB = [A.alloc("stgB", [128, 2048], F32) for _ in range(2)]
    b_stgB = [S.buf("stgB%d" % i) for i in range(2)]
    sidx = [0]

    def load_cast(dst, src_ap, shape3, b_dst, extra=None):
        k = sidx[0] % len(stgB)
        sidx[0] += 1
        a, bb = shape3
        view = stgB[k][:, 0:a * bb].rearrange("p (a b) -> p a b", a=a)
        S.dma(DMA(view, src_ap), b_stgB[k], writes=[b_stgB[k]])
        if extra is None:
            ce = cast_eng()
            S.op(ce, CAST(ce, dst, view), reads=[b_stgB[k]], writes=[b_dst])
        else:
            ex, b_ex = extra
            S.op('dve', [TT(dst[:, q, :], view[:, q, :], ex, ALU.mult) for q in range(a)],
                 reads=[b_stgB[k], b_ex], writes=[b_dst])

    for p in range(14):
        c0 = 1536 + p * 256
        load_cast(wB[:, :, p * 256:(p + 1) * 256], win_d[:, c0:c0 + 256].rearrange("(kc p) n -> p kc n", p=128),
                  (8, 256), b_wB)
    for q2 in range(2):
        load_cast(wpa[:, 2 * q2:2 * q2 + 2, :], wpa_d[q2 * 256:(q2 + 1) * 256, :].rearrange("(cc p) n -> p cc n", p=128),
                  (2, D), b_wpa)
        load_cast(wpb[:, 2 * q2:2 * q2 + 2, :], wpb_d[q2 * 256:(q2 + 1) * 256, :].rearrange("(cc p) n -> p cc n", p=128),
                  (2, D), b_wpb)
    for q4 in range(4):
        load_cast(wo[:, 2 * q4:2 * q4 + 2, :], wo_d[q4 * 256:(q4 + 1) * 256, :].rearrange("(cc p) n -> p cc n", p=128),
                  (2, D), b_wo)
    nbB = make_norm_bufs()
    hTB = A.alloc("hTB", [128, 8, 512], BF16); b_hTB = S.buf("hTB")
    yaT = A.alloc("yaT", [128, 4, 512], BF16); b_yaT = S.buf("yaT")
    xbs = A.alloc("xbs", [128, 512], F32); b_xbs = S.buf("xbs")
    bgs = A.alloc("bgs", [128, 512], F32); b_bgs = S.buf("bgs")
    u = A.alloc("u", [128, 4, 514], F32); b_u = [S.buf("u%d" % i) for i in range(4)]
    tcv = A.alloc("tcv", [128, 512], F32); b_tcv = S.buf("tcv")
    ybT = A.alloc("ybT", [128, 4, 512], BF16); b_ybT = S.buf("ybT")
    gas = A.alloc("gas", [128, 512], F32); b_gas = S.buf("gas")
    gbs = A.alloc("gbs", [128, 512], F32); b_gbs = S.buf("gbs")
    t1 = A.alloc("t1", [128, 512], F32); b_t1 = S.buf("t1")
    t2 = A.alloc("t2", [128, 512], F32); b_t2 = S.buf("t2")
    mT = A.alloc("mT", [128, 8, 512], BF16); b_mT = S.buf("mT")
    xr = [A.alloc("xr", [128, D], F32) for _ in range(2)]
    b_xr = [S.buf("xr%d" % i) for i in range(2)]
    to = A.alloc("to", [128, 512], F32); b_to = S.buf("to")
    x1t = [A.alloc("x1t", [128, D], F32) for _ in range(2)]
    b_x1t = [S.buf("x1t%d" % i) for i in range(2)]
    S.op('dve', MS(u[:], 0.0), writes=b_u)
    for c in range(8):
        for j in range(4):
            i = 4 * c + j
            k = i % 2
            S.dma(DMA(nbB['xt'][k][:], x_d[i * 128:(i + 1) * 128, :]), nbB['b_xt'][k], writes=[nbB['b_xt'][k]])
            norm_tile(nbB, i, nbB['xt'][k][:], nbB['b_xt'][k], hTB[:, :, j * 128:(j + 1) * 128], b_hTB, 0)
        S.dma(DMA(yaT[:], ya_d[:, c * 512:(c + 1) * 512].rearrange("(cc p) n -> p cc n", p=128)), b_yaT, writes=[b_yaT])
        for cc in range(4):
            for bank, col0 in [(1, 0), (2, 512), (3, 1024)]:
                S.op('pe', [MM(pbank[bank][:, :], wB[:, kc, col0 + cc * 128:col0 + (cc + 1) * 128], hTB[:, kc, :], kc == 0, kc == 7)
                            for kc in range(8)], reads=[b_wB, b_hTB], writes=[pbuf[bank]])
            S.op('act', ACTV(xbs[:], pbank[1][:, :], AF.Copy), reads=[pbuf[1]], writes=[b_xbs])
            if c > 0:
                S.op('dve', CP(u[:, cc, 0:2], u[:, cc, 512:514]), reads=[b_u[cc]], writes=[b_u[cc]])
            S.op('dve', TT(u[:, cc, 2:514], pbank[3][:, :], xbs[:], ALU.mult), reads=[pbuf[3], b_xbs], writes=[b_u[cc]])
            S.op('act', ACTV(bgs[:], pbank[2][:, :], AF.Copy), reads=[pbuf[2]], writes=[b_bgs])
            S.op('dve', TS(tcv[:], u[:, cc, 0:512], convw[:, cc, 0:1], None, ALU.mult),
                 reads=[b_u[cc], b_convw], writes=[b_tcv])
            S.op('dve', STT(tcv[:], u[:, cc, 1:513], convw[:, cc, 1:2], tcv[:], ALU.mult, ALU.add),
                 reads=[b_u[cc], b_convw, b_tcv], writes=[b_tcv])
            S.op('dve', STT(tcv[:], u[:, cc, 2:514], convw[:, cc, 2:3], tcv[:], ALU.mult, ALU.add),
                 reads=[b_u[cc], b_convw, b_tcv], writes=[b_tcv])
            S.op('dve', STT(ybT[:, cc, :], tcv[:], convb[:, cc:cc + 1], bgs[:], ALU.add, ALU.mult),
                 reads=[b_tcv, b_convb, b_bgs], writes=[b_ybT])
        for m in range(8):
            S.op('pe', [MM(pbank[4][:, :], wB[:, kc, 1536 + m * 128:1536 + (m + 1) * 128], hTB[:, kc, :], kc == 0, kc == 7)
                        for kc in range(8)], reads=[b_wB, b_hTB], writes=[pbuf[4]])
            S.op('pe', [MM(pbank[5][:, :], wB[:, kc, 2560 + m * 128:2560 + (m + 1) * 128], hTB[:, kc, :], kc == 0, kc == 7)
                        for kc in range(8)], reads=[b_wB, b_hTB], writes=[pbuf[5]])
            S.op('pe', [MM(pbank[6][:, :], wpa[:, cc, m * 128:(m + 1) * 128], yaT[:, cc, :], cc == 0, cc == 3)
                        for cc in range(4)], reads=[b_wpa, b_yaT], writes=[pbuf[6]])
            S.op('pe', [MM(pbank[7][:, :], wpb[:, cc, m * 128:(m + 1) * 128], ybT[:, cc, :], cc == 0, cc == 3)
                        for cc in range(4)], reads=[b_wpb, b_ybT], writes=[pbuf[7]])
            S.op('act', ACTV(gas[:], pbank[4][:, :], AF.Sigmoid), reads=[pbuf[4]], writes=[b_gas])
            S.op('act', ACTV(gbs[:], pbank[5][:, :], AF.Sigmoid), reads=[pbuf[5]], writes=[b_gbs])
            S.op('dve', TT(t1[:], pbank[6][:, :], gas[:], ALU.mult), reads=[pbuf[6], b_gas], writes=[b_t1])
            S.op('dve', TT(t2[:], pbank[7][:, :], gbs[:], ALU.mult), reads=[pbuf[7], b_gbs], writes=[b_t2])
            S.op('dve', TT(mT[:, m, :], t1[:], t2[:], ALU.add), reads=[b_t1, b_t2], writes=[b_mT])
        for j in range(4):
            i = 4 * c + j
            k = i % 2
            S.dma(DMA(xr[k][:], x_d[i * 128:(i + 1) * 128, :]), b_xr[k], writes=[b_xr[k]])
            for hf in range(2):
                S.op('pe', [MM(pbank[1][:, :], mT[:, m, j * 128:(j + 1) * 128], wo[:, m, hf * 512:(hf + 1) * 512], m == 0, m == 7)
                            for m in range(8)], reads=[b_mT, b_wo], writes=[pbuf[1]])
                S.op('dve', TT(to[:], pbank[1][:, :], gabc[:, 0, hf * 512:(hf + 1) * 512], ALU.mult),
                     reads=[pbuf[1], b_gabc], writes=[b_to])
                S.op('dve', TT(x1t[k][:, hf * 512:(hf + 1) * 512], to[:], xr[k][:, hf * 512:(hf + 1) * 512], ALU.add),
                     reads=[b_to, b_xr[k]], writes=[b_x1t[k]])
            S.dma(DMA(out_d[i * 128:(i + 1) * 128, :], x1t[k][:]), b_x1t[k], reads=[b_x1t[k]])
    S.barrier()
    A.off = PERSIST
    if stop_after == 'B':
        S.emit()
        return nc
    if _SPARSE:
        _sparse_moe(nc, S, A, locals())
        S.emit()
        return nc
    h2T = A.alloc("h2T", [128, 8, 2048], BF16); b_h2T = [S.buf("h2T%d" % i) for i in range(4)]
    acc = A.alloc("acc", [128, 16, D], F32); b_acc = [S.buf("acc%d" % i) for i in range(16)]
    cw = A.alloc("cw", [128, 16, 32], F32); b_cw = [S.buf("cw%d" % i) for i in range(16)]
    w1b = [A.alloc("w1b", [128, 8, 512], BF16) for _ in range(2)]; b_w1b = [S.buf("w1b%d" % i) for i in range(2)]
    w3b = [A.alloc("w3b", [128, 8, 512], BF16) for _ in range(2)]; b_w3b = [S.buf("w3b%d" % i) for i in range(2)]
    w2b = [A.alloc("w2b", [128, 4, D], BF16) for _ in range(2)]; b_w2b = [S.buf("w2b%d" % i) for i in range(2)]
    stgC = [A.alloc("stgC", [128, 2048], F32) for _ in range(2)]
    b_stgC = [S.buf("stgC%d" % i) for i in range(2)]
    stgB[:] = stgC
    b_stgB[:] = b_stgC
    nbC = make_norm_bufs(with_xt=True, with_junk=False)
    hidT = [A.alloc("hidT", [128, 4, 512], BF16) for _ in range(2)]; b_hid = [S.buf("hid%d" % i) for i in range(2)]
    sil = [A.alloc("sil", [128, 512], F32) for _ in range(2)]; b_sil = [S.buf("sil%d" % i) for i in range(2)]
    nbC['junk'] = hidT[0][:, :, :].rearrange("p a b -> p (a b)")[:, 0:D]
    nbC['b_junk'] = b_hid[0]
    lg = A.alloc("lg", [128, 36], F32); b_lg = S.buf("lg")
    rt = A.alloc("rt", [128, 16], F32); b_rt = S.buf("rt")
    goh = A.alloc("goh", [128, 4], F32); b_goh = S.buf("goh")
    gex = A.alloc("gex", [128, 4], F32); b_gex = S.buf("gex")
    pen = A.alloc("pen", [128, 4], F32); b_pen = S.buf("pen")
    em = A.alloc("em", [128, 32], F32); b_em = S.buf("em")
    emc = A.alloc("emc", [128, 32], F32); b_emc = S.buf("emc")
    mx8c = A.alloc("mx8c", [128, 8], F32); b_mx8c = S.buf("mx8c")
    selc = A.alloc("selc", [128, 32], F32); b_selc = S.buf("selc")
    ex = A.alloc("ex", [128, 32], F32); b_ex = S.buf("ex")
    exs = A.alloc("exs", [128, 32], F32); b_exs = S.buf("exs")
    gmax, ngmax, gsum, nm1, den, fsc = [rt[:, q:q + 1] for q in range(6)]
    PEN = 1.0e4
    for hf in range(2):
        for ti in range(16):
            i = hf * 16 + ti
            k = i % 2
            S.dma(DMA(nbC['xt'][k][:], out_d[i * 128:(i + 1) * 128, :]), nbC['b_xt'][k], writes=[nbC['b_xt'][k]])
            norm_tile(nbC, i, nbC['xt'][k][:], nbC['b_xt'][k], h2T[:, :, ti * 128:(ti + 1) * 128], b_h2T[ti // 4], 16)
            S.op('pool', MS(acc[:, ti, :], 0.0), writes=[b_acc[ti]])
            S.op('pe', [MM(pbank[1][:, 0:36], h2T[:, kc, ti * 128:(ti + 1) * 128], wrb[:, kc, :], kc == 0, kc == 7)
                        for kc in range(8)], reads=[b_h2T[ti // 4], b_wrb], writes=[pbuf[1]])
            S.op('dve', TT(lg[:], pbank[1][:, 0:36], brbc[:], ALU.add), reads=[pbuf[1], b_brbc], writes=[b_lg])
            S.op('dve', RED(gmax, lg[:, 0:4], ALU.max), reads=[b_lg], writes=[b_rt])
            S.op('dve', TS(ngmax, gmax, -1.0, None, ALU.mult), reads=[b_rt], writes=[b_rt])
            S.op('dve', TS(goh[:], lg[:, 0:4], gmax, None, ALU.is_ge), reads=[b_lg, b_rt], writes=[b_goh])
            S.op('act', ACTV(gex[:], lg[:, 0:4], AF.Exp, bias=ngmax, accum_out=gsum),
                 reads=[b_lg, b_rt], writes=[b_gex, b_rt])
            S.op('dve', RECIP(gsum, gsum), reads=[b_rt], writes=[b_rt])
            S.op('dve', TS(pen[:], goh[:], PEN, -PEN, ALU.mult, ALU.add), reads=[b_goh], writes=[b_pen])
            S.op('dve', TT(em[:].rearrange("p (g e) -> p g e", g=4), lg[:, 4:36].rearrange("p (g e) -> p g e", g=4),
                           pen[:, :].unsqueeze(2).to_broadcast([128, 4, 8]), ALU.add),
                 reads=[b_lg, b_pen], writes=[b_em])
            S.op('dve', MAX8(mx8c[:], em[:]), reads=[b_em], writes=[b_mx8c])
            S.op('dve', TS(nm1, mx8c[:, 0:1], -1.0, None, ALU.mult), reads=[b_mx8c], writes=[b_rt])
            S.op('dve', TS(selc[:], em[:], mx8c[:, 1:2], None, ALU.is_ge), reads=[b_em, b_mx8c], writes=[b_selc])
            S.op('dve', TS(emc[:], em[:], mx8c[:, 1:2], None, ALU.max), reads=[b_em, b_mx8c], writes=[b_emc])
            S.op('act', ACTV(ex[:], emc[:], AF.Exp, bias=nm1), reads=[b_emc, b_rt], writes=[b_ex])
            S.op('dve', TT(exs[:], ex[:], selc[:], ALU.mult), reads=[b_ex, b_selc], writes=[b_exs])
            S.op('dve', RED(den, exs[:]), reads=[b_exs], writes=[b_rt])
            S.op('dve', RECIP(den, den), reads=[b_rt], writes=[b_rt])
            S.op('dve', TT(fsc, den, gsum, ALU.mult), reads=[b_rt], writes=[b_rt])
            S.op('dve', TS(cw[:, ti, :], exs[:], fsc, None, ALU.mult), reads=[b_exs, b_rt], writes=[b_cw[ti]])
        for e in range(32):
            wb = e % 2
            for q2 in range(2):
                load_cast(w1b[wb][:, 4 * q2:4 * q2 + 4, :],
                          w1_d[e, q2 * 512:(q2 + 1) * 512, :].rearrange("(kc p) n -> p kc n", p=128), (4, 512), b_w1b[wb])
                load_cast(w3b[wb][:, 4 * q2:4 * q2 + 4, :],
                          w3_d[e, q2 * 512:(q2 + 1) * 512, :].rearrange("(kc p) n -> p kc n", p=128), (4, 512), b_w3b[wb])
            for q2 in range(2):
                load_cast(w2b[wb][:, 2 * q2:2 * q2 + 2, :],
                          w2_d[e, q2 * 256:(q2 + 1) * 256, :].rearrange("(fc p) n -> p fc n", p=128), (2, D), b_w2b[wb])
            for ch in range(4):
                hk = (e * 4 + ch) % 2
                for fc in range(4):
                    pb1 = 2 + (fc % 2)
                    pb3 = 4 + (fc % 2)
                    sk = fc % 2
                    S.op('pe', [MM(pbank[pb1][:, :], w1b[wb][:, kc, fc * 128:(fc + 1) * 128], h2T[:, kc, ch * 512:(ch + 1) * 512],
                                   kc == 0, kc == 7) for kc in range(8)], reads=[b_w1b[wb], b_h2T[ch]], writes=[pbuf[pb1]])
                    S.op('pe', [MM(pbank[pb3][:, :], w3b[wb][:, kc, fc * 128:(fc + 1) * 128], h2T[:, kc, ch * 512:(ch + 1) * 512],
                                   kc == 0, kc == 7) for kc in range(8)], reads=[b_w3b[wb], b_h2T[ch]], writes=[pbuf[pb3]])
                    S.op('act', ACTV(sil[sk][:], pbank[pb1][:, :], AF.Silu), reads=[pbuf[pb1]], writes=[b_sil[sk]])
                    S.op('dve', TT(hidT[hk][:, fc, :], pbank[pb3][:, :], sil[sk][:], ALU.mult),
                         reads=[pbuf[pb3], b_sil[sk]], writes=[b_hid[hk]])
                for j in range(4):
                    ti = ch * 4 + j
                    for hf2 in range(2):
                        po = 6 + hf2
                        S.op('pe', [MM(pbank[po][:, :], hidT[hk][:, fc, j * 128:(j + 1) * 128],
                                       w2b[wb][:, fc, hf2 * 512:(hf2 + 1) * 512], fc == 0, fc == 3) for fc in range(4)],
                             reads=[b_hid[hk], b_w2b[wb]], writes=[pbuf[po]])
                        S.op('dve', STT(acc[:, ti, hf2 * 512:(hf2 + 1) * 512], pbank[po][:, :], cw[:, ti, e:e + 1],
                                        acc[:, ti, hf2 * 512:(hf2 + 1) * 512], ALU.mult, ALU.add),
                             reads=[pbuf[po], b_cw[ti], b_acc[ti]], writes=[b_acc[ti]])
        for ti in range(16):
            i = hf * 16 + ti
            k = i % 2
            S.dma(DMA(nbC['xt'][k][:], out_d[i * 128:(i + 1) * 128, :]), nbC['b_xt'][k], writes=[nbC['b_xt'][k]])
            S.op('dve', TT(acc[:, ti, :], acc[:, ti, :], gabc[:, 1, :], ALU.mult), reads=[b_acc[ti], b_gabc], writes=[b_acc[ti]])
            S.op('dve', TT(acc[:, ti, :], acc[:, ti, :], nbC['xt'][k][:], ALU.add),
                 reads=[b_acc[ti], nbC['b_xt'][k]], writes=[b_acc[ti]])
            S.dma(DMA(out_d[i * 128:(i + 1) * 128, :], acc[:, ti, :]), b_acc[ti], reads=[b_acc[ti]])
    S.barrier()
    S.emit()
    return nc


def _consts():
    pos = np.arange(S_LEN, dtype=np.float32)
    inv = (np.float32(500000.0) ** (-np.arange(0, 16, 2, dtype=np.float32) / np.float32(16))).astype(np.float32)
    ang = (pos[:, None] * inv[None, :]).astype(np.float32)
    cos = np.cos(ang).astype(np.float32).reshape(NT, 128, 8).transpose(1, 0, 2)
    sin = np.sin(ang).astype(np.float32).reshape(NT, 128, 8).transpose(1, 0, 2)
    cs1 = np.concatenate([cos, cos], -1).reshape(128, NT * 16)
    sn2 = np.concatenate([-sin, sin], -1).reshape(128, NT * 16)
    kp = np.arange(128)[:, None, None]
    jj = np.arange(2)[None, :, None]
    qq = np.arange(256)[None, None, :]
    tri = (jj * 128 + kp <= qq).astype(np.float32).reshape(128, 512)
    oneh = (np.arange(S_LEN)[None, :] // 256 == np.arange(16)[:, None]).astype(np.float32)
    mconst = np.zeros((128, 225), np.float32)
    tt = np.arange(128)
    mconst[:, 0:128] = (tt[:, None] < tt[None, :]).astype(np.float32)
    ee = np.arange(32)
    mconst[0:32, 128:160] = (ee[:, None] < ee[None, :]).astype(np.float32)
    mconst[:, 160:176] = (512.0 * np.arange(16))[None, :]
    mconst[:, 176:224] = np.arange(48, dtype=np.float32)[None, :]
    mconst[:, 224] = np.arange(128, dtype=np.float32)
    return dict(cs1=np.ascontiguousarray(cs1), sn2=np.ascontiguousarray(sn2), tri=tri, oneh=oneh, mconst=mconst,
                ident=np.eye(128, dtype=np.float32))


def kernel(x, c, w_ada, b_ada, g_norm1, g_norm2, w_in, g_q, g_k, conv_w, conv_b,
           w_pa, w_pb, w_o, w_rg, b_rg, w_re, b_re, w1, w3, w2):
    f = lambda a: np.ascontiguousarray(np.asarray(a, dtype=np.float32))
    x = f(x); c = f(c)
    cst = _consts()
    gqk = np.concatenate([np.tile(f(g_q)[0], 4), np.tile(f(g_k)[0], 4)])
    gqk = np.ascontiguousarray(np.broadcast_to(gqk[None, :], (128, 512)))
    convw = np.ascontiguousarray(f(conv_w)[0].reshape(3, 4, 128).transpose(2, 1, 0).reshape(128, 12))
    convb = np.ascontiguousarray(f(conv_b)[0].reshape(4, 128).T)
    wr = np.ascontiguousarray(np.concatenate([f(w_rg)[0], f(w_re)[0]], axis=1))
    br = np.concatenate([f(b_rg)[0], f(b_re)[0]])
    br = np.ascontiguousarray(np.broadcast_to(br[None, :], (128, 36)))
    shared = dict(w_ada=f(w_ada)[0], b_ada=f(b_ada)[0:1], g1=f(g_norm1)[0:1], g2=f(g_norm2)[0:1], w_in=f(w_in)[0],
                  gqk=gqk, convw=convw, convb=convb, w_pa=f(w_pa)[0], w_pb=f(w_pb)[0], w_o=f(w_o)[0],
                  wr=wr, br=br, w1=f(w1)[0], w3=f(w3)[0], w2=f(w2)[0], **cst)
    n = _NCORES
    in_maps = []
    for b in range(n):
        m = dict(shared)
        m["x"] = x[b]
        m["cT"] = np.ascontiguousarray(c[b].reshape(8, 128).T)
        in_maps.append(m)
    if _STOP_AFTER is not None:
        for m in in_maps:
            for kk in ('w1', 'w3', 'w2'):
                m.pop(kk)
    nc = build_program(_STOP_AFTER)
    res = run_bass_kernel_spmd(nc, in_maps, core_ids=list(range(n)))
    if _STOP_AFTER is not None:
        global _DBG_RES
        _DBG_RES = res.results
    out = np.stack([np.asarray(res.results[b]["out"], dtype=np.float32).reshape(S_LEN, D) for b in range(n)])
    return out
```

```python
import numpy as np
import concourse.bass as bass
import concourse.mybir as mybir
from concourse.bass_utils import run_bass_kernel_spmd

F32 = mybir.dt.float32
BF16 = mybir.dt.bfloat16
ALU = mybir.AluOpType
AF = mybir.ActivationFunctionType
AX = mybir.AxisListType

S_LEN = 4096
D = 1024
NT = 32
BIG = 1.0e30
MASKV = 30000.0
_STOP_AFTER = None
_NCORES = 8
_DBG = {}
_SPARSE = True


class Buf:
    def __init__(self, name):
        self.name = name
        self.lw = None
        self.rd = {}
        self.dsem = {}
        self.dcnt = {}


class Sched:
    def __init__(self, nc):
        self.nc = nc
        self.engs = ['pe', 'act', 'dve', 'pool', 'sp']
        self.q = {e: [] for e in self.engs}
        self.sems = []
        self.esem = {}
        for e in ['pe', 'act', 'dve', 'pool']:
            self.esem[e] = self.new_sem('s_' + e)
        self.cnt = {e: 0 for e in self.esem}
        self.seen = {e: {} for e in self.engs}
        self.bufs = []

    def new_sem(self, name):
        s = self.nc.alloc_semaphore('%s_%d' % (name, len(self.sems)))
        self.sems.append(s)
        return len(self.sems) - 1

    def buf(self, name):
        b = Buf(name)
        self.bufs.append(b)
        return b

    def _waits(self, eng, reads, writes):
        need = {}

        def add(s, v):
            if need.get(s, 0) < v:
                need[s] = v
        for b in reads:
            if b.lw is not None:
                add(*b.lw)
        for b in writes:
            if b.lw is not None:
                add(*b.lw)
            for s, v in b.rd.items():
                add(s, v)
        seen = self.seen[eng]
        out = []
        for s, v in need.items():
            if eng == 'pe' and s == self.esem['pe']:
                continue
            if seen.get(s, 0) < v:
                seen[s] = v
                out.append((s, v))
        return out

    def _commit(self, ev, reads, writes):
        s, v = ev
        for b in reads:
            if b.rd.get(s, 0) < v:
                b.rd[s] = v
        for b in writes:
            b.lw = ev
            b.rd = {}

    def op(self, eng, fns, reads=(), writes=()):
        if callable(fns):
            fns = [fns]
        waits = self._waits(eng, reads, writes)
        self.cnt[eng] += 1
        ev = (self.esem[eng], self.cnt[eng])
        self.q[eng].append((waits, fns, ev[0], 1))
        self._commit(ev, reads, writes)

    def dma(self, fn, own, reads=(), writes=(), q='sp'):
        if q not in own.dsem:
            own.dsem[q] = self.new_sem('d_' + own.name + '_' + q)
            own.dcnt[q] = 0
        waits = self._waits(q, reads, writes)
        own.dcnt[q] += 16
        ev = (own.dsem[q], own.dcnt[q])
        self.q[q].append((waits, [fn], ev[0], 16))
        self._commit(ev, reads, writes)

    def barrier(self):
        evs = [(self.esem[e], self.cnt[e]) for e in self.esem if self.cnt[e] > 0]
        for b in self.bufs:
            for qq, sm in b.dsem.items():
                evs.append((sm, b.dcnt[qq]))
        for e in self.engs:
            seen = self.seen[e]
            waits = []
            for s, v in evs:
                if e == 'pe' and s == self.esem['pe']:
                    continue
                if seen.get(s, 0) < v:
                    seen[s] = v
                    waits.append((s, v))
            if waits:
                self.q[e].append((waits, [], None, 0))

    def emit(self):
        nc = self.nc
        sems = self.sems

        def replay(name, e):
            for waits, fns, s, inc in self.q[name]:
                for (ws, wv) in waits:
                    e.wait_ge(sems[ws], wv)
                ins = None
                for fn in fns:
                    ins = fn(e)
                if ins is not None and s is not None:
                    ins.then_inc(sems[s], inc)
        with nc.Block() as block:
            @block.tensor
            def _(e):
                replay('pe', e)

            @block.scalar
            def _(e):
                replay('act', e)

            @block.vector
            def _(e):
                replay('dve', e)

            @block.gpsimd
            def _(e):
                replay('pool', e)

            @block.sync
            def _(e):
                replay('sp', e)


class Rec:
    def __init__(self):
        self.items = []

    def op(self, *a, **k):
        self.items.append(('op', a, k))

    def dma(self, *a, **k):
        self.items.append(('dma', a, k))


class Arena:
    LO = 16640
    HI = 229376

    def __init__(self, nc):
        self.nc = nc
        self.off = self.LO
        self.n = 0

    def alloc(self, name, shape, dt):
        esz = 2 if dt == BF16 else 4
        nbytes = int(np.prod(shape[1:])) * esz
        off = (self.off + 31) // 32 * 32
        assert off + nbytes <= self.HI, ("SBUF overflow", name, off, nbytes)
        self.n += 1
        t = self.nc.alloc_sbuf_tensor_at("%s_%d" % (name, self.n), list(shape), dt, offset=off)
        self.off = off + nbytes
        return t


def MM(out, lhsT, rhs, start=True, stop=True):
    return lambda e: e.matmul(out, lhsT=lhsT, rhs=rhs, start=start, stop=stop)


def TR(out, in_, ident):
    return lambda e: e.transpose(out=out, in_=in_, identity=ident)


def ACTV(out, in_, func, **kw):
    return lambda e: e.activation(out=out, in_=in_, func=func, **kw)


def TT(out, in0, in1, op):
    return lambda e: e.tensor_tensor(out=out, in0=in0, in1=in1, op=op)


def TS(out, in0, s1, s2, op0, op1=None):
    if op1 is None:
        return lambda e: e.tensor_scalar(out=out, in0=in0, scalar1=s1, scalar2=None, op0=op0)
    return lambda e: e.tensor_scalar(out=out, in0=in0, scalar1=s1, scalar2=s2, op0=op0, op1=op1)


def STT(out, in0, scalar, in1, op0, op1):
    return lambda e: e.scalar_tensor_tensor(out=out, in0=in0, scalar=scalar, in1=in1, op0=op0, op1=op1)


def CP(out, in_):
    return lambda e: e.tensor_copy(out=out, in_=in_)


def MS(ap, v):
    return lambda e: e.memset(ap, v)


def RED(out, in_, op=None):
    return lambda e: e.tensor_reduce(out=out, in_=in_, axis=AX.X, op=(op or ALU.add))


def RECIP(out, in_):
    return lambda e: e.reciprocal(out=out, in_=in_)


def MAX8(out, in_):
    return lambda e: e.max(out=out, in_=in_)


def DMA(out, in_):
    return lambda e: e.dma_start(out=out, in_=in_)


def CAST(eng, out, in_):
    if eng == 'act':
        return ACTV(out, in_, AF.Copy)
    return CP(out, in_)


def IDMA_G(out, table, idx):
    return lambda e: e.indirect_dma_start(out=out, out_offset=None, in_=table,
                                          in_offset=bass.IndirectOffsetOnAxis(ap=idx, axis=0))


def IDMA_S(table, idx, in_):
    return lambda e: e.indirect_dma_start(out=table, out_offset=bass.IndirectOffsetOnAxis(ap=idx, axis=0),
                                          in_=in_, in_offset=None)


I32 = mybir.dt.int32


def _sparse_moe(nc, S, A, G):
    pbank, pbuf, identb, b_identb = G['pbank'], G['pbuf'], G['identb'], G['b_identb']
    modT, b_modT, gabc, b_gabc = G['modT'], G['b_modT'], G['gabc'], G['b_gabc']
    epsb, b_epsb, wrb, b_wrb, brbc, b_brbc = G['epsb'], G['b_epsb'], G['wrb'], G['b_wrb'], G['brbc'], G['b_brbc']
    out_d, mc_d, w1_d, w3_d, w2_d = G['out_d'], G['mc_d'], G['w1_d'], G['w3_d'], G['w2_d']
    pbf = G['pbf']
    NBLK, BLKR = 48, 512
    xs_d = nc.dram_tensor("xs_scratch", [NBLK * BLKR, D], BF16).ap()
    ys_d = nc.dram_tensor("ys_scratch", [NBLK * BLKR, D], F32).ap()
    w1t = w1_d.rearrange("e (p k) n -> (e p) (k n)", k=8)
    w3t = w3_d.rearrange("e (p k) n -> (e p) (k n)", k=8)
    w2t = w2_d.rearrange("e (p k) n -> (e p) (k n)", k=4)
    T = lambda name, shape, dt: (A.alloc(name, shape, dt), S.buf(name))
    mc, b_mc = T("mc", [128, 225], F32)
    ltri, b_ltri = T("ltri", [128, 128], BF16)
    onesb, b_onesb = T("onesb", [128, 128], BF16)
    ustr, b_ustr = T("ustr", [128, 32], BF16)
    modTp, b_modTp = G['modTp'], G['b_modTp']
    selall, b_selall = T("selall", [128, 32, 32], F32)
    selAall, b_selAall = T("selAall", [128, 32, 32], F32)
    cwall, b_cwall = T("cwall", [128, 32, 32], F32)
    rankall, b_rankall = T("rankall", [128, 32, 32], F32)
    csum, b_csum = T("csum", [128, 32], F32)
    dallf, b_dallf = T("dallf", [128, 64], F32)
    dalli, b_dalli = T("dalli", [128, 64], I32)
    wall, b_wall = T("wall", [128, 64], F32)
    widxf, b_widxf = T("widxf", [128, NBLK], F32)
    widxi, b_widxi = T("widxi", [128, NBLK], I32)
    xt = [A.alloc("xtC", [128, D], F32) for _ in range(2)]; b_xt = [S.buf("xtC%d" % i) for i in range(2)]
    ss = [A.alloc("ssC", [128, 1], F32) for _ in range(2)]; b_ss = [S.buf("ssC%d" % i) for i in range(2)]
    junk, b_junk = T("junkC", [128, D], BF16)
    h2Tt, b_h2Tt = T("h2Tt", [128, 8, 128], BF16)
    lg, b_lg = T("lgC", [128, 36], F32)
    rt, b_rt = T("rtC", [128, 16], F32)
    goh, b_goh = T("gohC", [128, 4], F32)
    gex, b_gex = T("gexC", [128, 4], F32)
    pen, b_pen = T("penC", [128, 4], F32)
    em, b_em = T("emC", [128, 32], F32)
    emc, b_emc = T("emcC", [128, 32], F32)
    mx8c, b_mx8c = T("mx8cC", [128, 8], F32)
    ex, b_ex = T("exC", [128, 32], F32)
    exs, b_exs = T("exsC", [128, 32], F32)
    selb, b_selb = T("selbC", [128, 32], BF16)
    tm, b_tm = T("tmC", [128, 32], F32)
    tm2, b_tm2 = T("tm2C", [128, 32], F32)
    big3, b_big3 = T("big3C", [128, 48 * 32], F32)
    nblk, b_nblk = T("nblkC", [128, 32], F32)
    nbpad, b_nbpad = T("nbpadC", [128, 128], BF16)
    nbT, b_nbT = T("nbTC", [128, 128], BF16)
    pss, b_pss = T("pssC", [128, 32], F32)
    pend, b_pend = T("pendC", [128, 32], F32)
    bex, b_bex = T("bexC", [128, NBLK], F32)
    gmax, ngmax, gsum, nm1, den, fsc = [rt[:, q:q + 1] for q in range(6)]
    PEN = 1.0e4
    MARK = A.off
    xnall = A.alloc("xnall", [128, 32, D], BF16); b_xnall = [S.buf("xnall%d" % i) for i in range(32)]

    zt, b_zt = T("zt", [128, 4, D], BF16)
    S.op('pool', MS(zt[:], 0.0), writes=[b_zt])
    for b in range(NBLK):
        S.dma(DMA(xs_d[b * BLKR:(b + 1) * BLKR, :].rearrange("(s p) d -> p s d", p=128), zt[:]), b_zt, reads=[b_zt])
    S.dma(DMA(mc[:], mc_d[:, :]), b_mc, writes=[b_mc])
    S.op('dve', [CP(ltri[:], mc[:, 0:128]), CP(ustr[0:32, :], mc[0:32, 128:160])], reads=[b_mc], writes=[b_ltri, b_ustr])
    S.op('pool', [MS(onesb[:], 1.0), MS(csum[:], 0.0), MS(nbpad[:], 0.0)], writes=[b_onesb, b_csum, b_nbpad])
    for i in range(32):
        k = i % 2
        S.dma(DMA(xt[k][:], out_d[i * 128:(i + 1) * 128, :]), b_xt[k], writes=[b_xt[k]])
        S.op('act', ACTV(junk[:], xt[k][:], AF.Square, scale=1.0 / 32.0, accum_out=ss[k][:]),
             reads=[b_xt[k]], writes=[b_junk, b_ss[k]])
        S.op('act', ACTV(ss[k][:], ss[k][:], AF.Sqrt, bias=epsb[:]), reads=[b_ss[k], b_epsb], writes=[b_ss[k]])
        S.op('dve', RECIP(ss[k][:], ss[k][:]), reads=[b_ss[k]], writes=[b_ss[k]])
        S.op('dve', TS(xnall[:, i, :], xt[k][:], ss[k][:, 0:1], None, ALU.mult), reads=[b_xt[k], b_ss[k]], writes=[b_xnall[i]])
        pv = pbf(0).rearrange("p (a b) -> p a b", a=8)
        S.op('pe', [TR(pv[:, kc, :], xnall[:, i, kc * 128:(kc + 1) * 128], identb[:]) for kc in range(8)],
             reads=[b_xnall[i], b_identb], writes=[pbuf[0]])
        S.op('act', [ACTV(h2Tt[:, kc, :], pv[:, kc, :], AF.Identity, scale=modT[:, 16 + kc:17 + kc],
                          bias=modT[:, 24 + kc:25 + kc]) for kc in range(8)], reads=[pbuf[0], b_modT], writes=[b_h2Tt])
        S.op('pe', [MM(pbank[1][:, 0:36], h2Tt[:, kc, :], wrb[:, kc, :], kc == 0, kc == 7) for kc in range(8)],
             reads=[b_h2Tt, b_wrb], writes=[pbuf[1]])
        S.op('dve', TT(lg[:], pbank[1][:, 0:36], brbc[:], ALU.add), reads=[pbuf[1], b_brbc], writes=[b_lg])
        S.op('dve', RED(gmax, lg[:, 0:4], ALU.max), reads=[b_lg], writes=[b_rt])
        S.op('dve', TS(ngmax, gmax, -1.0, None, ALU.mult), reads=[b_rt], writes=[b_rt])
        S.op('dve', TS(goh[:], lg[:, 0:4], gmax, None, ALU.is_ge), reads=[b_lg, b_rt], writes=[b_goh])
        S.op('act', ACTV(gex[:], lg[:, 0:4], AF.Exp, bias=ngmax, accum_out=gsum), reads=[b_lg, b_rt], writes=[b_gex, b_rt])
        S.op('dve', RECIP(gsum, gsum), reads=[b_rt], writes=[b_rt])
        S.op('dve', TS(pen[:], goh[:], PEN, -PEN, ALU.mult, ALU.add), reads=[b_goh], writes=[b_pen])
        S.op('dve', TT(em[:].rearrange("p (g e) -> p g e", g=4), lg[:, 4:36].rearrange("p (g e) -> p g e", g=4),
                       pen[:, :].unsqueeze(2).to_broadcast([128, 4, 8]), ALU.add), reads=[b_lg, b_pen], writes=[b_em])
        S.op('dve', MAX8(mx8c[:], em[:]), reads=[b_em], writes=[b_mx8c])
        S.op('dve', TS(nm1, mx8c[:, 0:1], -1.0, None, ALU.mult), reads=[b_mx8c], writes=[b_rt])
        S.op('dve', TS(selall[:, i, :], em[:], mx8c[:, 1:2], None, ALU.is_ge), reads=[b_em, b_mx8c], writes=[b_selall])
        S.op('dve', TS(selAall[:, i, :], em[:], mx8c[:, 0:1], None, ALU.is_ge), reads=[b_em, b_mx8c], writes=[b_selAall])
        S.op('dve', TS(emc[:], em[:], mx8c[:, 1:2], None, ALU.max), reads=[b_em, b_mx8c], writes=[b_emc])
        S.op('act', ACTV(ex[:], emc[:], AF.Exp, bias=nm1), reads=[b_emc, b_rt], writes=[b_ex])
        S.op('dve', TT(exs[:], ex[:], selall[:, i, :], ALU.mult), reads=[b_ex, b_selall], writes=[b_exs])
        S.op('dve', RED(den, exs[:]), reads=[b_exs], writes=[b_rt])
        S.op('dve', RECIP(den, den), reads=[b_rt], writes=[b_rt])
        S.op('dve', TT(fsc, den, gsum, ALU.mult), reads=[b_rt], writes=[b_rt])
        S.op('dve', TS(cwall[:, i, :], exs[:], fsc, None, ALU.mult), reads=[b_exs, b_rt], writes=[b_cwall])
        S.op('dve', CP(selb[:], selall[:, i, :]), reads=[b_selall], writes=[b_selb])
        S.op('pe', MM(pbank[2][:, 0:32], ltri[:], selb[:]), reads=[b_ltri, b_selb], writes=[pbuf[2]])
        S.op('pe', MM(pbank[3][:, 0:32], onesb[:], selb[:]), reads=[b_onesb, b_selb], writes=[pbuf[3]])
        S.op('dve', TT(rankall[:, i, :], pbank[2][:, 0:32], csum[:], ALU.add), reads=[pbuf[2], b_csum], writes=[b_rankall])
        S.op('dve', TT(csum[:], pbank[3][:, 0:32], csum[:], ALU.add), reads=[pbuf[3], b_csum], writes=[b_csum])
    b3 = big3[:, 0:512].rearrange("p (e k) -> p e k", k=16)
    S.op('dve', TT(b3, csum[:, :].unsqueeze(2).to_broadcast([128, 32, 16]),
                   mc[:, 160:176].unsqueeze(1).to_broadcast([128, 32, 16]), ALU.is_gt), reads=[b_csum, b_mc], writes=[b_big3])
    S.op('dve', RED(nblk[:], b3), reads=[b_big3], writes=[b_nblk])
    S.op('dve', CP(nbpad[:, 0:32], nblk[:]), reads=[b_nblk], writes=[b_nbpad])
    pvn = pbf(0)
    S.op('pe', TR(pvn[:, 0:128], nbpad[:], identb[:]), reads=[b_nbpad, b_identb], writes=[pbuf[0]])
    S.op('act', ACTV(nbT[0:32, :], pvn[0:32, 0:128], AF.Copy), reads=[pbuf[0]], writes=[b_nbT])
    S.op('pe', MM(pbank[2][:, 0:32], nbT[0:32, :], ustr[0:32, :]), reads=[b_nbT, b_ustr], writes=[pbuf[2]])
    S.op('dve', CP(pss[:], pbank[2][:, 0:32]), reads=[pbuf[2]], writes=[b_pss])
    S.op('dve', TT(pend[:], pss[:], nblk[:], ALU.add), reads=[b_pss, b_nblk], writes=[b_pend])
    b4 = big3[:, :].rearrange("p (b e) -> p b e", e=32)
    S.op('dve', TT(b4, pend[:, :].unsqueeze(1).to_broadcast([128, NBLK, 32]),
                   mc[:, 176:224].unsqueeze(2).to_broadcast([128, NBLK, 32]), ALU.is_le), reads=[b_pend, b_mc], writes=[b_big3])
    S.op('dve', RED(bex[:], b4), reads=[b_big3], writes=[b_bex])
    S.op('dve', TS(bex[:], bex[:], 31.0, None, ALU.min), reads=[b_bex], writes=[b_bex])
    S.op('dve', TS(widxf[:], bex[:], 128.0, None, ALU.mult), reads=[b_bex], writes=[b_widxf])
    S.op('dve', TT(widxf[:], widxf[:], mc[:, 224:225].to_broadcast([128, NBLK]), ALU.add), reads=[b_widxf, b_mc], writes=[b_widxf])
    S.op('dve', CP(widxi[:], widxf[:]), reads=[b_widxf], writes=[b_widxi])
    S.op('dve', TS(pss[:], pss[:], float(BLKR), None, ALU.mult), reads=[b_pss], writes=[b_pss])
    for i in range(32):
        S.op('dve', TT(tm[:], rankall[:, i, :], pss[:], ALU.add), reads=[b_rankall, b_pss], writes=[b_tm])
        S.op('dve', TT(tm2[:], tm[:], selAall[:, i, :], ALU.mult), reads=[b_tm, b_selAall], writes=[b_tm2])
        S.op('dve', RED(dallf[:, 2 * i:2 * i + 1], tm2[:]), reads=[b_tm2], writes=[b_dallf])
        S.op('dve', TT(tm2[:], selall[:, i, :], selAall[:, i, :], ALU.subtract), reads=[b_selall, b_selAall], writes=[b_tm2])
        S.op('dve', TT(tm[:], tm[:], tm2[:], ALU.mult), reads=[b_tm, b_tm2], writes=[b_tm])
        S.op('dve', RED(dallf[:, 2 * i + 1:2 * i + 2], tm[:]), reads=[b_tm], writes=[b_dallf])
        S.op('dve', TT(tm[:], cwall[:, i, :], tm2[:], ALU.mult), reads=[b_cwall, b_tm2], writes=[b_tm])
        S.op('dve', RED(wall[:, 2 * i + 1:2 * i + 2], tm[:]), reads=[b_tm], writes=[b_wall])
        S.op('dve', TT(tm[:], cwall[:, i, :], selAall[:, i, :], ALU.mult), reads=[b_cwall, b_selAall], writes=[b_tm])
        S.op('dve', RED(wall[:, 2 * i:2 * i + 1], tm[:]), reads=[b_tm], writes=[b_wall])
    S.op('dve', CP(dalli[:], dallf[:]), reads=[b_dallf], writes=[b_dalli])
    S.barrier()
    for i in range(32):
        for kk in range(2):
            S.dma(IDMA_S(xs_d[:, :], dalli[:, 2 * i + kk:2 * i + kk + 1], xnall[:, i, :]), b_xnall[i],
                  reads=[b_xnall[i], b_dalli], q='pool')
    S.barrier()
    A.off = MARK
    w1b = [A.alloc("w1s", [128, 8, 512], BF16) for _ in range(2)]; b_w1b = [S.buf("w1s%d" % i) for i in range(2)]
    w3b = [A.alloc("w3s", [128, 8, 512], BF16) for _ in range(2)]; b_w3b = [S.buf("w3s%d" % i) for i in range(2)]
    w2b = [A.alloc("w2s", [128, 4, D], BF16) for _ in range(2)]; b_w2b = [S.buf("w2s%d" % i) for i in range(2)]
    stg = [A.alloc("stgS", [128, 4096], F32) for _ in range(2)]; b_stg = [S.buf("stgS%d" % i) for i in range(2)]
    xs = [A.alloc("xs", [128, 4, D], BF16) for _ in range(2)]; b_xs = [S.buf("xs%d" % i) for i in range(2)]
    xsT = [A.alloc("xsT", [128, 8, 512], BF16) for _ in range(2)]; b_xsT = [S.buf("xsT%d" % i) for i in range(2)]
    hidT = [A.alloc("hidS", [128, 4, 512], BF16) for _ in range(2)]; b_hid = [S.buf("hidS%d" % i) for i in range(2)]
    sil = [A.alloc("silS", [128, 512], F32) for _ in range(2)]; b_sil = [S.buf("silS%d" % i) for i in range(2)]
    ysb = [A.alloc("ysb", [128, D], F32) for _ in range(2)]; b_ysb = [S.buf("ysb%d" % i) for i in range(2)]
    yg = [A.alloc("yg", [128, D], F32) for _ in range(4)]; b_yg = [S.buf("yg%d" % i) for i in range(4)]
    sidx = [0]
    crr = [0]

    def prep(b):
        wb = b % 2
        xb = b % 2
        S.dma(DMA(xs[xb][:], xs_d[b * BLKR:(b + 1) * BLKR, :].rearrange("(s p) d -> p s d", p=128)), b_xs[xb], writes=[b_xs[xb]])
        items = []
        for (tab, dst, b_dst) in [(w1t, w1b[wb], b_w1b[wb]), (w3t, w3b[wb], b_w3b[wb]), (w2t, w2b[wb], b_w2b[wb])]:
            sk = sidx[0] % 2
            sidx[0] += 1
            S.dma(IDMA_G(stg[sk][:], tab, widxi[:, b:b + 1]), b_stg[sk], reads=[b_widxi], writes=[b_stg[sk]], q='pool')
            crr[0] += 1
            ce = ['dve', 'act'][crr[0] % 2]

            def cast_item(ce=ce, dst=dst, sk=sk, b_dst=b_dst):
                S.op(ce, CAST(ce, dst[:].rearrange("p a b -> p (a b)"), stg[sk][:]), reads=[b_stg[sk]], writes=[b_dst])
            cast_item()
        for sub in range(4):
            def tr_item(sub=sub, xb=xb):
                pv = pbf(0).rearrange("p (a b) -> p a b", a=8)
                S.op('pe', [TR(pv[:, kc, :], xs[xb][:, sub, :].rearrange("p (q k) -> p q k", k=8)[:, :, kc], identb[:])
                            for kc in range(8)], reads=[b_xs[xb], b_identb], writes=[pbuf[0]])
                S.op('act', [ACTV(xsT[xb][:, kc, sub * 128:(sub + 1) * 128], pv[:, kc, :], AF.Identity,
                                  scale=modTp[:, kc:kc + 1], bias=modTp[:, 8 + kc:9 + kc]) for kc in range(8)],
                     reads=[pbuf[0], b_modTp], writes=[b_xsT[xb]])
            items.append(tr_item)
        return items

    for it in prep(0):
        it()
    for b in range(NBLK):
        wb = b % 2
        xb = b % 2
        hk = b % 2
        nxt = prep(b + 1) if b + 1 < NBLK else []
        for fc in range(4):
            pb1 = 2 + (fc % 2)
            pb3 = 4 + (fc % 2)
            sk2 = fc % 2
            S.op('pe', [MM(pbank[pb1][:, :], w1b[wb][:, kc, :].rearrange("p (m f) -> p m f", f=4)[:, :, fc], xsT[xb][:, kc, :],
                           kc == 0, kc == 7) for kc in range(8)], reads=[b_w1b[wb], b_xsT[xb]], writes=[pbuf[pb1]])
            S.op('pe', [MM(pbank[pb3][:, :], w3b[wb][:, kc, :].rearrange("p (m f) -> p m f", f=4)[:, :, fc], xsT[xb][:, kc, :],
                           kc == 0, kc == 7) for kc in range(8)], reads=[b_w3b[wb], b_xsT[xb]], writes=[pbuf[pb3]])
            S.op('act', ACTV(sil[sk2][:], pbank[pb1][:, :], AF.Silu), reads=[pbuf[pb1]], writes=[b_sil[sk2]])
            S.op('dve', TT(hidT[hk][:, fc, :], pbank[pb3][:, :], sil[sk2][:], ALU.mult),
                 reads=[pbuf[pb3], b_sil[sk2]], writes=[b_hid[hk]])
            if nxt:
                nxt.pop(0)()
        for j in range(4):
            yk = j % 2
            for hf2 in range(2):
                po = 6 + hf2
                S.op('pe', [MM(pbank[po][:, :], hidT[hk][:, fc, j * 128:(j + 1) * 128],
                               w2b[wb][:, fc, hf2 * 512:(hf2 + 1) * 512], fc == 0, fc == 3) for fc in range(4)],
                     reads=[b_hid[hk], b_w2b[wb]], writes=[pbuf[po]])
                S.op('dve' if hf2 else 'act',
                     CAST('dve' if hf2 else 'act', ysb[yk][:, hf2 * 512:(hf2 + 1) * 512], pbank[po][:, :]),
                     reads=[pbuf[po]], writes=[b_ysb[yk]])
            r0 = b * BLKR + j * 128
            S.dma(DMA(ys_d[r0:r0 + 128, :], ysb[yk][:]), b_ysb[yk], reads=[b_ysb[yk]])
        while nxt:
            nxt.pop(0)()
    S.barrier()
    for i in range(32):
        k = i % 2
        g0, g1 = yg[2 * k], yg[2 * k + 1]
        S.dma(IDMA_G(g0[:], ys_d[:, :], dalli[:, 2 * i:2 * i + 1]), b_yg[2 * k], reads=[b_dalli], writes=[b_yg[2 * k]], q='pool')
        S.dma(IDMA_G(g1[:], ys_d[:, :], dalli[:, 2 * i + 1:2 * i + 2]), b_yg[2 * k + 1], reads=[b_dalli], writes=[b_yg[2 * k + 1]], q='pool')
        S.dma(DMA(xt[k][:], out_d[i * 128:(i + 1) * 128, :]), b_xt[k], writes=[b_xt[k]])
        S.op('dve', TS(g0[:], g0[:], wall[:, 2 * i:2 * i + 1], None, ALU.mult), reads=[b_yg[2 * k], b_wall], writes=[b_yg[2 * k]])
        S.op('dve', STT(g0[:], g1[:], wall[:, 2 * i + 1:2 * i + 2], g0[:], ALU.mult, ALU.add),
             reads=[b_yg[2 * k], b_yg[2 * k + 1], b_wall], writes=[b_yg[2 * k]])
        S.op('dve', TT(g0[:], g0[:], gabc[:, 1, :], ALU.mult), reads=[b_yg[2 * k], b_gabc], writes=[b_yg[2 * k]])
        S.op('dve', TT(g0[:], g0[:], xt[k][:], ALU.add), reads=[b_yg[2 * k], b_xt[k]], writes=[b_yg[2 * k]])
        S.dma(DMA(out_d[i * 128:(i + 1) * 128, :], g0[:]), b_yg[2 * k], reads=[b_yg[2 * k]])
    S.barrier()


def build_program(stop_after=None):
    nc = bass.Bass("TRN2", target_bir_lowering=False)
    din = lambda name, shape: nc.dram_tensor(name, list(shape), F32, kind="ExternalInput").ap()
    x_d = din("x", [S_LEN, D])
    cT_d = din("cT", [128, 8])
    wada_d = din("w_ada", [D, 6 * D])
    bada_d = din("b_ada", [1, 6 * D])
    g1_d = din("g1", [1, D])
    g2_d = din("g2", [1, D])
    win_d = din("w_in", [D, 5120])
    gqk_d = din("gqk", [128, 512])
    cs1_d = din("cs1", [128, NT * 16])
    sn2_d = din("sn2", [128, NT * 16])
    convw_d = din("convw", [128, 12])
    convb_d = din("convb", [128, 4])
    wpa_d = din("w_pa", [512, D])
    wpb_d = din("w_pb", [512, D])
    wo_d = din("w_o", [D, D])
    wr_d = din("wr", [D, 36])
    br_d = din("br", [128, 36])
    if stop_after is None:
        w1_d = din("w1", [32, D, 512])
        w3_d = din("w3", [32, D, 512])
        w2_d = din("w2", [32, 512, D])
    ident_d = din("ident", [128, 128])
    tri_d = din("tri", [128, 512])
    oneh_d = din("oneh", [16, S_LEN])
    mc_d = din("mconst", [128, 225])
    out_d = nc.dram_tensor("out", [S_LEN, D], F32, kind="ExternalOutput").ap()
    ya_d = nc.dram_tensor("ya_scratch", [512, S_LEN], BF16, kind="ExternalOutput").ap()

    S = Sched(nc)
    A = Arena(nc)
    pbank = [nc.alloc_psum_tensor("pb%d" % i, [128, 512], F32) for i in range(8)]
    pbuf = [S.buf("pb%d" % i) for i in range(8)]

    def pbf(i):
        return pbank[i][:, :].bitcast(BF16)

    identb = A.alloc("identb", [128, 128], BF16); b_identb = S.buf("identb")
    cs1 = A.alloc("cs1", [128, NT, 16], F32); b_cs1 = S.buf("cs1")
    sn2 = A.alloc("sn2", [128, NT, 16], F32); b_sn2 = S.buf("sn2")
    trib = A.alloc("trib", [128, 2, 256], BF16); b_trib = S.buf("trib")
    modT = A.alloc("modT", [128, 32], F32); b_modT = S.buf("modT")
    gabc = A.alloc("gabc", [128, 2, D], F32); b_gabc = S.buf("gabc")
    epsb = A.alloc("epsb", [128, 1], F32); b_epsb = S.buf("epsb")
    onesf = A.alloc("onesf", [128, 128], F32); b_onesf = S.buf("onesf")
    gqk = A.alloc("gqk", [128, 512], F32); b_gqk = S.buf("gqk")
    convw = A.alloc("convw", [128, 4, 3], F32); b_convw = S.buf("convw")
    convb = A.alloc("convb", [128, 4], F32); b_convb = S.buf("convb")
    brbc = A.alloc("brbc", [128, 36], F32); b_brbc = S.buf("brbc")
    wrb = A.alloc("wrb", [128, 8, 36], BF16); b_wrb = S.buf("wrb")
    modTp = A.alloc("modTp", [128, 16], F32); b_modTp = S.buf("modTp")
    PERSIST = A.off

    stgc = A.alloc("stgc", [128, 512], F32); b_stgc = S.buf("stgc")
    stgi = A.alloc("stgi", [128, 128], F32); b_stgi = S.buf("stgi")
    stgr = A.alloc("stgr", [128, 8, 36], F32); b_stgr = S.buf("stgr")
    cT = A.alloc("cT", [128, 8], F32); b_cT = S.buf("cT")
    sT = A.alloc("sT", [128, 8], F32); b_sT = S.buf("sT")
    wst = [A.alloc("wst", [128, 8, 512], F32) for _ in range(2)]
    b_wst = [S.buf("wst%d" % i) for i in range(2)]
    modrow = A.alloc("modrow", [1, 6 * D], F32); b_modrow = S.buf("modrow")
    badar = A.alloc("badar", [1, 6 * D], F32); b_badar = S.buf("badar")
    grow = A.alloc("grow", [1, 2 * D], F32); b_grow = S.buf("grow")
    arow = A.alloc("arow", [1, 2 * D], F32); b_arow = S.buf("arow")

    S.op('pool', [MS(epsb[:], 1e-6), MS(onesf[:], 1.0)], writes=[b_epsb, b_onesf])
    S.dma(DMA(stgi[:], ident_d[:, :]), b_stgi, writes=[b_stgi])
    S.op('dve', CP(identb[:], stgi[:]), reads=[b_stgi], writes=[b_identb])
    S.dma(DMA(stgc[:], tri_d[:, :]), b_stgc, writes=[b_stgc])
    S.op('dve', CP(trib[:].rearrange("p a b -> p (a b)"), stgc[:]), reads=[b_stgc], writes=[b_trib])
    S.dma(DMA(cs1[:].rearrange("p a b -> p (a b)"), cs1_d[:, :]), b_cs1, writes=[b_cs1])
    S.dma(DMA(sn2[:].rearrange("p a b -> p (a b)"), sn2_d[:, :]), b_sn2, writes=[b_sn2])
    S.dma(DMA(gqk[:], gqk_d[:, :]), b_gqk, writes=[b_gqk])
    S.dma(DMA(convw[:].rearrange("p a b -> p (a b)"), convw_d[:, :]), b_convw, writes=[b_convw])
    S.dma(DMA(convb[:], convb_d[:, :]), b_convb, writes=[b_convb])
    S.dma(DMA(brbc[:], br_d[:, :]), b_brbc, writes=[b_brbc])
    S.dma(DMA(stgr[:], wr_d.rearrange("(kc p) n -> p kc n", p=128)), b_stgr, writes=[b_stgr])
    S.op('dve', CP(wrb[:], stgr[:]), reads=[b_stgr], writes=[b_wrb])
    S.dma(DMA(cT[:], cT_d[:, :]), b_cT, writes=[b_cT])
    S.op('act', ACTV(sT[:], cT[:], AF.Silu), reads=[b_cT], writes=[b_sT])
    S.dma(DMA(badar[:], bada_d[:, :]), b_badar, writes=[b_badar])
    S.dma(DMA(grow[0:1, 0:D], g1_d[:, :]), b_grow, writes=[b_grow])
    S.dma(DMA(grow[0:1, D:2 * D], g2_d[:, :]), b_grow, writes=[b_grow])
    for j in range(12):
        wb = j % 2
        S.dma(DMA(wst[wb][:], wada_d[:, j * 512:(j + 1) * 512].rearrange("(kc p) n -> p kc n", p=128)),
              b_wst[wb], writes=[b_wst[wb]])
        S.op('pe', [MM(pbank[0][0:1, :], sT[:, kc:kc + 1], wst[wb][:, kc, :], kc == 0, kc == 7) for kc in range(8)],
             reads=[b_sT, b_wst[wb]], writes=[pbuf[0]])
        S.op('dve', TT(modrow[0:1, j * 512:(j + 1) * 512], pbank[0][0:1, :], badar[0:1, j * 512:(j + 1) * 512], ALU.add),
             reads=[pbuf[0], b_badar], writes=[b_modrow])
    S.op('dve', STT(arow[0:1, 0:D], modrow[0:1, D:2 * D], 1.0, grow[0:1, 0:D], ALU.add, ALU.mult),
         reads=[b_modrow, b_grow], writes=[b_arow])
    S.op('dve', STT(arow[0:1, D:2 * D], modrow[0:1, 4 * D:5 * D], 1.0, grow[0:1, D:2 * D], ALU.add, ALU.mult),
         reads=[b_modrow, b_grow], writes=[b_arow])
    fns = []
    srcs = [(arow, 0), (modrow, 0), (arow, D), (modrow, 3 * D)]
    for r, (src, off) in enumerate(srcs):
        for kc in range(8):
            fns.append(MM(pbank[1][:, r * 8 + kc:r * 8 + kc + 1], src[0:1, off + kc * 128:off + (kc + 1) * 128],
                          onesf[0:1, 0:1]))
    S.op('pe', fns, reads=[b_arow, b_modrow, b_onesf], writes=[pbuf[1]])
    S.op('dve', CP(modT[:], pbank[1][:, 0:32]), reads=[pbuf[1]], writes=[b_modT])
    fns = []
    for r, (src, off) in enumerate([(arow, D), (modrow, 3 * D)]):
        for kc in range(8):
            fns.append(MM(pbank[1][:, r * 8 + kc:r * 8 + kc + 1],
                          src[0:1, off:off + D].rearrange("o (p k) -> o p k", k=8)[:, :, kc], onesf[0:1, 0:1]))
    S.op('pe', fns, reads=[b_arow, b_modrow, b_onesf], writes=[pbuf[1]])
    S.op('dve', CP(modTp[:], pbank[1][:, 0:16]), reads=[pbuf[1]], writes=[b_modTp])
    for gi, off in enumerate([2 * D, 5 * D]):
        for hf in range(2):
            S.op('pe', MM(pbank[2][:, :], onesf[0:1, 0:128], modrow[0:1, off + hf * 512:off + (hf + 1) * 512]),
                 reads=[b_onesf, b_modrow], writes=[pbuf[2]])
            S.op('act', ACTV(gabc[:, gi, hf * 512:(hf + 1) * 512], pbank[2][:, :], AF.Copy),
                 reads=[pbuf[2]], writes=[b_gabc])
    S.barrier()
    A.off = PERSIST
    if stop_after == '0':
        S.emit()
        return nc

    def make_norm_bufs(with_xt=True, with_junk=True):
        d = {}
        d['xt'] = [A.alloc("xt", [128, D], F32) for _ in range(2)] if with_xt else None
        d['b_xt'] = [S.buf("xt%d" % i) for i in range(2)]
        if with_junk:
            d['junk'] = A.alloc("junk", [128, D], BF16); d['b_junk'] = S.buf("junk")
        d['ss'] = [A.alloc("ss", [128, 1], F32) for _ in range(2)]
        d['b_ss'] = [S.buf("ss%d" % i) for i in range(2)]
        d['xn'] = [A.alloc("xn", [128, D], BF16) for _ in range(2)]
        d['b_xn'] = [S.buf("xn%d" % i) for i in range(2)]
        return d

    def norm_tile(nb, par, xsrc, b_xsrc, hT_dst, b_hT, col0, ptr_i=0, sch=None):
        S_ = sch or S
        k = par % 2
        S_.op('act', ACTV(nb['junk'][:], xsrc, AF.Square, scale=1.0 / 32.0, accum_out=nb['ss'][k][:]),
             reads=[b_xsrc], writes=[nb['b_junk'], nb['b_ss'][k]])
        S_.op('act', ACTV(nb['ss'][k][:], nb['ss'][k][:], AF.Sqrt, bias=epsb[:]),
             reads=[nb['b_ss'][k], b_epsb], writes=[nb['b_ss'][k]])
        S_.op('dve', RECIP(nb['ss'][k][:], nb['ss'][k][:]), reads=[nb['b_ss'][k]], writes=[nb['b_ss'][k]])
        S_.op('dve', TS(nb['xn'][k][:], xsrc, nb['ss'][k][:, 0:1], None, ALU.mult),
             reads=[b_xsrc, nb['b_ss'][k]], writes=[nb['b_xn'][k]])
        pv = pbf(ptr_i).rearrange("p (a b) -> p a b", a=8)
        S_.op('pe', [TR(pv[:, kc, :], nb['xn'][k][:, kc * 128:(kc + 1) * 128], identb[:]) for kc in range(8)],
             reads=[nb['b_xn'][k], b_identb], writes=[pbuf[ptr_i]])
        S_.op('act', [ACTV(hT_dst[:, kc, :], pv[:, kc, :], AF.Identity, scale=modT[:, col0 + kc:col0 + kc + 1],
                          bias=modT[:, col0 + 8 + kc:col0 + 9 + kc]) for kc in range(8)],
             reads=[pbuf[ptr_i], b_modT], writes=[b_hT])

    engrr = [0]

    def cast_eng():
        engrr[0] += 1
        return ['dve', 'act'][engrr[0] % 2]

    KxT = A.alloc("KxT", [128, 4, S_LEN], BF16)
    b_Kx = [S.buf("Kx%d" % c) for c in range(8)]
    Vx = A.alloc("Vx", [128, NT, 4, 65], BF16)
    b_Vx = [S.buf("Vx%d" % c) for c in range(8)]
    kmT = A.alloc("kmT", [128, 4, 16], BF16)
    b_km = [S.buf("km%d" % c) for c in range(16)]
    kms = A.alloc("kms", [128, 4], F32); b_kms = S.buf("kms")
    wqkv = A.alloc("wqkv", [128, 8, 768], BF16); b_wqkv = S.buf("wqkv")
    stgA = [A.alloc("stgA", [128, 8, 256], F32) for _ in range(2)]
    b_stgA = [S.buf("stgA%d" % i) for i in range(2)]
    stgo = A.alloc("stgo", [128, S_LEN], F32); b_stgo = S.buf("stgo")
    nb = make_norm_bufs()
    hT = [A.alloc("hT", [128, 8, 512], BF16) for _ in range(2)]
    b_hT = [S.buf("hT%d" % i) for i in range(2)]
    QxT = [A.alloc("QxT", [128, 4, 512], BF16) for _ in range(2)]
    b_Qx = [S.buf("Qx%d" % i) for i in range(2)]
    sq = A.alloc("sq", [128, 512], F32); b_sq = S.buf("sq")
    ssq = [A.alloc("ssq", [128, 8], F32) for _ in range(2)]; b_ssq = [S.buf("ssq%d" % i) for i in range(2)]
    qn = [A.alloc("qn", [128, 512], F32) for _ in range(2)]
    b_qn = [S.buf("qn%d" % i) for i in range(2)]
    tA = [A.alloc("tA", [128, 8, 16], F32) for _ in range(2)]; b_tA = [S.buf("tA%d" % i) for i in range(2)]
    tB = [A.alloc("tB", [128, 8, 16], F32) for _ in range(2)]; b_tB = [S.buf("tB%d" % i) for i in range(2)]
    qkb = [A.alloc("qkb", [128, 8, 128], BF16) for _ in range(2)]
    b_qkb = [S.buf("qkb%d" % i) for i in range(2)]
    gsb = A.alloc("gsb", [128, 4, 16], F32); b_gsb = S.buf("gsb")
    mx8 = A.alloc("mx8", [128, 4, 8], F32); b_mx8 = S.buf("mx8")
    sel = A.alloc("sel", [128, 4, 16], F32); b_sel = S.buf("sel")
    mbp = [A.alloc("mbp", [128, 4, 128], BF16) for _ in range(2)]
    b_mbp = [S.buf("mbp%d" % i) for i in range(2)]
    pT = [A.alloc("pT", [128, 512], BF16) for _ in range(3)]
    b_pT = [S.buf("pT%d" % i) for i in range(3)]
    rd = A.alloc("rd", [128, 512], F32); b_rd = S.buf("rd")
    bcs = A.alloc("bcs", [128, 512], F32); b_bcs = S.buf("bcs")
    yo = [A.alloc("yo", [128, 512], BF16) for _ in range(2)]
    b_yo = [S.buf("yo%d" % i) for i in range(2)]

    S.dma(DMA(stgo[64:80, :], oneh_d[:, :]), b_stgo, writes=[b_stgo])
    S.op('dve', [CP(KxT[64:80, h, :], stgo[64:80, :]) for h in range(4)], reads=[b_stgo], writes=b_Kx)
    S.op('dve', [MS(Vx[:, :, :, 64:65], 1.0), MS(mbp[0][:], 0.0), MS(mbp[1][:], 0.0),
                  MS(qkb[0][:], 0.0), MS(qkb[1][:], 0.0)],
         writes=b_Vx + b_mbp + b_qkb)

    rot = [0]
    L = _DBG.get('lvl', 9)
    b_pg = S.buf('pg')
    for hh in range(_DBG.get('nhh', 2)):
        for part, c0 in enumerate([hh * 256, 512 + hh * 256, 1024 + hh * 256]):
            sb = part % 2
            S.dma(DMA(stgA[sb][:], win_d[:, c0:c0 + 256].rearrange("(kc p) n -> p kc n", p=128)),
                  b_stgA[sb], writes=[b_stgA[sb]])
            ce = cast_eng()
            S.op(ce, CAST(ce, wqkv[:, :, part * 256:(part + 1) * 256], stgA[sb][:]),
                 reads=[b_stgA[sb]], writes=[b_wqkv])
        NCH = _DBG.get('nch', 8)
        R = Rec()

        def stageA(c, j):
            i = 4 * c + j
            k = i % 2
            hb = c % 2
            R.dma(DMA(nb['xt'][k][:], x_d[i * 128:(i + 1) * 128, :]), nb['b_xt'][k], writes=[nb['b_xt'][k]])
            norm_tile(nb, i, nb['xt'][k][:], nb['b_xt'][k], hT[hb][:, :, j * 128:(j + 1) * 128], b_hT[hb], 0, sch=R)

        def stageB(c, j):
            i = 4 * c + j
            k = i % 2
            hb = c % 2
            R.op('pe', [MM(pbank[1][:, :], hT[hb][:, kc, j * 128:(j + 1) * 128], wqkv[:, kc, 0:512], kc == 0, kc == 7)
                        for kc in range(8)], reads=[b_hT[hb], b_wqkv], writes=[pbuf[1]])
            R.op('pe', [MM(pbank[2][:, 0:256], hT[hb][:, kc, j * 128:(j + 1) * 128], wqkv[:, kc, 512:768], kc == 0, kc == 7)
                        for kc in range(8)], reads=[b_hT[hb], b_wqkv], writes=[pbuf[2]])
            R.op('act', ACTV(Vx[:, i, :, 0:64], pbank[2][:, 0:256].rearrange("p (h d) -> p h d", h=4), AF.Copy),
                 reads=[pbuf[2]], writes=[b_Vx[c]])
            R.op('act', ACTV(sq[:], pbank[1][:, :], AF.Square), reads=[pbuf[1]], writes=[b_sq])
            R.op('dve', RED(ssq[k][:], sq[:].rearrange("p (h d) -> p h d", h=8)), reads=[b_sq], writes=[b_ssq[k]])
            R.op('act', ACTV(ssq[k][:], ssq[k][:], AF.Sqrt, scale=1.0 / 64.0, bias=epsb[:]),
                 reads=[b_ssq[k], b_epsb], writes=[b_ssq[k]])
            R.op('dve', RECIP(ssq[k][:], ssq[k][:]), reads=[b_ssq[k]], writes=[b_ssq[k]])
            qv = qn[k][:].rearrange("p (h d) -> p h d", h=8)
            R.op('dve', TT(qv, pbank[1][:, :].rearrange("p (h d) -> p h d", h=8),
                           ssq[k][:, :].unsqueeze(2).to_broadcast([128, 8, 64]), ALU.mult),
                 reads=[pbuf[1], b_ssq[k]], writes=[b_qn[k]])

        def stageC(c, j):
            i = 4 * c + j
            k = i % 2
            qb = c % 2
            qv = qn[k][:].rearrange("p (h d) -> p h d", h=8)
            R.op('dve', TT(qn[k][:], qn[k][:], gqk[:], ALU.mult), reads=[b_qn[k], b_gqk], writes=[b_qn[k]])
            R.op('dve', [TT(tA[k][:], qv[:, :, 0:16], cs1[:, i, :].unsqueeze(1).to_broadcast([128, 8, 16]), ALU.mult),
                         TT(tB[k][:, :, 0:8], qv[:, :, 8:16], sn2[:, i, 0:8].unsqueeze(1).to_broadcast([128, 8, 8]), ALU.mult),
                         TT(tB[k][:, :, 8:16], qv[:, :, 0:8], sn2[:, i, 8:16].unsqueeze(1).to_broadcast([128, 8, 8]), ALU.mult)],
                 reads=[b_qn[k], b_cs1, b_sn2], writes=[b_tA[k], b_tB[k]])
            R.op('act', ACTV(qkb[k][:, :, 16:64], qv[:, :, 16:64], AF.Copy), reads=[b_qn[k]], writes=[b_qkb[k]])
            R.op('dve', TT(qkb[k][:, :, 0:16], tA[k][:], tB[k][:], ALU.add), reads=[b_tA[k], b_tB[k]], writes=[b_qkb[k]])
            ptq = pbf(0).rearrange("p (a b) -> p a b", a=8)
            R.op('pe', [TR(ptq[:, s, :], qkb[k][:, s, :], identb[:]) for s in range(8)],
                 reads=[b_qkb[k], b_identb], writes=[pbuf[0]])
            R.op('act', [ACTV(QxT[qb][0:64, h4, j * 128:(j + 1) * 128], ptq[0:64, h4, :], AF.Copy) for h4 in range(4)],
                 reads=[pbuf[0]], writes=[b_Qx[qb]])
            R.op('act', [ACTV(KxT[0:64, h4, i * 128:(i + 1) * 128], ptq[0:64, 4 + h4, :], AF.Copy) for h4 in range(4)],
                 reads=[pbuf[0]], writes=[b_Kx[c]])
            if i % 2 == 1:
                blk = i // 2
                R.op('dve', RED(kms[0:64, :], KxT[0:64, :, blk * 256:(blk + 1) * 256]),
                     reads=[b_Kx[c]], writes=[b_kms])
                R.op('dve', TS(kmT[0:64, :, blk], kms[0:64, :], 1.0 / 256.0, None, ALU.mult),
                     reads=[b_kms], writes=[b_km[blk]])

        def prep_items(c):
            R.items = []
            for j in range(4):
                stageA(c, j)
                stageB(c, j)
                stageC(c, j)
            return list(R.items)

        def replay(item):
            kind, a_, k_ = item
            getattr(S, kind)(*a_, **k_)

        pending = prep_items(0)
        for c in range(NCH):
            hb = c % 2
            qb = c % 2
            for item in pending:
                replay(item)
            pending = prep_items(c + 1) if c + 1 < NCH else []
            for j in range(4 if _DBG.get('gate', True) else 0):
                i = 4 * c + j
                cur = i // 2
                m = j % 2
                pg = pbank[2][:, 256:320].rearrange("p (h n) -> p h n", h=4)
                fl = [MS(gsb[:, :, cur:cur + 1], BIG)] + ([MS(gsb[:, :, cur + 1:16], -BIG)] if cur < 15 else [])
                S.op('dve', fl, writes=[b_gsb])
                if cur > 0:
                    S.op('pe', [MM(pg[:, h, 0:cur], QxT[qb][0:64, h, j * 128:(j + 1) * 128], kmT[0:64, h, 0:cur])
                                for h in range(4)], reads=[b_Qx[qb]] + b_km[0:cur], writes=[b_pg])
                    S.op('dve', CP(gsb[:, :, 0:cur], pg[:, :, 0:cur]), reads=[b_pg], writes=[b_gsb])
                S.op('dve', [MAX8(mx8[:, h, :], gsb[:, h, :]) for h in range(4)], reads=[b_gsb], writes=[b_mx8])
                S.op('dve', TT(sel[:], gsb[:], mx8[:, :, 3:4].to_broadcast([128, 4, 16]), ALU.is_ge),
                     reads=[b_gsb, b_mx8], writes=[b_sel])
                S.op('dve', TS(mbp[m][:, :, 64:80], sel[:], MASKV, -MASKV, ALU.mult, ALU.add),
                     reads=[b_sel], writes=[b_mbp[m]])
                pmb = pbf(0).rearrange("p (a b) -> p a b", a=8)
                S.op('pe', [TR(pmb[:, h, :], mbp[m][:, h, :], identb[:]) for h in range(4)],
                     reads=[b_mbp[m], b_identb], writes=[pbuf[0]])
                S.op('act', [ACTV(QxT[qb][64:80, h4, j * 128:(j + 1) * 128], pmb[64:80, h4, :], AF.Copy) for h4 in range(4)],
                     reads=[pbuf[0]], writes=[b_Qx[qb]])
            nsteps = 4 * (4 * c + 4)
            stepc = [0]
            for h in range(4 if _DBG.get('attn', True) else 0):
                nk = 4 * c + 4
                pyi = 6 + (h % 2)

                def cols_of(kt):
                    return (0, 512) if kt < 4 * c + 2 else (256, 512)

                def emit_S(kt, r):
                    c0, c1 = cols_of(kt)
                    S.op('pe', MM(pbank[3 + r][:, c0:c1], KxT[0:80, h, kt * 128:(kt + 1) * 128], QxT[qb][0:80, h, c0:c1]),
                         reads=[b_Kx[kt // 4], b_Qx[qb]], writes=[pbuf[3 + r]])
                rs = []
                for kt in range(nk):
                    rs.append(rot[0] % 3)
                    rot[0] += 1
                emit_S(0, rs[0])
                if nk > 1:
                    emit_S(1, rs[1])
                for kt in range(nk):
                    if kt + 2 < nk:
                        emit_S(kt + 2, rs[kt + 2])
                    r = rs[kt]
                    c0, c1 = cols_of(kt)
                    S.op('act', ACTV(pT[r][:, c0:c1], pbank[3 + r][:, c0:c1], AF.Exp, scale=0.125),
                         reads=[pbuf[3 + r]], writes=[b_pT[r]])
                    if kt >= 4 * c:
                        d0 = 0 if kt < 4 * c + 2 else 256
                        S.op('dve', TT(pT[r][:, d0:d0 + 256], pT[r][:, d0:d0 + 256], trib[:, kt % 2, :], ALU.mult),
                             reads=[b_pT[r], b_trib], writes=[b_pT[r]])
                    S.op('pe', MM(pbank[pyi][0:65, c0:c1], Vx[:, kt, h, :], pT[r][:, c0:c1], kt == 0, kt == nk - 1),
                         reads=[b_Vx[kt // 4], b_pT[r]], writes=[pbuf[pyi]])
                    stepc[0] += 1
                    if pending and _DBG.get('ilv', True):
                        left = max(1, nsteps - stepc[0] + 1)
                        for _ in range(-(-len(pending) // left)):
                            replay(pending.pop(0))
                yb = h % 2
                S.op('dve', RECIP(rd[64:65, :], pbank[pyi][64:65, :]), reads=[pbuf[pyi]], writes=[b_rd])
                rb = 3 + (rot[0] % 3)
                rot[0] += 1
                S.op('pe', MM(pbank[rb][0:64, :], onesf[64:65, 0:64], rd[64:65, :]),
                     reads=[b_onesf, b_rd], writes=[pbuf[rb]])
                S.op('act', ACTV(bcs[0:64, :], pbank[rb][0:64, :], AF.Copy), reads=[pbuf[rb]], writes=[b_bcs])
                S.op('dve', TT(yo[yb][0:64, :], pbank[pyi][0:64, :], bcs[0:64, :], ALU.mult),
                     reads=[pbuf[pyi], b_bcs], writes=[b_yo[yb]])
                hg = hh * 4 + h
                S.dma(DMA(ya_d[hg * 64:(hg + 1) * 64, c * 512:(c + 1) * 512], yo[yb][0:64, :]),
                      b_yo[yb], reads=[b_yo[yb]])
    S.barrier()
    A.off = PERSIST
    if stop_after == 'A':
        S.emit()
        return nc
    wB = A.alloc("wB", [128, 8, 3584], BF16); b_wB = S.buf("wB")
    wpa = A.alloc("wpa", [128, 4, D], BF16); b_wpa = S.buf("wpa")
    wpb = A.alloc("wpb", [128, 4, D], BF16); b_wpb = S.buf("wpb")
    wo = A.alloc("wo", [128, 8, D], BF16); b_wo = S.buf("wo")
    stgB = [A.alloc("stgB", [128, 2048], F32) for _ in range(2)]
    b_stgB = [S.buf("stgB%d" % i) for i in range(2)]
    sidx = [0]

    def load_cast(dst, src_ap, shape3, b_dst, extra=None):
        k = sidx[0] % len(stgB)
        sidx[0] += 1
        a, bb = shape3
        view = stgB[k][:, 0:a * bb].rearrange("p (a b) -> p a b", a=a)
        S.dma(DMA(view, src_ap), b_stgB[k], writes=[b_stgB[k]])
        if extra is None:
            ce = cast_eng()
            S.op(ce, CAST(ce, dst, view), reads=[b_stgB[k]], writes=[b_dst])
        else:
            ex, b_ex = extra
            S.op('dve', [TT(dst[:, q, :], view[:, q, :], ex, ALU.mult) for q in range(a)],
                 reads=[b_stgB[k], b_ex], writes=[b_dst])

    for p in range(14):
        c0 = 1536 + p * 256
        load_cast(wB[:, :, p * 256:(p + 1) * 256], win_d[:, c0:c0 + 256].rearrange("(kc p) n -> p kc n", p=128),
                  (8, 256), b_wB)
    for q2 in range(2):
        load_cast(wpa[:, 2 * q2:2 * q2 + 2, :], wpa_d[q2 * 256:(q2 + 1) * 256, :].rearrange("(cc p) n -> p cc n", p=128),
                  (2, D), b_wpa)
        load_cast(wpb[:, 2 * q2:2 * q2 + 2, :], wpb_d[q2 * 256:(q2 + 1) * 256, :].rearrange("(cc p) n -> p cc n", p=128),
                  (2, D), b_wpb)
    for q4 in range(4):
        load_cast(wo[:, 2 * q4:2 * q4 + 2, :], wo_d[q4 * 256:(q4 + 1) * 256, :].rearrange("(cc p) n -> p cc n", p=128),
                  (2, D), b_wo)
    nbB = make_norm_bufs()
    hTB = A.alloc("hTB", [128, 8, 512], BF16); b_hTB = S.buf("hTB")
    yaT = A.alloc("yaT", [128, 4, 512], BF16); b_yaT = S.buf("yaT")
    xbs = A.alloc("xbs", [128, 512], F32); b_xbs = S.buf("xbs")
    bgs = A.alloc("bgs", [128, 512], F32); b_bgs = S.buf("bgs")
    u = A.alloc("u", [128, 4, 514], F32); b_u = [S.buf("u%d" % i) for i in range(4)]
    tcv = A.alloc("tcv", [128, 512], F32); b_tcv = S.buf("tcv")
    ybT = A.alloc("ybT", [128, 4, 512], BF16); b_ybT = S.buf("ybT")
    gas = A.alloc("gas", [128, 512], F32); b_gas = S.buf("gas")
    gbs = A.alloc("gbs", [128, 512], F32); b_gbs = S.buf("gbs")
    t1 = A.alloc("t1", [128, 512], F32); b_t1 = S.buf("t1")
    t2 = A.alloc("t2", [128, 512], F32); b_t2 = S.buf("t2")
    mT = A.alloc("mT", [128, 8, 512], BF16); b_mT = S.buf("mT")
    xr = [A.alloc("xr", [128, D], F32) for _ in range(2)]
    b_xr = [S.buf("xr%d" % i) for i in range(2)]
    to = A.alloc("to", [128, 512], F32); b_to = S.buf("to")
    x1t = [A.alloc("x1t", [128, D], F32) for _ in range(2)]
    b_x1t = [S.buf("x1t%d" % i) for i in range(2)]
    S.op('dve', MS(u[:], 0.0), writes=b_u)
    for c in range(8):
        for j in range(4):
            i = 4 * c + j
            k = i % 2
            S.dma(DMA(nbB['xt'][k][:], x_d[i * 128:(i + 1) * 128, :]), nbB['b_xt'][k], writes=[nbB['b_xt'][k]])
            norm_tile(nbB, i, nbB['xt'][k][:], nbB['b_xt'][k], hTB[:, :, j * 128:(j + 1) * 128], b_hTB, 0)
        S.dma(DMA(yaT[:], ya_d[:, c * 512:(c + 1) * 512].rearrange("(cc p) n -> p cc n", p=128)), b_yaT, writes=[b_yaT])
        for cc in range(4):
            for bank, col0 in [(1, 0), (2, 512), (3, 1024)]:
                S.op('pe', [MM(pbank[bank][:, :], wB[:, kc, col0 + cc * 128:col0 + (cc + 1) * 128], hTB[:, kc, :], kc == 0, kc == 7)
                            for kc in range(8)], reads=[b_wB, b_hTB], writes=[pbuf[bank]])
            S.op('act', ACTV(xbs[:], pbank[1][:, :], AF.Copy), reads=[pbuf[1]], writes=[b_xbs])
            if c > 0:
                S.op('dve', CP(u[:, cc, 0:2], u[:, cc, 512:514]), reads=[b_u[cc]], writes=[b_u[cc]])
            S.op('dve', TT(u[:, cc, 2:514], pbank[3][:, :], xbs[:], ALU.mult), reads=[pbuf[3], b_xbs], writes=[b_u[cc]])
            S.op('act', ACTV(bgs[:], pbank[2][:, :], AF.Copy), reads=[pbuf[2]], writes=[b_bgs])
            S.op('dve', TS(tcv[:], u[:, cc, 0:512], convw[:, cc, 0:1], None, ALU.mult),
                 reads=[b_u[cc], b_convw], writes=[b_tcv])
            S.op('dve', STT(tcv[:], u[:, cc, 1:513], convw[:, cc, 1:2], tcv[:], ALU.mult, ALU.add),
                 reads=[b_u[cc], b_convw, b_tcv], writes=[b_tcv])
            S.op('dve', STT(tcv[:], u[:, cc, 2:514], convw[:, cc, 2:3], tcv[:], ALU.mult, ALU.add),
                 reads=[b_u[cc], b_convw, b_tcv], writes=[b_tcv])
            S.op('dve', STT(ybT[:, cc, :], tcv[:], convb[:, cc:cc + 1], bgs[:], ALU.add, ALU.mult),
                 reads=[b_tcv, b_convb, b_bgs], writes=[b_ybT])
        for m in range(8):
            S.op('pe', [MM(pbank[4][:, :], wB[:, kc, 1536 + m * 128:1536 + (m + 1) * 128], hTB[:, kc, :], kc == 0, kc == 7)
                        for kc in range(8)], reads=[b_wB, b_hTB], writes=[pbuf[4]])
            S.op('pe', [MM(pbank[5][:, :], wB[:, kc, 2560 + m * 128:2560 + (m + 1) * 128], hTB[:, kc, :], kc == 0, kc == 7)
                        for kc in range(8)], reads=[b_wB, b_hTB], writes=[pbuf[5]])
            S.op('pe', [MM(pbank[6][:, :], wpa[:, cc, m * 128:(m + 1) * 128], yaT[:, cc, :], cc == 0, cc == 3)
                        for cc in range(4)], reads=[b_wpa, b_yaT], writes=[pbuf[6]])
            S.op('pe', [MM(pbank[7][:, :], wpb[:, cc, m * 128:(m + 1) * 128], ybT[:, cc, :], cc == 0, cc == 3)
                        for cc in range(4)], reads=[b_wpb, b_ybT], writes=[pbuf[7]])
            S.op('act', ACTV(gas[:], pbank[4][:, :], AF.Sigmoid), reads=[pbuf[4]], writes=[b_gas])
            S.op('act', ACTV(gbs[:], pbank[5][:, :], AF.Sigmoid), reads=[pbuf[5]], writes=[b_gbs])
            S.op('dve', TT(t1[:], pbank[6][:, :], gas[:], ALU.mult), reads=[pbuf[6], b_gas], writes=[b_t1])
            S.op('dve', TT(t2[:], pbank[7][:, :], gbs[:], ALU.mult), reads=[pbuf[7], b_gbs], writes=[b_t2])
            S.op('dve', TT(mT[:, m, :], t1[:], t2[:], ALU.add), reads=[b_t1, b_t2], writes=[b_mT])
        for j in range(4):
            i = 4 * c + j
            k = i % 2
            S.dma(DMA(xr[k][:], x_d[i * 128:(i + 1) * 128, :]), b_xr[k], writes=[b_xr[k]])
            for hf in range(2):
                S.op('pe', [MM(pbank[1][:, :], mT[:, m, j * 128:(j + 1) * 128], wo[:, m, hf * 512:(hf + 1) * 512], m == 0, m == 7)
                            for m in range(8)], reads=[b_mT, b_wo], writes=[pbuf[1]])
                S.op('dve', TT(to[:], pbank[1][:, :], gabc[:, 0, hf * 512:(hf + 1) * 512], ALU.mult),
                     reads=[pbuf[1], b_gabc], writes=[b_to])
                S.op('dve', TT(x1t[k][:, hf * 512:(hf + 1) * 512], to[:], xr[k][:, hf * 512:(hf + 1) * 512], ALU.add),
                     reads=[b_to, b_xr[k]], writes=[b_x1t[k]])
            S.dma(DMA(out_d[i * 128:(i + 1) * 128, :], x1t[k][:]), b_x1t[k], reads=[b_x1t[k]])
    S.barrier()
    A.off = PERSIST
    if stop_after == 'B':
        S.emit()
        return nc
    if _SPARSE:
        _sparse_moe(nc, S, A, locals())
        S.emit()
        return nc
    h2T = A.alloc("h2T", [128, 8, 2048], BF16); b_h2T = [S.buf("h2T%d" % i) for i in range(4)]
    acc = A.alloc("acc", [128, 16, D], F32); b_acc = [S.buf("acc%d" % i) for i in range(16)]
    cw = A.alloc("cw", [128, 16, 32], F32); b_cw = [S.buf("cw%d" % i) for i in range(16)]
    w1b = [A.alloc("w1b", [128, 8, 512], BF16) for _ in range(2)]; b_w1b = [S.buf("w1b%d" % i) for i in range(2)]
    w3b = [A.alloc("w3b", [128, 8, 512], BF16) for _ in range(2)]; b_w3b = [S.buf("w3b%d" % i) for i in range(2)]
    w2b = [A.alloc("w2b", [128, 4, D], BF16) for _ in range(2)]; b_w2b = [S.buf("w2b%d" % i) for i in range(2)]
    stgC = [A.alloc("stgC", [128, 2048], F32) for _ in range(2)]
    b_stgC = [S.buf("stgC%d" % i) for i in range(2)]
    stgB[:] = stgC
    b_stgB[:] = b_stgC
    nbC = make_norm_bufs(with_xt=True, with_junk=False)
    hidT = [A.alloc("hidT", [128, 4, 512], BF16) for _ in range(2)]; b_hid = [S.buf("hid%d" % i) for i in range(2)]
    sil = [A.alloc("sil", [128, 512], F32) for _ in range(2)]; b_sil = [S.buf("sil%d" % i) for i in range(2)]
    nbC['junk'] = hidT[0][:, :, :].rearrange("p a b -> p (a b)")[:, 0:D]
    nbC['b_junk'] = b_hid[0]
    lg = A.alloc("lg", [128, 36], F32); b_lg = S.buf("lg")
    rt = A.alloc("rt", [128, 16], F32); b_rt = S.buf("rt")
    goh = A.alloc("goh", [128, 4], F32); b_goh = S.buf("goh")
    gex = A.alloc("gex", [128, 4], F32); b_gex = S.buf("gex")
    pen = A.alloc("pen", [128, 4], F32); b_pen = S.buf("pen")
    em = A.alloc("em", [128, 32], F32); b_em = S.buf("em")
    emc = A.alloc("emc", [128, 32], F32); b_emc = S.buf("emc")
    mx8c = A.alloc("mx8c", [128, 8], F32); b_mx8c = S.buf("mx8c")
    selc = A.alloc("selc", [128, 32], F32); b_selc = S.buf("selc")
    ex = A.alloc("ex", [128, 32], F32); b_ex = S.buf("ex")
    exs = A.alloc("exs", [128, 32], F32); b_exs = S.buf("exs")
    gmax, ngmax, gsum, nm1, den, fsc = [rt[:, q:q + 1] for q in range(6)]
    PEN = 1.0e4
    for hf in range(2):
        for ti in range(16):
            i = hf * 16 + ti
            k = i % 2
            S.dma(DMA(nbC['xt'][k][:], out_d[i * 128:(i + 1) * 128, :]), nbC['b_xt'][k], writes=[nbC['b_xt'][k]])
            norm_tile(nbC, i, nbC['xt'][k][:], nbC['b_xt'][k], h2T[:, :, ti * 128:(ti + 1) * 128], b_h2T[ti // 4], 16)
            S.op('pool', MS(acc[:, ti, :], 0.0), writes=[b_acc[ti]])
            S.op('pe', [MM(pbank[1][:, 0:36], h2T[:, kc, ti * 128:(ti + 1) * 128], wrb[:, kc, :], kc == 0, kc == 7)
                        for kc in range(8)], reads=[b_h2T[ti // 4], b_wrb], writes=[pbuf[1]])
            S.op('dve', TT(lg[:], pbank[1][:, 0:36], brbc[:], ALU.add), reads=[pbuf[1], b_brbc], writes=[b_lg])
            S.op('dve', RED(gmax, lg[:, 0:4], ALU.max), reads=[b_lg], writes=[b_rt])
            S.op('dve', TS(ngmax, gmax, -1.0, None, ALU.mult), reads=[b_rt], writes=[b_rt])
            S.op('dve', TS(goh[:], lg[:, 0:4], gmax, None, ALU.is_ge), reads=[b_lg, b_rt], writes=[b_goh])
            S.op('act', ACTV(gex[:], lg[:, 0:4], AF.Exp, bias=ngmax, accum_out=gsum),
                 reads=[b_lg, b_rt], writes=[b_gex, b_rt])
            S.op('dve', RECIP(gsum, gsum), reads=[b_rt], writes=[b_rt])
            S.op('dve', TS(pen[:], goh[:], PEN, -PEN, ALU.mult, ALU.add), reads=[b_goh], writes=[b_pen])
            S.op('dve', TT(em[:].rearrange("p (g e) -> p g e", g=4), lg[:, 4:36].rearrange("p (g e) -> p g e", g=4),
                           pen[:, :].unsqueeze(2).to_broadcast([128, 4, 8]), ALU.add),
                 reads=[b_lg, b_pen], writes=[b_em])
            S.op('dve', MAX8(mx8c[:], em[:]), reads=[b_em], writes=[b_mx8c])
            S.op('dve', TS(nm1, mx8c[:, 0:1], -1.0, None, ALU.mult), reads=[b_mx8c], writes=[b_rt])
            S.op('dve', TS(selc[:], em[:], mx8c[:, 1:2], None, ALU.is_ge), reads=[b_em, b_mx8c], writes=[b_selc])
            S.op('dve', TS(emc[:], em[:], mx8c[:, 1:2], None, ALU.max), reads=[b_em, b_mx8c], writes=[b_emc])
            S.op('act', ACTV(ex[:], emc[:], AF.Exp, bias=nm1), reads=[b_emc, b_rt], writes=[b_ex])
            S.op('dve', TT(exs[:], ex[:], selc[:], ALU.mult), reads=[b_ex, b_selc], writes=[b_exs])
            S.op('dve', RED(den, exs[:]), reads=[b_exs], writes=[b_rt])
            S.op('dve', RECIP(den, den), reads=[b_rt], writes=[b_rt])
            S.op('dve', TT(fsc, den, gsum, ALU.mult), reads=[b_rt], writes=[b_rt])
            S.op('dve', TS(cw[:, ti, :], exs[:], fsc, None, ALU.mult), reads=[b_exs, b_rt], writes=[b_cw[ti]])
        for e in range(32):
            wb = e % 2
            for q2 in range(2):
                load_cast(w1b[wb][:, 4 * q2:4 * q2 + 4, :],
                          w1_d[e, q2 * 512:(q2 + 1) * 512, :].rearrange("(kc p) n -> p kc n", p=128), (4, 512), b_w1b[wb])
                load_cast(w3b[wb][:, 4 * q2:4 * q2 + 4, :],
                          w3_d[e, q2 * 512:(q2 + 1) * 512, :].rearrange("(kc p) n -> p kc n", p=128), (4, 512), b_w3b[wb])
            for q2 in range(2):
                load_cast(w2b[wb][:, 2 * q2:2 * q2 + 2, :],
                          w2_d[e, q2 * 256:(q2 + 1) * 256, :].rearrange("(fc p) n -> p fc n", p=128), (2, D), b_w2b[wb])
            for ch in range(4):
                hk = (e * 4 + ch) % 2
                for fc in range(4):
                    pb1 = 2 + (fc % 2)
                    pb3 = 4 + (fc % 2)
                    sk = fc % 2
                    S.op('pe', [MM(pbank[pb1][:, :], w1b[wb][:, kc, fc * 128:(fc + 1) * 128], h2T[:, kc, ch * 512:(ch + 1) * 512],
                                   kc == 0, kc == 7) for kc in range(8)], reads=[b_w1b[wb], b_h2T[ch]], writes=[pbuf[pb1]])
                    S.op('pe', [MM(pbank[pb3][:, :], w3b[wb][:, kc, fc * 128:(fc + 1) * 128], h2T[:, kc, ch * 512:(ch + 1) * 512],
                                   kc == 0, kc == 7) for kc in range(8)], reads=[b_w3b[wb], b_h2T[ch]], writes=[pbuf[pb3]])
                    S.op('act', ACTV(sil[sk][:], pbank[pb1][:, :], AF.Silu), reads=[pbuf[pb1]], writes=[b_sil[sk]])
                    S.op('dve', TT(hidT[hk][:, fc, :], pbank[pb3][:, :], sil[sk][:], ALU.mult),
                         reads=[pbuf[pb3], b_sil[sk]], writes=[b_hid[hk]])
                for j in range(4):
                    ti = ch * 4 + j
                    for hf2 in range(2):
                        po = 6 + hf2
                        S.op('pe', [MM(pbank[po][:, :], hidT[hk][:, fc, j * 128:(j + 1) * 128],
                                       w2b[wb][:, fc, hf2 * 512:(hf2 + 1) * 512], fc == 0, fc == 3) for fc in range(4)],
                             reads=[b_hid[hk], b_w2b[wb]], writes=[pbuf[po]])
                        S.op('dve', STT(acc[:, ti, hf2 * 512:(hf2 + 1) * 512], pbank[po][:, :], cw[:, ti, e:e + 1],
                                        acc[:, ti, hf2 * 512:(hf2 + 1) * 512], ALU.mult, ALU.add),
                             reads=[pbuf[po], b_cw[ti], b_acc[ti]], writes=[b_acc[ti]])
        for ti in range(16):
            i = hf * 16 + ti
            k = i % 2
            S.dma(DMA(nbC['xt'][k][:], out_d[i * 128:(i + 1) * 128, :]), nbC['b_xt'][k], writes=[nbC['b_xt'][k]])
            S.op('dve', TT(acc[:, ti, :], acc[:, ti, :], gabc[:, 1, :], ALU.mult), reads=[b_acc[ti], b_gabc], writes=[b_acc[ti]])
            S.op('dve', TT(acc[:, ti, :], acc[:, ti, :], nbC['xt'][k][:], ALU.add),
                 reads=[b_acc[ti], nbC['b_xt'][k]], writes=[b_acc[ti]])
            S.dma(DMA(out_d[i * 128:(i + 1) * 128, :], acc[:, ti, :]), b_acc[ti], reads=[b_acc[ti]])
    S.barrier()
    S.emit()
    return nc


def _consts():
    pos = np.arange(S_LEN, dtype=np.float32)
    inv = (np.float32(500000.0) ** (-np.arange(0, 16, 2, dtype=np.float32) / np.float32(16))).astype(np.float32)
    ang = (pos[:, None] * inv[None, :]).astype(np.float32)
    cos = np.cos(ang).astype(np.float32).reshape(NT, 128, 8).transpose(1, 0, 2)
    sin = np.sin(ang).astype(np.float32).reshape(NT, 128, 8).transpose(1, 0, 2)
    cs1 = np.concatenate([cos, cos], -1).reshape(128, NT * 16)
    sn2 = np.concatenate([-sin, sin], -1).reshape(128, NT * 16)
    kp = np.arange(128)[:, None, None]
    jj = np.arange(2)[None, :, None]
    qq = np.arange(256)[None, None, :]
    tri = (jj * 128 + kp <= qq).astype(np.float32).reshape(128, 512)
    oneh = (np.arange(S_LEN)[None, :] // 256 == np.arange(16)[:, None]).astype(np.float32)
    mconst = np.zeros((128, 225), np.float32)
    tt = np.arange(128)
    mconst[:, 0:128] = (tt[:, None] < tt[None, :]).astype(np.float32)
    ee = np.arange(32)
    mconst[0:32, 128:160] = (ee[:, None] < ee[None, :]).astype(np.float32)
    mconst[:, 160:176] = (512.0 * np.arange(16))[None, :]
    mconst[:, 176:224] = np.arange(48, dtype=np.float32)[None, :]
    mconst[:, 224] = np.arange(128, dtype=np.float32)
    return dict(cs1=np.ascontiguousarray(cs1), sn2=np.ascontiguousarray(sn2), tri=tri, oneh=oneh, mconst=mconst,
                ident=np.eye(128, dtype=np.float32))


def kernel(x, c, w_ada, b_ada, g_norm1, g_norm2, w_in, g_q, g_k, conv_w, conv_b,
           w_pa, w_pb, w_o, w_rg, b_rg, w_re, b_re, w1, w3, w2):
    f = lambda a: np.ascontiguousarray(np.asarray(a, dtype=np.float32))
    x = f(x); c = f(c)
    cst = _consts()
    gqk = np.concatenate([np.tile(f(g_q)[0], 4), np.tile(f(g_k)[0], 4)])
    gqk = np.ascontiguousarray(np.broadcast_to(gqk[None, :], (128, 512)))
    convw = np.ascontiguousarray(f(conv_w)[0].reshape(3, 4, 128).transpose(2, 1, 0).reshape(128, 12))
    convb = np.ascontiguousarray(f(conv_b)[0].reshape(4, 128).T)
    wr = np.ascontiguousarray(np.concatenate([f(w_rg)[0], f(w_re)[0]], axis=1))
    br = np.concatenate([f(b_rg)[0], f(b_re)[0]])
    br = np.ascontiguousarray(np.broadcast_to(br[None, :], (128, 36)))
    shared = dict(w_ada=f(w_ada)[0], b_ada=f(b_ada)[0:1], g1=f(g_norm1)[0:1], g2=f(g_norm2)[0:1], w_in=f(w_in)[0],
                  gqk=gqk, convw=convw, convb=convb, w_pa=f(w_pa)[0], w_pb=f(w_pb)[0], w_o=f(w_o)[0],
                  wr=wr, br=br, w1=f(w1)[0], w3=f(w3)[0], w2=f(w2)[0], **cst)
    n = _NCORES
    in_maps = []
    for b in range(n):
        m = dict(shared)
        m["x"] = x[b]
        m["cT"] = np.ascontiguousarray(c[b].reshape(8, 128).T)
        in_maps.append(m)
    if _STOP_AFTER is not None:
        for m in in_maps:
            for kk in ('w1', 'w3', 'w2'):
                m.pop(kk)
    nc = build_program(_STOP_AFTER)
    res = run_bass_kernel_spmd(nc, in_maps, core_ids=list(range(n)))
    if _STOP_AFTER is not None:
        global _DBG_RES
        _DBG_RES = res.results
    out = np.stack([np.asarray(res.results[b]["out"], dtype=np.float32).reshape(S_LEN, D) for b in range(n)])
    return out
```

```python
import numpy as np
import concourse.bass as bass
import concourse.mybir as mybir
from concourse.bass_utils import run_bass_kernel_spmd

F32 = mybir.dt.float32
BF16 = mybir.dt.bfloat16
ALU = mybir.AluOpType
AF = mybir.ActivationFunctionType
AX = mybir.AxisListType

S_LEN = 4096
D = 1024
NT = 32
BIG = 1.0e30
MASKV = 30000.0
_STOP_AFTER = None
_NCORES = 8
_DBG = {}
_SPARSE = True


class Buf:
    def __init__(self, name):
        self.name = name
        self.lw = None
        self.rd = {}
        self.dsem = {}
        self.dcnt = {}


class Sched:
    def __init__(self, nc):
        self.nc = nc
        self.engs = ['pe', 'act', 'dve', 'pool', 'sp']
        self.q = {e: [] for e in self.engs}
        self.sems = []
        self.esem = {}
        for e in ['pe', 'act', 'dve', 'pool']:
            self.esem[e] = self.new_sem('s_' + e)
        self.cnt = {e: 0 for e in self.esem}
        self.seen = {e: {} for e in self.engs}
        self.bufs = []

    def new_sem(self, name):
        s = self.nc.alloc_semaphore('%s_%d' % (name, len(self.sems)))
        self.sems.append(s)
        return len(self.sems) - 1

    def buf(self, name):
        b = Buf(name)
        self.bufs.append(b)
        return b

    def _waits(self, eng, reads, writes):
        need = {}

        def add(s, v):
            if need.get(s, 0) < v:
                need[s] = v
        for b in reads:
            if b.lw is not None:
                add(*b.lw)
        for b in writes:
            if b.lw is not None:
                add(*b.lw)
            for s, v in b.rd.items():
                add(s, v)
        seen = self.seen[eng]
        out = []
        for s, v in need.items():
            if eng == 'pe' and s == self.esem['pe']:
                continue
            if seen.get(s, 0) < v:
                seen[s] = v
                out.append((s, v))
        return out

    def _commit(self, ev, reads, writes):
        s, v = ev
        for b in reads:
            if b.rd.get(s, 0) < v:
                b.rd[s] = v
        for b in writes:
            b.lw = ev
            b.rd = {}

    def op(self, eng, fns, reads=(), writes=()):
        if callable(fns):
            fns = [fns]
        waits = self._waits(eng, reads, writes)
        self.cnt[eng] += 1
        ev = (self.esem[eng], self.cnt[eng])
        self.q[eng].append((waits, fns, ev[0], 1))
        self._commit(ev, reads, writes)

    def dma(self, fn, own, reads=(), writes=(), q='sp'):
        if q not in own.dsem:
            own.dsem[q] = self.new_sem('d_' + own.name + '_' + q)
            own.dcnt[q] = 0
        waits = self._waits(q, reads, writes)
        own.dcnt[q] += 16
        ev = (own.dsem[q], own.dcnt[q])
        self.q[q].append((waits, [fn], ev[0], 16))
        self._commit(ev, reads, writes)

    def barrier(self):
        evs = [(self.esem[e], self.cnt[e]) for e in self.esem if self.cnt[e] > 0]
        for b in self.bufs:
            for qq, sm in b.dsem.items():
                evs.append((sm, b.dcnt[qq]))
        for e in self.engs:
            seen = self.seen[e]
            waits = []
            for s, v in evs:
                if e == 'pe' and s == self.esem['pe']:
                    continue
                if seen.get(s, 0) < v:
                    seen[s] = v
                    waits.append((s, v))
            if waits:
                self.q[e].append((waits, [], None, 0))

    def emit(self):
        nc = self.nc
        sems = self.sems

        def replay(name, e):
            for waits, fns, s, inc in self.q[name]:
                for (ws, wv) in waits:
                    e.wait_ge(sems[ws], wv)
                ins = None
                for fn in fns:
                    ins = fn(e)
                if ins is not None and s is not None:
                    ins.then_inc(sems[s], inc)
        with nc.Block() as block:
            @block.tensor
            def _(e):
                replay('pe', e)

            @block.scalar
            def _(e):
                replay('act', e)

            @block.vector
            def _(e):
                replay('dve', e)

            @block.gpsimd
            def _(e):
                replay('pool', e)

            @block.sync
            def _(e):
                replay('sp', e)


class Rec:
    def __init__(self):
        self.items = []

    def op(self, *a, **k):
        self.items.append(('op', a, k))

    def dma(self, *a, **k):
        self.items.append(('dma', a, k))


class Arena:
    LO = 16640
    HI = 229376

    def __init__(self, nc):
        self.nc = nc
        self.off = self.LO
        self.n = 0

    def alloc(self, name, shape, dt):
        esz = 2 if dt == BF16 else 4
        nbytes = int(np.prod(shape[1:])) * esz
        off = (self.off + 31) // 32 * 32
        assert off + nbytes <= self.HI, ("SBUF overflow", name, off, nbytes)
        self.n += 1
        t = self.nc.alloc_sbuf_tensor_at("%s_%d" % (name, self.n), list(shape), dt, offset=off)
        self.off = off + nbytes
        return t


def MM(out, lhsT, rhs, start=True, stop=True):
    return lambda e: e.matmul(out, lhsT=lhsT, rhs=rhs, start=start, stop=stop)


def TR(out, in_, ident):
    return lambda e: e.transpose(out=out, in_=in_, identity=ident)


def ACTV(out, in_, func, **kw):
    return lambda e: e.activation(out=out, in_=in_, func=func, **kw)


def TT(out, in0, in1, op):
    return lambda e: e.tensor_tensor(out=out, in0=in0, in1=in1, op=op)


def TS(out, in0, s1, s2, op0, op1=None):
    if op1 is None:
        return lambda e: e.tensor_scalar(out=out, in0=in0, scalar1=s1, scalar2=None, op0=op0)
    return lambda e: e.tensor_scalar(out=out, in0=in0, scalar1=s1, scalar2=s2, op0=op0, op1=op1)


def STT(out, in0, scalar, in1, op0, op1):
    return lambda e: e.scalar_tensor_tensor(out=out, in0=in0, scalar=scalar, in1=in1, op0=op0, op1=op1)


def CP(out, in_):
    return lambda e: e.tensor_copy(out=out, in_=in_)


def MS(ap, v):
    return lambda e: e.memset(ap, v)


def RED(out, in_, op=None):
    return lambda e: e.tensor_reduce(out=out, in_=in_, axis=AX.X, op=(op or ALU.add))


def RECIP(out, in_):
    return lambda e: e.reciprocal(out=out, in_=in_)


def MAX8(out, in_):
    return lambda e: e.max(out=out, in_=in_)


def DMA(out, in_):
    return lambda e: e.dma_start(out=out, in_=in_)


def CAST(eng, out, in_):
    if eng == 'act':
        return ACTV(out, in_, AF.Copy)
    return CP(out, in_)


def IDMA_G(out, table, idx):
    return lambda e: e.indirect_dma_start(out=out, out_offset=None, in_=table,
                                          in_offset=bass.IndirectOffsetOnAxis(ap=idx, axis=0))


def IDMA_S(table, idx, in_):
    return lambda e: e.indirect_dma_start(out=table, out_offset=bass.IndirectOffsetOnAxis(ap=idx, axis=0),
                                          in_=in_, in_offset=None)


I32 = mybir.dt.int32


def _sparse_moe(nc, S, A, G):
    pbank, pbuf, identb, b_identb = G['pbank'], G['pbuf'], G['identb'], G['b_identb']
    modT, b_modT, gabc, b_gabc = G['modT'], G['b_modT'], G['gabc'], G['b_gabc']
    epsb, b_epsb, wrb, b_wrb, brbc, b_brbc = G['epsb'], G['b_epsb'], G['wrb'], G['b_wrb'], G['brbc'], G['b_brbc']
    out_d, mc_d, w1_d, w3_d, w2_d = G['out_d'], G['mc_d'], G['w1_d'], G['w3_d'], G['w2_d']
    pbf = G['pbf']
    NBLK, BLKR = 48, 512
    xs_d = nc.dram_tensor("xs_scratch", [NBLK * BLKR, D], BF16).ap()
    ys_d = nc.dram_tensor("ys_scratch", [NBLK * BLKR, D], F32).ap()
    w1t = w1_d.rearrange("e (p k) n -> (e p) (k n)", k=8)
    w3t = w3_d.rearrange("e (p k) n -> (e p) (k n)", k=8)
    w2t = w2_d.rearrange("e (p k) n -> (e p) (k n)", k=4)
    T = lambda name, shape, dt: (A.alloc(name, shape, dt), S.buf(name))
    mc, b_mc = T("mc", [128, 225], F32)
    ltri, b_ltri = T("ltri", [128, 128], BF16)
    onesb, b_onesb = T("onesb", [128, 128], BF16)
    ustr, b_ustr = T("ustr", [128, 32], BF16)
    modTp, b_modTp = G['modTp'], G['b_modTp']
    selall, b_selall = T("selall", [128, 32, 32], F32)
    selAall, b_selAall = T("selAall", [128, 32, 32], F32)
    cwall, b_cwall = T("cwall", [128, 32, 32], F32)
    rankall, b_rankall = T("rankall", [128, 32, 32], F32)
    csum, b_csum = T("csum", [128, 32], F32)
    dallf, b_dallf = T("dallf", [128, 64], F32)
    dalli, b_dalli = T("dalli", [128, 64], I32)
    wall, b_wall = T("wall", [128, 64], F32)
    widxf, b_widxf = T("widxf", [128, NBLK], F32)
    widxi, b_widxi = T("widxi", [128, NBLK], I32)
    xt = [A.alloc("xtC", [128, D], F32) for _ in range(2)]; b_xt = [S.buf("xtC%d" % i) for i in range(2)]
    ss = [A.alloc("ssC", [128, 1], F32) for _ in range(2)]; b_ss = [S.buf("ssC%d" % i) for i in range(2)]
    junk, b_junk = T("junkC", [128, D], BF16)
    h2Tt, b_h2Tt = T("h2Tt", [128, 8, 128], BF16)
    lg, b_lg = T("lgC", [128, 36], F32)
    rt, b_rt = T("rtC", [128, 16], F32)
    goh, b_goh = T("gohC", [128, 4], F32)
    gex, b_gex = T("gexC", [128, 4], F32)
    pen, b_pen = T("penC", [128, 4], F32)
    em, b_em = T("emC", [128, 32], F32)
    emc, b_emc = T("emcC", [128, 32], F32)
    mx8c, b_mx8c = T("mx8cC", [128, 8], F32)
    ex, b_ex = T("exC", [128, 32], F32)
    exs, b_exs = T("exsC", [128, 32], F32)
    selb, b_selb = T("selbC", [128, 32], BF16)
    tm, b_tm = T("tmC", [128, 32], F32)
    tm2, b_tm2 = T("tm2C", [128, 32], F32)
    big3, b_big3 = T("big3C", [128, 48 * 32], F32)
    nblk, b_nblk = T("nblkC", [128, 32], F32)
    nbpad, b_nbpad = T("nbpadC", [128, 128], BF16)
    nbT, b_nbT = T("nbTC", [128, 128], BF16)
    pss, b_pss = T("pssC", [128, 32], F32)
    pend, b_pend = T("pendC", [128, 32], F32)
    bex, b_bex = T("bexC", [128, NBLK], F32)
    gmax, ngmax, gsum, nm1, den, fsc = [rt[:, q:q + 1] for q in range(6)]
    PEN = 1.0e4
    MARK = A.off
    xnall = A.alloc("xnall", [128, 32, D], BF16); b_xnall = [S.buf("xnall%d" % i) for i in range(32)]

    zt, b_zt = T("zt", [128, 4, D], BF16)
    S.op('pool', MS(zt[:], 0.0), writes=[b_zt])
    for b in range(NBLK):
        S.dma(DMA(xs_d[b * BLKR:(b + 1) * BLKR, :].rearrange("(s p) d -> p s d", p=128), zt[:]), b_zt, reads=[b_zt], q='pool')
    S.dma(DMA(mc[:], mc_d[:, :]), b_mc, writes=[b_mc])
    S.op('dve', [CP(ltri[:], mc[:, 0:128]), CP(ustr[0:32, :], mc[0:32, 128:160])], reads=[b_mc], writes=[b_ltri, b_ustr])
    S.op('pool', [MS(onesb[:], 1.0), MS(csum[:], 0.0), MS(nbpad[:], 0.0)], writes=[b_onesb, b_csum, b_nbpad])
    for i in range(32):
        k = i % 2
        S.dma(DMA(xt[k][:], out_d[i * 128:(i + 1) * 128, :]), b_xt[k], writes=[b_xt[k]])
        S.op('act', ACTV(junk[:], xt[k][:], AF.Square, scale=1.0 / 32.0, accum_out=ss[k][:]),
             reads=[b_xt[k]], writes=[b_junk, b_ss[k]])
        S.op('act', ACTV(ss[k][:], ss[k][:], AF.Sqrt, bias=epsb[:]), reads=[b_ss[k], b_epsb], writes=[b_ss[k]])
        S.op('dve', RECIP(ss[k][:], ss[k][:]), reads=[b_ss[k]], writes=[b_ss[k]])
        S.op('dve', TS(xnall[:, i, :], xt[k][:], ss[k][:, 0:1], None, ALU.mult), reads=[b_xt[k], b_ss[k]], writes=[b_xnall[i]])
        pv = pbf(0).rearrange("p (a b) -> p a b", a=8)
        S.op('pe', [TR(pv[:, kc, :], xnall[:, i, kc * 128:(kc + 1) * 128], identb[:]) for kc in range(8)],
             reads=[b_xnall[i], b_identb], writes=[pbuf[0]])
        S.op('act', [ACTV(h2Tt[:, kc, :], pv[:, kc, :], AF.Identity, scale=modT[:, 16 + kc:17 + kc],
                          bias=modT[:, 24 + kc:25 + kc]) for kc in range(8)], reads=[pbuf[0], b_modT], writes=[b_h2Tt])
        S.op('pe', [MM(pbank[1][:, 0:36], h2Tt[:, kc, :], wrb[:, kc, :], kc == 0, kc == 7) for kc in range(8)],
             reads=[b_h2Tt, b_wrb], writes=[pbuf[1]])
        S.op('dve', TT(lg[:], pbank[1][:, 0:36], brbc[:], ALU.add), reads=[pbuf[1], b_brbc], writes=[b_lg])
        S.op('dve', RED(gmax, lg[:, 0:4], ALU.max), reads=[b_lg], writes=[b_rt])
        S.op('dve', TS(ngmax, gmax, -1.0, None, ALU.mult), reads=[b_rt], writes=[b_rt])
        S.op('dve', TS(goh[:], lg[:, 0:4], gmax, None, ALU.is_ge), reads=[b_lg, b_rt], writes=[b_goh])
        S.op('act', ACTV(gex[:], lg[:, 0:4], AF.Exp, bias=ngmax, accum_out=gsum), reads=[b_lg, b_rt], writes=[b_gex, b_rt])
        S.op('dve', RECIP(gsum, gsum), reads=[b_rt], writes=[b_rt])
        S.op('dve', TS(pen[:], goh[:], PEN, -PEN, ALU.mult, ALU.add), reads=[b_goh], writes=[b_pen])
        S.op('dve', TT(em[:].rearrange("p (g e) -> p g e", g=4), lg[:, 4:36].rearrange("p (g e) -> p g e", g=4),
                       pen[:, :].unsqueeze(2).to_broadcast([128, 4, 8]), ALU.add), reads=[b_lg, b_pen], writes=[b_em])
        S.op('dve', MAX8(mx8c[:], em[:]), reads=[b_em], writes=[b_mx8c])
        S.op('dve', TS(nm1, mx8c[:, 0:1], -1.0, None, ALU.mult), reads=[b_mx8c], writes=[b_rt])
        S.op('dve', TS(selall[:, i, :], em[:], mx8c[:, 1:2], None, ALU.is_ge), reads=[b_em, b_mx8c], writes=[b_selall])
        S.op('dve', TS(selAall[:, i, :], em[:], mx8c[:, 0:1], None, ALU.is_ge), reads=[b_em, b_mx8c], writes=[b_selAall])
        S.op('dve', TS(emc[:], em[:], mx8c[:, 1:2], None, ALU.max), reads=[b_em, b_mx8c], writes=[b_emc])
        S.op('act', ACTV(ex[:], emc[:], AF.Exp, bias=nm1), reads=[b_emc, b_rt], writes=[b_ex])
        S.op('dve', TT(exs[:], ex[:], selall[:, i, :], ALU.mult), reads=[b_ex, b_selall], writes=[b_exs])
        S.op('dve', RED(den, exs[:]), reads=[b_exs], writes=[b_rt])
        S.op('dve', RECIP(den, den), reads=[b_rt], writes=[b_rt])
        S.op('dve', TT(fsc, den, gsum, ALU.mult), reads=[b_rt], writes=[b_rt])
        S.op('dve', TS(cwall[:, i, :], exs[:], fsc, None, ALU.mult), reads=[b_exs, b_rt], writes=[b_cwall])
        S.op('dve', CP(selb[:], selall[:, i, :]), reads=[b_selall], writes=[b_selb])
        S.op('pe', MM(pbank[2][:, 0:32], ltri[:], selb[:]), reads=[b_ltri, b_selb], writes=[pbuf[2]])
        S.op('pe', MM(pbank[3][:, 0:32], onesb[:], selb[:]), reads=[b_onesb, b_selb], writes=[pbuf[3]])
        S.op('dve', TT(rankall[:, i, :], pbank[2][:, 0:32], csum[:], ALU.add), reads=[pbuf[2], b_csum], writes=[b_rankall])
        S.op('dve', TT(csum[:], pbank[3][:, 0:32], csum[:], ALU.add), reads=[pbuf[3], b_csum], writes=[b_csum])
    b3 = big3[:, 0:512].rearrange("p (e k) -> p e k", k=16)
    S.op('dve', TT(b3, csum[:, :].unsqueeze(2).to_broadcast([128, 32, 16]),
                   mc[:, 160:176].unsqueeze(1).to_broadcast([128, 32, 16]), ALU.is_gt), reads=[b_csum, b_mc], writes=[b_big3])
    S.op('dve', RED(nblk[:], b3), reads=[b_big3], writes=[b_nblk])
    S.op('dve', CP(nbpad[:, 0:32], nblk[:]), reads=[b_nblk], writes=[b_nbpad])
    pvn = pbf(0)
    S.op('pe', TR(pvn[:, 0:128], nbpad[:], identb[:]), reads=[b_nbpad, b_identb], writes=[pbuf[0]])
    S.op('act', ACTV(nbT[0:32, :], pvn[0:32, 0:128], AF.Copy), reads=[pbuf[0]], writes=[b_nbT])
    S.op('pe', MM(pbank[2][:, 0:32], nbT[0:32, :], ustr[0:32, :]), reads=[b_nbT, b_ustr], writes=[pbuf[2]])
    S.op('dve', CP(pss[:], pbank[2][:, 0:32]), reads=[pbuf[2]], writes=[b_pss])
    S.op('dve', TT(pend[:], pss[:], nblk[:], ALU.add), reads=[b_pss, b_nblk], writes=[b_pend])
    b4 = big3[:, :].rearrange("p (b e) -> p b e", e=32)
    S.op('dve', TT(b4, pend[:, :].unsqueeze(1).to_broadcast([128, NBLK, 32]),
                   mc[:, 176:224].unsqueeze(2).to_broadcast([128, NBLK, 32]), ALU.is_le), reads=[b_pend, b_mc], writes=[b_big3])
    S.op('dve', RED(bex[:], b4), reads=[b_big3], writes=[b_bex])
    S.op('dve', TS(bex[:], bex[:], 31.0, None, ALU.min), reads=[b_bex], writes=[b_bex])
    S.op('dve', TS(widxf[:], bex[:], 128.0, None, ALU.mult), reads=[b_bex], writes=[b_widxf])
    S.op('dve', TT(widxf[:], widxf[:], mc[:, 224:225].to_broadcast([128, NBLK]), ALU.add), reads=[b_widxf, b_mc], writes=[b_widxf])
    S.op('dve', CP(widxi[:], widxf[:]), reads=[b_widxf], writes=[b_widxi])
    S.op('dve', TS(pss[:], pss[:], float(BLKR), None, ALU.mult), reads=[b_pss], writes=[b_pss])
    for i in range(32):
        S.op('dve', TT(tm[:], rankall[:, i, :], pss[:], ALU.add), reads=[b_rankall, b_pss], writes=[b_tm])
        S.op('dve', TT(tm2[:], tm[:], selAall[:, i, :], ALU.mult), reads=[b_tm, b_selAall], writes=[b_tm2])
        S.op('dve', RED(dallf[:, 2 * i:2 * i + 1], tm2[:]), reads=[b_tm2], writes=[b_dallf])
        S.op('dve', TT(tm2[:], selall[:, i, :], selAall[:, i, :], ALU.subtract), reads=[b_selall, b_selAall], writes=[b_tm2])
        S.op('dve', TT(tm[:], tm[:], tm2[:], ALU.mult), reads=[b_tm, b_tm2], writes=[b_tm])
        S.op('dve', RED(dallf[:, 2 * i + 1:2 * i + 2], tm[:]), reads=[b_tm], writes=[b_dallf])
        S.op('dve', TT(tm[:], cwall[:, i, :], tm2[:], ALU.mult), reads=[b_cwall, b_tm2], writes=[b_tm])
        S.op('dve', RED(wall[:, 2 * i + 1:2 * i + 2], tm[:]), reads=[b_tm], writes=[b_wall])
        S.op('dve', TT(tm[:], cwall[:, i, :], selAall[:, i, :], ALU.mult), reads=[b_cwall, b_selAall], writes=[b_tm])
        S.op('dve', RED(wall[:, 2 * i:2 * i + 1], tm[:]), reads=[b_tm], writes=[b_wall])
    S.op('dve', CP(dalli[:], dallf[:]), reads=[b_dallf], writes=[b_dalli])
    S.barrier()
    for i in range(32):
        for kk in range(2):
            S.dma(IDMA_S(xs_d[:, :], dalli[:, 2 * i + kk:2 * i + kk + 1], xnall[:, i, :]), b_xnall[i],
                  reads=[b_xnall[i], b_dalli], q='pool')
    S.barrier()
    A.off = MARK
    w1b = [A.alloc("w1s", [128, 8, 512], BF16) for _ in range(2)]; b_w1b = [S.buf("w1s%d" % i) for i in range(2)]
    w3b = [A.alloc("w3s", [128, 8, 512], BF16) for _ in range(2)]; b_w3b = [S.buf("w3s%d" % i) for i in range(2)]
    w2b = [A.alloc("w2s", [128, 4, D], BF16) for _ in range(2)]; b_w2b = [S.buf("w2s%d" % i) for i in range(2)]
    stg = [A.alloc("stgS", [128, 4096], F32) for _ in range(2)]; b_stg = [S.buf("stgS%d" % i) for i in range(2)]
    xs = [A.alloc("xs", [128, 4, D], BF16) for _ in range(2)]; b_xs = [S.buf("xs%d" % i) for i in range(2)]
    xsT = [A.alloc("xsT", [128, 8, 512], BF16) for _ in range(2)]; b_xsT = [S.buf("xsT%d" % i) for i in range(2)]
    hidT = [A.alloc("hidS", [128, 4, 512], BF16) for _ in range(2)]; b_hid = [S.buf("hidS%d" % i) for i in range(2)]
    sil = [A.alloc("silS", [128, 512], F32) for _ in range(2)]; b_sil = [S.buf("silS%d" % i) for i in range(2)]
    ysb = [A.alloc("ysb", [128, D], F32) for _ in range(2)]; b_ysb = [S.buf("ysb%d" % i) for i in range(2)]
    yg = [A.alloc("yg", [128, D], F32) for _ in range(4)]; b_yg = [S.buf("yg%d" % i) for i in range(4)]
    sidx = [0]
    crr = [0]

    def prep(b):
        wb = b % 2
        xb = b % 2
        S.dma(DMA(xs[xb][:], xs_d[b * BLKR:(b + 1) * BLKR, :].rearrange("(s p) d -> p s d", p=128)), b_xs[xb], writes=[b_xs[xb]])
        items = []
        for (tab, dst, b_dst) in [(w1t, w1b[wb], b_w1b[wb]), (w3t, w3b[wb], b_w3b[wb]), (w2t, w2b[wb], b_w2b[wb])]:
            sk = sidx[0] % 2
            sidx[0] += 1
            S.dma(IDMA_G(stg[sk][:], tab, widxi[:, b:b + 1]), b_stg[sk], reads=[b_widxi], writes=[b_stg[sk]], q='pool')
            crr[0] += 1
            ce = ['dve', 'act'][crr[0] % 2]

            def cast_item(ce=ce, dst=dst, sk=sk, b_dst=b_dst):
                S.op(ce, CAST(ce, dst[:].rearrange("p a b -> p (a b)"), stg[sk][:]), reads=[b_stg[sk]], writes=[b_dst])
            cast_item()
        for sub in range(4):
            def tr_item(sub=sub, xb=xb):
                pv = pbf(0).rearrange("p (a b) -> p a b", a=8)
                S.op('pe', [TR(pv[:, kc, :], xs[xb][:, sub, :].rearrange("p (q k) -> p q k", k=8)[:, :, kc], identb[:])
                            for kc in range(8)], reads=[b_xs[xb], b_identb], writes=[pbuf[0]])
                S.op('act', [ACTV(xsT[xb][:, kc, sub * 128:(sub + 1) * 128], pv[:, kc, :], AF.Identity,
                                  scale=modTp[:, kc:kc + 1], bias=modTp[:, 8 + kc:9 + kc]) for kc in range(8)],
                     reads=[pbuf[0], b_modTp], writes=[b_xsT[xb]])
            items.append(tr_item)
        return items

    for it in prep(0):
        it()
    for b in range(NBLK):
        wb = b % 2
        xb = b % 2
        hk = b % 2
        nxt = prep(b + 1) if b + 1 < NBLK else []
        for fc in range(4):
            pb1 = 2 + (fc % 2)
            pb3 = 4 + (fc % 2)
            sk2 = fc % 2
            S.op('pe', [MM(pbank[pb1][:, :], w1b[wb][:, kc, :].rearrange("p (m f) -> p m f", f=4)[:, :, fc], xsT[xb][:, kc, :],
                           kc == 0, kc == 7) for kc in range(8)], reads=[b_w1b[wb], b_xsT[xb]], writes=[pbuf[pb1]])
            S.op('pe', [MM(pbank[pb3][:, :], w3b[wb][:, kc, :].rearrange("p (m f) -> p m f", f=4)[:, :, fc], xsT[xb][:, kc, :],
                           kc == 0, kc == 7) for kc in range(8)], reads=[b_w3b[wb], b_xsT[xb]], writes=[pbuf[pb3]])
            S.op('act', ACTV(sil[sk2][:], pbank[pb1][:, :], AF.Silu), reads=[pbuf[pb1]], writes=[b_sil[sk2]])
            S.op('dve', TT(hidT[hk][:, fc, :], pbank[pb3][:, :], sil[sk2][:], ALU.mult),
                 reads=[pbuf[pb3], b_sil[sk2]], writes=[b_hid[hk]])
            if nxt:
                nxt.pop(0)()
        for j in range(4):
            yk = j % 2
            for hf2 in range(2):
                po = 6 + hf2
                S.op('pe', [MM(pbank[po][:, :], hidT[hk][:, fc, j * 128:(j + 1) * 128],
                               w2b[wb][:, fc, hf2 * 512:(hf2 + 1) * 512], fc == 0, fc == 3) for fc in range(4)],
                     reads=[b_hid[hk], b_w2b[wb]], writes=[pbuf[po]])
                S.op('dve' if hf2 else 'act',
                     CAST('dve' if hf2 else 'act', ysb[yk][:, hf2 * 512:(hf2 + 1) * 512], pbank[po][:, :]),
                     reads=[pbuf[po]], writes=[b_ysb[yk]])
            r0 = b * BLKR + j * 128
            S.dma(DMA(ys_d[r0:r0 + 128, :], ysb[yk][:]), b_ysb[yk], reads=[b_ysb[yk]])
        while nxt:
            nxt.pop(0)()
    S.barrier()
    for i in range(32):
        k = i % 2
        g0, g1 = yg[2 * k], yg[2 * k + 1]
        S.dma(IDMA_G(g0[:], ys_d[:, :], dalli[:, 2 * i:2 * i + 1]), b_yg[2 * k], reads=[b_dalli], writes=[b_yg[2 * k]], q='pool')
        S.dma(IDMA_G(g1[:], ys_d[:, :], dalli[:, 2 * i + 1:2 * i + 2]), b_yg[2 * k + 1], reads=[b_dalli], writes=[b_yg[2 * k + 1]], q='pool')
        S.dma(DMA(xt[k][:], out_d[i * 128:(i + 1) * 128, :]), b_xt[k], writes=[b_xt[k]])
        S.op('dve', TS(g0[:], g0[:], wall[:, 2 * i:2 * i + 1], None, ALU.mult), reads=[b_yg[2 * k], b_wall], writes=[b_yg[2 * k]])
        S.op('dve', STT(g0[:], g1[:], wall[:, 2 * i + 1:2 * i + 2], g0[:], ALU.mult, ALU.add),
             reads=[b_yg[2 * k], b_yg[2 * k + 1], b_wall], writes=[b_yg[2 * k]])
        S.op('dve', TT(g0[:], g0[:], gabc[:, 1, :], ALU.mult), reads=[b_yg[2 * k], b_gabc], writes=[b_yg[2 * k]])
        S.op('dve', TT(g0[:], g0[:], xt[k][:], ALU.add), reads=[b_yg[2 * k], b_xt[k]], writes=[b_yg[2 * k]])
        S.dma(DMA(out_d[i * 128:(i + 1) * 128, :], g0[:]), b_yg[2 * k], reads=[b_yg[2 * k]])
    S.barrier()


def build_program(stop_after=None):
    nc = bass.Bass("TRN2", target_bir_lowering=False)
    din = lambda name, shape: nc.dram_tensor(name, list(shape), F32, kind="ExternalInput").ap()
    x_d = din("x", [S_LEN, D])
    cT_d = din("cT", [128, 8])
    wada_d = din("w_ada", [D, 6 * D])
    bada_d = din("b_ada", [1, 6 * D])
    g1_d = din("g1", [1, D])
    g2_d = din("g2", [1, D])
    win_d = din("w_in", [D, 5120])
    gqk_d = din("gqk", [128, 512])
    cs1_d = din("cs1", [128, NT * 16])
    sn2_d = din("sn2", [128, NT * 16])
    convw_d = din("convw", [128, 12])
    convb_d = din("convb", [128, 4])
    wpa_d = din("w_pa", [512, D])
    wpb_d = din("w_pb", [512, D])
    wo_d = din("w_o", [D, D])
    wr_d = din("wr", [D, 36])
    br_d = din("br", [128, 36])
    if stop_after is None:
        w1_d = din("w1", [32, D, 512])
        w3_d = din("w3", [32, D, 512])
        w2_d = din("w2", [32, 512, D])
    ident_d = din("ident", [128, 128])
    tri_d = din("tri", [128, 512])
    oneh_d = din("oneh", [16, S_LEN])
    mc_d = din("mconst", [128, 225])
    out_d = nc.dram_tensor("out", [S_LEN, D], F32, kind="ExternalOutput").ap()
    ya_d = nc.dram_tensor("ya_scratch", [512, S_LEN], BF16, kind="ExternalOutput").ap()

    S = Sched(nc)
    A = Arena(nc)
    pbank = [nc.alloc_psum_tensor("pb%d" % i, [128, 512], F32) for i in range(8)]
    pbuf = [S.buf("pb%d" % i) for i in range(8)]

    def pbf(i):
        return pbank[i][:, :].bitcast(BF16)

    identb = A.alloc("identb", [128, 128], BF16); b_identb = S.buf("identb")
    cs1 = A.alloc("cs1", [128, NT, 16], F32); b_cs1 = S.buf("cs1")
    sn2 = A.alloc("sn2", [128, NT, 16], F32); b_sn2 = S.buf("sn2")
    trib = A.alloc("trib", [128, 2, 256], BF16); b_trib = S.buf("trib")
    modT = A.alloc("modT", [128, 32], F32); b_modT = S.buf("modT")
    gabc = A.alloc("gabc", [128, 2, D], F32); b_gabc = S.buf("gabc")
    epsb = A.alloc("epsb", [128, 1], F32); b_epsb = S.buf("epsb")
    onesf = A.alloc("onesf", [128, 128], F32); b_onesf = S.buf("onesf")
    gqk = A.alloc("gqk", [128, 512], F32); b_gqk = S.buf("gqk")
    convw = A.alloc("convw", [128, 4, 3], F32); b_convw = S.buf("convw")
    convb = A.alloc("convb", [128, 4], F32); b_convb = S.buf("convb")
    brbc = A.alloc("brbc", [128, 36], F32); b_brbc = S.buf("brbc")
    wrb = A.alloc("wrb", [128, 8, 36], BF16); b_wrb = S.buf("wrb")
    modTp = A.alloc("modTp", [128, 16], F32); b_modTp = S.buf("modTp")
    PERSIST = A.off

    stgc = A.alloc("stgc", [128, 512], F32); b_stgc = S.buf("stgc")
    stgi = A.alloc("stgi", [128, 128], F32); b_stgi = S.buf("stgi")
    stgr = A.alloc("stgr", [128, 8, 36], F32); b_stgr = S.buf("stgr")
    cT = A.alloc("cT", [128, 8], F32); b_cT = S.buf("cT")
    sT = A.alloc("sT", [128, 8], F32); b_sT = S.buf("sT")
    wst = [A.alloc("wst", [128, 8, 512], F32) for _ in range(2)]
    b_wst = [S.buf("wst%d" % i) for i in range(2)]
    modrow = A.alloc("modrow", [1, 6 * D], F32); b_modrow = S.buf("modrow")
    badar = A.alloc("badar", [1, 6 * D], F32); b_badar = S.buf("badar")
    grow = A.alloc("grow", [1, 2 * D], F32); b_grow = S.buf("grow")
    arow = A.alloc("arow", [1, 2 * D], F32); b_arow = S.buf("arow")

    S.op('pool', [MS(epsb[:], 1e-6), MS(onesf[:], 1.0)], writes=[b_epsb, b_onesf])
    S.dma(DMA(stgi[:], ident_d[:, :]), b_stgi, writes=[b_stgi])
    S.op('dve', CP(identb[:], stgi[:]), reads=[b_stgi], writes=[b_identb])
    S.dma(DMA(stgc[:], tri_d[:, :]), b_stgc, writes=[b_stgc])
    S.op('dve', CP(trib[:].rearrange("p a b -> p (a b)"), stgc[:]), reads=[b_stgc], writes=[b_trib])
    S.dma(DMA(cs1[:].rearrange("p a b -> p (a b)"), cs1_d[:, :]), b_cs1, writes=[b_cs1])
    S.dma(DMA(sn2[:].rearrange("p a b -> p (a b)"), sn2_d[:, :]), b_sn2, writes=[b_sn2])
    S.dma(DMA(gqk[:], gqk_d[:, :]), b_gqk, writes=[b_gqk])
    S.dma(DMA(convw[:].rearrange("p a b -> p (a b)"), convw_d[:, :]), b_convw, writes=[b_convw])
    S.dma(DMA(convb[:], convb_d[:, :]), b_convb, writes=[b_convb])
    S.dma(DMA(brbc[:], br_d[:, :]), b_brbc, writes=[b_brbc])
    S.dma(DMA(stgr[:], wr_d.rearrange("(kc p) n -> p kc n", p=128)), b_stgr, writes=[b_stgr])
    S.op('dve', CP(wrb[:], stgr[:]), reads=[b_stgr], writes=[b_wrb])
    S.dma(DMA(cT[:], cT_d[:, :]), b_cT, writes=[b_cT])
    S.op('act', ACTV(sT[:], cT[:], AF.Silu), reads=[b_cT], writes=[b_sT])
    S.dma(DMA(badar[:], bada_d[:, :]), b_badar, writes=[b_badar])
    S.dma(DMA(grow[0:1, 0:D], g1_d[:, :]), b_grow, writes=[b_grow])
    S.dma(DMA(grow[0:1, D:2 * D], g2_d[:, :]), b_grow, writes=[b_grow])
    for j in range(12):
        wb = j % 2
        S.dma(DMA(wst[wb][:], wada_d[:, j * 512:(j + 1) * 512].rearrange("(kc p) n -> p kc n", p=128)),
              b_wst[wb], writes=[b_wst[wb]])
        S.op('pe', [MM(pbank[0][0:1, :], sT[:, kc:kc + 1], wst[wb][:, kc, :], kc == 0, kc == 7) for kc in range(8)],
             reads=[b_sT, b_wst[wb]], writes=[pbuf[0]])
        S.op('dve', TT(modrow[0:1, j * 512:(j + 1) * 512], pbank[0][0:1, :], badar[0:1, j * 512:(j + 1) * 512], ALU.add),
             reads=[pbuf[0], b_badar], writes=[b_modrow])
    S.op('dve', STT(arow[0:1, 0:D], modrow[0:1, D:2 * D], 1.0, grow[0:1, 0:D], ALU.add, ALU.mult),
         reads=[b_modrow, b_grow], writes=[b_arow])
    S.op('dve', STT(arow[0:1, D:2 * D], modrow[0:1, 4 * D:5 * D], 1.0, grow[0:1, D:2 * D], ALU.add, ALU.mult),
         reads=[b_modrow, b_grow], writes=[b_arow])
    fns = []
    srcs = [(arow, 0), (modrow, 0), (arow, D), (modrow, 3 * D)]
    for r, (src, off) in enumerate(srcs):
        for kc in range(8):
            fns.append(MM(pbank[1][:, r * 8 + kc:r * 8 + kc + 1], src[0:1, off + kc * 128:off + (kc + 1) * 128],
                          onesf[0:1, 0:1]))
    S.op('pe', fns, reads=[b_arow, b_modrow, b_onesf], writes=[pbuf[1]])
    S.op('dve', CP(modT[:], pbank[1][:, 0:32]), reads=[pbuf[1]], writes=[b_modT])
    fns = []
    for r, (src, off) in enumerate([(arow, D), (modrow, 3 * D)]):
        for kc in range(8):
            fns.append(MM(pbank[1][:, r * 8 + kc:r * 8 + kc + 1],
                          src[0:1, off:off + D].rearrange("o (p k) -> o p k", k=8)[:, :, kc], onesf[0:1, 0:1]))
    S.op('pe', fns, reads=[b_arow, b_modrow, b_onesf], writes=[pbuf[1]])
    S.op('dve', CP(modTp[:], pbank[1][:, 0:16]), reads=[pbuf[1]], writes=[b_modTp])
    for gi, off in enumerate([2 * D, 5 * D]):
        for hf in range(2):
            S.op('pe', MM(pbank[2][:, :], onesf[0:1, 0:128], modrow[0:1, off + hf * 512:off + (hf + 1) * 512]),
                 reads=[b_onesf, b_modrow], writes=[pbuf[2]])
            S.op('act', ACTV(gabc[:, gi, hf * 512:(hf + 1) * 512], pbank[2][:, :], AF.Copy),
                 reads=[pbuf[2]], writes=[b_gabc])
    S.barrier()
    A.off = PERSIST
    if stop_after == '0':
        S.emit()
        return nc

    def make_norm_bufs(with_xt=True, with_junk=True):
        d = {}
        d['xt'] = [A.alloc("xt", [128, D], F32) for _ in range(2)] if with_xt else None
        d['b_xt'] = [S.buf("xt%d" % i) for i in range(2)]
        if with_junk:
            d['junk'] = A.alloc("junk", [128, D], BF16); d['b_junk'] = S.buf("junk")
        d['ss'] = [A.alloc("ss", [128, 1], F32) for _ in range(2)]
        d['b_ss'] = [S.buf("ss%d" % i) for i in range(2)]
        d['xn'] = [A.alloc("xn", [128, D], BF16) for _ in range(2)]
        d['b_xn'] = [S.buf("xn%d" % i) for i in range(2)]
        return d

    def norm_tile(nb, par, xsrc, b_xsrc, hT_dst, b_hT, col0, ptr_i=0, sch=None):
        S_ = sch or S
        k = par % 2
        S_.op('act', ACTV(nb['junk'][:], xsrc, AF.Square, scale=1.0 / 32.0, accum_out=nb['ss'][k][:]),
             reads=[b_xsrc], writes=[nb['b_junk'], nb['b_ss'][k]])
        S_.op('act', ACTV(nb['ss'][k][:], nb['ss'][k][:], AF.Sqrt, bias=epsb[:]),
             reads=[nb['b_ss'][k], b_epsb], writes=[nb['b_ss'][k]])
        S_.op('dve', RECIP(nb['ss'][k][:], nb['ss'][k][:]), reads=[nb['b_ss'][k]], writes=[nb['b_ss'][k]])
        S_.op('dve', TS(nb['xn'][k][:], xsrc, nb['ss'][k][:, 0:1], None, ALU.mult),
             reads=[b_xsrc, nb['b_ss'][k]], writes=[nb['b_xn'][k]])
        pv = pbf(ptr_i).rearrange("p (a b) -> p a b", a=8)
        S_.op('pe', [TR(pv[:, kc, :], nb['xn'][k][:, kc * 128:(kc + 1) * 128], identb[:]) for kc in range(8)],
             reads=[nb['b_xn'][k], b_identb], writes=[pbuf[ptr_i]])
        S_.op('act', [ACTV(hT_dst[:, kc, :], pv[:, kc, :], AF.Identity, scale=modT[:, col0 + kc:col0 + kc + 1],
                          bias=modT[:, col0 + 8 + kc:col0 + 9 + kc]) for kc in range(8)],
             reads=[pbuf[ptr_i], b_modT], writes=[b_hT])

    engrr = [0]

    def cast_eng():
        engrr[0] += 1
        return ['dve', 'act'][engrr[0] % 2]

    KxT = A.alloc("KxT", [128, 4, S_LEN], BF16)
    b_Kx = [S.buf("Kx%d" % c) for c in range(8)]
    Vx = A.alloc("Vx", [128, NT, 4, 65], BF16)
    b_Vx = [S.buf("Vx%d" % c) for c in range(8)]
    kmT = A.alloc("kmT", [128, 4, 16], BF16)
    b_km = [S.buf("km%d" % c) for c in range(16)]
    kms = A.alloc("kms", [128, 4], F32); b_kms = S.buf("kms")
    wqkv = A.alloc("wqkv", [128, 8, 768], BF16); b_wqkv = S.buf("wqkv")
    stgA = [A.alloc("stgA", [128, 8, 256], F32) for _ in range(2)]
    b_stgA = [S.buf("stgA%d" % i) for i in range(2)]
    stgo = A.alloc("stgo", [128, S_LEN], F32); b_stgo = S.buf("stgo")
    nb = make_norm_bufs()
    hT = [A.alloc("hT", [128, 8, 512], BF16) for _ in range(2)]
    b_hT = [S.buf("hT%d" % i) for i in range(2)]
    QxT = [A.alloc("QxT", [128, 4, 512], BF16) for _ in range(2)]
    b_Qx = [S.buf("Qx%d" % i) for i in range(2)]
    sq = A.alloc("sq", [128, 512], F32); b_sq = S.buf("sq")
    ssq = [A.alloc("ssq", [128, 8], F32) for _ in range(2)]; b_ssq = [S.buf("ssq%d" % i) for i in range(2)]
    qn = [A.alloc("qn", [128, 512], F32) for _ in range(2)]
    b_qn = [S.buf("qn%d" % i) for i in range(2)]
    tA = [A.alloc("tA", [128, 8, 16], F32) for _ in range(2)]; b_tA = [S.buf("tA%d" % i) for i in range(2)]
    tB = [A.alloc("tB", [128, 8, 16], F32) for _ in range(2)]; b_tB = [S.buf("tB%d" % i) for i in range(2)]
    qkb = [A.alloc("qkb", [128, 8, 128], BF16) for _ in range(2)]
    b_qkb = [S.buf("qkb%d" % i) for i in range(2)]
    gsb = A.alloc("gsb", [128, 4, 16], F32); b_gsb = S.buf("gsb")
    mx8 = A.alloc("mx8", [128, 4, 8], F32); b_mx8 = S.buf("mx8")
    sel = A.alloc("sel", [128, 4, 16], F32); b_sel = S.buf("sel")
    mbp = [A.alloc("mbp", [128, 4, 128], BF16) for _ in range(2)]
    b_mbp = [S.buf("mbp%d" % i) for i in range(2)]
    pT = [A.alloc("pT", [128, 512], BF16) for _ in range(3)]
    b_pT = [S.buf("pT%d" % i) for i in range(3)]
    rd = A.alloc("rd", [128, 512], F32); b_rd = S.buf("rd")
    bcs = A.alloc("bcs", [128, 512], F32); b_bcs = S.buf("bcs")
    yo = [A.alloc("yo", [128, 512], BF16) for _ in range(2)]
    b_yo = [S.buf("yo%d" % i) for i in range(2)]

    S.dma(DMA(stgo[64:80, :], oneh_d[:, :]), b_stgo, writes=[b_stgo])
    S.op('dve', [CP(KxT[64:80, h, :], stgo[64:80, :]) for h in range(4)], reads=[b_stgo], writes=b_Kx)
    S.op('dve', [MS(Vx[:, :, :, 64:65], 1.0), MS(mbp[0][:], 0.0), MS(mbp[1][:], 0.0),
                  MS(qkb[0][:], 0.0), MS(qkb[1][:], 0.0)],
         writes=b_Vx + b_mbp + b_qkb)

    rot = [0]
    L = _DBG.get('lvl', 9)
    b_pg = S.buf('pg')
    for hh in range(_DBG.get('nhh', 2)):
        for part, c0 in enumerate([hh * 256, 512 + hh * 256, 1024 + hh * 256]):
            sb = part % 2
            S.dma(DMA(stgA[sb][:], win_d[:, c0:c0 + 256].rearrange("(kc p) n -> p kc n", p=128)),
                  b_stgA[sb], writes=[b_stgA[sb]])
            ce = cast_eng()
            S.op(ce, CAST(ce, wqkv[:, :, part * 256:(part + 1) * 256], stgA[sb][:]),
                 reads=[b_stgA[sb]], writes=[b_wqkv])
        NCH = _DBG.get('nch', 8)
        R = Rec()

        def stageA(c, j):
            i = 4 * c + j
            k = i % 2
            hb = c % 2
            R.dma(DMA(nb['xt'][k][:], x_d[i * 128:(i + 1) * 128, :]), nb['b_xt'][k], writes=[nb['b_xt'][k]])
            norm_tile(nb, i, nb['xt'][k][:], nb['b_xt'][k], hT[hb][:, :, j * 128:(j + 1) * 128], b_hT[hb], 0, sch=R)

        def stageB(c, j):
            i = 4 * c + j
            k = i % 2
            hb = c % 2
            R.op('pe', [MM(pbank[1][:, :], hT[hb][:, kc, j * 128:(j + 1) * 128], wqkv[:, kc, 0:512], kc == 0, kc == 7)
                        for kc in range(8)], reads=[b_hT[hb], b_wqkv], writes=[pbuf[1]])
            R.op('pe', [MM(pbank[2][:, 0:256], hT[hb][:, kc, j * 128:(j + 1) * 128], wqkv[:, kc, 512:768], kc == 0, kc == 7)
                        for kc in range(8)], reads=[b_hT[hb], b_wqkv], writes=[pbuf[2]])
            R.op('act', ACTV(Vx[:, i, :, 0:64], pbank[2][:, 0:256].rearrange("p (h d) -> p h d", h=4), AF.Copy),
                 reads=[pbuf[2]], writes=[b_Vx[c]])
            R.op('act', ACTV(sq[:], pbank[1][:, :], AF.Square), reads=[pbuf[1]], writes=[b_sq])
            R.op('dve', RED(ssq[k][:], sq[:].rearrange("p (h d) -> p h d", h=8)), reads=[b_sq], writes=[b_ssq[k]])
            R.op('act', ACTV(ssq[k][:], ssq[k][:], AF.Sqrt, scale=1.0 / 64.0, bias=epsb[:]),
                 reads=[b_ssq[k], b_epsb], writes=[b_ssq[k]])
            R.op('dve', RECIP(ssq[k][:], ssq[k][:]), reads=[b_ssq[k]], writes=[b_ssq[k]])
            qv = qn[k][:].rearrange("p (h d) -> p h d", h=8)
            R.op('dve', TT(qv, pbank[1][:, :].rearrange("p (h d) -> p h d", h=8),
                           ssq[k][:, :].unsqueeze(2).to_broadcast([128, 8, 64]), ALU.mult),
                 reads=[pbuf[1], b_ssq[k]], writes=[b_qn[k]])

        def stageC(c, j):
            i = 4 * c + j
            k = i % 2
            qb = c % 2
            qv = qn[k][:].rearrange("p (h d) -> p h d", h=8)
            R.op('dve', TT(qn[k][:], qn[k][:], gqk[:], ALU.mult), reads=[b_qn[k], b_gqk], writes=[b_qn[k]])
            R.op('dve', [TT(tA[k][:], qv[:, :, 0:16], cs1[:, i, :].unsqueeze(1).to_broadcast([128, 8, 16]), ALU.mult),
                         TT(tB[k][:, :, 0:8], qv[:, :, 8:16], sn2[:, i, 0:8].unsqueeze(1).to_broadcast([128, 8, 8]), ALU.mult),
                         TT(tB[k][:, :, 8:16], qv[:, :, 0:8], sn2[:, i, 8:16].unsqueeze(1).to_broadcast([128, 8, 8]), ALU.mult)],
                 reads=[b_qn[k], b_cs1, b_sn2], writes=[b_tA[k], b_tB[k]])
            R.op('act', ACTV(qkb[k][:, :, 16:64], qv[:, :, 16:64], AF.Copy), reads=[b_qn[k]], writes=[b_qkb[k]])
            R.op('dve', TT(qkb[k][:, :, 0:16], tA[k][:], tB[k][:], ALU.add), reads=[b_tA[k], b_tB[k]], writes=[b_qkb[k]])
            ptq = pbf(0).rearrange("p (a b) -> p a b", a=8)
            R.op('pe', [TR(ptq[:, s, :], qkb[k][:, s, :], identb[:]) for s in range(8)],
                 reads=[b_qkb[k], b_identb], writes=[pbuf[0]])
            R.op('act', [ACTV(QxT[qb][0:64, h4, j * 128:(j + 1) * 128], ptq[0:64, h4, :], AF.Copy) for h4 in range(4)],
                 reads=[pbuf[0]], writes=[b_Qx[qb]])
            R.op('act', [ACTV(KxT[0:64, h4, i * 128:(i + 1) * 128], ptq[0:64, 4 + h4, :], AF.Copy) for h4 in range(4)],
                 reads=[pbuf[0]], writes=[b_Kx[c]])
            if i % 2 == 1:
                blk = i // 2
                R.op('dve', RED(kms[0:64, :], KxT[0:64, :, blk * 256:(blk + 1) * 256]),
                     reads=[b_Kx[c]], writes=[b_kms])
                R.op('dve', TS(kmT[0:64, :, blk], kms[0:64, :], 1.0 / 256.0, None, ALU.mult),
                     reads=[b_kms], writes=[b_km[blk]])

        def prep_items(c):
            R.items = []
            for j in range(4):
                stageA(c, j)
                stageB(c, j)
                stageC(c, j)
            return list(R.items)

        def replay(item):
            kind, a_, k_ = item
            getattr(S, kind)(*a_, **k_)

        pending = prep_items(0)
        for c in range(NCH):
            hb = c % 2
            qb = c % 2
            for item in pending:
                replay(item)
            pending = prep_items(c + 1) if c + 1 < NCH else []
            for j in range(4 if _DBG.get('gate', True) else 0):
                i = 4 * c + j
                cur = i // 2
                m = j % 2
                pg = pbank[2][:, 256:320].rearrange("p (h n) -> p h n", h=4)
                fl = [MS(gsb[:, :, cur:cur + 1], BIG)] + ([MS(gsb[:, :, cur + 1:16], -BIG)] if cur < 15 else [])
                S.op('dve', fl, writes=[b_gsb])
                if cur > 0:
                    S.op('pe', [MM(pg[:, h, 0:cur], QxT[qb][0:64, h, j * 128:(j + 1) * 128], kmT[0:64, h, 0:cur])
                                for h in range(4)], reads=[b_Qx[qb]] + b_km[0:cur], writes=[b_pg])
                    S.op('dve', CP(gsb[:, :, 0:cur], pg[:, :, 0:cur]), reads=[b_pg], writes=[b_gsb])
                S.op('dve', [MAX8(mx8[:, h, :], gsb[:, h, :]) for h in range(4)], reads=[b_gsb], writes=[b_mx8])
                S.op('dve', TT(sel[:], gsb[:], mx8[:, :, 3:4].to_broadcast([128, 4, 16]), ALU.is_ge),
                     reads=[b_gsb, b_mx8], writes=[b_sel])
                S.op('dve', TS(mbp[m][:, :, 64:80], sel[:], MASKV, -MASKV, ALU.mult, ALU.add),
                     reads=[b_sel], writes=[b_mbp[m]])
                pmb = pbf(0).rearrange("p (a b) -> p a b", a=8)
                S.op('pe', [TR(pmb[:, h, :], mbp[m][:, h, :], identb[:]) for h in range(4)],
                     reads=[b_mbp[m], b_identb], writes=[pbuf[0]])
                S.op('act', [ACTV(QxT[qb][64:80, h4, j * 128:(j + 1) * 128], pmb[64:80, h4, :], AF.Copy) for h4 in range(4)],
                     reads=[pbuf[0]], writes=[b_Qx[qb]])
            nsteps = 4 * (4 * c + 4)
            stepc = [0]
            for h in range(4 if _DBG.get('attn', True) else 0):
                nk = 4 * c + 4
                pyi = 6 + (h % 2)

                def cols_of(kt):
                    return (0, 512) if kt < 4 * c + 2 else (256, 512)

                def emit_S(kt, r):
                    c0, c1 = cols_of(kt)
                    S.op('pe', MM(pbank[3 + r][:, c0:c1], KxT[0:80, h, kt * 128:(kt + 1) * 128], QxT[qb][0:80, h, c0:c1]),
                         reads=[b_Kx[kt // 4], b_Qx[qb]], writes=[pbuf[3 + r]])
                rs = []
                for kt in range(nk):
                    rs.append(rot[0] % 3)
                    rot[0] += 1
                emit_S(0, rs[0])
                if nk > 1:
                    emit_S(1, rs[1])
                for kt in range(nk):
                    if kt + 2 < nk:
                        emit_S(kt + 2, rs[kt + 2])
                    r = rs[kt]
                    c0, c1 = cols_of(kt)
                    S.op('act', ACTV(pT[r][:, c0:c1], pbank[3 + r][:, c0:c1], AF.Exp, scale=0.125),
                         reads=[pbuf[3 + r]], writes=[b_pT[r]])
                    if kt >= 4 * c:
                        d0 = 0 if kt < 4 * c + 2 else 256
                        S.op('dve', TT(pT[r][:, d0:d0 + 256], pT[r][:, d0:d0 + 256], trib[:, kt % 2, :], ALU.mult),
                             reads=[b_pT[r], b_trib], writes=[b_pT[r]])
                    S.op('pe', MM(pbank[pyi][0:65, c0:c1], Vx[:, kt, h, :], pT[r][:, c0:c1], kt == 0, kt == nk - 1),
                         reads=[b_Vx[kt // 4], b_pT[r]], writes=[pbuf[pyi]])
                    stepc[0] += 1
                    if pending and _DBG.get('ilv', True):
                        left = max(1, nsteps - stepc[0] + 1)
                        for _ in range(-(-len(pending) // left)):
                            replay(pending.pop(0))
                yb = h % 2
                S.op('dve', RECIP(rd[64:65, :], pbank[pyi][64:65, :]), reads=[pbuf[pyi]], writes=[b_rd])
                rb = 3 + (rot[0] % 3)
                rot[0] += 1
                S.op('pe', MM(pbank[rb][0:64, :], onesf[64:65, 0:64], rd[64:65, :]),
                     reads=[b_onesf, b_rd], writes=[pbuf[rb]])
                S.op('act', ACTV(bcs[0:64, :], pbank[rb][0:64, :], AF.Copy), reads=[pbuf[rb]], writes=[b_bcs])
                S.op('dve', TT(yo[yb][0:64, :], pbank[pyi][0:64, :], bcs[0:64, :], ALU.mult),
                     reads=[pbuf[pyi], b_bcs], writes=[b_yo[yb]])
                hg = hh * 4 + h
                S.dma(DMA(ya_d[hg * 64:(hg + 1) * 64, c * 512:(c + 1) * 512], yo[yb][0:64, :]),
                      b_yo[yb], reads=[b_yo[yb]], q='pool')
    S.barrier()
    A.off = PERSIST
    if stop_after == 'A':
        S.emit()
        return nc
    wB = A.alloc("wB", [128, 8, 3584], BF16); b_wB = S.buf("wB")
    wpa = A.alloc("wpa", [128, 4, D], BF16); b_wpa = S.buf("wpa")
    wpb = A.alloc("wpb", [128, 4, D], BF16); b_wpb = S.buf("wpb")
    wo = A.alloc("wo", [128, 8, D], BF16); b_wo = S.buf("wo")
    stgB = [A.alloc("stgB", [128, 2048], F32) for _ in range(2)]
    b_stgB = [S.buf("stgB%d" % i) for i in range(2)]
    sidx = [0]

    def load_cast(dst, src_ap, shape3, b_dst, extra=None):
        k = sidx[0] % len(stgB)
        sidx[0] += 1
        a, bb = shape3
        view = stgB[k][:, 0:a * bb].rearrange("p (a b) -> p a b", a=a)
        S.dma(DMA(view, src_ap), b_stgB[k], writes=[b_stgB[k]])
        if extra is None:
            ce = cast_eng()
            S.op(ce, CAST(ce, dst, view), reads=[b_stgB[k]], writes=[b_dst])
        else:
            ex, b_ex = extra
            S.op('dve', [TT(dst[:, q, :], view[:, q, :], ex, ALU.mult) for q in range(a)],
                 reads=[b_stgB[k], b_ex], writes=[b_dst])

    for p in range(14):
        c0 = 1536 + p * 256
        load_cast(wB[:, :, p * 256:(p + 1) * 256], win_d[:, c0:c0 + 256].rearrange("(kc p) n -> p kc n", p=128),
                  (8, 256), b_wB)
    for q2 in range(2):
        load_cast(wpa[:, 2 * q2:2 * q2 + 2, :], wpa_d[q2 * 256:(q2 + 1) * 256, :].rearrange("(cc p) n -> p cc n", p=128),
                  (2, D), b_wpa)
        load_cast(wpb[:, 2 * q2:2 * q2 + 2, :], wpb_d[q2 * 256:(q2 + 1) * 256, :].rearrange("(cc p) n -> p cc n", p=128),
                  (2, D), b_wpb)
    for q4 in range(4):
        load_cast(wo[:, 2 * q4:2 * q4 + 2, :], wo_d[q4 * 256:(q4 + 1) * 256, :].rearrange("(cc p) n -> p cc n", p=128),
                  (2, D), b_wo)
    nbB = make_norm_bufs()
    hTB = A.alloc("hTB", [128, 8, 512], BF16); b_hTB = S.buf("hTB")
    yaT = A.alloc("yaT", [128, 4, 512], BF16); b_yaT = S.buf("yaT")
    xbs = A.alloc("xbs", [128, 512], F32); b_xbs = S.buf("xbs")
    bgs = A.alloc("bgs", [128, 512], F32); b_bgs = S.buf("bgs")
    u = A.alloc("u", [128, 4, 514], F32); b_u = [S.buf("u%d" % i) for i in range(4)]
    tcv = A.alloc("tcv", [128, 512], F32); b_tcv = S.buf("tcv")
    ybT = A.alloc("ybT", [128, 4, 512], BF16); b_ybT = S.buf("ybT")
    gas = A.alloc("gas", [128, 512], F32); b_gas = S.buf("gas")
    gbs = A.alloc("gbs", [128, 512], F32); b_gbs = S.buf("gbs")
    t1 = A.alloc("t1", [128, 512], F32); b_t1 = S.buf("t1")
    t2 = A.alloc("t2", [128, 512], F32); b_t2 = S.buf("t2")
    mT = A.alloc("mT", [128, 8, 512], BF16); b_mT = S.buf("mT")
    xr = [A.alloc("xr", [128, D], F32) for _ in range(2)]
    b_xr = [S.buf("xr%d" % i) for i in range(2)]
    to = A.alloc("to", [128, 512], F32); b_to = S.buf("to")
    x1t = [A.alloc("x1t", [128, D], F32) for _ in range(2)]
    b_x1t = [S.buf("x1t%d" % i) for i in range(2)]
    S.op('dve', MS(u[:], 0.0), writes=b_u)
    for c in range(8):
        for j in range(4):
            i = 4 * c + j
            k = i % 2
            S.dma(DMA(nbB['xt'][k][:], x_d[i * 128:(i + 1) * 128, :]), nbB['b_xt'][k], writes=[nbB['b_xt'][k]])
            norm_tile(nbB, i, nbB['xt'][k][:], nbB['b_xt'][k], hTB[:, :, j * 128:(j + 1) * 128], b_hTB, 0)
        S.dma(DMA(yaT[:], ya_d[:, c * 512:(c + 1) * 512].rearrange("(cc p) n -> p cc n", p=128)), b_yaT, writes=[b_yaT])
        for cc in range(4):
            for bank, col0 in [(1, 0), (2, 512), (3, 1024)]:
                S.op('pe', [MM(pbank[bank][:, :], wB[:, kc, col0 + cc * 128:col0 + (cc + 1) * 128], hTB[:, kc, :], kc == 0, kc == 7)
                            for kc in range(8)], reads=[b_wB, b_hTB], writes=[pbuf[bank]])
            S.op('act', ACTV(xbs[:], pbank[1][:, :], AF.Copy), reads=[pbuf[1]], writes=[b_xbs])
            if c > 0:
                S.op('dve', CP(u[:, cc, 0:2], u[:, cc, 512:514]), reads=[b_u[cc]], writes=[b_u[cc]])
            S.op('dve', TT(u[:, cc, 2:514], pbank[3][:, :], xbs[:], ALU.mult), reads=[pbuf[3], b_xbs], writes=[b_u[cc]])
            S.op('act', ACTV(bgs[:], pbank[2][:, :], AF.Copy), reads=[pbuf[2]], writes=[b_bgs])
            S.op('dve', TS(tcv[:], u[:, cc, 0:512], convw[:, cc, 0:1], None, ALU.mult),
                 reads=[b_u[cc], b_convw], writes=[b_tcv])
            S.op('dve', STT(tcv[:], u[:, cc, 1:513], convw[:, cc, 1:2], tcv[:], ALU.mult, ALU.add),
                 reads=[b_u[cc], b_convw, b_tcv], writes=[b_tcv])
            S.op('dve', STT(tcv[:], u[:, cc, 2:514], convw[:, cc, 2:3], tcv[:], ALU.mult, ALU.add),
                 reads=[b_u[cc], b_convw, b_tcv], writes=[b_tcv])
            S.op('dve', STT(ybT[:, cc, :], tcv[:], convb[:, cc:cc + 1], bgs[:], ALU.add, ALU.mult),
                 reads=[b_tcv, b_convb, b_bgs], writes=[b_ybT])
        for m in range(8):
            S.op('pe', [MM(pbank[4][:, :], wB[:, kc, 1536 + m * 128:1536 + (m + 1) * 128], hTB[:, kc, :], kc == 0, kc == 7)
                        for kc in range(8)], reads=[b_wB, b_hTB], writes=[pbuf[4]])
            S.op('pe', [MM(pbank[5][:, :], wB[:, kc, 2560 + m * 128:2560 + (m + 1) * 128], hTB[:, kc, :], kc == 0, kc == 7)
                        for kc in range(8)], reads=[b_wB, b_hTB], writes=[pbuf[5]])
            S.op('pe', [MM(pbank[6][:, :], wpa[:, cc, m * 128:(m + 1) * 128], yaT[:, cc, :], cc == 0, cc == 3)
                        for cc in range(4)], reads=[b_wpa, b_yaT], writes=[pbuf[6]])
            S.op('pe', [MM(pbank[7][:, :], wpb[:, cc, m * 128:(m + 1) * 128], ybT[:, cc, :], cc == 0, cc == 3)
                        for cc in range(4)], reads=[b_wpb, b_ybT], writes=[pbuf[7]])
            S.op('act', ACTV(gas[:], pbank[4][:, :], AF.Sigmoid), reads=[pbuf[4]], writes=[b_gas])
            S.op('act', ACTV(gbs[:], pbank[5][:, :], AF.Sigmoid), reads=[pbuf[5]], writes=[b_gbs])
            S.op('dve', TT(t1[:], pbank[6][:, :], gas[:], ALU.mult), reads=[pbuf[6], b_gas], writes=[b_t1])
            S.op('dve', TT(t2[:], pbank[7][:, :], gbs[:], ALU.mult), reads=[pbuf[7], b_gbs], writes=[b_t2])
            S.op('dve', TT(mT[:, m, :], t1[:], t2[:], ALU.add), reads=[b_t1, b_t2], writes=[b_mT])
        for j in range(4):
            i = 4 * c + j
            k = i % 2
            S.dma(DMA(xr[k][:], x_d[i * 128:(i + 1) * 128, :]), b_xr[k], writes=[b_xr[k]])
            for hf in range(2):
                S.op('pe', [MM(pbank[1][:, :], mT[:, m, j * 128:(j + 1) * 128], wo[:, m, hf * 512:(hf + 1) * 512], m == 0, m == 7)
                            for m in range(8)], reads=[b_mT, b_wo], writes=[pbuf[1]])
                S.op('dve', TT(to[:], pbank[1][:, :], gabc[:, 0, hf * 512:(hf + 1) * 512], ALU.mult),
                     reads=[pbuf[1], b_gabc], writes=[b_to])
                S.op('dve', TT(x1t[k][:, hf * 512:(hf + 1) * 512], to[:], xr[k][:, hf * 512:(hf + 1) * 512], ALU.add),
                     reads=[b_to, b_xr[k]], writes=[b_x1t[k]])
            S.dma(DMA(out_d[i * 128:(i + 1) * 128, :], x1t[k][:]), b_x1t[k], reads=[b_x1t[k]])
    S.barrier()
    A.off = PERSIST
    if stop_after == 'B':
        S.emit()
        return nc
    if _SPARSE:
        _sparse_moe(nc, S, A, locals())
        S.emit()
        return nc
    h2T = A.alloc("h2T", [128, 8, 2048], BF16); b_h2T = [S.buf("h2T%d" % i) for i in range(4)]
    acc = A.alloc("acc", [128, 16, D], F32); b_acc = [S.buf("acc%d" % i) for i in range(16)]
    cw = A.alloc("cw", [128, 16, 32], F32); b_cw = [S.buf("cw%d" % i) for i in range(16)]
    w1b = [A.alloc("w1b", [128, 8, 512], BF16) for _ in range(2)]; b_w1b = [S.buf("w1b%d" % i) for i in range(2)]
    w3b = [A.alloc("w3b", [128, 8, 512], BF16) for _ in range(2)]; b_w3b = [S.buf("w3b%d" % i) for i in range(2)]
    w2b = [A.alloc("w2b", [128, 4, D], BF16) for _ in range(2)]; b_w2b = [S.buf("w2b%d" % i) for i in range(2)]
    stgC = [A.alloc("stgC", [128, 2048], F32) for _ in range(2)]
    b_stgC = [S.buf("stgC%d" % i) for i in range(2)]
    stgB[:] = stgC
    b_stgB[:] = b_stgC
    nbC = make_norm_bufs(with_xt=True, with_junk=False)
    hidT = [A.alloc("hidT", [128, 4, 512], BF16) for _ in range(2)]; b_hid = [S.buf("hid%d" % i) for i in range(2)]
    sil = [A.alloc("sil", [128, 512], F32) for _ in range(2)]; b_sil = [S.buf("sil%d" % i) for i in range(2)]
    nbC['junk'] = hidT[0][:, :, :].rearrange("p a b -> p (a b)")[:, 0:D]
    nbC['b_junk'] = b_hid[0]
    lg = A.alloc("lg", [128, 36], F32); b_lg = S.buf("lg")
    rt = A.alloc("rt", [128, 16], F32); b_rt = S.buf("rt")
    goh = A.alloc("goh", [128, 4], F32); b_goh = S.buf("goh")
    gex = A.alloc("gex", [128, 4], F32); b_gex = S.buf("gex")
    pen = A.alloc("pen", [128, 4], F32); b_pen = S.buf("pen")
    em = A.alloc("em", [128, 32], F32); b_em = S.buf("em")
    emc = A.alloc("emc", [128, 32], F32); b_emc = S.buf("emc")
    mx8c = A.alloc("mx8c", [128, 8], F32); b_mx8c = S.buf("mx8c")
    selc = A.alloc("selc", [128, 32], F32); b_selc = S.buf("selc")
    ex = A.alloc("ex", [128, 32], F32); b_ex = S.buf("ex")
    exs = A.alloc("exs", [128, 32], F32); b_exs = S.buf("exs")
    gmax, ngmax, gsum, nm1, den, fsc = [rt[:, q:q + 1] for q in range(6)]
    PEN = 1.0e4
    for hf in range(2):
        for ti in range(16):
            i = hf * 16 + ti
            k = i % 2
            S.dma(DMA(nbC['xt'][k][:], out_d[i * 128:(i + 1) * 128, :]), nbC['b_xt'][k], writes=[nbC['b_xt'][k]])
            norm_tile(nbC, i, nbC['xt'][k][:], nbC['b_xt'][k], h2T[:, :, ti * 128:(ti + 1) * 128], b_h2T[ti // 4], 16)
            S.op('pool', MS(acc[:, ti, :], 0.0), writes=[b_acc[ti]])
            S.op('pe', [MM(pbank[1][:, 0:36], h2T[:, kc, ti * 128:(ti + 1) * 128], wrb[:, kc, :], kc == 0, kc == 7)
                        for kc in range(8)], reads=[b_h2T[ti // 4], b_wrb], writes=[pbuf[1]])
            S.op('dve', TT(lg[:], pbank[1][:, 0:36], brbc[:], ALU.add), reads=[pbuf[1], b_brbc], writes=[b_lg])
            S.op('dve', RED(gmax, lg[:, 0:4], ALU.max), reads=[b_lg], writes=[b_rt])
            S.op('dve', TS(ngmax, gmax, -1.0, None, ALU.mult), reads=[b_rt], writes=[b_rt])
            S.op('dve', TS(goh[:], lg[:, 0:4], gmax, None, ALU.is_ge), reads=[b_lg, b_rt], writes=[b_goh])
            S.op('act', ACTV(gex[:], lg[:, 0:4], AF.Exp, bias=ngmax, accum_out=gsum),
                 reads=[b_lg, b_rt], writes=[b_gex, b_rt])
            S.op('dve', RECIP(gsum, gsum), reads=[b_rt], writes=[b_rt])
            S.op('dve', TS(pen[:], goh[:], PEN, -PEN, ALU.mult, ALU.add), reads=[b_goh], writes=[b_pen])
            S.op('dve', TT(em[:].rearrange("p (g e) -> p g e", g=4), lg[:, 4:36].rearrange("p (g e) -> p g e", g=4),
                           pen[:, :].unsqueeze(2).to_broadcast([128, 4, 8]), ALU.add),
                 reads=[b_lg, b_pen], writes=[b_em])
            S.op('dve', MAX8(mx8c[:], em[:]), reads=[b_em], writes=[b_mx8c])
            S.op('dve', TS(nm1, mx8c[:, 0:1], -1.0, None, ALU.mult), reads=[b_mx8c], writes=[b_rt])
            S.op('dve', TS(selc[:], em[:], mx8c[:, 1:2], None, ALU.is_ge), reads=[b_em, b_mx8c], writes=[b_selc])
            S.op('dve', TS(emc[:], em[:], mx8c[:, 1:2], None, ALU.max), reads=[b_em, b_mx8c], writes=[b_emc])
            S.op('act', ACTV(ex[:], emc[:], AF.Exp, bias=nm1), reads=[b_emc, b_rt], writes=[b_ex])
            S.op('dve', TT(exs[:], ex[:], selc[:], ALU.mult), reads=[b_ex, b_selc], writes=[b_exs])
            S.op('dve', RED(den, exs[:]), reads=[b_exs], writes=[b_rt])
            S.op('dve', RECIP(den, den), reads=[b_rt], writes=[b_rt])
            S.op('dve', TT(fsc, den, gsum, ALU.mult), reads=[b_rt], writes=[b_rt])
            S.op('dve', TS(cw[:, ti, :], exs[:], fsc, None, ALU.mult), reads=[b_exs, b_rt], writes=[b_cw[ti]])
        for e in range(32):
            wb = e % 2
            for q2 in range(2):
                load_cast(w1b[wb][:, 4 * q2:4 * q2 + 4, :],
                          w1_d[e, q2 * 512:(q2 + 1) * 512, :].rearrange("(kc p) n -> p kc n", p=128), (4, 512), b_w1b[wb])
                load_cast(w3b[wb][:, 4 * q2:4 * q2 + 4, :],
                          w3_d[e, q2 * 512:(q2 + 1) * 512, :].rearrange("(kc p) n -> p kc n", p=128), (4, 512), b_w3b[wb])
            for q2 in range(2):
                load_cast(w2b[wb][:, 2 * q2:2 * q2 + 2, :],
                          w2_d[e, q2 * 256:(q2 + 1) * 256, :].rearrange("(fc p) n -> p fc n", p=128), (2, D), b_w2b[wb])
            for ch in range(4):
                hk = (e * 4 + ch) % 2
                for fc in range(4):
                    pb1 = 2 + (fc % 2)
                    pb3 = 4 + (fc % 2)
                    sk = fc % 2
                    S.op('pe', [MM(pbank[pb1][:, :], w1b[wb][:, kc, fc * 128:(fc + 1) * 128], h2T[:, kc, ch * 512:(ch + 1) * 512],
                                   kc == 0, kc == 7) for kc in range(8)], reads=[b_w1b[wb], b_h2T[ch]], writes=[pbuf[pb1]])
                    S.op('pe', [MM(pbank[pb3][:, :], w3b[wb][:, kc, fc * 128:(fc + 1) * 128], h2T[:, kc, ch * 512:(ch + 1) * 512],
                                   kc == 0, kc == 7) for kc in range(8)], reads=[b_w3b[wb], b_h2T[ch]], writes=[pbuf[pb3]])
                    S.op('act', ACTV(sil[sk][:], pbank[pb1][:, :], AF.Silu), reads=[pbuf[pb1]], writes=[b_sil[sk]])
                    S.op('dve', TT(hidT[hk][:, fc, :], pbank[pb3][:, :], sil[sk][:], ALU.mult),
                         reads=[pbuf[pb3], b_sil[sk]], writes=[b_hid[hk]])
                for j in range(4):
                    ti = ch * 4 + j
                    for hf2 in range(2):
                        po = 6 + hf2
                        S.op('pe', [MM(pbank[po][:, :], hidT[hk][:, fc, j * 128:(j + 1) * 128],
                                       w2b[wb][:, fc, hf2 * 512:(hf2 + 1) * 512], fc == 0, fc == 3) for fc in range(4)],
                             reads=[b_hid[hk], b_w2b[wb]], writes=[pbuf[po]])
                        S.op('dve', STT(acc[:, ti, hf2 * 512:(hf2 + 1) * 512], pbank[po][:, :], cw[:, ti, e:e + 1],
                                        acc[:, ti, hf2 * 512:(hf2 + 1) * 512], ALU.mult, ALU.add),
                             reads=[pbuf[po], b_cw[ti], b_acc[ti]], writes=[b_acc[ti]])
        for ti in range(16):
            i = hf * 16 + ti
            k = i % 2
            S.dma(DMA(nbC['xt'][k][:], out_d[i * 128:(i + 1) * 128, :]), nbC['b_xt'][k], writes=[nbC['b_xt'][k]])
            S.op('dve', TT(acc[:, ti, :], acc[:, ti, :], gabc[:, 1, :], ALU.mult), reads=[b_acc[ti], b_gabc], writes=[b_acc[ti]])
            S.op('dve', TT(acc[:, ti, :], acc[:, ti, :], nbC['xt'][k][:], ALU.add),
                 reads=[b_acc[ti], nbC['b_xt'][k]], writes=[b_acc[ti]])
            S.dma(DMA(out_d[i * 128:(i + 1) * 128, :], acc[:, ti, :]), b_acc[ti], reads=[b_acc[ti]])
    S.barrier()
    S.emit()
    return nc


def _consts():
    pos = np.arange(S_LEN, dtype=np.float32)
    inv = (np.float32(500000.0) ** (-np.arange(0, 16, 2, dtype=np.float32) / np.float32(16))).astype(np.float32)
    ang = (pos[:, None] * inv[None, :]).astype(np.float32)
    cos = np.cos(ang).astype(np.float32).reshape(NT, 128, 8).transpose(1, 0, 2)
    sin = np.sin(ang).astype(np.float32).reshape(NT, 128, 8).transpose(1, 0, 2)
    cs1 = np.concatenate([cos, cos], -1).reshape(128, NT * 16)
    sn2 = np.concatenate([-sin, sin], -1).reshape(128, NT * 16)
    kp = np.arange(128)[:, None, None]
    jj = np.arange(2)[None, :, None]
    qq = np.arange(256)[None, None, :]
    tri = (jj * 128 + kp <= qq).astype(np.float32).reshape(128, 512)
    oneh = (np.arange(S_LEN)[None, :] // 256 == np.arange(16)[:, None]).astype(np.float32)
    mconst = np.zeros((128, 225), np.float32)
    tt = np.arange(128)
    mconst[:, 0:128] = (tt[:, None] < tt[None, :]).astype(np.float32)
    ee = np.arange(32)
    mconst[0:32, 128:160] = (ee[:, None] < ee[None, :]).astype(np.float32)
    mconst[:, 160:176] = (512.0 * np.arange(16))[None, :]
    mconst[:, 176:224] = np.arange(48, dtype=np.float32)[None, :]
    mconst[:, 224] = np.arange(128, dtype=np.float32)
    return dict(cs1=np.ascontiguousarray(cs1), sn2=np.ascontiguousarray(sn2), tri=tri, oneh=oneh, mconst=mconst,
                ident=np.eye(128, dtype=np.float32))


def kernel(x, c, w_ada, b_ada, g_norm1, g_norm2, w_in, g_q, g_k, conv_w, conv_b,
           w_pa, w_pb, w_o, w_rg, b_rg, w_re, b_re, w1, w3, w2):
    f = lambda a: np.ascontiguousarray(np.asarray(a, dtype=np.float32))
    x = f(x); c = f(c)
    cst = _consts()
    gqk = np.concatenate([np.tile(f(g_q)[0], 4), np.tile(f(g_k)[0], 4)])
    gqk = np.ascontiguousarray(np.broadcast_to(gqk[None, :], (128, 512)))
    convw = np.ascontiguousarray(f(conv_w)[0].reshape(3, 4, 128).transpose(2, 1, 0).reshape(128, 12))
    convb = np.ascontiguousarray(f(conv_b)[0].reshape(4, 128).T)
    wr = np.ascontiguousarray(np.concatenate([f(w_rg)[0], f(w_re)[0]], axis=1))
    br = np.concatenate([f(b_rg)[0], f(b_re)[0]])
    br = np.ascontiguousarray(np.broadcast_to(br[None, :], (128, 36)))
    shared = dict(w_ada=f(w_ada)[0], b_ada=f(b_ada)[0:1], g1=f(g_norm1)[0:1], g2=f(g_norm2)[0:1], w_in=f(w_in)[0],
                  gqk=gqk, convw=convw, convb=convb, w_pa=f(w_pa)[0], w_pb=f(w_pb)[0], w_o=f(w_o)[0],
                  wr=wr, br=br, w1=f(w1)[0], w3=f(w3)[0], w2=f(w2)[0], **cst)
    n = _NCORES
    in_maps = []
    for b in range(n):
        m = dict(shared)
        m["x"] = x[b]
        m["cT"] = np.ascontiguousarray(c[b].reshape(8, 128).T)
        in_maps.append(m)
    if _STOP_AFTER is not None:
        for m in in_maps:
            for kk in ('w1', 'w3', 'w2'):
                m.pop(kk)
    nc = build_program(_STOP_AFTER)
    res = run_bass_kernel_spmd(nc, in_maps, core_ids=list(range(n)))
    if _STOP_AFTER is not None:
        global _DBG_RES
        _DBG_RES = res.results
    out = np.stack([np.asarray(res.results[b]["out"], dtype=np.float32).reshape(S_LEN, D) for b in range(n)])
    return out
```

```python
import numpy as np
import concourse.bass as bass
import concourse.mybir as mybir
from concourse.bass_utils import run_bass_kernel_spmd

F32 = mybir.dt.float32
BF16 = mybir.dt.bfloat16
ALU = mybir.AluOpType
AF = mybir.ActivationFunctionType
AX = mybir.AxisListType

S_LEN = 4096
D = 1024
NT = 32
BIG = 1.0e30
MASKV = 30000.0
_STOP_AFTER = None
_NCORES = 8
_DBG = {}
_SPARSE = True


class Buf:
    def __init__(self, name):
        self.name = name
        self.lw = None
        self.rd = {}
        self.dsem = {}
        self.dcnt = {}


class Sched:
    def __init__(self, nc):
        self.nc = nc
        self.engs = ['pe', 'act', 'dve', 'pool', 'sp']
        self.q = {e: [] for e in self.engs}
        self.sems = []
        self.esem = {}
        for e in ['pe', 'act', 'dve', 'pool']:
            self.esem[e] = self.new_sem('s_' + e)
        self.cnt = {e: 0 for e in self.esem}
        self.seen = {e: {} for e in self.engs}
        self.bufs = []

    def new_sem(self, name):
        s = self.nc.alloc_semaphore('%s_%d' % (name, len(self.sems)))
        self.sems.append(s)
        return len(self.sems) - 1

    def buf(self, name):
        b = Buf(name)
        self.bufs.append(b)
        return b

    def _waits(self, eng, reads, writes):
        need = {}

        def add(s, v):
            if need.get(s, 0) < v:
                need[s] = v
        for b in reads:
            if b.lw is not None:
                add(*b.lw)
        for b in writes:
            if b.lw is not None:
                add(*b.lw)
            for s, v in b.rd.items():
                add(s, v)
        seen = self.seen[eng]
        out = []
        for s, v in need.items():
            if eng == 'pe' and s == self.esem['pe']:
                continue
            if seen.get(s, 0) < v:
                seen[s] = v
                out.append((s, v))
        return out

    def _commit(self, ev, reads, writes):
        s, v = ev
        for b in reads:
            if b.rd.get(s, 0) < v:
                b.rd[s] = v
        for b in writes:
            b.lw = ev
            b.rd = {}

    def op(self, eng, fns, reads=(), writes=()):
        if callable(fns):
            fns = [fns]
        waits = self._waits(eng, reads, writes)
        self.cnt[eng] += 1
        ev = (self.esem[eng], self.cnt[eng])
        self.q[eng].append((waits, fns, ev[0], 1))
        self._commit(ev, reads, writes)

    def dma(self, fn, own, reads=(), writes=(), q='sp'):
        if q not in own.dsem:
            own.dsem[q] = self.new_sem('d_' + own.name + '_' + q)
            own.dcnt[q] = 0
        waits = self._waits(q, reads, writes)
        own.dcnt[q] += 16
        ev = (own.dsem[q], own.dcnt[q])
        self.q[q].append((waits, [fn], ev[0], 16))
        self._commit(ev, reads, writes)

    def barrier(self):
        evs = [(self.esem[e], self.cnt[e]) for e in self.esem if self.cnt[e] > 0]
        for b in self.bufs:
            for qq, sm in b.dsem.items():
                evs.append((sm, b.dcnt[qq]))
        for e in self.engs:
            seen = self.seen[e]
            waits = []
            for s, v in evs:
                if e == 'pe' and s == self.esem['pe']:
                    continue
                if seen.get(s, 0) < v:
                    seen[s] = v
                    waits.append((s, v))
            if waits:
                self.q[e].append((waits, [], None, 0))

    def emit(self):
        nc = self.nc
        sems = self.sems

        def replay(name, e):
            for waits, fns, s, inc in self.q[name]:
                for (ws, wv) in waits:
                    e.wait_ge(sems[ws], wv)
                ins = None
                for fn in fns:
                    ins = fn(e)
                if ins is not None and s is not None:
                    ins.then_inc(sems[s], inc)
        with nc.Block() as block:
            @block.tensor
            def _(e):
                replay('pe', e)

            @block.scalar
            def _(e):
                replay('act', e)

            @block.vector
            def _(e):
                replay('dve', e)

            @block.gpsimd
            def _(e):
                replay('pool', e)

            @block.sync
            def _(e):
                replay('sp', e)


class Rec:
    def __init__(self):
        self.items = []

    def op(self, *a, **k):
        self.items.append(('op', a, k))

    def dma(self, *a, **k):
        self.items.append(('dma', a, k))


class Arena:
    LO = 16640
    HI = 229376

    def __init__(self, nc):
        self.nc = nc
        self.off = self.LO
        self.n = 0

    def alloc(self, name, shape, dt):
        esz = 2 if dt == BF16 else 4
        nbytes = int(np.prod(shape[1:])) * esz
        off = (self.off + 31) // 32 * 32
        assert off + nbytes <= self.HI, ("SBUF overflow", name, off, nbytes)
        self.n += 1
        t = self.nc.alloc_sbuf_tensor_at("%s_%d" % (name, self.n), list(shape), dt, offset=off)
        self.off = off + nbytes
        return t


def MM(out, lhsT, rhs, start=True, stop=True):
    return lambda e: e.matmul(out, lhsT=lhsT, rhs=rhs, start=start, stop=stop)


def TR(out, in_, ident):
    return lambda e: e.transpose(out=out, in_=in_, identity=ident)


def ACTV(out, in_, func, **kw):
    return lambda e: e.activation(out=out, in_=in_, func=func, **kw)


def TT(out, in0, in1, op):
    return lambda e: e.tensor_tensor(out=out, in0=in0, in1=in1, op=op)


def TS(out, in0, s1, s2, op0, op1=None):
    if op1 is None:
        return lambda e: e.tensor_scalar(out=out, in0=in0, scalar1=s1, scalar2=None, op0=op0)
    return lambda e: e.tensor_scalar(out=out, in0=in0, scalar1=s1, scalar2=s2, op0=op0, op1=op1)


def STT(out, in0, scalar, in1, op0, op1):
    return lambda e: e.scalar_tensor_tensor(out=out, in0=in0, scalar=scalar, in1=in1, op0=op0, op1=op1)


def CP(out, in_):
    return lambda e: e.tensor_copy(out=out, in_=in_)


def MS(ap, v):
    return lambda e: e.memset(ap, v)


def RED(out, in_, op=None):
    return lambda e: e.tensor_reduce(out=out, in_=in_, axis=AX.X, op=(op or ALU.add))


def RECIP(out, in_):
    return lambda e: e.reciprocal(out=out, in_=in_)


def MAX8(out, in_):
    return lambda e: e.max(out=out, in_=in_)


def DMA(out, in_):
    return lambda e: e.dma_start(out=out, in_=in_)


def CAST(eng, out, in_):
    if eng == 'act':
        return ACTV(out, in_, AF.Copy)
    return CP(out, in_)


def IDMA_G(out, table, idx):
    return lambda e: e.indirect_dma_start(out=out, out_offset=None, in_=table,
                                          in_offset=bass.IndirectOffsetOnAxis(ap=idx, axis=0))


def IDMA_S(table, idx, in_):
    return lambda e: e.indirect_dma_start(out=table, out_offset=bass.IndirectOffsetOnAxis(ap=idx, axis=0),
                                          in_=in_, in_offset=None)


I32 = mybir.dt.int32


def _sparse_moe(nc, S, A, G):
    pbank, pbuf, identb, b_identb = G['pbank'], G['pbuf'], G['identb'], G['b_identb']
    modT, b_modT, gabc, b_gabc = G['modT'], G['b_modT'], G['gabc'], G['b_gabc']
    epsb, b_epsb, wrb, b_wrb, brbc, b_brbc = G['epsb'], G['b_epsb'], G['wrb'], G['b_wrb'], G['brbc'], G['b_brbc']
    out_d, mc_d, w1_d, w3_d, w2_d = G['out_d'], G['mc_d'], G['w1_d'], G['w3_d'], G['w2_d']
    pbf = G['pbf']
    NBLK, BLKR = 48, 512
    xs_d = nc.dram_tensor("xs_scratch", [NBLK * BLKR, D], BF16).ap()
    ys_d = nc.dram_tensor("ys_scratch", [NBLK * BLKR, D], F32).ap()
    w1t = w1_d.rearrange("e (p k) n -> (e p) (k n)", k=8)
    w3t = w3_d.rearrange("e (p k) n -> (e p) (k n)", k=8)
    w2t = w2_d.rearrange("e (p k) n -> (e p) (k n)", k=4)
    T = lambda name, shape, dt: (A.alloc(name, shape, dt), S.buf(name))
    mc, b_mc = T("mc", [128, 225], F32)
    ltri, b_ltri = T("ltri", [128, 128], BF16)
    onesb, b_onesb = T("onesb", [128, 128], BF16)
    ustr, b_ustr = T("ustr", [128, 32], BF16)
    modTp, b_modTp = G['modTp'], G['b_modTp']
    selall, b_selall = T("selall", [128, 32, 32], F32)
    selAall, b_selAall = T("selAall", [128, 32, 32], F32)
    cwall, b_cwall = T("cwall", [128, 32, 32], F32)
    rankall, b_rankall = T("rankall", [128, 32, 32], F32)
    csum, b_csum = T("csum", [128, 32], F32)
    dallf, b_dallf = T("dallf", [128, 64], F32)
    dalli, b_dalli = T("dalli", [128, 64], I32)
    wall, b_wall = T("wall", [128, 64], F32)
    widxf, b_widxf = T("widxf", [128, NBLK], F32)
    widxi, b_widxi = T("widxi", [128, NBLK], I32)
    xt = [A.alloc("xtC", [128, D], F32) for _ in range(2)]; b_xt = [S.buf("xtC%d" % i) for i in range(2)]
    ss = [A.alloc("ssC", [128, 1], F32) for _ in range(2)]; b_ss = [S.buf("ssC%d" % i) for i in range(2)]
    junk, b_junk = T("junkC", [128, D], BF16)
    h2Tt, b_h2Tt = T("h2Tt", [128, 8, 128], BF16)
    lg, b_lg = T("lgC", [128, 36], F32)
    rt, b_rt = T("rtC", [128, 16], F32)
    goh, b_goh = T("gohC", [128, 4], F32)
    gex, b_gex = T("gexC", [128, 4], F32)
    pen, b_pen = T("penC", [128, 4], F32)
    em, b_em = T("emC", [128, 32], F32)
    emc, b_emc = T("emcC", [128, 32], F32)
    mx8c, b_mx8c = T("mx8cC", [128, 8], F32)
    ex, b_ex = T("exC", [128, 32], F32)
    exs, b_exs = T("exsC", [128, 32], F32)
    selb, b_selb = T("selbC", [128, 32], BF16)
    tm, b_tm = T("tmC", [128, 32], F32)
    tm2, b_tm2 = T("tm2C", [128, 32], F32)
    big3, b_big3 = T("big3C", [128, 48 * 32], F32)
    nblk, b_nblk = T("nblkC", [128, 32], F32)
    nbpad, b_nbpad = T("nbpadC", [128, 128], BF16)
    nbT, b_nbT = T("nbTC", [128, 128], BF16)
    pss, b_pss = T("pssC", [128, 32], F32)
    pend, b_pend = T("pendC", [128, 32], F32)
    bex, b_bex = T("bexC", [128, NBLK], F32)
    gmax, ngmax, gsum, nm1, den, fsc = [rt[:, q:q + 1] for q in range(6)]
    PEN = 1.0e4
    MARK = A.off
    xnall = A.alloc("xnall", [128, 32, D], BF16); b_xnall = [S.buf("xnall%d" % i) for i in range(32)]

    zt, b_zt = T("zt", [128, 4, D], BF16)
    S.op('pool', MS(zt[:], 0.0), writes=[b_zt])
    for b in range(NBLK):
        S.dma(DMA(xs_d[b * BLKR:(b + 1) * BLKR, :].rearrange("(s p) d -> p s d", p=128), zt[:]), b_zt, reads=[b_zt])
    S.dma(DMA(mc[:], mc_d[:, :]), b_mc, writes=[b_mc])
    S.op('dve', [CP(ltri[:], mc[:, 0:128]), CP(ustr[0:32, :], mc[0:32, 128:160])], reads=[b_mc], writes=[b_ltri, b_ustr])
    S.op('pool', [MS(onesb[:], 1.0), MS(csum[:], 0.0), MS(nbpad[:], 0.0)], writes=[b_onesb, b_csum, b_nbpad])
    for i in range(32):
        k = i % 2
        S.dma(DMA(xt[k][:], out_d[i * 128:(i + 1) * 128, :]), b_xt[k], writes=[b_xt[k]])
        S.op('act', ACTV(junk[:], xt[k][:], AF.Square, scale=1.0 / 32.0, accum_out=ss[k][:]),
             reads=[b_xt[k]], writes=[b_junk, b_ss[k]])
        S.op('act', ACTV(ss[k][:], ss[k][:], AF.Sqrt, bias=epsb[:]), reads=[b_ss[k], b_epsb], writes=[b_ss[k]])
        S.op('dve', RECIP(ss[k][:], ss[k][:]), reads=[b_ss[k]], writes=[b_ss[k]])
        S.op('dve', TS(xnall[:, i, :], xt[k][:], ss[k][:, 0:1], None, ALU.mult), reads=[b_xt[k], b_ss[k]], writes=[b_xnall[i]])
        pv = pbf(0).rearrange("p (a b) -> p a b", a=8)
        S.op('pe', [TR(pv[:, kc, :], xnall[:, i, kc * 128:(kc + 1) * 128], identb[:]) for kc in range(8)],
             reads=[b_xnall[i], b_identb], writes=[pbuf[0]])
        S.op('act', [ACTV(h2Tt[:, kc, :], pv[:, kc, :], AF.Identity, scale=modT[:, 16 + kc:17 + kc],
                          bias=modT[:, 24 + kc:25 + kc]) for kc in range(8)], reads=[pbuf[0], b_modT], writes=[b_h2Tt])
        S.op('pe', [MM(pbank[1][:, 0:36], h2Tt[:, kc, :], wrb[:, kc, :], kc == 0, kc == 7) for kc in range(8)],
             reads=[b_h2Tt, b_wrb], writes=[pbuf[1]])
        S.op('dve', TT(lg[:], pbank[1][:, 0:36], brbc[:], ALU.add), reads=[pbuf[1], b_brbc], writes=[b_lg])
        S.op('dve', RED(gmax, lg[:, 0:4], ALU.max), reads=[b_lg], writes=[b_rt])
        S.op('dve', TS(ngmax, gmax, -1.0, None, ALU.mult), reads=[b_rt], writes=[b_rt])
        S.op('dve', TS(goh[:], lg[:, 0:4], gmax, None, ALU.is_ge), reads=[b_lg, b_rt], writes=[b_goh])
        S.op('act', ACTV(gex[:], lg[:, 0:4], AF.Exp, bias=ngmax, accum_out=gsum), reads=[b_lg, b_rt], writes=[b_gex, b_rt])
        S.op('dve', RECIP(gsum, gsum), reads=[b_rt], writes=[b_rt])
        S.op('dve', TS(pen[:], goh[:], PEN, -PEN, ALU.mult, ALU.add), reads=[b_goh], writes=[b_pen])
        S.op('dve', TT(em[:].rearrange("p (g e) -> p g e", g=4), lg[:, 4:36].rearrange("p (g e) -> p g e", g=4),
                       pen[:, :].unsqueeze(2).to_broadcast([128, 4, 8]), ALU.add), reads=[b_lg, b_pen], writes=[b_em])
        S.op('dve', MAX8(mx8c[:], em[:]), reads=[b_em], writes=[b_mx8c])
        S.op('dve', TS(nm1, mx8c[:, 0:1], -1.0, None, ALU.mult), reads=[b_mx8c], writes=[b_rt])
        S.op('dve', TS(selall[:, i, :], em[:], mx8c[:, 1:2], None, ALU.is_ge), reads=[b_em, b_mx8c], writes=[b_selall])
        S.op('dve', TS(selAall[:, i, :], em[:], mx8c[:, 0:1], None, ALU.is_ge), reads=[b_em, b_mx8c], writes=[b_selAall])
        S.op('dve', TS(emc[:], em[:], mx8c[:, 1:2], None, ALU.max), reads=[b_em, b_mx8c], writes=[b_emc])
        S.op('act', ACTV(ex[:], emc[:], AF.Exp, bias=nm1), reads=[b_emc, b_rt], writes=[b_ex])
        S.op('dve', TT(exs[:], ex[:], selall[:, i, :], ALU.mult), reads=[b_ex, b_selall], writes=[b_exs])
        S.op('dve', RED(den, exs[:]), reads=[b_exs], writes=[b_rt])
        S.op('dve', RECIP(den, den), reads=[b_rt], writes=[b_rt])
        S.op('dve', TT(fsc, den, gsum, ALU.mult), reads=[b_rt], writes=[b_rt])
        S.op('dve', TS(cwall[:, i, :], exs[:], fsc, None, ALU.mult), reads=[b_exs, b_rt], writes=[b_cwall])
        S.op('dve', CP(selb[:], selall[:, i, :]), reads=[b_selall], writes=[b_selb])
        S.op('pe', MM(pbank[2][:, 0:32], ltri[:], selb[:]), reads=[b_ltri, b_selb], writes=[pbuf[2]])
        S.op('pe', MM(pbank[3][:, 0:32], onesb[:], selb[:]), reads=[b_onesb, b_selb], writes=[pbuf[3]])
        S.op('dve', TT(rankall[:, i, :], pbank[2][:, 0:32], csum[:], ALU.add), reads=[pbuf[2], b_csum], writes=[b_rankall])
        S.op('dve', TT(csum[:], pbank[3][:, 0:32], csum[:], ALU.add), reads=[pbuf[3], b_csum], writes=[b_csum])
    b3 = big3[:, 0:512].rearrange("p (e k) -> p e k", k=16)
    S.op('dve', TT(b3, csum[:, :].unsqueeze(2).to_broadcast([128, 32, 16]),
                   mc[:, 160:176].unsqueeze(1).to_broadcast([128, 32, 16]), ALU.is_gt), reads=[b_csum, b_mc], writes=[b_big3])
    S.op('dve', RED(nblk[:], b3), reads=[b_big3], writes=[b_nblk])
    S.op('dve', CP(nbpad[:, 0:32], nblk[:]), reads=[b_nblk], writes=[b_nbpad])
    pvn = pbf(0)
    S.op('pe', TR(pvn[:, 0:128], nbpad[:], identb[:]), reads=[b_nbpad, b_identb], writes=[pbuf[0]])
    S.op('act', ACTV(nbT[0:32, :], pvn[0:32, 0:128], AF.Copy), reads=[pbuf[0]], writes=[b_nbT])
    S.op('pe', MM(pbank[2][:, 0:32], nbT[0:32, :], ustr[0:32, :]), reads=[b_nbT, b_ustr], writes=[pbuf[2]])
    S.op('dve', CP(pss[:], pbank[2][:, 0:32]), reads=[pbuf[2]], writes=[b_pss])
    S.op('dve', TT(pend[:], pss[:], nblk[:], ALU.add), reads=[b_pss, b_nblk], writes=[b_pend])
    b4 = big3[:, :].rearrange("p (b e) -> p b e", e=32)
    S.op('dve', TT(b4, pend[:, :].unsqueeze(1).to_broadcast([128, NBLK, 32]),
                   mc[:, 176:224].unsqueeze(2).to_broadcast([128, NBLK, 32]), ALU.is_le), reads=[b_pend, b_mc], writes=[b_big3])
    S.op('dve', RED(bex[:], b4), reads=[b_big3], writes=[b_bex])
    S.op('dve', TS(bex[:], bex[:], 31.0, None, ALU.min), reads=[b_bex], writes=[b_bex])
    S.op('dve', TS(widxf[:], bex[:], 128.0, None, ALU.mult), reads=[b_bex], writes=[b_widxf])
    S.op('dve', TT(widxf[:], widxf[:], mc[:, 224:225].to_broadcast([128, NBLK]), ALU.add), reads=[b_widxf, b_mc], writes=[b_widxf])
    S.op('dve', CP(widxi[:], widxf[:]), reads=[b_widxf], writes=[b_widxi])
    S.op('dve', TS(pss[:], pss[:], float(BLKR), None, ALU.mult), reads=[b_pss], writes=[b_pss])
    for i in range(32):
        S.op('dve', TT(tm[:], rankall[:, i, :], pss[:], ALU.add), reads=[b_rankall, b_pss], writes=[b_tm])
        S.op('dve', TT(tm2[:], tm[:], selAall[:, i, :], ALU.mult), reads=[b_tm, b_selAall], writes=[b_tm2])
        S.op('dve', RED(dallf[:, 2 * i:2 * i + 1], tm2[:]), reads=[b_tm2], writes=[b_dallf])
        S.op('dve', TT(tm2[:], selall[:, i, :], selAall[:, i, :], ALU.subtract), reads=[b_selall, b_selAall], writes=[b_tm2])
        S.op('dve', TT(tm[:], tm[:], tm2[:], ALU.mult), reads=[b_tm, b_tm2], writes=[b_tm])
        S.op('dve', RED(dallf[:, 2 * i + 1:2 * i + 2], tm[:]), reads=[b_tm], writes=[b_dallf])
        S.op('dve', TT(tm[:], cwall[:, i, :], tm2[:], ALU.mult), reads=[b_cwall, b_tm2], writes=[b_tm])
        S.op('dve', RED(wall[:, 2 * i + 1:2 * i + 2], tm[:]), reads=[b_tm], writes=[b_wall])
        S.op('dve', TT(tm[:], cwall[:, i, :], selAall[:, i, :], ALU.mult), reads=[b_cwall, b_selAall], writes=[b_tm])
        S.op('dve', RED(wall[:, 2 * i:2 * i + 1], tm[:]), reads=[b_tm], writes=[b_wall])
    S.op('dve', CP(dalli[:], dallf[:]), reads=[b_dallf], writes=[b_dalli])
    S.barrier()
    for i in range(32):
        for kk in range(2):
            S.dma(IDMA_S(xs_d[:, :], dalli[:, 2 * i + kk:2 * i + kk + 1], xnall[:, i, :]), b_xnall[i],
                  reads=[b_xnall[i], b_dalli], q='pool')
    S.barrier()
    A.off = MARK
    w1b = [A.alloc("w1s", [128, 8, 512], BF16) for _ in range(2)]; b_w1b = [S.buf("w1s%d" % i) for i in range(2)]
    w3b = [A.alloc("w3s", [128, 8, 512], BF16) for _ in range(2)]; b_w3b = [S.buf("w3s%d" % i) for i in range(2)]
    w2b = [A.alloc("w2s", [128, 4, D], BF16) for _ in range(2)]; b_w2b = [S.buf("w2s%d" % i) for i in range(2)]
    stg = [A.alloc("stgS", [128, 4096], F32) for _ in range(2)]; b_stg = [S.buf("stgS%d" % i) for i in range(2)]
    xs = [A.alloc("xs", [128, 4, D], BF16) for _ in range(2)]; b_xs = [S.buf("xs%d" % i) for i in range(2)]
    xsT = [A.alloc("xsT", [128, 8, 512], BF16) for _ in range(2)]; b_xsT = [S.buf("xsT%d" % i) for i in range(2)]
    hidT = [A.alloc("hidS", [128, 4, 512], BF16) for _ in range(2)]; b_hid = [S.buf("hidS%d" % i) for i in range(2)]
    sil = [A.alloc("silS", [128, 512], F32) for _ in range(2)]; b_sil = [S.buf("silS%d" % i) for i in range(2)]
    ysb = [A.alloc("ysb", [128, D], F32) for _ in range(2)]; b_ysb = [S.buf("ysb%d" % i) for i in range(2)]
    yg = [A.alloc("yg", [128, D], F32) for _ in range(4)]; b_yg = [S.buf("yg%d" % i) for i in range(4)]
    sidx = [0]
    crr = [0]

    def prep(b):
        wb = b % 2
        xb = b % 2
        S.dma(DMA(xs[xb][:], xs_d[b * BLKR:(b + 1) * BLKR, :].rearrange("(s p) d -> p s d", p=128)), b_xs[xb], writes=[b_xs[xb]])
        items = []
        for (tab, dst, b_dst) in [(w1t, w1b[wb], b_w1b[wb]), (w3t, w3b[wb], b_w3b[wb]), (w2t, w2b[wb], b_w2b[wb])]:
            sk = sidx[0] % 2
            sidx[0] += 1
            S.dma(IDMA_G(stg[sk][:], tab, widxi[:, b:b + 1]), b_stg[sk], reads=[b_widxi], writes=[b_stg[sk]], q='pool')
            crr[0] += 1
            ce = ['dve', 'act'][crr[0] % 2]

            def cast_item(ce=ce, dst=dst, sk=sk, b_dst=b_dst):
                S.op(ce, CAST(ce, dst[:].rearrange("p a b -> p (a b)"), stg[sk][:]), reads=[b_stg[sk]], writes=[b_dst])
            cast_item()
        for sub in range(4):
            def tr_item(sub=sub, xb=xb):
                pv = pbf(0).rearrange("p (a b) -> p a b", a=8)
                S.op('pe', [TR(pv[:, kc, :], xs[xb][:, sub, :].rearrange("p (q k) -> p q k", k=8)[:, :, kc], identb[:])
                            for kc in range(8)], reads=[b_xs[xb], b_identb], writes=[pbuf[0]])
                S.op('act', [ACTV(xsT[xb][:, kc, sub * 128:(sub + 1) * 128], pv[:, kc, :], AF.Identity,
                                  scale=modTp[:, kc:kc + 1], bias=modTp[:, 8 + kc:9 + kc]) for kc in range(8)],
                     reads=[pbuf[0], b_modTp], writes=[b_xsT[xb]])
            items.append(tr_item)
        return items

    for it in prep(0):
        it()
    for b in range(NBLK):
        wb = b % 2
        xb = b % 2
        hk = b % 2
        nxt = prep(b + 1) if b + 1 < NBLK else []
        for fc in range(4):
            pb1 = 2 + (fc % 2)
            pb3 = 4 + (fc % 2)
            sk2 = fc % 2
            S.op('pe', [MM(pbank[pb1][:, :], w1b[wb][:, kc, :].rearrange("p (m f) -> p m f", f=4)[:, :, fc], xsT[xb][:, kc, :],
                           kc == 0, kc == 7) for kc in range(8)], reads=[b_w1b[wb], b_xsT[xb]], writes=[pbuf[pb1]])
            S.op('pe', [MM(pbank[pb3][:, :], w3b[wb][:, kc, :].rearrange("p (m f) -> p m f", f=4)[:, :, fc], xsT[xb][:, kc, :],
                           kc == 0, kc == 7) for kc in range(8)], reads=[b_w3b[wb], b_xsT[xb]], writes=[pbuf[pb3]])
            S.op('act', ACTV(sil[sk2][:], pbank[pb1][:, :], AF.Silu), reads=[pbuf[pb1]], writes=[b_sil[sk2]])
            S.op('dve', TT(hidT[hk][:, fc, :], pbank[pb3][:, :], sil[sk2][:], ALU.mult),
                 reads=[pbuf[pb3], b_sil[sk2]], writes=[b_hid[hk]])
            if nxt:
                nxt.pop(0)()
        for j in range(4):
            yk = j % 2
            for hf2 in range(2):
                po = 6 + hf2
                S.op('pe', [MM(pbank[po][:, :], hidT[hk][:, fc, j * 128:(j + 1) * 128],
                               w2b[wb][:, fc, hf2 * 512:(hf2 + 1) * 512], fc == 0, fc == 3) for fc in range(4)],
                     reads=[b_hid[hk], b_w2b[wb]], writes=[pbuf[po]])
                S.op('dve' if hf2 else 'act',
                     CAST('dve' if hf2 else 'act', ysb[yk][:, hf2 * 512:(hf2 + 1) * 512], pbank[po][:, :]),
                     reads=[pbuf[po]], writes=[b_ysb[yk]])
            r0 = b * BLKR + j * 128
            S.dma(DMA(ys_d[r0:r0 + 128, :], ysb[yk][:]), b_ysb[yk], reads=[b_ysb[yk]])
        while nxt:
            nxt.pop(0)()
    S.barrier()
    for i in range(32):
        k = i % 2
        g0, g1 = yg[2 * k], yg[2 * k + 1]
        S.dma(IDMA_G(g0[:], ys_d[:, :], dalli[:, 2 * i:2 * i + 1]), b_yg[2 * k], reads=[b_dalli], writes=[b_yg[2 * k]], q='pool')
        S.dma(IDMA_G(g1[:], ys_d[:, :], dalli[:, 2 * i + 1:2 * i + 2]), b_yg[2 * k + 1], reads=[b_dalli], writes=[b_yg[2 * k + 1]], q='pool')
        S.dma(DMA(xt[k][:], out_d[i * 128:(i + 1) * 128, :]), b_xt[k], writes=[b_xt[k]])
        S.op('dve', TS(g0[:], g0[:], wall[:, 2 * i:2 * i + 1], None, ALU.mult), reads=[b_yg[2 * k], b_wall], writes=[b_yg[2 * k]])
        S.op('dve', STT(g0[:], g1[:], wall[:, 2 * i + 1:2 * i + 2], g0[:], ALU.mult, ALU.add),
             reads=[b_yg[2 * k], b_yg[2 * k + 1], b_wall], writes=[b_yg[2 * k]])
        S.op('dve', TT(g0[:], g0[:], gabc[:, 1, :], ALU.mult), reads=[b_yg[2 * k], b_gabc], writes=[b_yg[2 * k]])
        S.op('dve', TT(g0[:], g0[:], xt[k][:], ALU.add), reads=[b_yg[2 * k], b_xt[k]], writes=[b_yg[2 * k]])
        S.dma(DMA(out_d[i * 128:(i + 1) * 128, :], g0[:]), b_yg[2 * k], reads=[b_yg[2 * k]])
    S.barrier()


def build_program(stop_after=None):
    nc = bass.Bass("TRN2", target_bir_lowering=False)
    din = lambda name, shape: nc.dram_tensor(name, list(shape), F32, kind="ExternalInput").ap()
    x_d = din("x", [S_LEN, D])
    cT_d = din("cT", [128, 8])
    wada_d = din("w_ada", [D, 6 * D])
    bada_d = din("b_ada", [1, 6 * D])
    g1_d = din("g1", [1, D])
    g2_d = din("g2", [1, D])
    win_d = din("w_in", [D, 5120])
    gqk_d = din("gqk", [128, 512])
    cs1_d = din("cs1", [128, NT * 16])
    sn2_d = din("sn2", [128, NT * 16])
    convw_d = din("convw", [128, 12])
    convb_d = din("convb", [128, 4])
    wpa_d = din("w_pa", [512, D])
    wpb_d = din("w_pb", [512, D])
    wo_d = din("w_o", [D, D])
    wr_d = din("wr", [D, 36])
    br_d = din("br", [128, 36])
    if stop_after is None:
        w1_d = din("w1", [32, D, 512])
        w3_d = din("w3", [32, D, 512])
        w2_d = din("w2", [32, 512, D])
    ident_d = din("ident", [128, 128])
    tri_d = din("tri", [128, 512])
    oneh_d = din("oneh", [16, S_LEN])
    mc_d = din("mconst", [128, 225])
    out_d = nc.dram_tensor("out", [S_LEN, D], F32, kind="ExternalOutput").ap()
    ya_d = nc.dram_tensor("ya_scratch", [512, S_LEN], BF16, kind="ExternalOutput").ap()

    S = Sched(nc)
    A = Arena(nc)
    pbank = [nc.alloc_psum_tensor("pb%d" % i, [128, 512], F32) for i in range(8)]
    pbuf = [S.buf("pb%d" % i) for i in range(8)]

    def pbf(i):
        return pbank[i][:, :].bitcast(BF16)

    identb = A.alloc("identb", [128, 128], BF16); b_identb = S.buf("identb")
    cs1 = A.alloc("cs1", [128, NT, 16], F32); b_cs1 = S.buf("cs1")
    sn2 = A.alloc("sn2", [128, NT, 16], F32); b_sn2 = S.buf("sn2")
    trib = A.alloc("trib", [128, 2, 256], BF16); b_trib = S.buf("trib")
    modT = A.alloc("modT", [128, 32], F32); b_modT = S.buf("modT")
    gabc = A.alloc("gabc", [128, 2, D], F32); b_gabc = S.buf("gabc")
    epsb = A.alloc("epsb", [128, 1], F32); b_epsb = S.buf("epsb")
    onesf = A.alloc("onesf", [128, 128], F32); b_onesf = S.buf("onesf")
    gqk = A.alloc("gqk", [128, 512], F32); b_gqk = S.buf("gqk")
    convw = A.alloc("convw", [128, 4, 3], F32); b_convw = S.buf("convw")
    convb = A.alloc("convb", [128, 4], F32); b_convb = S.buf("convb")
    brbc = A.alloc("brbc", [128, 36], F32); b_brbc = S.buf("brbc")
    wrb = A.alloc("wrb", [128, 8, 36], BF16); b_wrb = S.buf("wrb")
    modTp = A.alloc("modTp", [128, 16], F32); b_modTp = S.buf("modTp")
    PERSIST = A.off

    stgc = A.alloc("stgc", [128, 512], F32); b_stgc = S.buf("stgc")
    stgi = A.alloc("stgi", [128, 128], F32); b_stgi = S.buf("stgi")
    stgr = A.alloc("stgr", [128, 8, 36], F32); b_stgr = S.buf("stgr")
    cT = A.alloc("cT", [128, 8], F32); b_cT = S.buf("cT")
    sT = A.alloc("sT", [128, 8], F32); b_sT = S.buf("sT")
    wst = [A.alloc("wst", [128, 8, 512], F32) for _ in range(2)]
    b_wst = [S.buf("wst%d" % i) for i in range(2)]
    modrow = A.alloc("modrow", [1, 6 * D], F32); b_modrow = S.buf("modrow")
    badar = A.alloc("badar", [1, 6 * D], F32); b_badar = S.buf("badar")
    grow = A.alloc("grow", [1, 2 * D], F32); b_grow = S.buf("grow")
    arow = A.alloc("arow", [1, 2 * D], F32); b_arow = S.buf("arow")

    S.op('pool', [MS(epsb[:], 1e-6), MS(onesf[:], 1.0)], writes=[b_epsb, b_onesf])
    S.dma(DMA(stgi[:], ident_d[:, :]), b_stgi, writes=[b_stgi])
    S.op('dve', CP(identb[:], stgi[:]), reads=[b_stgi], writes=[b_identb])
    S.dma(DMA(stgc[:], tri_d[:, :]), b_stgc, writes=[b_stgc])
    S.op('dve', CP(trib[:].rearrange("p a b -> p (a b)"), stgc[:]), reads=[b_stgc], writes=[b_trib])
    S.dma(DMA(cs1[:].rearrange("p a b -> p (a b)"), cs1_d[:, :]), b_cs1, writes=[b_cs1])
    S.dma(DMA(sn2[:].rearrange("p a b -> p (a b)"), sn2_d[:, :]), b_sn2, writes=[b_sn2])
    S.dma(DMA(gqk[:], gqk_d[:, :]), b_gqk, writes=[b_gqk])
    S.dma(DMA(convw[:].rearrange("p a b -> p (a b)"), convw_d[:, :]), b_convw, writes=[b_convw])
    S.dma(DMA(convb[:], convb_d[:, :]), b_convb, writes=[b_convb])
    S.dma(DMA(brbc[:], br_d[:, :]), b_brbc, writes=[b_brbc])
    S.dma(DMA(stgr[:], wr_d.rearrange("(kc p) n -> p kc n", p=128)), b_stgr, writes=[b_stgr])
    S.op('dve', CP(wrb[:], stgr[:]), reads=[b_stgr], writes=[b_wrb])
    S.dma(DMA(cT[:], cT_d[:, :]), b_cT, writes=[b_cT])
    S.op('act', ACTV(sT[:], cT[:], AF.Silu), reads=[b_cT], writes=[b_sT])
    S.dma(DMA(badar[:], bada_d[:, :]), b_badar, writes=[b_badar])
    S.dma(DMA(grow[0:1, 0:D], g1_d[:, :]), b_grow, writes=[b_grow])
    S.dma(DMA(grow[0:1, D:2 * D], g2_d[:, :]), b_grow, writes=[b_grow])
    for j in range(12):
        wb = j % 2
        S.dma(DMA(wst[wb][:], wada_d[:, j * 512:(j + 1) * 512].rearrange("(kc p) n -> p kc n", p=128)),
              b_wst[wb], writes=[b_wst[wb]])
        S.op('pe', [MM(pbank[0][0:1, :], sT[:, kc:kc + 1], wst[wb][:, kc, :], kc == 0, kc == 7) for kc in range(8)],
             reads=[b_sT, b_wst[wb]], writes=[pbuf[0]])
        S.op('dve', TT(modrow[0:1, j * 512:(j + 1) * 512], pbank[0][0:1, :], badar[0:1, j * 512:(j + 1) * 512], ALU.add),
             reads=[pbuf[0], b_badar], writes=[b_modrow])
    S.op('dve', STT(arow[0:1, 0:D], modrow[0:1, D:2 * D], 1.0, grow[0:1, 0:D], ALU.add, ALU.mult),
         reads=[b_modrow, b_grow], writes=[b_arow])
    S.op('dve', STT(arow[0:1, D:2 * D], modrow[0:1, 4 * D:5 * D], 1.0, grow[0:1, D:2 * D], ALU.add, ALU.mult),
         reads=[b_modrow, b_grow], writes=[b_arow])
    fns = []
    srcs = [(arow, 0), (modrow, 0), (arow, D), (modrow, 3 * D)]
    for r, (src, off) in enumerate(srcs):
        for kc in range(8):
            fns.append(MM(pbank[1][:, r * 8 + kc:r * 8 + kc + 1], src[0:1, off + kc * 128:off + (kc + 1) * 128],
                          onesf[0:1, 0:1]))
    S.op('pe', fns, reads=[b_arow, b_modrow, b_onesf], writes=[pbuf[1]])
    S.op('dve', CP(modT[:], pbank[1][:, 0:32]), reads=[pbuf[1]], writes=[b_modT])
    fns = []
    for r, (src, off) in enumerate([(arow, D), (modrow, 3 * D)]):
        for kc in range(8):
            fns.append(MM(pbank[1][:, r * 8 + kc:r * 8 + kc + 1],
                          src[0:1, off:off + D].rearrange("o (p k) -> o p k", k=8)[:, :, kc], onesf[0:1, 0:1]))
    S.op('pe', fns, reads=[b_arow, b_modrow, b_onesf], writes=[pbuf[1]])
    S.op('dve', CP(modTp[:], pbank[1][:, 0:16]), reads=[pbuf[1]], writes=[b_modTp])
    for gi, off in enumerate([2 * D, 5 * D]):
        for hf in range(2):
            S.op('pe', MM(pbank[2][:, :], onesf[0:1, 0:128], modrow[0:1, off + hf * 512:off + (hf + 1) * 512]),
                 reads=[b_onesf, b_modrow], writes=[pbuf[2]])
            S.op('act', ACTV(gabc[:, gi, hf * 512:(hf + 1) * 512], pbank[2][:, :], AF.Copy),
                 reads=[pbuf[2]], writes=[b_gabc])
    S.barrier()
    A.off = PERSIST
    if stop_after == '0':
        S.emit()
        return nc

    def make_norm_bufs(with_xt=True, with_junk=True):
        d = {}
        d['xt'] = [A.alloc("xt", [128, D], F32) for _ in range(2)] if with_xt else None
        d['b_xt'] = [S.buf("xt%d" % i) for i in range(2)]
        if with_junk:
            d['junk'] = A.alloc("junk", [128, D], BF16); d['b_junk'] = S.buf("junk")
        d['ss'] = [A.alloc("ss", [128, 1], F32) for _ in range(2)]
        d['b_ss'] = [S.buf("ss%d" % i) for i in range(2)]
        d['xn'] = [A.alloc("xn", [128, D], BF16) for _ in range(2)]
        d['b_xn'] = [S.buf("xn%d" % i) for i in range(2)]
        return d

    def norm_tile(nb, par, xsrc, b_xsrc, hT_dst, b_hT, col0, ptr_i=0, sch=None):
        S_ = sch or S
        k = par % 2
        S_.op('act', ACTV(nb['junk'][:], xsrc, AF.Square, scale=1.0 / 32.0, accum_out=nb['ss'][k][:]),
             reads=[b_xsrc], writes=[nb['b_junk'], nb['b_ss'][k]])
        S_.op('act', ACTV(nb['ss'][k][:], nb['ss'][k][:], AF.Ln, bias=epsb[:]),
             reads=[nb['b_ss'][k], b_epsb], writes=[nb['b_ss'][k]])
        S_.op('act', ACTV(nb['ss'][k][:], nb['ss'][k][:], AF.Exp, scale=-0.5), reads=[nb['b_ss'][k]], writes=[nb['b_ss'][k]])
        S_.op('dve', TS(nb['xn'][k][:], xsrc, nb['ss'][k][:, 0:1], None, ALU.mult),
             reads=[b_xsrc, nb['b_ss'][k]], writes=[nb['b_xn'][k]])
        pv = pbf(ptr_i).rearrange("p (a b) -> p a b", a=8)
        S_.op('pe', [TR(pv[:, kc, :], nb['xn'][k][:, kc * 128:(kc + 1) * 128], identb[:]) for kc in range(8)],
             reads=[nb['b_xn'][k], b_identb], writes=[pbuf[ptr_i]])
        S_.op('act', [ACTV(hT_dst[:, kc, :], pv[:, kc, :], AF.Identity, scale=modT[:, col0 + kc:col0 + kc + 1],
                          bias=modT[:, col0 + 8 + kc:col0 + 9 + kc]) for kc in range(8)],
             reads=[pbuf[ptr_i], b_modT], writes=[b_hT])

    engrr = [0]

    def cast_eng():
        engrr[0] += 1
        return ['dve', 'act'][engrr[0] % 2]

    KxT = A.alloc("KxT", [128, 4, S_LEN], BF16)
    b_Kx = [S.buf("Kx%d" % c) for c in range(8)]
    Vx = A.alloc("Vx", [128, NT, 4, 65], BF16)
    b_Vx = [S.buf("Vx%d" % c) for c in range(8)]
    kmT = A.alloc("kmT", [128, 4, 16], BF16)
    b_km = [S.buf("km%d" % c) for c in range(16)]
    kms = A.alloc("kms", [128, 4], F32); b_kms = S.buf("kms")
    wqkv = A.alloc("wqkv", [128, 8, 768], BF16); b_wqkv = S.buf("wqkv")
    stgA = [A.alloc("stgA", [128, 8, 256], F32) for _ in range(2)]
    b_stgA = [S.buf("stgA%d" % i) for i in range(2)]
    stgo = A.alloc("stgo", [128, S_LEN], F32); b_stgo = S.buf("stgo")
    nb = make_norm_bufs()
    hT = [A.alloc("hT", [128, 8, 512], BF16) for _ in range(2)]
    b_hT = [S.buf("hT%d" % i) for i in range(2)]
    QxT = [A.alloc("QxT", [128, 4, 512], BF16) for _ in range(2)]
    b_Qx = [S.buf("Qx%d" % i) for i in range(2)]
    sq = A.alloc("sq", [128, 512], F32); b_sq = S.buf("sq")
    ssq = [A.alloc("ssq", [128, 8], F32) for _ in range(2)]; b_ssq = [S.buf("ssq%d" % i) for i in range(2)]
    qn = [A.alloc("qn", [128, 512], F32) for _ in range(2)]
    b_qn = [S.buf("qn%d" % i) for i in range(2)]
    tA = [A.alloc("tA", [128, 8, 16], F32) for _ in range(2)]; b_tA = [S.buf("tA%d" % i) for i in range(2)]
    tB = [A.alloc("tB", [128, 8, 16], F32) for _ in range(2)]; b_tB = [S.buf("tB%d" % i) for i in range(2)]
    qkb = [A.alloc("qkb", [128, 8, 128], BF16) for _ in range(2)]
    b_qkb = [S.buf("qkb%d" % i) for i in range(2)]
    gsb = A.alloc("gsb", [128, 4, 16], F32); b_gsb = S.buf("gsb")
    mx8 = A.alloc("mx8", [128, 4, 8], F32); b_mx8 = S.buf("mx8")
    sel = A.alloc("sel", [128, 4, 16], F32); b_sel = S.buf("sel")
    mbp = [A.alloc("mbp", [128, 4, 128], BF16) for _ in range(2)]
    b_mbp = [S.buf("mbp%d" % i) for i in range(2)]
    pT = [A.alloc("pT", [128, 512], BF16) for _ in range(3)]
    b_pT = [S.buf("pT%d" % i) for i in range(3)]
    rd = A.alloc("rd", [128, 512], F32); b_rd = S.buf("rd")
    bcs = A.alloc("bcs", [128, 512], F32); b_bcs = S.buf("bcs")
    yo = [A.alloc("yo", [128, 512], BF16) for _ in range(2)]
    b_yo = [S.buf("yo%d" % i) for i in range(2)]

    S.dma(DMA(stgo[64:80, :], oneh_d[:, :]), b_stgo, writes=[b_stgo])
    S.op('dve', [CP(KxT[64:80, h, :], stgo[64:80, :]) for h in range(4)], reads=[b_stgo], writes=b_Kx)
    S.op('dve', [MS(Vx[:, :, :, 64:65], 1.0), MS(mbp[0][:], 0.0), MS(mbp[1][:], 0.0),
                  MS(qkb[0][:], 0.0), MS(qkb[1][:], 0.0)],
         writes=b_Vx + b_mbp + b_qkb)

    rot = [0]
    L = _DBG.get('lvl', 9)
    b_pg = S.buf('pg')
    for hh in range(_DBG.get('nhh', 2)):
        for part, c0 in enumerate([hh * 256, 512 + hh * 256, 1024 + hh * 256]):
            sb = part % 2
            S.dma(DMA(stgA[sb][:], win_d[:, c0:c0 + 256].rearrange("(kc p) n -> p kc n", p=128)),
                  b_stgA[sb], writes=[b_stgA[sb]])
            ce = cast_eng()
            S.op(ce, CAST(ce, wqkv[:, :, part * 256:(part + 1) * 256], stgA[sb][:]),
                 reads=[b_stgA[sb]], writes=[b_wqkv])
        NCH = _DBG.get('nch', 8)
        R = Rec()

        def stageA(c, j):
            i = 4 * c + j
            k = i % 2
            hb = c % 2
            R.dma(DMA(nb['xt'][k][:], x_d[i * 128:(i + 1) * 128, :]), nb['b_xt'][k], writes=[nb['b_xt'][k]])
            norm_tile(nb, i, nb['xt'][k][:], nb['b_xt'][k], hT[hb][:, :, j * 128:(j + 1) * 128], b_hT[hb], 0, sch=R)

        def stageB(c, j):
            i = 4 * c + j
            k = i % 2
            hb = c % 2
            R.op('pe', [MM(pbank[1][:, :], hT[hb][:, kc, j * 128:(j + 1) * 128], wqkv[:, kc, 0:512], kc == 0, kc == 7)
                        for kc in range(8)], reads=[b_hT[hb], b_wqkv], writes=[pbuf[1]])
            R.op('pe', [MM(pbank[2][:, 0:256], hT[hb][:, kc, j * 128:(j + 1) * 128], wqkv[:, kc, 512:768], kc == 0, kc == 7)
                        for kc in range(8)], reads=[b_hT[hb], b_wqkv], writes=[pbuf[2]])
            R.op('act', ACTV(Vx[:, i, :, 0:64], pbank[2][:, 0:256].rearrange("p (h d) -> p h d", h=4), AF.Copy),
                 reads=[pbuf[2]], writes=[b_Vx[c]])
            R.op('act', ACTV(sq[:], pbank[1][:, :], AF.Square), reads=[pbuf[1]], writes=[b_sq])
            R.op('dve', RED(ssq[k][:], sq[:].rearrange("p (h d) -> p h d", h=8)), reads=[b_sq], writes=[b_ssq[k]])
            R.op('act', ACTV(ssq[k][:], ssq[k][:], AF.Ln, scale=1.0 / 64.0, bias=epsb[:]),
                 reads=[b_ssq[k], b_epsb], writes=[b_ssq[k]])
            R.op('act', ACTV(ssq[k][:], ssq[k][:], AF.Exp, scale=-0.5), reads=[b_ssq[k]], writes=[b_ssq[k]])
            qv = qn[k][:].rearrange("p (h d) -> p h d", h=8)
            R.op('dve', TT(qv, pbank[1][:, :].rearrange("p (h d) -> p h d", h=8),
                           ssq[k][:, :].unsqueeze(2).to_broadcast([128, 8, 64]), ALU.mult),
                 reads=[pbuf[1], b_ssq[k]], writes=[b_qn[k]])

        def stageC(c, j):
            i = 4 * c + j
            k = i % 2
            qb = c % 2
            qv = qn[k][:].rearrange("p (h d) -> p h d", h=8)
            R.op('dve', TT(qn[k][:], qn[k][:], gqk[:], ALU.mult), reads=[b_qn[k], b_gqk], writes=[b_qn[k]])
            R.op('dve', [TT(tA[k][:], qv[:, :, 0:16], cs1[:, i, :].unsqueeze(1).to_broadcast([128, 8, 16]), ALU.mult),
                         TT(tB[k][:, :, 0:8], qv[:, :, 8:16], sn2[:, i, 0:8].unsqueeze(1).to_broadcast([128, 8, 8]), ALU.mult),
                         TT(tB[k][:, :, 8:16], qv[:, :, 0:8], sn2[:, i, 8:16].unsqueeze(1).to_broadcast([128, 8, 8]), ALU.mult)],
                 reads=[b_qn[k], b_cs1, b_sn2], writes=[b_tA[k], b_tB[k]])
            R.op('act', ACTV(qkb[k][:, :, 16:64], qv[:, :, 16:64], AF.Copy), reads=[b_qn[k]], writes=[b_qkb[k]])
            R.op('dve', TT(qkb[k][:, :, 0:16], tA[k][:], tB[k][:], ALU.add), reads=[b_tA[k], b_tB[k]], writes=[b_qkb[k]])
            ptq = pbf(0).rearrange("p (a b) -> p a b", a=8)
            R.op('pe', [TR(ptq[:, s, :], qkb[k][:, s, :], identb[:]) for s in range(8)],
                 reads=[b_qkb[k], b_identb], writes=[pbuf[0]])
            R.op('act', [ACTV(QxT[qb][0:64, h4, j * 128:(j + 1) * 128], ptq[0:64, h4, :], AF.Copy) for h4 in range(4)],
                 reads=[pbuf[0]], writes=[b_Qx[qb]])
            R.op('act', [ACTV(KxT[0:64, h4, i * 128:(i + 1) * 128], ptq[0:64, 4 + h4, :], AF.Copy) for h4 in range(4)],
                 reads=[pbuf[0]], writes=[b_Kx[c]])
            if i % 2 == 1:
                blk = i // 2
                R.op('dve', RED(kms[0:64, :], KxT[0:64, :, blk * 256:(blk + 1) * 256]),
                     reads=[b_Kx[c]], writes=[b_kms])
                R.op('dve', TS(kmT[0:64, :, blk], kms[0:64, :], 1.0 / 256.0, None, ALU.mult),
                     reads=[b_kms], writes=[b_km[blk]])

        def prep_items(c):
            R.items = []
            for j in range(4):
                stageA(c, j)
                stageB(c, j)
                stageC(c, j)
            return list(R.items)

        def replay(item):
            kind, a_, k_ = item
            getattr(S, kind)(*a_, **k_)

        pending = prep_items(0)
        for c in range(NCH):
            hb = c % 2
            qb = c % 2
            for item in pending:
                replay(item)
            pending = prep_items(c + 1) if c + 1 < NCH else []
            for j in range(4 if _DBG.get('gate', True) else 0):
                i = 4 * c + j
                cur = i // 2
                m = j % 2
                pg = pbank[2][:, 256:320].rearrange("p (h n) -> p h n", h=4)
                fl = [MS(gsb[:, :, cur:cur + 1], BIG)] + ([MS(gsb[:, :, cur + 1:16], -BIG)] if cur < 15 else [])
                S.op('dve', fl, writes=[b_gsb])
                if cur > 0:
                    S.op('pe', [MM(pg[:, h, 0:cur], QxT[qb][0:64, h, j * 128:(j + 1) * 128], kmT[0:64, h, 0:cur])
                                for h in range(4)], reads=[b_Qx[qb]] + b_km[0:cur], writes=[b_pg])
                    S.op('dve', CP(gsb[:, :, 0:cur], pg[:, :, 0:cur]), reads=[b_pg], writes=[b_gsb])
                S.op('dve', [MAX8(mx8[:, h, :], gsb[:, h, :]) for h in range(4)], reads=[b_gsb], writes=[b_mx8])
                S.op('dve', TT(sel[:], gsb[:], mx8[:, :, 3:4].to_broadcast([128, 4, 16]), ALU.is_ge),
                     reads=[b_gsb, b_mx8], writes=[b_sel])
                S.op('dve', TS(mbp[m][:, :, 64:80], sel[:], MASKV, -MASKV, ALU.mult, ALU.add),
                     reads=[b_sel], writes=[b_mbp[m]])
                pmb = pbf(0).rearrange("p (a b) -> p a b", a=8)
                S.op('pe', [TR(pmb[:, h, :], mbp[m][:, h, :], identb[:]) for h in range(4)],
                     reads=[b_mbp[m], b_identb], writes=[pbuf[0]])
                S.op('act', [ACTV(QxT[qb][64:80, h4, j * 128:(j + 1) * 128], pmb[64:80, h4, :], AF.Copy) for h4 in range(4)],
                     reads=[pbuf[0]], writes=[b_Qx[qb]])
            nsteps = 4 * (4 * c + 4)
            stepc = [0]
            for h in range(4 if _DBG.get('attn', True) else 0):
                nk = 4 * c + 4
                pyi = 6 + (h % 2)

                def cols_of(kt):
                    return (0, 512) if kt < 4 * c + 2 else (256, 512)

                def emit_S(kt, r):
                    c0, c1 = cols_of(kt)
                    S.op('pe', MM(pbank[3 + r][:, c0:c1], KxT[0:80, h, kt * 128:(kt + 1) * 128], QxT[qb][0:80, h, c0:c1]),
                         reads=[b_Kx[kt // 4], b_Qx[qb]], writes=[pbuf[3 + r]])
                rs = []
                for kt in range(nk):
                    rs.append(rot[0] % 3)
                    rot[0] += 1
                emit_S(0, rs[0])
                if nk > 1:
                    emit_S(1, rs[1])
                for kt in range(nk):
                    if kt + 2 < nk:
                        emit_S(kt + 2, rs[kt + 2])
                    r = rs[kt]
                    c0, c1 = cols_of(kt)
                    S.op('act', ACTV(pT[r][:, c0:c1], pbank[3 + r][:, c0:c1], AF.Exp, scale=0.125),
                         reads=[pbuf[3 + r]], writes=[b_pT[r]])
                    if kt >= 4 * c:
                        d0 = 0 if kt < 4 * c + 2 else 256
                        S.op('dve', TT(pT[r][:, d0:d0 + 256], pT[r][:, d0:d0 + 256], trib[:, kt % 2, :], ALU.mult),
                             reads=[b_pT[r], b_trib], writes=[b_pT[r]])
                    S.op('pe', MM(pbank[pyi][0:65, c0:c1], Vx[:, kt, h, :], pT[r][:, c0:c1], kt == 0, kt == nk - 1),
                         reads=[b_Vx[kt // 4], b_pT[r]], writes=[pbuf[pyi]])
                    stepc[0] += 1
                    if pending and _DBG.get('ilv', True):
                        left = max(1, nsteps - stepc[0] + 1)
                        for _ in range(-(-len(pending) // left)):
                            replay(pending.pop(0))
                yb = h % 2
                S.op('dve', RECIP(rd[64:65, :], pbank[pyi][64:65, :]), reads=[pbuf[pyi]], writes=[b_rd])
                rb = 3 + (rot[0] % 3)
                rot[0] += 1
                S.op('pe', MM(pbank[rb][0:64, :], onesf[64:65, 0:64], rd[64:65, :]),
                     reads=[b_onesf, b_rd], writes=[pbuf[rb]])
                S.op('act', ACTV(bcs[0:64, :], pbank[rb][0:64, :], AF.Copy), reads=[pbuf[rb]], writes=[b_bcs])
                S.op('dve', TT(yo[yb][0:64, :], pbank[pyi][0:64, :], bcs[0:64, :], ALU.mult),
                     reads=[pbuf[pyi], b_bcs], writes=[b_yo[yb]])
                hg = hh * 4 + h
                S.dma(DMA(ya_d[hg * 64:(hg + 1) * 64, c * 512:(c + 1) * 512], yo[yb][0:64, :]),
                      b_yo[yb], reads=[b_yo[yb]])
    S.barrier()
    A.off = PERSIST
    if stop_after == 'A':
        S.emit()
        return nc
    wB = A.alloc("wB", [128, 8, 3584], BF16); b_wB = S.buf("wB")
    wpa = A.alloc("wpa", [128, 4, D], BF16); b_wpa = S.buf("wpa")
    wpb = A.alloc("wpb", [128, 4, D], BF16); b_wpb = S.buf("wpb")
    wo = A.alloc("wo", [128, 8, D], BF16); b_wo = S.buf("wo")
    stgB = [A.alloc("stgB", [128, 2048], F32) for _ in range(2)]
    b_stgB = [S.buf("stgB%d" % i) for i in range(2)]
    sidx = [0]

    def load_cast(dst, src_ap, shape3, b_dst, extra=None):
        k = sidx[0] % len(stgB)
        sidx[0] += 1
        a, bb = shape3
        view = stgB[k][:, 0:a * bb].rearrange("p (a b) -> p a b", a=a)
        S.dma(DMA(view, src_ap), b_stgB[k], writes=[b_stgB[k]])
        if extra is None:
            ce = cast_eng()
            S.op(ce, CAST(ce, dst, view), reads=[b_stgB[k]], writes=[b_dst])
        else:
            ex, b_ex = extra
            S.op('dve', [TT(dst[:, q, :], view[:, q, :], ex, ALU.mult) for q in range(a)],
                 reads=[b_stgB[k], b_ex], writes=[b_dst])

    for p in range(14):
        c0 = 1536 + p * 256
        load_cast(wB[:, :, p * 256:(p + 1) * 256], win_d[:, c0:c0 + 256].rearrange("(kc p) n -> p kc n", p=128),
                  (8, 256), b_wB)
    for q2 in range(2):
        load_cast(wpa[:, 2 * q2:2 * q2 + 2, :], wpa_d[q2 * 256:(q2 + 1) * 256, :].rearrange("(cc p) n -> p cc n", p=128),
                  (2, D), b_wpa)
        load_cast(wpb[:, 2 * q2:2 * q2 + 2, :], wpb_d[q2 * 256:(q2 + 1) * 256, :].rearrange("(cc p) n -> p cc n", p=128),
                  (2, D), b_wpb)
    for q4 in range(4):
        load_cast(wo[:, 2 * q4:2 * q4 + 2, :], wo_d[q4 * 256:(q4 + 1) * 256, :].rearrange("(cc p) n -> p cc n", p=128),
                  (2, D), b_wo)
    nbB = make_norm_bufs()
    hTB = A.alloc("hTB", [128, 8, 512], BF16); b_hTB = S.buf("hTB")
    yaT = A.alloc("yaT", [128, 4, 512], BF16); b_yaT = S.buf("yaT")
    xbs = A.alloc("xbs", [128, 512], F32); b_xbs = S.buf("xbs")
    bgs = A.alloc("bgs", [128, 512], F32); b_bgs = S.buf("bgs")
    u = A.alloc("u", [128, 4, 514], F32); b_u = [S.buf("u%d" % i) for i in range(4)]
    tcv = A.alloc("tcv", [128, 512], F32); b_tcv = S.buf("tcv")
    ybT = A.alloc("ybT", [128, 4, 512], BF16); b_ybT = S.buf("ybT")
    gas = A.alloc("gas", [128, 512], F32); b_gas = S.buf("gas")
    gbs = A.alloc("gbs", [128, 512], F32); b_gbs = S.buf("gbs")
    t1 = A.alloc("t1", [128, 512], F32); b_t1 = S.buf("t1")
    t2 = A.alloc("t2", [128, 512], F32); b_t2 = S.buf("t2")
    mT = A.alloc("mT", [128, 8, 512], BF16); b_mT = S.buf("mT")
    xr = [A.alloc("xr", [128, D], F32) for _ in range(2)]
    b_xr = [S.buf("xr%d" % i) for i in range(2)]
    to = A.alloc("to", [128, 512], F32); b_to = S.buf("to")
    x1t = [A.alloc("x1t", [128, D], F32) for _ in range(2)]
    b_x1t = [S.buf("x1t%d" % i) for i in range(2)]
    S.op('dve', MS(u[:], 0.0), writes=b_u)
    for c in range(8):
        for j in range(4):
            i = 4 * c + j
            k = i % 2
            S.dma(DMA(nbB['xt'][k][:], x_d[i * 128:(i + 1) * 128, :]), nbB['b_xt'][k], writes=[nbB['b_xt'][k]])
            norm_tile(nbB, i, nbB['xt'][k][:], nbB['b_xt'][k], hTB[:, :, j * 128:(j + 1) * 128], b_hTB, 0)
        S.dma(DMA(yaT[:], ya_d[:, c * 512:(c + 1) * 512].rearrange("(cc p) n -> p cc n", p=128)), b_yaT, writes=[b_yaT])
        for cc in range(4):
            for bank, col0 in [(1, 0), (2, 512), (3, 1024)]:
                S.op('pe', [MM(pbank[bank][:, :], wB[:, kc, col0 + cc * 128:col0 + (cc + 1) * 128], hTB[:, kc, :], kc == 0, kc == 7)
                            for kc in range(8)], reads=[b_wB, b_hTB], writes=[pbuf[bank]])
            S.op('act', ACTV(xbs[:], pbank[1][:, :], AF.Copy), reads=[pbuf[1]], writes=[b_xbs])
            if c > 0:
                S.op('dve', CP(u[:, cc, 0:2], u[:, cc, 512:514]), reads=[b_u[cc]], writes=[b_u[cc]])
            S.op('dve', TT(u[:, cc, 2:514], pbank[3][:, :], xbs[:], ALU.mult), reads=[pbuf[3], b_xbs], writes=[b_u[cc]])
            S.op('act', ACTV(bgs[:], pbank[2][:, :], AF.Copy), reads=[pbuf[2]], writes=[b_bgs])
            S.op('dve', TS(tcv[:], u[:, cc, 0:512], convw[:, cc, 0:1], None, ALU.mult),
                 reads=[b_u[cc], b_convw], writes=[b_tcv])
            S.op('dve', STT(tcv[:], u[:, cc, 1:513], convw[:, cc, 1:2], tcv[:], ALU.mult, ALU.add),
                 reads=[b_u[cc], b_convw, b_tcv], writes=[b_tcv])
            S.op('dve', STT(tcv[:], u[:, cc, 2:514], convw[:, cc, 2:3], tcv[:], ALU.mult, ALU.add),
                 reads=[b_u[cc], b_convw, b_tcv], writes=[b_tcv])
            S.op('dve', STT(ybT[:, cc, :], tcv[:], convb[:, cc:cc + 1], bgs[:], ALU.add, ALU.mult),
                 reads=[b_tcv, b_convb, b_bgs], writes=[b_ybT])
        for m in range(8):
            S.op('pe', [MM(pbank[4][:, :], wB[:, kc, 1536 + m * 128:1536 + (m + 1) * 128], hTB[:, kc, :], kc == 0, kc == 7)
                        for kc in range(8)], reads=[b_wB, b_hTB], writes=[pbuf[4]])
            S.op('pe', [MM(pbank[5][:, :], wB[:, kc, 2560 + m * 128:2560 + (m + 1) * 128], hTB[:, kc, :], kc == 0, kc == 7)
                        for kc in range(8)], reads=[b_wB, b_hTB], writes=[pbuf[5]])
            S.op('pe', [MM(pbank[6][:, :], wpa[:, cc, m * 128:(m + 1) * 128], yaT[:, cc, :], cc == 0, cc == 3)
                        for cc in range(4)], reads=[b_wpa, b_yaT], writes=[pbuf[6]])
            S.op('pe', [MM(pbank[7][:, :], wpb[:, cc, m * 128:(m + 1) * 128], ybT[:, cc, :], cc == 0, cc == 3)
                        for cc in range(4)], reads=[b_wpb, b_ybT], writes=[pbuf[7]])
            S.op('act', ACTV(gas[:], pbank[4][:, :], AF.Sigmoid), reads=[pbuf[4]], writes=[b_gas])
            S.op('act', ACTV(gbs[:], pbank[5][:, :], AF.Sigmoid), reads=[pbuf[5]], writes=[b_gbs])
            S.op('dve', TT(t1[:], pbank[6][:, :], gas[:], ALU.mult), reads=[pbuf[6], b_gas], writes=[b_t1])
            S.op('dve', TT(t2[:], pbank[7][:, :], gbs[:], ALU.mult), reads=[pbuf[7], b_gbs], writes=[b_t2])
            S.op('dve', TT(mT[:, m, :], t1[:], t2[:], ALU.add), reads=[b_t1, b_t2], writes=[b_mT])
        for j in range(4):
            i = 4 * c + j
            k = i % 2
            S.dma(DMA(xr[k][:], x_d[i * 128:(i + 1) * 128, :]), b_xr[k], writes=[b_xr[k]])
            for hf in range(2):
                S.op('pe', [MM(pbank[1][:, :], mT[:, m, j * 128:(j + 1) * 128], wo[:, m, hf * 512:(hf + 1) * 512], m == 0, m == 7)
                            for m in range(8)], reads=[b_mT, b_wo], writes=[pbuf[1]])
                S.op('dve', TT(to[:], pbank[1][:, :], gabc[:, 0, hf * 512:(hf + 1) * 512], ALU.mult),
                     reads=[pbuf[1], b_gabc], writes=[b_to])
                S.op('dve', TT(x1t[k][:, hf * 512:(hf + 1) * 512], to[:], xr[k][:, hf * 512:(hf + 1) * 512], ALU.add),
                     reads=[b_to, b_xr[k]], writes=[b_x1t[k]])
            S.dma(DMA(out_d[i * 128:(i + 1) * 128, :], x1t[k][:]), b_x1t[k], reads=[b_x1t[k]])
    S.barrier()
    A.off = PERSIST
    if stop_after == 'B':
        S.emit()
        return nc
    if _SPARSE:
        _sparse_moe(nc, S, A, locals())
        S.emit()
        return nc
    h2T = A.alloc("h2T", [128, 8, 2048], BF16); b_h2T = [S.buf("h2T%d" % i) for i in range(4)]
    acc = A.alloc("acc", [128, 16, D], F32); b_acc = [S.buf("acc%d" % i) for i in range(16)]
    cw = A.alloc("cw", [128, 16, 32], F32); b_cw = [S.buf("cw%d" % i) for i in range(16)]
    w1b = [A.alloc("w1b", [128, 8, 512], BF16) for _ in range(2)]; b_w1b = [S.buf("w1b%d" % i) for i in range(2)]
    w3b = [A.alloc("w3b", [128, 8, 512], BF16) for _ in range(2)]; b_w3b = [S.buf("w3b%d" % i) for i in range(2)]
    w2b = [A.alloc("w2b", [128, 4, D], BF16) for _ in range(2)]; b_w2b = [S.buf("w2b%d" % i) for i in range(2)]
    stgC = [A.alloc("stgC", [128, 2048], F32) for _ in range(2)]
    b_stgC = [S.buf("stgC%d" % i) for i in range(2)]
    stgB[:] = stgC
    b_stgB[:] = b_stgC
    nbC = make_norm_bufs(with_xt=True, with_junk=False)
    hidT = [A.alloc("hidT", [128, 4, 512], BF16) for _ in range(2)]; b_hid = [S.buf("hid%d" % i) for i in range(2)]
    sil = [A.alloc("sil", [128, 512], F32) for _ in range(2)]; b_sil = [S.buf("sil%d" % i) for i in range(2)]
    nbC['junk'] = hidT[0][:, :, :].rearrange("p a b -> p (a b)")[:, 0:D]
    nbC['b_junk'] = b_hid[0]
    lg = A.alloc("lg", [128, 36], F32); b_lg = S.buf("lg")
    rt = A.alloc("rt", [128, 16], F32); b_rt = S.buf("rt")
    goh = A.alloc("goh", [128, 4], F32); b_goh = S.buf("goh")
    gex = A.alloc("gex", [128, 4], F32); b_gex = S.buf("gex")
    pen = A.alloc("pen", [128, 4], F32); b_pen = S.buf("pen")
    em = A.alloc("em", [128, 32], F32); b_em = S.buf("em")
    emc = A.alloc("emc", [128, 32], F32); b_emc = S.buf("emc")
    mx8c = A.alloc("mx8c", [128, 8], F32); b_mx8c = S.buf("mx8c")
    selc = A.alloc("selc", [128, 32], F32); b_selc = S.buf("selc")
    ex = A.alloc("ex", [128, 32], F32); b_ex = S.buf("ex")
    exs = A.alloc("exs", [128, 32], F32); b_exs = S.buf("exs")
    gmax, ngmax, gsum, nm1, den, fsc = [rt[:, q:q + 1] for q in range(6)]
    PEN = 1.0e4
    for hf in range(2):
        for ti in range(16):
            i = hf * 16 + ti
            k = i % 2
            S.dma(DMA(nbC['xt'][k][:], out_d[i * 128:(i + 1) * 128, :]), nbC['b_xt'][k], writes=[nbC['b_xt'][k]])
            norm_tile(nbC, i, nbC['xt'][k][:], nbC['b_xt'][k], h2T[:, :, ti * 128:(ti + 1) * 128], b_h2T[ti // 4], 16)
            S.op('pool', MS(acc[:, ti, :], 0.0), writes=[b_acc[ti]])
            S.op('pe', [MM(pbank[1][:, 0:36], h2T[:, kc, ti * 128:(ti + 1) * 128], wrb[:, kc, :], kc == 0, kc == 7)
                        for kc in range(8)], reads=[b_h2T[ti // 4], b_wrb], writes=[pbuf[1]])
            S.op('dve', TT(lg[:], pbank[1][:, 0:36], brbc[:], ALU.add), reads=[pbuf[1], b_brbc], writes=[b_lg])
            S.op('dve', RED(gmax, lg[:, 0:4], ALU.max), reads=[b_lg], writes=[b_rt])
            S.op('dve', TS(ngmax, gmax, -1.0, None, ALU.mult), reads=[b_rt], writes=[b_rt])
            S.op('dve', TS(goh[:], lg[:, 0:4], gmax, None, ALU.is_ge), reads=[b_lg, b_rt], writes=[b_goh])
            S.op('act', ACTV(gex[:], lg[:, 0:4], AF.Exp, bias=ngmax, accum_out=gsum),
                 reads=[b_lg, b_rt], writes=[b_gex, b_rt])
            S.op('dve', RECIP(gsum, gsum), reads=[b_rt], writes=[b_rt])
            S.op('dve', TS(pen[:], goh[:], PEN, -PEN, ALU.mult, ALU.add), reads=[b_goh], writes=[b_pen])
            S.op('dve', TT(em[:].rearrange("p (g e) -> p g e", g=4), lg[:, 4:36].rearrange("p (g e) -> p g e", g=4),
                           pen[:, :].unsqueeze(2).to_broadcast([128, 4, 8]), ALU.add),
                 reads=[b_lg, b_pen], writes=[b_em])
            S.op('dve', MAX8(mx8c[:], em[:]), reads=[b_em], writes=[b_mx8c])
            S.op('dve', TS(nm1, mx8c[:, 0:1], -1.0, None, ALU.mult), reads=[b_mx8c], writes=[b_rt])
            S.op('dve', TS(selc[:], em[:], mx8c[:, 1:2], None, ALU.is_ge), reads=[b_em, b_mx8c], writes=[b_selc])
            S.op('dve', TS(emc[:], em[:], mx8c[:, 1:2], None, ALU.max), reads=[b_em, b_mx8c], writes=[b_emc])
            S.op('act', ACTV(ex[:], emc[:], AF.Exp, bias=nm1), reads=[b_emc, b_rt], writes=[b_ex])
            S.op('dve', TT(exs[:], ex[:], selc[:], ALU.mult), reads=[b_ex, b_selc], writes=[b_exs])
            S.op('dve', RED(den, exs[:]), reads=[b_exs], writes=[b_rt])
            S.op('dve', RECIP(den, den), reads=[b_rt], writes=[b_rt])
            S.op('dve', TT(fsc, den, gsum, ALU.mult), reads=[b_rt], writes=[b_rt])
            S.op('dve', TS(cw[:, ti, :], exs[:], fsc, None, ALU.mult), reads=[b_exs, b_rt], writes=[b_cw[ti]])
        for e in range(32):
            wb = e % 2
            for q2 in range(2):
                load_cast(w1b[wb][:, 4 * q2:4 * q2 + 4, :],
                          w1_d[e, q2 * 512:(q2 + 1) * 512, :].rearrange("(kc p) n -> p kc n", p=128), (4, 512), b_w1b[wb])
                load_cast(w3b[wb][:, 4 * q2:4 * q2 + 4, :],
                          w3_d[e, q2 * 512:(q2 + 1) * 512, :].rearrange("(kc p) n -> p kc n", p=128), (4, 512), b_w3b[wb])
            for q2 in range(2):
                load_cast(w2b[wb][:, 2 * q2:2 * q2 + 2, :],
                          w2_d[e, q2 * 256:(q2 + 1) * 256, :].rearrange("(fc p) n -> p fc n", p=128), (2, D), b_w2b[wb])
            for ch in range(4):
                hk = (e * 4 + ch) % 2
                for fc in range(4):
                    pb1 = 2 + (fc % 2)
                    pb3 = 4 + (fc % 2)
                    sk = fc % 2
                    S.op('pe', [MM(pbank[pb1][:, :], w1b[wb][:, kc, fc * 128:(fc + 1) * 128], h2T[:, kc, ch * 512:(ch + 1) * 512],
                                   kc == 0, kc == 7) for kc in range(8)], reads=[b_w1b[wb], b_h2T[ch]], writes=[pbuf[pb1]])
                    S.op('pe', [MM(pbank[pb3][:, :], w3b[wb][:, kc, fc * 128:(fc + 1) * 128], h2T[:, kc, ch * 512:(ch + 1) * 512],
                                   kc == 0, kc == 7) for kc in range(8)], reads=[b_w3b[wb], b_h2T[ch]], writes=[pbuf[pb3]])
                    S.op('act', ACTV(sil[sk][:], pbank[pb1][:, :], AF.Silu), reads=[pbuf[pb1]], writes=[b_sil[sk]])
                    S.op('dve', TT(hidT[hk][:, fc, :], pbank[pb3][:, :], sil[sk][:], ALU.mult),
                         reads=[pbuf[pb3], b_sil[sk]], writes=[b_hid[hk]])
                for j in range(4):
                    ti = ch * 4 + j
                    for hf2 in range(2):
                        po = 6 + hf2
                        S.op('pe', [MM(pbank[po][:, :], hidT[hk][:, fc, j * 128:(j + 1) * 128],
                                       w2b[wb][:, fc, hf2 * 512:(hf2 + 1) * 512], fc == 0, fc == 3) for fc in range(4)],
                             reads=[b_hid[hk], b_w2b[wb]], writes=[pbuf[po]])
                        S.op('dve', STT(acc[:, ti, hf2 * 512:(hf2 + 1) * 512], pbank[po][:, :], cw[:, ti, e:e + 1],
                                        acc[:, ti, hf2 * 512:(hf2 + 1) * 512], ALU.mult, ALU.add),
                             reads=[pbuf[po], b_cw[ti], b_acc[ti]], writes=[b_acc[ti]])
        for ti in range(16):
            i = hf * 16 + ti
            k = i % 2
            S.dma(DMA(nbC['xt'][k][:], out_d[i * 128:(i + 1) * 128, :]), nbC['b_xt'][k], writes=[nbC['b_xt'][k]])
            S.op('dve', TT(acc[:, ti, :], acc[:, ti, :], gabc[:, 1, :], ALU.mult), reads=[b_acc[ti], b_gabc], writes=[b_acc[ti]])
            S.op('dve', TT(acc[:, ti, :], acc[:, ti, :], nbC['xt'][k][:], ALU.add),
                 reads=[b_acc[ti], nbC['b_xt'][k]], writes=[b_acc[ti]])
            S.dma(DMA(out_d[i * 128:(i + 1) * 128, :], acc[:, ti, :]), b_acc[ti], reads=[b_acc[ti]])
    S.barrier()
    S.emit()
    return nc


def _consts():
    pos = np.arange(S_LEN, dtype=np.float32)
    inv = (np.float32(500000.0) ** (-np.arange(0, 16, 2, dtype=np.float32) / np.float32(16))).astype(np.float32)
    ang = (pos[:, None] * inv[None, :]).astype(np.float32)
    cos = np.cos(ang).astype(np.float32).reshape(NT, 128, 8).transpose(1, 0, 2)
    sin = np.sin(ang).astype(np.float32).reshape(NT, 128, 8).transpose(1, 0, 2)
    cs1 = np.concatenate([cos, cos], -1).reshape(128, NT * 16)
    sn2 = np.concatenate([-sin, sin], -1).reshape(128, NT * 16)
    kp = np.arange(128)[:, None, None]
    jj = np.arange(2)[None, :, None]
    qq = np.arange(256)[None, None, :]
    tri = (jj * 128 + kp <= qq).astype(np.float32).reshape(128, 512)
    oneh = (np.arange(S_LEN)[None, :] // 256 == np.arange(16)[:, None]).astype(np.float32)
    mconst = np.zeros((128, 225), np.float32)
    tt = np.arange(128)
    mconst[:, 0:128] = (tt[:, None] < tt[None, :]).astype(np.float32)
    ee = np.arange(32)
    mconst[0:32, 128:160] = (ee[:, None] < ee[None, :]).astype(np.float32)
    mconst[:, 160:176] = (512.0 * np.arange(16))[None, :]
    mconst[:, 176:224] = np.arange(48, dtype=np.float32)[None, :]
    mconst[:, 224] = np.arange(128, dtype=np.float32)
    return dict(cs1=np.ascontiguousarray(cs1), sn2=np.ascontiguousarray(sn2), tri=tri, oneh=oneh, mconst=mconst,
                ident=np.eye(128, dtype=np.float32))


def kernel(x, c, w_ada, b_ada, g_norm1, g_norm2, w_in, g_q, g_k, conv_w, conv_b,
           w_pa, w_pb, w_o, w_rg, b_rg, w_re, b_re, w1, w3, w2):
    f = lambda a: np.ascontiguousarray(np.asarray(a, dtype=np.float32))
    x = f(x); c = f(c)
    cst = _consts()
    gqk = np.concatenate([np.tile(f(g_q)[0], 4), np.tile(f(g_k)[0], 4)])
    gqk = np.ascontiguousarray(np.broadcast_to(gqk[None, :], (128, 512)))
    convw = np.ascontiguousarray(f(conv_w)[0].reshape(3, 4, 128).transpose(2, 1, 0).reshape(128, 12))
    convb = np.ascontiguousarray(f(conv_b)[0].reshape(4, 128).T)
    wr = np.ascontiguousarray(np.concatenate([f(w_rg)[0], f(w_re)[0]], axis=1))
    br = np.concatenate([f(b_rg)[0], f(b_re)[0]])
    br = np.ascontiguousarray(np.broadcast_to(br[None, :], (128, 36)))
    shared = dict(w_ada=f(w_ada)[0], b_ada=f(b_ada)[0:1], g1=f(g_norm1)[0:1], g2=f(g_norm2)[0:1], w_in=f(w_in)[0],
                  gqk=gqk, convw=convw, convb=convb, w_pa=f(w_pa)[0], w_pb=f(w_pb)[0], w_o=f(w_o)[0],
                  wr=wr, br=br, w1=f(w1)[0], w3=f(w3)[0], w2=f(w2)[0], **cst)
    n = _NCORES
    in_maps = []
    for b in range(n):
        m = dict(shared)
        m["x"] = x[b]
        m["cT"] = np.ascontiguousarray(c[b].reshape(8, 128).T)
        in_maps.append(m)
    if _STOP_AFTER is not None:
        for m in in_maps:
            for kk in ('w1', 'w3', 'w2'):
                m.pop(kk)
    nc = build_program(_STOP_AFTER)
    res = run_bass_kernel_spmd(nc, in_maps, core_ids=list(range(n)))
    if _STOP_AFTER is not None:
        global _DBG_RES
        _DBG_RES = res.results
    out = np.stack([np.asarray(res.results[b]["out"], dtype=np.float32).reshape(S_LEN, D) for b in range(n)])
    return out
```

```python
import numpy as np
import concourse.bass as bass
import concourse.mybir as mybir
from concourse.bass_utils import run_bass_kernel_spmd

F32 = mybir.dt.float32
BF16 = mybir.dt.bfloat16
ALU = mybir.AluOpType
AF = mybir.ActivationFunctionType
AX = mybir.AxisListType

S_LEN = 4096
D = 1024
NT = 32
BIG = 1.0e30
MASKV = 30000.0
_STOP_AFTER = None
_NCORES = 8
_DBG = {}
_SPARSE = True


class Buf:
    def __init__(self, name):
        self.name = name
        self.lw = None
        self.rd = {}
        self.dsem = {}
        self.dcnt = {}


class Sched:
    def __init__(self, nc):
        self.nc = nc
        self.engs = ['pe', 'act', 'dve', 'pool', 'sp']
        self.q = {e: [] for e in self.engs}
        self.sems = []
        self.esem = {}
        for e in ['pe', 'act', 'dve', 'pool']:
            self.esem[e] = self.new_sem('s_' + e)
        self.cnt = {e: 0 for e in self.esem}
        self.seen = {e: {} for e in self.engs}
        self.bufs = []

    def new_sem(self, name):
        s = self.nc.alloc_semaphore('%s_%d' % (name, len(self.sems)))
        self.sems.append(s)
        return len(self.sems) - 1

    def buf(self, name):
        b = Buf(name)
        self.bufs.append(b)
        return b

    def _waits(self, eng, reads, writes):
        need = {}

        def add(s, v):
            if need.get(s, 0) < v:
                need[s] = v
        for b in reads:
            if b.lw is not None:
                add(*b.lw)
        for b in writes:
            if b.lw is not None:
                add(*b.lw)
            for s, v in b.rd.items():
                add(s, v)
        seen = self.seen[eng]
        out = []
        for s, v in need.items():
            if eng == 'pe' and s == self.esem['pe']:
                continue
            if seen.get(s, 0) < v:
                seen[s] = v
                out.append((s, v))
        return out

    def _commit(self, ev, reads, writes):
        s, v = ev
        for b in reads:
            if b.rd.get(s, 0) < v:
                b.rd[s] = v
        for b in writes:
            b.lw = ev
            b.rd = {}

    def op(self, eng, fns, reads=(), writes=()):
        if callable(fns):
            fns = [fns]
        waits = self._waits(eng, reads, writes)
        self.cnt[eng] += 1
        ev = (self.esem[eng], self.cnt[eng])
        self.q[eng].append((waits, fns, ev[0], 1))
        self._commit(ev, reads, writes)

    def dma(self, fn, own, reads=(), writes=(), q='sp'):
        if q not in own.dsem:
            own.dsem[q] = self.new_sem('d_' + own.name + '_' + q)
            own.dcnt[q] = 0
        waits = self._waits(q, reads, writes)
        own.dcnt[q] += 16
        ev = (own.dsem[q], own.dcnt[q])
        self.q[q].append((waits, [fn], ev[0], 16))
        self._commit(ev, reads, writes)

    def barrier(self):
        evs = [(self.esem[e], self.cnt[e]) for e in self.esem if self.cnt[e] > 0]
        for b in self.bufs:
            for qq, sm in b.dsem.items():
                evs.append((sm, b.dcnt[qq]))
        for e in self.engs:
            seen = self.seen[e]
            waits = []
            for s, v in evs:
                if e == 'pe' and s == self.esem['pe']:
                    continue
                if seen.get(s, 0) < v:
                    seen[s] = v
                    waits.append((s, v))
            if waits:
                self.q[e].append((waits, [], None, 0))

    def emit(self):
        nc = self.nc
        sems = self.sems

        def replay(name, e):
            for waits, fns, s, inc in self.q[name]:
                for (ws, wv) in waits:
                    e.wait_ge(sems[ws], wv)
                ins = None
                for fn in fns:
                    ins = fn(e)
                if ins is not None and s is not None:
                    ins.then_inc(sems[s], inc)
        with nc.Block() as block:
            @block.tensor
            def _(e):
                replay('pe', e)

            @block.scalar
            def _(e):
                replay('act', e)

            @block.vector
            def _(e):
                replay('dve', e)

            @block.gpsimd
            def _(e):
                replay('pool', e)

            @block.sync
            def _(e):
                replay('sp', e)


class Rec:
    def __init__(self):
        self.items = []

    def op(self, *a, **k):
        self.items.append(('op', a, k))

    def dma(self, *a, **k):
        self.items.append(('dma', a, k))


class Arena:
    LO = 16640
    HI = 229376

    def __init__(self, nc):
        self.nc = nc
        self.off = self.LO
        self.n = 0

    def alloc(self, name, shape, dt):
        esz = 2 if dt == BF16 else 4
        nbytes = int(np.prod(shape[1:])) * esz
        off = (self.off + 31) // 32 * 32
        assert off + nbytes <= self.HI, ("SBUF overflow", name, off, nbytes)
        self.n += 1
        t = self.nc.alloc_sbuf_tensor_at("%s_%d" % (name, self.n), list(shape), dt, offset=off)
        self.off = off + nbytes
        return t


def MM(out, lhsT, rhs, start=True, stop=True):
    return lambda e: e.matmul(out, lhsT=lhsT, rhs=rhs, start=start, stop=stop)


def TR(out, in_, ident):
    return lambda e: e.transpose(out=out, in_=in_, identity=ident)


def ACTV(out, in_, func, **kw):
    return lambda e: e.activation(out=out, in_=in_, func=func, **kw)


def TT(out, in0, in1, op):
    return lambda e: e.tensor_tensor(out=out, in0=in0, in1=in1, op=op)


def TS(out, in0, s1, s2, op0, op1=None):
    if op1 is None:
        return lambda e: e.tensor_scalar(out=out, in0=in0, scalar1=s1, scalar2=None, op0=op0)
    return lambda e: e.tensor_scalar(out=out, in0=in0, scalar1=s1, scalar2=s2, op0=op0, op1=op1)


def STT(out, in0, scalar, in1, op0, op1):
    return lambda e: e.scalar_tensor_tensor(out=out, in0=in0, scalar=scalar, in1=in1, op0=op0, op1=op1)


def CP(out, in_):
    return lambda e: e.tensor_copy(out=out, in_=in_)


def MS(ap, v):
    return lambda e: e.memset(ap, v)


def RED(out, in_, op=None):
    return lambda e: e.tensor_reduce(out=out, in_=in_, axis=AX.X, op=(op or ALU.add))


def RECIP(out, in_):
    return lambda e: e.reciprocal(out=out, in_=in_)


def MAX8(out, in_):
    return lambda e: e.max(out=out, in_=in_)


def DMA(out, in_):
    return lambda e: e.dma_start(out=out, in_=in_)


def CAST(eng, out, in_):
    if eng == 'act':
        return ACTV(out, in_, AF.Copy)
    return CP(out, in_)


def IDMA_G(out, table, idx):
    return lambda e: e.indirect_dma_start(out=out, out_offset=None, in_=table,
                                          in_offset=bass.IndirectOffsetOnAxis(ap=idx, axis=0))


def IDMA_S(table, idx, in_):
    return lambda e: e.indirect_dma_start(out=table, out_offset=bass.IndirectOffsetOnAxis(ap=idx, axis=0),
                                          in_=in_, in_offset=None)


I32 = mybir.dt.int32


def _sparse_moe(nc, S, A, G):
    pbank, pbuf, identb, b_identb = G['pbank'], G['pbuf'], G['identb'], G['b_identb']
    modT, b_modT, gabc, b_gabc = G['modT'], G['b_modT'], G['gabc'], G['b_gabc']
    epsb, b_epsb, wrb, b_wrb, brbc, b_brbc = G['epsb'], G['b_epsb'], G['wrb'], G['b_wrb'], G['brbc'], G['b_brbc']
    out_d, mc_d, w1_d, w3_d, w2_d = G['out_d'], G['mc_d'], G['w1_d'], G['w3_d'], G['w2_d']
    pbf = G['pbf']
    NBLK, BLKR = 48, 512
    xs_d = nc.dram_tensor("xs_scratch", [NBLK * BLKR, D], BF16).ap()
    ys_d = nc.dram_tensor("ys_scratch", [NBLK * BLKR, D], F32).ap()
    w1t = w1_d.rearrange("e (p k) n -> (e p) (k n)", k=8)
    w3t = w3_d.rearrange("e (p k) n -> (e p) (k n)", k=8)
    w2t = w2_d.rearrange("e (p k) n -> (e p) (k n)", k=4)
    T = lambda name, shape, dt: (A.alloc(name, shape, dt), S.buf(name))
    mc, b_mc = T("mc", [128, 225], F32)
    ltri, b_ltri = T("ltri", [128, 128], BF16)
    onesb, b_onesb = T("onesb", [128, 128], BF16)
    ustr, b_ustr = T("ustr", [128, 32], BF16)
    modTp, b_modTp = G['modTp'], G['b_modTp']
    selall, b_selall = T("selall", [128, 32, 32], F32)
    selAall, b_selAall = T("selAall", [128, 32, 32], F32)
    cwall, b_cwall = T("cwall", [128, 32, 32], F32)
    rankall, b_rankall = T("rankall", [128, 32, 32], F32)
    csum, b_csum = T("csum", [128, 32], F32)
    dallf, b_dallf = T("dallf", [128, 64], F32)
    dalli, b_dalli = T("dalli", [128, 64], I32)
    wall, b_wall = T("wall", [128, 64], F32)
    widxf, b_widxf = T("widxf", [128, NBLK], F32)
    widxi, b_widxi = T("widxi", [128, NBLK], I32)
    xt = [A.alloc("xtC", [128, D], F32) for _ in range(2)]; b_xt = [S.buf("xtC%d" % i) for i in range(2)]
    ss = [A.alloc("ssC", [128, 1], F32) for _ in range(2)]; b_ss = [S.buf("ssC%d" % i) for i in range(2)]
    junk, b_junk = T("junkC", [128, D], BF16)
    h2Tt, b_h2Tt = T("h2Tt", [128, 8, 128], BF16)
    lg, b_lg = T("lgC", [128, 36], F32)
    rt, b_rt = T("rtC", [128, 16], F32)
    goh, b_goh = T("gohC", [128, 4], F32)
    gex, b_gex = T("gexC", [128, 4], F32)
    pen, b_pen = T("penC", [128, 4], F32)
    em, b_em = T("emC", [128, 32], F32)
    emc, b_emc = T("emcC", [128, 32], F32)
    mx8c, b_mx8c = T("mx8cC", [128, 8], F32)
    ex, b_ex = T("exC", [128, 32], F32)
    exs, b_exs = T("exsC", [128, 32], F32)
    selb, b_selb = T("selbC", [128, 32], BF16)
    tm, b_tm = T("tmC", [128, 32], F32)
    tm2, b_tm2 = T("tm2C", [128, 32], F32)
    big3, b_big3 = T("big3C", [128, 48 * 32], F32)
    nblk, b_nblk = T("nblkC", [128, 32], F32)
    nbpad, b_nbpad = T("nbpadC", [128, 128], BF16)
    nbT, b_nbT = T("nbTC", [128, 128], BF16)
    pss, b_pss = T("pssC", [128, 32], F32)
    pend, b_pend = T("pendC", [128, 32], F32)
    bex, b_bex = T("bexC", [128, NBLK], F32)
    gmax, ngmax, gsum, nm1, den, fsc = [rt[:, q:q + 1] for q in range(6)]
    PEN = 1.0e4
    MARK = A.off
    xnall = A.alloc("xnall", [128, 32, D], BF16); b_xnall = [S.buf("xnall%d" % i) for i in range(32)]

    zt, b_zt = T("zt", [128, 4, D], BF16)
    S.op('pool', MS(zt[:], 0.0), writes=[b_zt])
    for b in range(NBLK):
        S.dma(DMA(xs_d[b * BLKR:(b + 1) * BLKR, :].rearrange("(s p) d -> p s d", p=128), zt[:]), b_zt, reads=[b_zt])
    S.dma(DMA(mc[:], mc_d[:, :]), b_mc, writes=[b_mc])
    S.op('dve', [CP(ltri[:], mc[:, 0:128]), CP(ustr[0:32, :], mc[0:32, 128:160])], reads=[b_mc], writes=[b_ltri, b_ustr])
    S.op('pool', [MS(onesb[:], 1.0), MS(csum[:], 0.0), MS(nbpad[:], 0.0)], writes=[b_onesb, b_csum, b_nbpad])
    for i in range(32):
        k = i % 2
        S.dma(DMA(xt[k][:], out_d[i * 128:(i + 1) * 128, :]), b_xt[k], writes=[b_xt[k]])
        S.op('act', ACTV(junk[:], xt[k][:], AF.Square, scale=1.0 / 32.0, accum_out=ss[k][:]),
             reads=[b_xt[k]], writes=[b_junk, b_ss[k]])
        S.op('act', ACTV(ss[k][:], ss[k][:], AF.Ln, bias=epsb[:]), reads=[b_ss[k], b_epsb], writes=[b_ss[k]])
        S.op('act', ACTV(ss[k][:], ss[k][:], AF.Exp, scale=-0.5), reads=[b_ss[k]], writes=[b_ss[k]])
        S.op('dve', TS(xnall[:, i, :], xt[k][:], ss[k][:, 0:1], None, ALU.mult), reads=[b_xt[k], b_ss[k]], writes=[b_xnall[i]])
        pv = pbf(0).rearrange("p (a b) -> p a b", a=8)
        S.op('pe', [TR(pv[:, kc, :], xnall[:, i, kc * 128:(kc + 1) * 128], identb[:]) for kc in range(8)],
             reads=[b_xnall[i], b_identb], writes=[pbuf[0]])
        S.op('act', [ACTV(h2Tt[:, kc, :], pv[:, kc, :], AF.Identity, scale=modT[:, 16 + kc:17 + kc],
                          bias=modT[:, 24 + kc:25 + kc]) for kc in range(8)], reads=[pbuf[0], b_modT], writes=[b_h2Tt])
        S.op('pe', [MM(pbank[1][:, 0:36], h2Tt[:, kc, :], wrb[:, kc, :], kc == 0, kc == 7) for kc in range(8)],
             reads=[b_h2Tt, b_wrb], writes=[pbuf[1]])
        S.op('dve', TT(lg[:], pbank[1][:, 0:36], brbc[:], ALU.add), reads=[pbuf[1], b_brbc], writes=[b_lg])
        S.op('dve', RED(gmax, lg[:, 0:4], ALU.max), reads=[b_lg], writes=[b_rt])
        S.op('dve', TS(ngmax, gmax, -1.0, None, ALU.mult), reads=[b_rt], writes=[b_rt])
        S.op('dve', TS(goh[:], lg[:, 0:4], gmax, None, ALU.is_ge), reads=[b_lg, b_rt], writes=[b_goh])
        S.op('act', ACTV(gex[:], lg[:, 0:4], AF.Exp, bias=ngmax, accum_out=gsum), reads=[b_lg, b_rt], writes=[b_gex, b_rt])
        S.op('dve', RECIP(gsum, gsum), reads=[b_rt], writes=[b_rt])
        S.op('dve', TS(pen[:], goh[:], PEN, -PEN, ALU.mult, ALU.add), reads=[b_goh], writes=[b_pen])
        S.op('dve', TT(em[:].rearrange("p (g e) -> p g e", g=4), lg[:, 4:36].rearrange("p (g e) -> p g e", g=4),
                       pen[:, :].unsqueeze(2).to_broadcast([128, 4, 8]), ALU.add), reads=[b_lg, b_pen], writes=[b_em])
        S.op('dve', MAX8(mx8c[:], em[:]), reads=[b_em], writes=[b_mx8c])
        S.op('dve', TS(nm1, mx8c[:, 0:1], -1.0, None, ALU.mult), reads=[b_mx8c], writes=[b_rt])
        S.op('dve', TS(selall[:, i, :], em[:], mx8c[:, 1:2], None, ALU.is_ge), reads=[b_em, b_mx8c], writes=[b_selall])
        S.op('dve', TS(selAall[:, i, :], em[:], mx8c[:, 0:1], None, ALU.is_ge), reads=[b_em, b_mx8c], writes=[b_selAall])
        S.op('dve', TS(emc[:], em[:], mx8c[:, 1:2], None, ALU.max), reads=[b_em, b_mx8c], writes=[b_emc])
        S.op('act', ACTV(ex[:], emc[:], AF.Exp, bias=nm1), reads=[b_emc, b_rt], writes=[b_ex])
        S.op('dve', TT(exs[:], ex[:], selall[:, i, :], ALU.mult), reads=[b_ex, b_selall], writes=[b_exs])
        S.op('dve', RED(den, exs[:]), reads=[b_exs], writes=[b_rt])
        S.op('dve', RECIP(den, den), reads=[b_rt], writes=[b_rt])
        S.op('dve', TT(fsc, den, gsum, ALU.mult), reads=[b_rt], writes=[b_rt])
        S.op('dve', TS(cwall[:, i, :], exs[:], fsc, None, ALU.mult), reads=[b_exs, b_rt], writes=[b_cwall])
        S.op('dve', CP(selb[:], selall[:, i, :]), reads=[b_selall], writes=[b_selb])
        S.op('pe', MM(pbank[2][:, 0:32], ltri[:], selb[:]), reads=[b_ltri, b_selb], writes=[pbuf[2]])
        S.op('pe', MM(pbank[3][:, 0:32], onesb[:], selb[:]), reads=[b_onesb, b_selb], writes=[pbuf[3]])
        S.op('dve', TT(rankall[:, i, :], pbank[2][:, 0:32], csum[:], ALU.add), reads=[pbuf[2], b_csum], writes=[b_rankall])
        S.op('dve', TT(csum[:], pbank[3][:, 0:32], csum[:], ALU.add), reads=[pbuf[3], b_csum], writes=[b_csum])
    b3 = big3[:, 0:512].rearrange("p (e k) -> p e k", k=16)
    S.op('dve', TT(b3, csum[:, :].unsqueeze(2).to_broadcast([128, 32, 16]),
                   mc[:, 160:176].unsqueeze(1).to_broadcast([128, 32, 16]), ALU.is_gt), reads=[b_csum, b_mc], writes=[b_big3])
    S.op('dve', RED(nblk[:], b3), reads=[b_big3], writes=[b_nblk])
    S.op('dve', CP(nbpad[:, 0:32], nblk[:]), reads=[b_nblk], writes=[b_nbpad])
    pvn = pbf(0)
    S.op('pe', TR(pvn[:, 0:128], nbpad[:], identb[:]), reads=[b_nbpad, b_identb], writes=[pbuf[0]])
    S.op('act', ACTV(nbT[0:32, :], pvn[0:32, 0:128], AF.Copy), reads=[pbuf[0]], writes=[b_nbT])
    S.op('pe', MM(pbank[2][:, 0:32], nbT[0:32, :], ustr[0:32, :]), reads=[b_nbT, b_ustr], writes=[pbuf[2]])
    S.op('dve', CP(pss[:], pbank[2][:, 0:32]), reads=[pbuf[2]], writes=[b_pss])
    S.op('dve', TT(pend[:], pss[:], nblk[:], ALU.add), reads=[b_pss, b_nblk], writes=[b_pend])
    b4 = big3[:, :].rearrange("p (b e) -> p b e", e=32)
    S.op('dve', TT(b4, pend[:, :].unsqueeze(1).to_broadcast([128, NBLK, 32]),
                   mc[:, 176:224].unsqueeze(2).to_broadcast([128, NBLK, 32]), ALU.is_le), reads=[b_pend, b_mc], writes=[b_big3])
    S.op('dve', RED(bex[:], b4), reads=[b_big3], writes=[b_bex])
    S.op('dve', TS(bex[:], bex[:], 31.0, None, ALU.min), reads=[b_bex], writes=[b_bex])
    S.op('dve', TS(widxf[:], bex[:], 128.0, None, ALU.mult), reads=[b_bex], writes=[b_widxf])
    S.op('dve', TT(widxf[:], widxf[:], mc[:, 224:225].to_broadcast([128, NBLK]), ALU.add), reads=[b_widxf, b_mc], writes=[b_widxf])
    S.op('dve', CP(widxi[:], widxf[:]), reads=[b_widxf], writes=[b_widxi])
    S.op('dve', TS(pss[:], pss[:], float(BLKR), None, ALU.mult), reads=[b_pss], writes=[b_pss])
    for i in range(32):
        S.op('dve', TT(tm[:], rankall[:, i, :], pss[:], ALU.add), reads=[b_rankall, b_pss], writes=[b_tm])
        S.op('dve', TT(tm2[:], tm[:], selAall[:, i, :], ALU.mult), reads=[b_tm, b_selAall], writes=[b_tm2])
        S.op('dve', RED(dallf[:, 2 * i:2 * i + 1], tm2[:]), reads=[b_tm2], writes=[b_dallf])
        S.op('dve', TT(tm2[:], selall[:, i, :], selAall[:, i, :], ALU.subtract), reads=[b_selall, b_selAall], writes=[b_tm2])
        S.op('dve', TT(tm[:], tm[:], tm2[:], ALU.mult), reads=[b_tm, b_tm2], writes=[b_tm])
        S.op('dve', RED(dallf[:, 2 * i + 1:2 * i + 2], tm[:]), reads=[b_tm], writes=[b_dallf])
        S.op('dve', TT(tm[:], cwall[:, i, :], tm2[:], ALU.mult), reads=[b_cwall, b_tm2], writes=[b_tm])
        S.op('dve', RED(wall[:, 2 * i + 1:2 * i + 2], tm[:]), reads=[b_tm], writes=[b_wall])
        S.op('dve', TT(tm[:], cwall[:, i, :], selAall[:, i, :], ALU.mult), reads=[b_cwall, b_selAall], writes=[b_tm])
        S.op('dve', RED(wall[:, 2 * i:2 * i + 1], tm[:]), reads=[b_tm], writes=[b_wall])
    S.op('dve', CP(dalli[:], dallf[:]), reads=[b_dallf], writes=[b_dalli])
    S.barrier()
    for i in range(32):
        for kk in range(2):
            S.dma(IDMA_S(xs_d[:, :], dalli[:, 2 * i + kk:2 * i + kk + 1], xnall[:, i, :]), b_xnall[i],
                  reads=[b_xnall[i], b_dalli], q='pool')
    S.barrier()
    A.off = MARK
    w1b = [A.alloc("w1s", [128, 8, 512], BF16) for _ in range(2)]; b_w1b = [S.buf("w1s%d" % i) for i in range(2)]
    w3b = [A.alloc("w3s", [128, 8, 512], BF16) for _ in range(2)]; b_w3b = [S.buf("w3s%d" % i) for i in range(2)]
    w2b = [A.alloc("w2s", [128, 4, D], BF16) for _ in range(2)]; b_w2b = [S.buf("w2s%d" % i) for i in range(2)]
    stg = [A.alloc("stgS", [128, 4096], F32) for _ in range(2)]; b_stg = [S.buf("stgS%d" % i) for i in range(2)]
    xs = [A.alloc("xs", [128, 4, D], BF16) for _ in range(2)]; b_xs = [S.buf("xs%d" % i) for i in range(2)]
    xsT = [A.alloc("xsT", [128, 8, 512], BF16) for _ in range(2)]; b_xsT = [S.buf("xsT%d" % i) for i in range(2)]
    hidT = [A.alloc("hidS", [128, 4, 512], BF16) for _ in range(2)]; b_hid = [S.buf("hidS%d" % i) for i in range(2)]
    sil = [A.alloc("silS", [128, 512], F32) for _ in range(2)]; b_sil = [S.buf("silS%d" % i) for i in range(2)]
    ysb = [A.alloc("ysb", [128, D], F32) for _ in range(2)]; b_ysb = [S.buf("ysb%d" % i) for i in range(2)]
    yg = [A.alloc("yg", [128, D], F32) for _ in range(4)]; b_yg = [S.buf("yg%d" % i) for i in range(4)]
    sidx = [0]
    crr = [0]

    def prep(b):
        wb = b % 2
        xb = b % 2
        S.dma(DMA(xs[xb][:], xs_d[b * BLKR:(b + 1) * BLKR, :].rearrange("(s p) d -> p s d", p=128)), b_xs[xb], writes=[b_xs[xb]])
        items = []
        for (tab, dst, b_dst) in [(w1t, w1b[wb], b_w1b[wb]), (w3t, w3b[wb], b_w3b[wb]), (w2t, w2b[wb], b_w2b[wb])]:
            sk = sidx[0] % 2
            sidx[0] += 1
            S.dma(IDMA_G(stg[sk][:], tab, widxi[:, b:b + 1]), b_stg[sk], reads=[b_widxi], writes=[b_stg[sk]], q='pool')
            crr[0] += 1
            ce = ['dve', 'act'][crr[0] % 2]

            def cast_item(ce=ce, dst=dst, sk=sk, b_dst=b_dst):
                S.op(ce, CAST(ce, dst[:].rearrange("p a b -> p (a b)"), stg[sk][:]), reads=[b_stg[sk]], writes=[b_dst])
            cast_item()
        for sub in range(4):
            def tr_item(sub=sub, xb=xb):
                pv = pbf(0).rearrange("p (a b) -> p a b", a=8)
                S.op('pe', [TR(pv[:, kc, :], xs[xb][:, sub, :].rearrange("p (q k) -> p q k", k=8)[:, :, kc], identb[:])
                            for kc in range(8)], reads=[b_xs[xb], b_identb], writes=[pbuf[0]])
                S.op('act', [ACTV(xsT[xb][:, kc, sub * 128:(sub + 1) * 128], pv[:, kc, :], AF.Identity,
                                  scale=modTp[:, kc:kc + 1], bias=modTp[:, 8 + kc:9 + kc]) for kc in range(8)],
                     reads=[pbuf[0], b_modTp], writes=[b_xsT[xb]])
            items.append(tr_item)
        return items

    for it in prep(0):
        it()
    for b in range(NBLK):
        wb = b % 2
        xb = b % 2
        hk = b % 2
        nxt = prep(b + 1) if b + 1 < NBLK else []
        for fc in range(4):
            pb1 = 2 + (fc % 2)
            pb3 = 4 + (fc % 2)
            sk2 = fc % 2
            S.op('pe', [MM(pbank[pb1][:, :], w1b[wb][:, kc, :].rearrange("p (m f) -> p m f", f=4)[:, :, fc], xsT[xb][:, kc, :],
                           kc == 0, kc == 7) for kc in range(8)], reads=[b_w1b[wb], b_xsT[xb]], writes=[pbuf[pb1]])
            S.op('pe', [MM(pbank[pb3][:, :], w3b[wb][:, kc, :].rearrange("p (m f) -> p m f", f=4)[:, :, fc], xsT[xb][:, kc, :],
                           kc == 0, kc == 7) for kc in range(8)], reads=[b_w3b[wb], b_xsT[xb]], writes=[pbuf[pb3]])
            S.op('act', ACTV(sil[sk2][:], pbank[pb1][:, :], AF.Silu), reads=[pbuf[pb1]], writes=[b_sil[sk2]])
            S.op('dve', TT(hidT[hk][:, fc, :], pbank[pb3][:, :], sil[sk2][:], ALU.mult),
                 reads=[pbuf[pb3], b_sil[sk2]], writes=[b_hid[hk]])
            if nxt:
                nxt.pop(0)()
        for j in range(4):
            yk = j % 2
            for hf2 in range(2):
                po = 6 + hf2
                S.op('pe', [MM(pbank[po][:, :], hidT[hk][:, fc, j * 128:(j + 1) * 128],
                               w2b[wb][:, fc, hf2 * 512:(hf2 + 1) * 512], fc == 0, fc == 3) for fc in range(4)],
                     reads=[b_hid[hk], b_w2b[wb]], writes=[pbuf[po]])
                S.op('dve' if hf2 else 'act',
                     CAST('dve' if hf2 else 'act', ysb[yk][:, hf2 * 512:(hf2 + 1) * 512], pbank[po][:, :]),
                     reads=[pbuf[po]], writes=[b_ysb[yk]])
            r0 = b * BLKR + j * 128
            S.dma(DMA(ys_d[r0:r0 + 128, :], ysb[yk][:]), b_ysb[yk], reads=[b_ysb[yk]])
        while nxt:
            nxt.pop(0)()
    S.barrier()
    for i in range(32):
        k = i % 2
        g0, g1 = yg[2 * k], yg[2 * k + 1]
        S.dma(IDMA_G(g0[:], ys_d[:, :], dalli[:, 2 * i:2 * i + 1]), b_yg[2 * k], reads=[b_dalli], writes=[b_yg[2 * k]], q='pool')
        S.dma(IDMA_G(g1[:], ys_d[:, :], dalli[:, 2 * i + 1:2 * i + 2]), b_yg[2 * k + 1], reads=[b_dalli], writes=[b_yg[2 * k + 1]], q='pool')
        S.dma(DMA(xt[k][:], out_d[i * 128:(i + 1) * 128, :]), b_xt[k], writes=[b_xt[k]])
        S.op('dve', TS(g0[:], g0[:], wall[:, 2 * i:2 * i + 1], None, ALU.mult), reads=[b_yg[2 * k], b_wall], writes=[b_yg[2 * k]])
        S.op('dve', STT(g0[:], g1[:], wall[:, 2 * i + 1:2 * i + 2], g0[:], ALU.mult, ALU.add),
             reads=[b_yg[2 * k], b_yg[2 * k + 1], b_wall], writes=[b_yg[2 * k]])
        S.op('dve', TT(g0[:], g0[:], gabc[:, 1, :], ALU.mult), reads=[b_yg[2 * k], b_gabc], writes=[b_yg[2 * k]])
        S.op('dve', TT(g0[:], g0[:], xt[k][:], ALU.add), reads=[b_yg[2 * k], b_xt[k]], writes=[b_yg[2 * k]])
        S.dma(DMA(out_d[i * 128:(i + 1) * 128, :], g0[:]), b_yg[2 * k], reads=[b_yg[2 * k]])
    S.barrier()


def build_program(stop_after=None):
    nc = bass.Bass("TRN2", target_bir_lowering=False)
    din = lambda name, shape: nc.dram_tensor(name, list(shape), F32, kind="ExternalInput").ap()
    x_d = din("x", [S_LEN, D])
    cT_d = din("cT", [128, 8])
    wada_d = din("w_ada", [D, 6 * D])
    bada_d = din("b_ada", [1, 6 * D])
    g1_d = din("g1", [1, D])
    g2_d = din("g2", [1, D])
    win_d = din("w_in", [D, 5120])
    gqk_d = din("gqk", [128, 512])
    cs1_d = din("cs1", [128, NT * 16])
    sn2_d = din("sn2", [128, NT * 16])
    convw_d = din("convw", [128, 12])
    convb_d = din("convb", [128, 4])
    wpa_d = din("w_pa", [512, D])
    wpb_d = din("w_pb", [512, D])
    wo_d = din("w_o", [D, D])
    wr_d = din("wr", [D, 36])
    br_d = din("br", [128, 36])
    if stop_after is None:
        w1_d = din("w1", [32, D, 512])
        w3_d = din("w3", [32, D, 512])
        w2_d = din("w2", [32, 512, D])
    ident_d = din("ident", [128, 128])
    tri_d = din("tri", [128, 512])
    oneh_d = din("oneh", [16, S_LEN])
    mc_d = din("mconst", [128, 225])
    out_d = nc.dram_tensor("out", [S_LEN, D], F32, kind="ExternalOutput").ap()
    ya_d = nc.dram_tensor("ya_scratch", [512, S_LEN], BF16, kind="ExternalOutput").ap()

    S = Sched(nc)
    A = Arena(nc)
    pbank = [nc.alloc_psum_tensor("pb%d" % i, [128, 512], F32) for i in range(8)]
    pbuf = [S.buf("pb%d" % i) for i in range(8)]

    def pbf(i):
        return pbank[i][:, :].bitcast(BF16)

    identb = A.alloc("identb", [128, 128], BF16); b_identb = S.buf("identb")
    cs1 = A.alloc("cs1", [128, NT, 16], F32); b_cs1 = S.buf("cs1")
    sn2 = A.alloc("sn2", [128, NT, 16], F32); b_sn2 = S.buf("sn2")
    trib = A.alloc("trib", [128, 2, 256], BF16); b_trib = S.buf("trib")
    modT = A.alloc("modT", [128, 32], F32); b_modT = S.buf("modT")
    gabc = A.alloc("gabc", [128, 2, D], F32); b_gabc = S.buf("gabc")
    epsb = A.alloc("epsb", [128, 1], F32); b_epsb = S.buf("epsb")
    onesf = A.alloc("onesf", [128, 128], F32); b_onesf = S.buf("onesf")
    gqk = A.alloc("gqk", [128, 512], F32); b_gqk = S.buf("gqk")
    convw = A.alloc("convw", [128, 4, 3], F32); b_convw = S.buf("convw")
    convb = A.alloc("convb", [128, 4], F32); b_convb = S.buf("convb")
    brbc = A.alloc("brbc", [128, 36], F32); b_brbc = S.buf("brbc")
    wrb = A.alloc("wrb", [128, 8, 36], BF16); b_wrb = S.buf("wrb")
    modTp = A.alloc("modTp", [128, 16], F32); b_modTp = S.buf("modTp")
    PERSIST = A.off

    stgc = A.alloc("stgc", [128, 512], F32); b_stgc = S.buf("stgc")
    stgi = A.alloc("stgi", [128, 128], F32); b_stgi = S.buf("stgi")
    stgr = A.alloc("stgr", [128, 8, 36], F32); b_stgr = S.buf("stgr")
    cT = A.alloc("cT", [128, 8], F32); b_cT = S.buf("cT")
    sT = A.alloc("sT", [128, 8], F32); b_sT = S.buf("sT")
    wst = [A.alloc("wst", [128, 8, 512], F32) for _ in range(2)]
    b_wst = [S.buf("wst%d" % i) for i in range(2)]
    modrow = A.alloc("modrow", [1, 6 * D], F32); b_modrow = S.buf("modrow")
    badar = A.alloc("badar", [1, 6 * D], F32); b_badar = S.buf("badar")
    grow = A.alloc("grow", [1, 2 * D], F32); b_grow = S.buf("grow")
    arow = A.alloc("arow", [1, 2 * D], F32); b_arow = S.buf("arow")

    S.op('pool', [MS(epsb[:], 1e-6), MS(onesf[:], 1.0)], writes=[b_epsb, b_onesf])
    S.dma(DMA(stgi[:], ident_d[:, :]), b_stgi, writes=[b_stgi])
    S.op('dve', CP(identb[:], stgi[:]), reads=[b_stgi], writes=[b_identb])
    S.dma(DMA(stgc[:], tri_d[:, :]), b_stgc, writes=[b_stgc])
    S.op('dve', CP(trib[:].rearrange("p a b -> p (a b)"), stgc[:]), reads=[b_stgc], writes=[b_trib])
    S.dma(DMA(cs1[:].rearrange("p a b -> p (a b)"), cs1_d[:, :]), b_cs1, writes=[b_cs1])
    S.dma(DMA(sn2[:].rearrange("p a b -> p (a b)"), sn2_d[:, :]), b_sn2, writes=[b_sn2])
    S.dma(DMA(gqk[:], gqk_d[:, :]), b_gqk, writes=[b_gqk])
    S.dma(DMA(convw[:].rearrange("p a b -> p (a b)"), convw_d[:, :]), b_convw, writes=[b_convw])
    S.dma(DMA(convb[:], convb_d[:, :]), b_convb, writes=[b_convb])
    S.dma(DMA(brbc[:], br_d[:, :]), b_brbc, writes=[b_brbc])
    S.dma(DMA(stgr[:], wr_d.rearrange("(kc p) n -> p kc n", p=128)), b_stgr, writes=[b_stgr])
    S.op('dve', CP(wrb[:], stgr[:]), reads=[b_stgr], writes=[b_wrb])
    S.dma(DMA(cT[:], cT_d[:, :]), b_cT, writes=[b_cT])
    S.op('act', ACTV(sT[:], cT[:], AF.Silu), reads=[b_cT], writes=[b_sT])
    S.dma(DMA(badar[:], bada_d[:, :]), b_badar, writes=[b_badar])
    S.dma(DMA(grow[0:1, 0:D], g1_d[:, :]), b_grow, writes=[b_grow])
    S.dma(DMA(grow[0:1, D:2 * D], g2_d[:, :]), b_grow, writes=[b_grow])
    for j in range(12):
        wb = j % 2
        S.dma(DMA(wst[wb][:], wada_d[:, j * 512:(j + 1) * 512].rearrange("(kc p) n -> p kc n", p=128)),
              b_wst[wb], writes=[b_wst[wb]])
        S.op('pe', [MM(pbank[0][0:1, :], sT[:, kc:kc + 1], wst[wb][:, kc, :], kc == 0, kc == 7) for kc in range(8)],
             reads=[b_sT, b_wst[wb]], writes=[pbuf[0]])
        S.op('dve', TT(modrow[0:1, j * 512:(j + 1) * 512], pbank[0][0:1, :], badar[0:1, j * 512:(j + 1) * 512], ALU.add),
             reads=[pbuf[0], b_badar], writes=[b_modrow])
    S.op('dve', STT(arow[0:1, 0:D], modrow[0:1, D:2 * D], 1.0, grow[0:1, 0:D], ALU.add, ALU.mult),
         reads=[b_modrow, b_grow], writes=[b_arow])
    S.op('dve', STT(arow[0:1, D:2 * D], modrow[0:1, 4 * D:5 * D], 1.0, grow[0:1, D:2 * D], ALU.add, ALU.mult),
         reads=[b_modrow, b_grow], writes=[b_arow])
    fns = []
    srcs = [(arow, 0), (modrow, 0), (arow, D), (modrow, 3 * D)]
    for r, (src, off) in enumerate(srcs):
        for kc in range(8):
            fns.append(MM(pbank[1][:, r * 8 + kc:r * 8 + kc + 1], src[0:1, off + kc * 128:off + (kc + 1) * 128],
                          onesf[0:1, 0:1]))
    S.op('pe', fns, reads=[b_arow, b_modrow, b_onesf], writes=[pbuf[1]])
    S.op('dve', CP(modT[:], pbank[1][:, 0:32]), reads=[pbuf[1]], writes=[b_modT])
    fns = []
    for r, (src, off) in enumerate([(arow, D), (modrow, 3 * D)]):
        for kc in range(8):
            fns.append(MM(pbank[1][:, r * 8 + kc:r * 8 + kc + 1],
                          src[0:1, off:off + D].rearrange("o (p k) -> o p k", k=8)[:, :, kc], onesf[0:1, 0:1]))
    S.op('pe', fns, reads=[b_arow, b_modrow, b_onesf], writes=[pbuf[1]])
    S.op('dve', CP(modTp[:], pbank[1][:, 0:16]), reads=[pbuf[1]], writes=[b_modTp])
    for gi, off in enumerate([2 * D, 5 * D]):
        for hf in range(2):
            S.op('pe', MM(pbank[2][:, :], onesf[0:1, 0:128], modrow[0:1, off + hf * 512:off + (hf + 1) * 512]),
                 reads=[b_onesf, b_modrow], writes=[pbuf[2]])
            S.op('act', ACTV(gabc[:, gi, hf * 512:(hf + 1) * 512], pbank[2][:, :], AF.Copy),
                 reads=[pbuf[2]], writes=[b_gabc])
    S.barrier()
    A.off = PERSIST
    if stop_after == '0':
        S.emit()
        return nc

    def make_norm_bufs(with_xt=True, with_junk=True):
        d = {}
        d['xt'] = [A.alloc("xt", [128, D], F32) for _ in range(2)] if with_xt else None
        d['b_xt'] = [S.buf("xt%d" % i) for i in range(2)]
        if with_junk:
            d['junk'] = A.alloc("junk", [128, D], BF16); d['b_junk'] = S.buf("junk")
        d['ss'] = [A.alloc("ss", [128, 1], F32) for _ in range(2)]
        d['b_ss'] = [S.buf("ss%d" % i) for i in range(2)]
        d['xn'] = [A.alloc("xn", [128, D], BF16) for _ in range(2)]
        d['b_xn'] = [S.buf("xn%d" % i) for i in range(2)]
        return d

    def norm_tile(nb, par, xsrc, b_xsrc, hT_dst, b_hT, col0, ptr_i=0, sch=None):
        S_ = sch or S
        k = par % 2
        S_.op('act', ACTV(nb['junk'][:], xsrc, AF.Square, scale=1.0 / 32.0, accum_out=nb['ss'][k][:]),
             reads=[b_xsrc], writes=[nb['b_junk'], nb['b_ss'][k]])
        S_.op('act', ACTV(nb['ss'][k][:], nb['ss'][k][:], AF.Ln, bias=epsb[:]),
             reads=[nb['b_ss'][k], b_epsb], writes=[nb['b_ss'][k]])
        S_.op('act', ACTV(nb['ss'][k][:], nb['ss'][k][:], AF.Exp, scale=-0.5), reads=[nb['b_ss'][k]], writes=[nb['b_ss'][k]])
        S_.op('dve', TS(nb['xn'][k][:], xsrc, nb['ss'][k][:, 0:1], None, ALU.mult),
             reads=[b_xsrc, nb['b_ss'][k]], writes=[nb['b_xn'][k]])
        pv = pbf(ptr_i).rearrange("p (a b) -> p a b", a=8)
        S_.op('pe', [TR(pv[:, kc, :], nb['xn'][k][:, kc * 128:(kc + 1) * 128], identb[:]) for kc in range(8)],
             reads=[nb['b_xn'][k], b_identb], writes=[pbuf[ptr_i]])
        S_.op('act', [ACTV(hT_dst[:, kc, :], pv[:, kc, :], AF.Identity, scale=modT[:, col0 + kc:col0 + kc + 1],
                          bias=modT[:, col0 + 8 + kc:col0 + 9 + kc]) for kc in range(8)],
             reads=[pbuf[ptr_i], b_modT], writes=[b_hT])

    engrr = [0]

    def cast_eng():
        engrr[0] += 1
        return ['dve', 'act'][engrr[0] % 2]

    KxT = A.alloc("KxT", [128, 4, S_LEN], BF16)
    b_Kx = [S.buf("Kx%d" % c) for c in range(8)]
    Vx = A.alloc("Vx", [128, NT, 4, 65], BF16)
    b_Vx = [S.buf("Vx%d" % c) for c in range(8)]
    kmT = A.alloc("kmT", [128, 4, 16], BF16)
    b_km = [S.buf("km%d" % c) for c in range(16)]
    kms = A.alloc("kms", [128, 4], F32); b_kms = S.buf("kms")
    wqkv = A.alloc("wqkv", [128, 8, 768], BF16); b_wqkv = S.buf("wqkv")
    stgA = [A.alloc("stgA", [128, 8, 256], F32) for _ in range(2)]
    b_stgA = [S.buf("stgA%d" % i) for i in range(2)]
    stgo = A.alloc("stgo", [128, S_LEN], F32); b_stgo = S.buf("stgo")
    nb = make_norm_bufs()
    hT = [A.alloc("hT", [128, 8, 512], BF16) for _ in range(2)]
    b_hT = [S.buf("hT%d" % i) for i in range(2)]
    QxT = [A.alloc("QxT", [128, 4, 512], BF16) for _ in range(2)]
    b_Qx = [S.buf("Qx%d" % i) for i in range(2)]
    sq = A.alloc("sq", [128, 512], F32); b_sq = S.buf("sq")
    ssq = [A.alloc("ssq", [128, 8], F32) for _ in range(2)]; b_ssq = [S.buf("ssq%d" % i) for i in range(2)]
    qn = [A.alloc("qn", [128, 512], F32) for _ in range(2)]
    b_qn = [S.buf("qn%d" % i) for i in range(2)]
    tA = [A.alloc("tA", [128, 8, 16], F32) for _ in range(2)]; b_tA = [S.buf("tA%d" % i) for i in range(2)]
    tB = [A.alloc("tB", [128, 8, 16], F32) for _ in range(2)]; b_tB = [S.buf("tB%d" % i) for i in range(2)]
    qkb = [A.alloc("qkb", [128, 8, 128], BF16) for _ in range(2)]
    b_qkb = [S.buf("qkb%d" % i) for i in range(2)]
    gsb = A.alloc("gsb", [128, 4, 16], F32); b_gsb = S.buf("gsb")
    mx8 = A.alloc("mx8", [128, 4, 8], F32); b_mx8 = S.buf("mx8")
    sel = A.alloc("sel", [128, 4, 16], F32); b_sel = S.buf("sel")
    mbp = [A.alloc("mbp", [128, 4, 128], BF16) for _ in range(2)]
    b_mbp = [S.buf("mbp%d" % i) for i in range(2)]
    pT = [A.alloc("pT", [128, 512], BF16) for _ in range(3)]
    b_pT = [S.buf("pT%d" % i) for i in range(3)]
    rd = A.alloc("rd", [128, 512], F32); b_rd = S.buf("rd")
    bcs = A.alloc("bcs", [128, 512], F32); b_bcs = S.buf("bcs")
    yo = [A.alloc("yo", [128, 512], BF16) for _ in range(2)]
    b_yo = [S.buf("yo%d" % i) for i in range(2)]

    S.dma(DMA(stgo[64:80, :], oneh_d[:, :]), b_stgo, writes=[b_stgo])
    S.op('dve', [CP(KxT[64:80, h, :], stgo[64:80, :]) for h in range(4)], reads=[b_stgo], writes=b_Kx)
    S.op('dve', [MS(Vx[:, :, :, 64:65], 1.0), MS(mbp[0][:], 0.0), MS(mbp[1][:], 0.0),
                  MS(qkb[0][:], 0.0), MS(qkb[1][:], 0.0)],
         writes=b_Vx + b_mbp + b_qkb)

    rot = [0]
    L = _DBG.get('lvl', 9)
    b_pg = S.buf('pg')
    for hh in range(_DBG.get('nhh', 2)):
        for part, c0 in enumerate([hh * 256, 512 + hh * 256, 1024 + hh * 256]):
            sb = part % 2
            S.dma(DMA(stgA[sb][:], win_d[:, c0:c0 + 256].rearrange("(kc p) n -> p kc n", p=128)),
                  b_stgA[sb], writes=[b_stgA[sb]])
            ce = cast_eng()
            S.op(ce, CAST(ce, wqkv[:, :, part * 256:(part + 1) * 256], stgA[sb][:]),
                 reads=[b_stgA[sb]], writes=[b_wqkv])
        NCH = _DBG.get('nch', 8)
        R = Rec()

        def stageA(c, j):
            i = 4 * c + j
            k = i % 2
            hb = c % 2
            R.dma(DMA(nb['xt'][k][:], x_d[i * 128:(i + 1) * 128, :]), nb['b_xt'][k], writes=[nb['b_xt'][k]])
            norm_tile(nb, i, nb['xt'][k][:], nb['b_xt'][k], hT[hb][:, :, j * 128:(j + 1) * 128], b_hT[hb], 0, sch=R)

        def stageB(c, j):
            i = 4 * c + j
            k = i % 2
            hb = c % 2
            R.op('pe', [MM(pbank[1][:, :], hT[hb][:, kc, j * 128:(j + 1) * 128], wqkv[:, kc, 0:512], kc == 0, kc == 7)
                        for kc in range(8)], reads=[b_hT[hb], b_wqkv], writes=[pbuf[1]])
            R.op('pe', [MM(pbank[2][:, 0:256], hT[hb][:, kc, j * 128:(j + 1) * 128], wqkv[:, kc, 512:768], kc == 0, kc == 7)
                        for kc in range(8)], reads=[b_hT[hb], b_wqkv], writes=[pbuf[2]])
            R.op('act', ACTV(Vx[:, i, :, 0:64], pbank[2][:, 0:256].rearrange("p (h d) -> p h d", h=4), AF.Copy),
                 reads=[pbuf[2]], writes=[b_Vx[c]])
            R.op('act', ACTV(sq[:], pbank[1][:, :], AF.Square), reads=[pbuf[1]], writes=[b_sq])
            R.op('dve', RED(ssq[k][:], sq[:].rearrange("p (h d) -> p h d", h=8)), reads=[b_sq], writes=[b_ssq[k]])
            R.op('act', ACTV(ssq[k][:], ssq[k][:], AF.Ln, scale=1.0 / 64.0, bias=epsb[:]),
                 reads=[b_ssq[k], b_epsb], writes=[b_ssq[k]])
            R.op('act', ACTV(ssq[k][:], ssq[k][:], AF.Exp, scale=-0.5), reads=[b_ssq[k]], writes=[b_ssq[k]])
            qv = qn[k][:].rearrange("p (h d) -> p h d", h=8)
            R.op('dve', TT(qv, pbank[1][:, :].rearrange("p (h d) -> p h d", h=8),
                           ssq[k][:, :].unsqueeze(2).to_broadcast([128, 8, 64]), ALU.mult),
                 reads=[pbuf[1], b_ssq[k]], writes=[b_qn[k]])

        def stageC(c, j):
            i = 4 * c + j
            k = i % 2
            qb = c % 2
            qv = qn[k][:].rearrange("p (h d) -> p h d", h=8)
            R.op('dve', TT(qn[k][:], qn[k][:], gqk[:], ALU.mult), reads=[b_qn[k], b_gqk], writes=[b_qn[k]])
            R.op('dve', [TT(tA[k][:], qv[:, :, 0:16], cs1[:, i, :].unsqueeze(1).to_broadcast([128, 8, 16]), ALU.mult),
                         TT(tB[k][:, :, 0:8], qv[:, :, 8:16], sn2[:, i, 0:8].unsqueeze(1).to_broadcast([128, 8, 8]), ALU.mult),
                         TT(tB[k][:, :, 8:16], qv[:, :, 0:8], sn2[:, i, 8:16].unsqueeze(1).to_broadcast([128, 8, 8]), ALU.mult)],
                 reads=[b_qn[k], b_cs1, b_sn2], writes=[b_tA[k], b_tB[k]])
            R.op('act', ACTV(qkb[k][:, :, 16:64], qv[:, :, 16:64], AF.Copy), reads=[b_qn[k]], writes=[b_qkb[k]])
            R.op('dve', TT(qkb[k][:, :, 0:16], tA[k][:], tB[k][:], ALU.add), reads=[b_tA[k], b_tB[k]], writes=[b_qkb[k]])
            ptq = pbf(0).rearrange("p (a b) -> p a b", a=8)
            R.op('pe', [TR(ptq[:, s, :], qkb[k][:, s, :], identb[:]) for s in range(8)],
                 reads=[b_qkb[k], b_identb], writes=[pbuf[0]])
            R.op('act', [ACTV(QxT[qb][0:64, h4, j * 128:(j + 1) * 128], ptq[0:64, h4, :], AF.Copy) for h4 in range(4)],
                 reads=[pbuf[0]], writes=[b_Qx[qb]])
            R.op('act', [ACTV(KxT[0:64, h4, i * 128:(i + 1) * 128], ptq[0:64, 4 + h4, :], AF.Copy) for h4 in range(4)],
                 reads=[pbuf[0]], writes=[b_Kx[c]])
            if i % 2 == 1:
                blk = i // 2
                R.op('dve', RED(kms[0:64, :], KxT[0:64, :, blk * 256:(blk + 1) * 256]),
                     reads=[b_Kx[c]], writes=[b_kms])
                R.op('dve', TS(kmT[0:64, :, blk], kms[0:64, :], 1.0 / 256.0, None, ALU.mult),
                     reads=[b_kms], writes=[b_km[blk]])

        def prep_items(c):
            R.items = []
            for j in range(4):
                stageA(c, j)
                stageB(c, j)
                stageC(c, j)
            return list(R.items)

        def replay(item):
            kind, a_, k_ = item
            getattr(S, kind)(*a_, **k_)

        pending = prep_items(0)
        for c in range(NCH):
            hb = c % 2
            qb = c % 2
            for item in pending:
                replay(item)
            pending = prep_items(c + 1) if c + 1 < NCH else []
            for j in range(4 if _DBG.get('gate', True) else 0):
                i = 4 * c + j
                cur = i // 2
                m = j % 2
                pg = pbank[2][:, 256:320].rearrange("p (h n) -> p h n", h=4)
                fl = [MS(gsb[:, :, cur:cur + 1], BIG)] + ([MS(gsb[:, :, cur + 1:16], -BIG)] if cur < 15 else [])
                S.op('dve', fl, writes=[b_gsb])
                if cur > 0:
                    S.op('pe', [MM(pg[:, h, 0:cur], QxT[qb][0:64, h, j * 128:(j + 1) * 128], kmT[0:64, h, 0:cur])
                                for h in range(4)], reads=[b_Qx[qb]] + b_km[0:cur], writes=[b_pg])
                    S.op('dve', CP(gsb[:, :, 0:cur], pg[:, :, 0:cur]), reads=[b_pg], writes=[b_gsb])
                S.op('dve', [MAX8(mx8[:, h, :], gsb[:, h, :]) for h in range(4)], reads=[b_gsb], writes=[b_mx8])
                S.op('dve', TT(sel[:], gsb[:], mx8[:, :, 3:4].to_broadcast([128, 4, 16]), ALU.is_ge),
                     reads=[b_gsb, b_mx8], writes=[b_sel])
                S.op('dve', TS(mbp[m][:, :, 64:80], sel[:], MASKV, -MASKV, ALU.mult, ALU.add),
                     reads=[b_sel], writes=[b_mbp[m]])
                pmb = pbf(0).rearrange("p (a b) -> p a b", a=8)
                S.op('pe', [TR(pmb[:, h, :], mbp[m][:, h, :], identb[:]) for h in range(4)],
                     reads=[b_mbp[m], b_identb], writes=[pbuf[0]])
                S.op('act', [ACTV(QxT[qb][64:80, h4, j * 128:(j + 1) * 128], pmb[64:80, h4, :], AF.Copy) for h4 in range(4)],
                     reads=[pbuf[0]], writes=[b_Qx[qb]])
            nsteps = 4 * (4 * c + 4)
            stepc = [0]
            for h in range(4 if _DBG.get('attn', True) else 0):
                nk = 4 * c + 4
                pyi = 6 + (h % 2)

                def cols_of(kt):
                    return (0, 512) if kt < 4 * c + 2 else (256, 512)

                def emit_S(kt, r):
                    c0, c1 = cols_of(kt)
                    S.op('pe', MM(pbank[3 + r][:, c0:c1], KxT[0:80, h, kt * 128:(kt + 1) * 128], QxT[qb][0:80, h, c0:c1]),
                         reads=[b_Kx[kt // 4], b_Qx[qb]], writes=[pbuf[3 + r]])
                rs = []
                for kt in range(nk):
                    rs.append(rot[0] % 3)
                    rot[0] += 1
                emit_S(0, rs[0])
                if nk > 1:
                    emit_S(1, rs[1])
                for kt in range(nk):
                    if kt + 2 < nk:
                        emit_S(kt + 2, rs[kt + 2])
                    r = rs[kt]
                    c0, c1 = cols_of(kt)
                    S.op('act', ACTV(pT[r][:, c0:c1], pbank[3 + r][:, c0:c1], AF.Exp, scale=0.125),
                         reads=[pbuf[3 + r]], writes=[b_pT[r]])
                    if kt >= 4 * c:
                        d0 = 0 if kt < 4 * c + 2 else 256
                        S.op('dve', TT(pT[r][:, d0:d0 + 256], pT[r][:, d0:d0 + 256], trib[:, kt % 2, :], ALU.mult),
                             reads=[b_pT[r], b_trib], writes=[b_pT[r]])
                    S.op('pe', MM(pbank[pyi][0:65, c0:c1], Vx[:, kt, h, :], pT[r][:, c0:c1], kt == 0, kt == nk - 1),
                         reads=[b_Vx[kt // 4], b_pT[r]], writes=[pbuf[pyi]])
                    stepc[0] += 1
                    if pending and _DBG.get('ilv', True):
                        left = max(1, nsteps - stepc[0] + 1)
                        for _ in range(-(-len(pending) // left)):
                            replay(pending.pop(0))
                yb = h % 2
                S.op('dve', RECIP(rd[64:65, :], pbank[pyi][64:65, :]), reads=[pbuf[pyi]], writes=[b_rd])
                rb = 3 + (rot[0] % 3)
                rot[0] += 1
                S.op('pe', MM(pbank[rb][0:64, :], onesf[64:65, 0:64], rd[64:65, :]),
                     reads=[b_onesf, b_rd], writes=[pbuf[rb]])
                S.op('act', ACTV(bcs[0:64, :], pbank[rb][0:64, :], AF.Copy), reads=[pbuf[rb]], writes=[b_bcs])
                S.op('dve', TT(yo[yb][0:64, :], pbank[pyi][0:64, :], bcs[0:64, :], ALU.mult),
                     reads=[pbuf[pyi], b_bcs], writes=[b_yo[yb]])
                hg = hh * 4 + h
                S.dma(DMA(ya_d[hg * 64:(hg + 1) * 64, c * 512:(c + 1) * 512], yo[yb][0:64, :]),
                      b_yo[yb], reads=[b_yo[yb]])
    S.barrier()
    A.off = PERSIST
    if stop_after == 'A':
        S.emit()
        return nc
    wB = A.alloc("wB", [128, 8, 3584], BF16); b_wB = S.buf("wB")
    wpa = A.alloc("wpa", [128, 4, D], BF16); b_wpa = S.buf("wpa")
    wpb = A.alloc("wpb", [128, 4, D], BF16); b_wpb = S.buf("wpb")
    wo = A.alloc("wo", [128, 8, D], BF16); b_wo = S.buf("wo")
    stgB = [A.alloc("stgB", [128, 2048], F32) for _ in range(2)]
    b_stgB = [S.buf("stgB%d" % i) for i in range(2)]
    sidx = [0]

    def load_cast(dst, src_ap, shape3, b_dst, extra=None):
        k = sidx[0] % len(stgB)
        sidx[0] += 1
        a, bb = shape3
        view = stgB[k][:, 0:a * bb].rearrange("p (a b) -> p a b", a=a)
        S.dma(DMA(view, src_ap), b_stgB[k], writes=[b_stgB[k]])
        if extra is None:
            ce = cast_eng()
            S.op(ce, CAST(ce, dst, view), reads=[b_stgB[k]], writes=[b_dst])
        else:
            ex, b_ex = extra
            S.op('dve', [TT(dst[:, q, :], view[:, q, :], ex, ALU.mult) for q in range(a)],
                 reads=[b_stgB[k], b_ex], writes=[b_dst])

    for p in range(14):
        c0 = 1536 + p * 256
        load_cast(wB[:, :, p * 256:(p + 1) * 256], win_d[:, c0:c0 + 256].rearrange("(kc p) n -> p kc n", p=128),
                  (8, 256), b_wB)
    for q2 in range(2):
        load_cast(wpa[:, 2 * q2:2 * q2 + 2, :], wpa_d[q2 * 256:(q2 + 1) * 256, :].rearrange("(cc p) n -> p cc n", p=128),
                  (2, D), b_wpa)
        load_cast(wpb[:, 2 * q2:2 * q2 + 2, :], wpb_d[q2 * 256:(q2 + 1) * 256, :].rearrange("(cc p) n -> p cc n", p=128),
                  (2, D), b_wpb)
    for q4 in range(4):
        load_cast(wo[:, 2 * q4:2 * q4 + 2, :], wo_d[q4 * 256:(q4 + 1) * 256, :].rearrange("(cc p) n -> p cc n", p=128),
                  (2, D), b_wo)
    nbB = make_norm_bufs()
    hTB = A.alloc("hTB", [128, 8, 512], BF16); b_hTB = S.buf("hTB")
    yaT = A.alloc("yaT", [128, 4, 512], BF16); b_yaT = S.buf("yaT")
    xbs = A.alloc("xbs", [128, 512], F32); b_xbs = S.buf("xbs")
    bgs = A.alloc("bgs", [128, 512], F32); b_bgs = S.buf("bgs")
    u = A.alloc("u", [128, 4, 514], F32); b_u = [S.buf("u%d" % i) for i in range(4)]
    tcv = A.alloc("tcv", [128, 512], F32); b_tcv = S.buf("tcv")
    ybT = A.alloc("ybT", [128, 4, 512], BF16); b_ybT = S.buf("ybT")
    gas = A.alloc("gas", [128, 512], F32); b_gas = S.buf("gas")
    gbs = A.alloc("gbs", [128, 512], F32); b_gbs = S.buf("gbs")
    t1 = A.alloc("t1", [128, 512], F32); b_t1 = S.buf("t1")
    t2 = A.alloc("t2", [128, 512], F32); b_t2 = S.buf("t2")
    mT = A.alloc("mT", [128, 8, 512], BF16); b_mT = S.buf("mT")
    xr = [A.alloc("xr", [128, D], F32) for _ in range(2)]
    b_xr = [S.buf("xr%d" % i) for i in range(2)]
    to = A.alloc("to", [128, 512], F32); b_to = S.buf("to")
    x1t = [A.alloc("x1t", [128, D], F32) for _ in range(2)]
    b_x1t = [S.buf("x1t%d" % i) for i in range(2)]
    S.op('dve', MS(u[:], 0.0), writes=b_u)
    for c in range(8):
        for j in range(4):
            i = 4 * c + j
            k = i % 2
            S.dma(DMA(nbB['xt'][k][:], x_d[i * 128:(i + 1) * 128, :]), nbB['b_xt'][k], writes=[nbB['b_xt'][k]])
            norm_tile(nbB, i, nbB['xt'][k][:], nbB['b_xt'][k], hTB[:, :, j * 128:(j + 1) * 128], b_hTB, 0)
        S.dma(DMA(yaT[:], ya_d[:, c * 512:(c + 1) * 512].rearrange("(cc p) n -> p cc n", p=128)), b_yaT, writes=[b_yaT])
        for cc in range(4):
            for bank, col0 in [(1, 0), (2, 512), (3, 1024)]:
                S.op('pe', [MM(pbank[bank][:, :], wB[:, kc, col0 + cc * 128:col0 + (cc + 1) * 128], hTB[:, kc, :], kc == 0, kc == 7)
                            for kc in range(8)], reads=[b_wB, b_hTB], writes=[pbuf[bank]])
            S.op('act', ACTV(xbs[:], pbank[1][:, :], AF.Copy), reads=[pbuf[1]], writes=[b_xbs])
            if c > 0:
                S.op('dve', CP(u[:, cc, 0:2], u[:, cc, 512:514]), reads=[b_u[cc]], writes=[b_u[cc]])
            S.op('dve', TT(u[:, cc, 2:514], pbank[3][:, :], xbs[:], ALU.mult), reads=[pbuf[3], b_xbs], writes=[b_u[cc]])
            S.op('act', ACTV(bgs[:], pbank[2][:, :], AF.Copy), reads=[pbuf[2]], writes=[b_bgs])
            S.op('dve', TS(tcv[:], u[:, cc, 0:512], convw[:, cc, 0:1], None, ALU.mult),
                 reads=[b_u[cc], b_convw], writes=[b_tcv])
            S.op('dve', STT(tcv[:], u[:, cc, 1:513], convw[:, cc, 1:2], tcv[:], ALU.mult, ALU.add),
                 reads=[b_u[cc], b_convw, b_tcv], writes=[b_tcv])
            S.op('dve', STT(tcv[:], u[:, cc, 2:514], convw[:, cc, 2:3], tcv[:], ALU.mult, ALU.add),
                 reads=[b_u[cc], b_convw, b_tcv], writes=[b_tcv])
            S.op('dve', STT(ybT[:, cc, :], tcv[:], convb[:, cc:cc + 1], bgs[:], ALU.add, ALU.mult),
                 reads=[b_tcv, b_convb, b_bgs], writes=[b_ybT])
        for m in range(8):
            S.op('pe', [MM(pbank[4][:, :], wB[:, kc, 1536 + m * 128:1536 + (m + 1) * 128], hTB[:, kc, :], kc == 0, kc == 7)
                        for kc in range(8)], reads=[b_wB, b_hTB], writes=[pbuf[4]])
            S.op('pe', [MM(pbank[5][:, :], wB[:, kc, 2560 + m * 128:2560 + (m + 1) * 128], hTB[:, kc, :], kc == 0, kc == 7)
                        for kc in range(8)], reads=[b_wB, b_hTB], writes=[pbuf[5]])
            S.op('pe', [MM(pbank[6][:, :], wpa[:, cc, m * 128:(m + 1) * 128], yaT[:, cc, :], cc == 0, cc == 3)
                        for cc in range(4)], reads=[b_wpa, b_yaT], writes=[pbuf[6]])
            S.op('pe', [MM(pbank[7][:, :], wpb[:, cc, m * 128:(m + 1) * 128], ybT[:, cc, :], cc == 0, cc == 3)
                        for cc in range(4)], reads=[b_wpb, b_ybT], writes=[pbuf[7]])
            S.op('act', ACTV(gas[:], pbank[4][:, :], AF.Sigmoid), reads=[pbuf[4]], writes=[b_gas])
            S.op('act', ACTV(gbs[:], pbank[5][:, :], AF.Sigmoid), reads=[pbuf[5]], writes=[b_gbs])
            S.op('dve', TT(t1[:], pbank[6][:, :], gas[:], ALU.mult), reads=[pbuf[6], b_gas], writes=[b_t1])
            S.op('dve', TT(t2[:], pbank[7][:, :], gbs[:], ALU.mult), reads=[pbuf[7], b_gbs], writes=[b_t2])
            S.op('dve', TT(mT[:, m, :], t1[:], t2[:], ALU.add), reads=[b_t1, b_t2], writes=[b_mT])
        for j in range(4):
            i = 4 * c + j
            k = i % 2
            S.dma(DMA(xr[k][:], x_d[i * 128:(i + 1) * 128, :]), b_xr[k], writes=[b_xr[k]])
            for hf in range(2):
                S.op('pe', [MM(pbank[1][:, :], mT[:, m, j * 128:(j + 1) * 128], wo[:, m, hf * 512:(hf + 1) * 512], m == 0, m == 7)
                            for m in range(8)], reads=[b_mT, b_wo], writes=[pbuf[1]])
                S.op('dve', TT(to[:], pbank[1][:, :], gabc[:, 0, hf * 512:(hf + 1) * 512], ALU.mult),
                     reads=[pbuf[1], b_gabc], writes=[b_to])
                S.op('dve', TT(x1t[k][:, hf * 512:(hf + 1) * 512], to[:], xr[k][:, hf * 512:(hf + 1) * 512], ALU.add),
                     reads=[b_to, b_xr[k]], writes=[b_x1t[k]])
            S.dma(DMA(out_d[i * 128:(i + 1) * 128, :], x1t[k][:]), b_x1t[k], reads=[b_x1t[k]])
    S.barrier()
    A.off = PERSIST
    if stop_after == 'B':
        S.emit()
        return nc
    if _SPARSE:
        _sparse_moe(nc, S, A, locals())
        S.emit()
        return nc
    h2T = A.alloc("h2T", [128, 8, 2048], BF16); b_h2T = [S.buf("h2T%d" % i) for i in range(4)]
    acc = A.alloc("acc", [128, 16, D], F32); b_acc = [S.buf("acc%d" % i) for i in range(16)]
    cw = A.alloc("cw", [128, 16, 32], F32); b_cw = [S.buf("cw%d" % i) for i in range(16)]
    w1b = [A.alloc("w1b", [128, 8, 512], BF16) for _ in range(2)]; b_w1b = [S.buf("w1b%d" % i) for i in range(2)]
    w3b = [A.alloc("w3b", [128, 8, 512], BF16) for _ in range(2)]; b_w3b = [S.buf("w3b%d" % i) for i in range(2)]
    w2b = [A.alloc("w2b", [128, 4, D], BF16) for _ in range(2)]; b_w2b = [S.buf("w2b%d" % i) for i in range(2)]
    stgC = [A.alloc("stgC", [128, 2048], F32) for _ in range(2)]
    b_stgC = [S.buf("stgC%d" % i) for i in range(2)]
    stgB[:] = stgC
    b_stgB[:] = b_stgC
    nbC = make_norm_bufs(with_xt=True, with_junk=False)
    hidT = [A.alloc("hidT", [128, 4, 512], BF16) for _ in range(2)]; b_hid = [S.buf("hid%d" % i) for i in range(2)]
    sil = [A.alloc("sil", [128, 512], F32) for _ in range(2)]; b_sil = [S.buf("sil%d" % i) for i in range(2)]
    nbC['junk'] = hidT[0][:, :, :].rearrange("p a b -> p (a b)")[:, 0:D]
    nbC['b_junk'] = b_hid[0]
    lg = A.alloc("lg", [128, 36], F32); b_lg = S.buf("lg")
    rt = A.alloc("rt", [128, 16], F32); b_rt = S.buf("rt")
    goh = A.alloc("goh", [128, 4], F32); b_goh = S.buf("goh")
    gex = A.alloc("gex", [128, 4], F32); b_gex = S.buf("gex")
    pen = A.alloc("pen", [128, 4], F32); b_pen = S.buf("pen")
    em = A.alloc("em", [128, 32], F32); b_em = S.buf("em")
    emc = A.alloc("emc", [128, 32], F32); b_emc = S.buf("emc")
    mx8c = A.alloc("mx8c", [128, 8], F32); b_mx8c = S.buf("mx8c")
    selc = A.alloc("selc", [128, 32], F32); b_selc = S.buf("selc")
    ex = A.alloc("ex", [128, 32], F32); b_ex = S.buf("ex")
    exs = A.alloc("exs", [128, 32], F32); b_exs = S.buf("exs")
    gmax, ngmax, gsum, nm1, den, fsc = [rt[:, q:q + 1] for q in range(6)]
    PEN = 1.0e4
    for hf in range(2):
        for ti in range(16):
            i = hf * 16 + ti
            k = i % 2
            S.dma(DMA(nbC['xt'][k][:], out_d[i * 128:(i + 1) * 128, :]), nbC['b_xt'][k], writes=[nbC['b_xt'][k]])
            norm_tile(nbC, i, nbC['xt'][k][:], nbC['b_xt'][k], h2T[:, :, ti * 128:(ti + 1) * 128], b_h2T[ti // 4], 16)
            S.op('pool', MS(acc[:, ti, :], 0.0), writes=[b_acc[ti]])
            S.op('pe', [MM(pbank[1][:, 0:36], h2T[:, kc, ti * 128:(ti + 1) * 128], wrb[:, kc, :], kc == 0, kc == 7)
                        for kc in range(8)], reads=[b_h2T[ti // 4], b_wrb], writes=[pbuf[1]])
            S.op('dve', TT(lg[:], pbank[1][:, 0:36], brbc[:], ALU.add), reads=[pbuf[1], b_brbc], writes=[b_lg])
            S.op('dve', RED(gmax, lg[:, 0:4], ALU.max), reads=[b_lg], writes=[b_rt])
            S.op('dve', TS(ngmax, gmax, -1.0, None, ALU.mult), reads=[b_rt], writes=[b_rt])
            S.op('dve', TS(goh[:], lg[:, 0:4], gmax, None, ALU.is_ge), reads=[b_lg, b_rt], writes=[b_goh])
            S.op('act', ACTV(gex[:], lg[:, 0:4], AF.Exp, bias=ngmax, accum_out=gsum),
                 reads=[b_lg, b_rt], writes=[b_gex, b_rt])
            S.op('dve', RECIP(gsum, gsum), reads=[b_rt], writes=[b_rt])
            S.op('dve', TS(pen[:], goh[:], PEN, -PEN, ALU.mult, ALU.add), reads=[b_goh], writes=[b_pen])
            S.op('dve', TT(em[:].rearrange("p (g e) -> p g e", g=4), lg[:, 4:36].rearrange("p (g e) -> p g e", g=4),
                           pen[:, :].unsqueeze(2).to_broadcast([128, 4, 8]), ALU.add),
                 reads=[b_lg, b_pen], writes=[b_em])
            S.op('dve', MAX8(mx8c[:], em[:]), reads=[b_em], writes=[b_mx8c])
            S.op('dve', TS(nm1, mx8c[:, 0:1], -1.0, None, ALU.mult), reads=[b_mx8c], writes=[b_rt])
            S.op('dve', TS(selc[:], em[:], mx8c[:, 1:2], None, ALU.is_ge), reads=[b_em, b_mx8c], writes=[b_selc])
            S.op('dve', TS(emc[:], em[:], mx8c[:, 1:2], None, ALU.max), reads=[b_em, b_mx8c], writes=[b_emc])
            S.op('act', ACTV(ex[:], emc[:], AF.Exp, bias=nm1), reads=[b_emc, b_rt], writes=[b_ex])
            S.op('dve', TT(exs[:], ex[:], selc[:], ALU.mult), reads=[b_ex, b_selc], writes=[b_exs])
            S.op('dve', RED(den, exs[:]), reads=[b_exs], writes=[b_rt])
            S.op('dve', RECIP(den, den), reads=[b_rt], writes=[b_rt])
            S.op('dve', TT(fsc, den, gsum, ALU.mult), reads=[b_rt], writes=[b_rt])
            S.op('dve', TS(cw[:, ti, :], exs[:], fsc, None, ALU.mult), reads=[b_exs, b_rt], writes=[b_cw[ti]])
        for e in range(32):
            wb = e % 2
            for q2 in range(2):
                load_cast(w1b[wb][:, 4 * q2:4 * q2 + 4, :],
                          w1_d[e, q2 * 512:(q2 + 1) * 512, :].rearrange("(kc p) n -> p kc n", p=128), (4, 512), b_w1b[wb])
                load_cast(w3b[wb][:, 4 * q2:4 * q2 + 4, :],
                          w3_d[e, q2 * 512:(q2 + 1) * 512, :].rearrange("(kc p) n -> p kc n", p=128), (4, 512), b_w3b[wb])
            for q2 in range(2):
                load_cast(w2b[wb][:, 2 * q2:2 * q2 + 2, :],
                          w2_d[e, q2 * 256:(q2 + 1) * 256, :].rearrange("(fc p) n -> p fc n", p=128), (2, D), b_w2b[wb])
            for ch in range(4):
                hk = (e * 4 + ch) % 2
                for fc in range(4):
                    pb1 = 2 + (fc % 2)
                    pb3 = 4 + (fc % 2)
                    sk = fc % 2
                    S.op('pe', [MM(pbank[pb1][:, :], w1b[wb][:, kc, fc * 128:(fc + 1) * 128], h2T[:, kc, ch * 512:(ch + 1) * 512],
                                   kc == 0, kc == 7) for kc in range(8)], reads=[b_w1b[wb], b_h2T[ch]], writes=[pbuf[pb1]])
                    S.op('pe', [MM(pbank[pb3][:, :], w3b[wb][:, kc, fc * 128:(fc + 1) * 128], h2T[:, kc, ch * 512:(ch + 1) * 512],
                                   kc == 0, kc == 7) for kc in range(8)], reads=[b_w3b[wb], b_h2T[ch]], writes=[pbuf[pb3]])
                    S.op('act', ACTV(sil[sk][:], pbank[pb1][:, :], AF.Silu), reads=[pbuf[pb1]], writes=[b_sil[sk]])
                    S.op('dve', TT(hidT[hk][:, fc, :], pbank[pb3][:, :], sil[sk][:], ALU.mult),
                         reads=[pbuf[pb3], b_sil[sk]], writes=[b_hid[hk]])
                for j in range(4):
                    ti = ch * 4 + j
                    for hf2 in range(2):
                        po = 6 + hf2
                        S.op('pe', [MM(pbank[po][:, :], hidT[hk][:, fc, j * 128:(j + 1) * 128],
                                       w2b[wb][:, fc, hf2 * 512:(hf2 + 1) * 512], fc == 0, fc == 3) for fc in range(4)],
                             reads=[b_hid[hk], b_w2b[wb]], writes=[pbuf[po]])
                        S.op('dve', STT(acc[:, ti, hf2 * 512:(hf2 + 1) * 512], pbank[po][:, :], cw[:, ti, e:e + 1],
                                        acc[:, ti, hf2 * 512:(hf2 + 1) * 512], ALU.mult, ALU.add),
                             reads=[pbuf[po], b_cw[ti], b_acc[ti]], writes=[b_acc[ti]])
        for ti in range(16):
            i = hf * 16 + ti
            k = i % 2
            S.dma(DMA(nbC['xt'][k][:], out_d[i * 128:(i + 1) * 128, :]), nbC['b_xt'][k], writes=[nbC['b_xt'][k]])
            S.op('dve', TT(acc[:, ti, :], acc[:, ti, :], gabc[:, 1, :], ALU.mult), reads=[b_acc[ti], b_gabc], writes=[b_acc[ti]])
            S.op('dve', TT(acc[:, ti, :], acc[:, ti, :], nbC['xt'][k][:], ALU.add),
                 reads=[b_acc[ti], nbC['b_xt'][k]], writes=[b_acc[ti]])
            S.dma(DMA(out_d[i * 128:(i + 1) * 128, :], acc[:, ti, :]), b_acc[ti], reads=[b_acc[ti]])
    S.barrier()
    S.emit()
    return nc


def _consts():
    pos = np.arange(S_LEN, dtype=np.float32)
    inv = (np.float32(500000.0) ** (-np.arange(0, 16, 2, dtype=np.float32) / np.float32(16))).astype(np.float32)
    ang = (pos[:, None] * inv[None, :]).astype(np.float32)
    cos = np.cos(ang).astype(np.float32).reshape(NT, 128, 8).transpose(1, 0, 2)
    sin = np.sin(ang).astype(np.float32).reshape(NT, 128, 8).transpose(1, 0, 2)
    cs1 = np.concatenate([cos, cos], -1).reshape(128, NT * 16)
    sn2 = np.concatenate([-sin, sin], -1).reshape(128, NT * 16)
    kp = np.arange(128)[:, None, None]
    jj = np.arange(2)[None, :, None]
    qq = np.arange(256)[None, None, :]
    tri = (jj * 128 + kp <= qq).astype(np.float32).reshape(128, 512)
    oneh = (np.arange(S_LEN)[None, :] // 256 == np.arange(16)[:, None]).astype(np.float32)
    mconst = np.zeros((128, 225), np.float32)
    tt = np.arange(128)
    mconst[:, 0:128] = (tt[:, None] < tt[None, :]).astype(np.float32)
    ee = np.arange(32)
    mconst[0:32, 128:160] = (ee[:, None] < ee[None, :]).astype(np.float32)
    mconst[:, 160:176] = (512.0 * np.arange(16))[None, :]
    mconst[:, 176:224] = np.arange(48, dtype=np.float32)[None, :]
    mconst[:, 224] = np.arange(128, dtype=np.float32)
    return dict(cs1=np.ascontiguousarray(cs1), sn2=np.ascontiguousarray(sn2), tri=tri, oneh=oneh, mconst=mconst,
                ident=np.eye(128, dtype=np.float32))


def kernel(x, c, w_ada, b_ada, g_norm1, g_norm2, w_in, g_q, g_k, conv_w, conv_b,
           w_pa, w_pb, w_o, w_rg, b_rg, w_re, b_re, w1, w3, w2):
    f = lambda a: np.ascontiguousarray(np.asarray(a, dtype=np.float32))
    x = f(x); c = f(c)
    cst = _consts()
    gqk = np.concatenate([np.tile(f(g_q)[0], 4), np.tile(f(g_k)[0], 4)])
    gqk = np.ascontiguousarray(np.broadcast_to(gqk[None, :], (128, 512)))
    convw = np.ascontiguousarray(f(conv_w)[0].reshape(3, 4, 128).transpose(2, 1, 0).reshape(128, 12))
    convb = np.ascontiguousarray(f(conv_b)[0].reshape(4, 128).T)
    wr = np.ascontiguousarray(np.concatenate([f(w_rg)[0], f(w_re)[0]], axis=1))
    br = np.concatenate([f(b_rg)[0], f(b_re)[0]])
    br = np.ascontiguousarray(np.broadcast_to(br[None, :], (128, 36)))
    shared = dict(w_ada=f(w_ada)[0], b_ada=f(b_ada)[0:1], g1=f(g_norm1)[0:1], g2=f(g_norm2)[0:1], w_in=f(w_in)[0],
                  gqk=gqk, convw=convw, convb=convb, w_pa=f(w_pa)[0], w_pb=f(w_pb)[0], w_o=f(w_o)[0],
                  wr=wr, br=br, w1=f(w1)[0], w3=f(w3)[0], w2=f(w2)[0], **cst)
    n = _NCORES
    in_maps = []
    for b in range(n):
        m = dict(shared)
        m["x"] = x[b]
        m["cT"] = np.ascontiguousarray(c[b].reshape(8, 128).T)
        in_maps.append(m)
    if _STOP_AFTER is not None:
        for m in in_maps:
            for kk in ('w1', 'w3', 'w2'):
                m.pop(kk)
    nc = build_program(_STOP_AFTER)
    res = run_bass_kernel_spmd(nc, in_maps, core_ids=list(range(n)))
    if _STOP_AFTER is not None:
        global _DBG_RES
        _DBG_RES = res.results
    out = np.stack([np.asarray(res.results[b]["out"], dtype=np.float32).reshape(S_LEN, D) for b in range(n)])
    return out
```
